# Optimizing a Trainium2 kernel written in Bass

```python
import math
import jax, jax.numpy as jnp
from jax import lax
import numpy as np

D_MODEL = 1024
BATCH = 16
SEQ = 2048
DEPTH = 1

D_MIX = D_MODEL
HEAD_DIM = 64
NSA_HEADS = 8
NSA_KV_HEADS = 2
NSA_WIDTH = NSA_HEADS * HEAD_DIM
KV_W = NSA_KV_HEADS * HEAD_DIM
CONV_GROUPS = 8
CONV_WIDTH = D_MIX - NSA_WIDTH
CONV_K = 3
CMP_BLK = 32
CMP_STRIDE = 16
SEL_BLK = 64
SEL_TOPN = 8
WINDOW = 512
Q_BLK = 64
FORCE_SCORE = 1e4
REL_BUCKETS = 32
REL_MAX_DIST = 128
PEER_HEADS = 8
PEER_NKEYS = 128
PEER_EXPERTS = PEER_NKEYS * PEER_NKEYS
PEER_QDIM = 128
PEER_TOPK = 16
PEER_CHUNK = 128
EPS = 1e-6

IN_SIZES = (NSA_WIDTH, KV_W, KV_W, KV_W, KV_W, KV_W, KV_W, NSA_HEADS * 3,
            CONV_WIDTH, CONV_WIDTH, CONV_WIDTH)
IN_COLS = sum(IN_SIZES)
SPLIT_POINTS = tuple(int(v) for v in np.cumsum(IN_SIZES)[:-1])

kernel_name = 'hybrid_nsa_shortconv_peer'


def rmsnorm(x, g):
    x32 = x.astype(jnp.float32)
    y = x32 * lax.rsqrt(jnp.mean(x32 * x32, axis=-1, keepdims=True) + EPS)
    return y.astype(x.dtype) * g


def masked_softmax(logits, mask):
    logits = jnp.where(mask, logits.astype(jnp.float32), -1e30)
    p = jax.nn.softmax(logits, axis=-1)
    return jnp.where(jnp.any(mask, axis=-1, keepdims=True), p, 0.0)


def rel_bucket(dist):
    n = jnp.maximum(dist, 0)
    exact = REL_BUCKETS // 2
    log_ratio = jnp.log(jnp.maximum(n, 1).astype(jnp.float32) / exact) / math.log(REL_MAX_DIST / exact)
    large = exact + (log_ratio * (REL_BUCKETS - exact)).astype(jnp.int32)
    return jnp.where(n < exact, n, jnp.minimum(large, REL_BUCKETS - 1))


def nsa_mixer(q, kc_tok, vc_tok, ks_tok, vs_tok, kw_tok, vw_tok, gates,
              cmp_pe_k, cmp_pe_v, cmp_wk1, cmp_wk2, cmp_wv1, cmp_wv2, rel_table):
    B, S = q.shape[0], q.shape[1]
    G, R, dh = NSA_KV_HEADS, NSA_HEADS // NSA_KV_HEADS, HEAD_DIM

    n_cmp = (S - CMP_BLK) // CMP_STRIDE + 1
    cmp_start = np.arange(n_cmp) * CMP_STRIDE
    tok_idx = cmp_start[:, None] + np.arange(CMP_BLK)[None, :]
    cmp_end = jnp.asarray(cmp_start + CMP_BLK - 1, jnp.int32)

    def compress(tok, pe, w1, w2):
        blk = tok[:, tok_idx] + pe[None, None, :, None, :]
        flat = blk.transpose(0, 1, 3, 2, 4).reshape(B, n_cmp, G, CMP_BLK * dh)
        return jax.nn.gelu(flat @ w1) @ w2

    kc = compress(kc_tok, cmp_pe_k, cmp_wk1, cmp_wk2)
    vc = compress(vc_tok, cmp_pe_v, cmp_wv1, cmp_wv2)

    n_slc = S // SEL_BLK
    n_sel = min(SEL_TOPN, n_slc)
    slc_start = np.arange(n_slc) * SEL_BLK
    overlap = (cmp_start[:, None] < slc_start[None, :] + SEL_BLK) & (cmp_start[:, None] + CMP_BLK > slc_start[None, :])
    cmp_to_slc = jnp.asarray(overlap, jnp.float32)
    ks_blk = ks_tok.reshape(B, n_slc, SEL_BLK, G, dh).transpose(0, 3, 1, 2, 4)
    vs_blk = vs_tok.reshape(B, n_slc, SEL_BLK, G, dh).transpose(0, 3, 1, 2, 4)

    kw_pad = jnp.pad(kw_tok, ((0, 0), (WINDOW, 0), (0, 0), (0, 0)))
    vw_pad = jnp.pad(vw_tok, ((0, 0), (WINDOW, 0), (0, 0), (0, 0)))

    b_ix = jnp.arange(B)[:, None, None]
    g_ix = jnp.arange(G)[None, :, None]
    rel_g = rel_table.reshape(REL_BUCKETS, G, R)
    blk_ids = jnp.arange(n_slc)
    scale = HEAD_DIM ** -0.5

    def head_bias(dist):
        return jnp.transpose(rel_table[rel_bucket(dist)], (2, 0, 1)).reshape(G, R, *dist.shape)

    def query_block(i):
        t0 = i * Q_BLK
        t = t0 + jnp.arange(Q_BLK)
        qb = lax.dynamic_slice_in_dim(q, t0, Q_BLK, axis=1).reshape(B, Q_BLK, G, R, dh) * scale
        gb = jax.nn.sigmoid(lax.dynamic_slice_in_dim(gates, t0, Q_BLK, axis=1).astype(jnp.float32))
        gb = gb.reshape(B, Q_BLK, G, R, 3)

        s = jnp.einsum('bqgrd,bngd->bgrqn', qb, kc) + head_bias(t[:, None] - cmp_end[None, :])
        p_cmp = masked_softmax(s, cmp_end[None, :] <= t[:, None])
        o_cmp = jnp.einsum('bgrqn,bngd->bqgrd', p_cmp, vc)

        imp = jnp.einsum('bgrqn,nj->bgqj', p_cmp, cmp_to_slc)
        cur = t // SEL_BLK
        forced = (blk_ids[None, :] == 0) | (blk_ids[None, :] == cur[:, None]) | (blk_ids[None, :] == cur[:, None] - 1)
        allowed = blk_ids[None, :] * SEL_BLK <= t[:, None]
        imp = jnp.where(forced, FORCE_SCORE, jnp.where(allowed, imp, -jnp.inf))
        _, sel = lax.top_k(imp, n_sel)
        L = n_sel * SEL_BLK
        sel_flat = sel.reshape(B, G, Q_BLK * n_sel)
        ks = ks_blk[b_ix, g_ix, sel_flat].reshape(B, G, Q_BLK, L, dh)
        vs = vs_blk[b_ix, g_ix, sel_flat].reshape(B, G, Q_BLK, L, dh)
        kpos = (sel[..., None] * SEL_BLK + jnp.arange(SEL_BLK)).reshape(B, G, Q_BLK, L)
        dist = t[:, None] - kpos
        bias = rel_g[rel_bucket(dist), jnp.arange(G)[:, None, None]]
        s = jnp.einsum('bqgrd,bgqld->bgrql', qb, ks) + bias.transpose(0, 1, 4, 2, 3)
        p = masked_softmax(s, (dist >= 0)[:, :, None])
        o_slc = jnp.einsum('bgrql,bgqld->bqgrd', p, vs)

        kw = lax.dynamic_slice_in_dim(kw_pad, t0, Q_BLK + WINDOW, axis=1)
        vw = lax.dynamic_slice_in_dim(vw_pad, t0, Q_BLK + WINDOW, axis=1)
        kpos_w = t0 - WINDOW + jnp.arange(Q_BLK + WINDOW)
        dist_w = t[:, None] - kpos_w[None, :]
        mask_w = (dist_w >= 0) & (dist_w < WINDOW) & (kpos_w[None, :] >= 0)
        s = jnp.einsum('bqgrd,blgd->bgrql', qb, kw) + head_bias(dist_w)
        p = masked_softmax(s, mask_w)
        o_win = jnp.einsum('bgrql,blgd->bqgrd', p, vw)

        o = gb[..., 0:1] * o_cmp + gb[..., 1:2] * o_slc + gb[..., 2:3] * o_win
        return o.reshape(B, Q_BLK, NSA_WIDTH)

    out = lax.map(query_block, jnp.arange(S // Q_BLK))
    return out.transpose(1, 0, 2, 3).reshape(B, S, NSA_WIDTH)


def short_conv_mixer(h, b_gate, c_gate, conv_w):
    z = c_gate * h
    y = lax.conv_general_dilated(z, conv_w.astype(z.dtype), window_strides=(1,),
                                 padding=((CONV_K - 1, 0),),
                                 dimension_numbers=('NWC', 'WIO', 'NWC'),
                                 feature_group_count=CONV_WIDTH)
    return b_gate * y


def peer_ffn(xn, w_q, sub_keys, expert_u, expert_v):
    B, S, D = xn.shape
    q = (xn @ w_q).reshape(B, S, PEER_HEADS, 2, PEER_QDIM // 2)
    s = jnp.einsum('bshpd,hpkd->bshpk', q, sub_keys)
    v_half, i_half = lax.top_k(s, PEER_TOPK)
    cand = v_half[..., 0, :, None] + v_half[..., 1, None, :]
    top_s, top_c = lax.top_k(cand.reshape(B, S, PEER_HEADS, PEER_TOPK * PEER_TOPK), PEER_TOPK)
    i1 = jnp.take_along_axis(i_half[..., 0, :], top_c // PEER_TOPK, axis=-1)
    i2 = jnp.take_along_axis(i_half[..., 1, :], top_c % PEER_TOPK, axis=-1)
    experts = i1 * PEER_NKEYS + i2
    g = jax.nn.softmax(top_s.astype(jnp.float32), axis=-1)
    E = PEER_HEADS * PEER_TOPK
    n_chunk = (B * S) // PEER_CHUNK
    xs = xn.reshape(n_chunk, PEER_CHUNK, D)
    es = experts.reshape(n_chunk, PEER_CHUNK, E)
    gs = g.reshape(n_chunk, PEER_CHUNK, E)

    def chunk(args):
        xc, ec, gc = args
        u = expert_u[ec]
        act = jax.nn.gelu(jnp.einsum('cd,ced->ce', xc, u).astype(jnp.float32))
        return jnp.einsum('ce,ced->cd', gc * act, expert_v[ec])

    return lax.map(chunk, (xs, es, gs)).reshape(B, S, D)


def setup_inputs(seed: int = 0) -> dict:
    key = jax.random.key(seed)
    ks = jax.random.split(key, 24)
    L, D, dh = DEPTH, D_MODEL, HEAD_DIM

    def nrm(k, shape, s):
        return jax.random.normal(k, shape, jnp.float32) * s

    def gain(k, shape):
        return 1.0 + 0.05 * jax.random.normal(k, shape, jnp.float32)

    return {
        'x': nrm(ks[0], (BATCH, SEQ, D), 1.0),
        'c': nrm(ks[1], (BATCH, D), 1.0),
        'ln_mix_g': gain(ks[2], (L, D)),
        'ln_ffn_g': gain(ks[3], (L, D)),
        'w_mod': nrm(ks[4], (L, D, 6 * D), 0.5 * D ** -0.5),
        'b_mod': nrm(ks[5], (L, 6 * D), 0.02),
        'w_in': nrm(ks[6], (L, D, IN_COLS), D ** -0.5),
        'cmp_pe_k': nrm(ks[7], (L, CMP_BLK, dh), 0.1),
        'cmp_pe_v': nrm(ks[8], (L, CMP_BLK, dh), 0.1),
        'cmp_wk1': nrm(ks[9], (L, CMP_BLK * dh, dh), (CMP_BLK * dh) ** -0.5),
        'cmp_wk2': nrm(ks[10], (L, dh, dh), dh ** -0.5),
        'cmp_wv1': nrm(ks[11], (L, CMP_BLK * dh, dh), (CMP_BLK * dh) ** -0.5),
        'cmp_wv2': nrm(ks[12], (L, dh, dh), dh ** -0.5),
        'conv_w': nrm(ks[13], (L, CONV_K, 1, CONV_WIDTH), CONV_K ** -0.5),
        'norm_attn_g': gain(ks[14], (L, NSA_WIDTH)),
        'norm_conv_g': gain(ks[15], (L, CONV_WIDTH)),
        'w_out': nrm(ks[16], (L, D_MIX, D), D_MIX ** -0.5),
        'peer_wq': nrm(ks[17], (L, D, PEER_HEADS * PEER_QDIM), D ** -0.5),
        'peer_keys': nrm(ks[18], (L, PEER_HEADS, 2, PEER_NKEYS, PEER_QDIM // 2), (PEER_QDIM // 2) ** -0.5),
        'peer_u': nrm(ks[19], (L, PEER_EXPERTS, D), D ** -0.5),
        'peer_v': nrm(ks[20], (L, PEER_EXPERTS, D), PEER_HEADS ** -0.5),
        'rel_table': nrm(ks[21], (REL_BUCKETS, NSA_HEADS), 0.5),
        'ln_final_g': gain(ks[22], (D,)),
    }


def reference(x, c, ln_mix_g, ln_ffn_g, w_mod, b_mod, w_in, cmp_pe_k, cmp_pe_v,
              cmp_wk1, cmp_wk2, cmp_wv1, cmp_wv2, conv_w, norm_attn_g, norm_conv_g,
              w_out, peer_wq, peer_keys, peer_u, peer_v, rel_table, ln_final_g):
    B, S = x.shape[0], x.shape[1]
    c_act = jax.nn.silu(c)
    for l in range(DEPTH):
        mod = (c_act @ w_mod[l] + b_mod[l])[:, None, :]
        sh1, sc1, gt1, sh2, sc2, gt2 = jnp.split(mod, 6, axis=-1)

        h = rmsnorm(x, ln_mix_g[l]) * (1 + sc1) + sh1
        proj = h @ w_in[l]
        q, kc, vc, ksl, vsl, kw, vw, gts, cb, cc, ch = jnp.split(proj, SPLIT_POINTS, axis=-1)
        kv = lambda a: a.reshape(B, S, NSA_KV_HEADS, HEAD_DIM)
        o_attn = nsa_mixer(q.reshape(B, S, NSA_HEADS, HEAD_DIM), kv(kc), kv(vc), kv(ksl), kv(vsl),
                           kv(kw), kv(vw), gts.reshape(B, S, NSA_HEADS, 3),
                           cmp_pe_k[l], cmp_pe_v[l], cmp_wk1[l], cmp_wk2[l], cmp_wv1[l], cmp_wv2[l],
                           rel_table)
        o_conv = short_conv_mixer(ch, cb, cc, conv_w[l])
        mixed = jnp.concatenate([rmsnorm(o_attn, norm_attn_g[l]), rmsnorm(o_conv, norm_conv_g[l])], axis=-1)
        x = x + gt1 * (mixed @ w_out[l])

        h2 = rmsnorm(x, ln_ffn_g[l]) * (1 + sc2) + sh2
        x = x + gt2 * peer_ffn(h2, peer_wq[l], peer_keys[l], peer_u[l], peer_v[l])
    return rmsnorm(x, ln_final_g)
```

```python
import math
import numpy as np
from contextlib import ExitStack
import concourse.bass as bass
import concourse.mybir as mybir
from concourse.bass_utils import run_bass_kernel_spmd

F32 = mybir.dt.float32
BF16 = mybir.dt.bfloat16
U32 = mybir.dt.uint32
AF = mybir.ActivationFunctionType
ALU = mybir.AluOpType
AX = mybir.AxisListType

S = 2048
D = 1024
NT = 16
EPS = 1e-6
NEG = -30000.0
LENG = "dve"
import os
PDBG = os.environ.get("PDBG", "")
SEM_CHUNK = 30000


class _Stop(Exception):
    pass


class Tok:
    __slots__ = ("writers", "readers")
    registry = []

    def __init__(self):
        self.writers = []
        self.readers = []
        Tok.registry.append(self)


class Op:
    __slots__ = ("eng", "sem", "val", "is_dma")


class Prog:
    ENGS = ("pe", "dve", "act", "pool", "sp")

    def __init__(self, nc, stack, n_dma_sems=8):
        self.nc = nc
        self.stack = stack
        self.eobj = {"pe": nc.tensor, "dve": nc.vector, "act": nc.scalar,
                     "pool": nc.gpsimd, "sp": nc.sync}
        self.esems = {e: [] for e in self.ENGS}
        self.ecount = {e: 0 for e in self.ENGS}
        self.waited = {e: {} for e in self.ENGS}
        self.dma_sems = {}
        self.dma_rr = {}
        self.n_dma_sems = n_dma_sems
        self.nwaits = 0

    def _esem(self, e, chunk):
        lst = self.esems[e]
        while len(lst) <= chunk:
            lst.append(self.stack.enter_context(self.nc.semaphore(f"s_{e}_{len(lst)}")))
        return lst[chunk]

    def _wait(self, e, sem, val):
        w = self.waited[e]
        k = id(sem)
        if w.get(k, 0) >= val:
            return
        w[k] = val
        self.eobj[e].wait_ge(sem, val)
        self.nwaits += 1

    def _deps(self, e, is_dma, reads, writes):
        for t in reads:
            for p in t.writers:
                if (not p.is_dma) and (not is_dma) and p.eng == e and e == "pe":
                    continue
                self._wait(e, p.sem, p.val)
        for t in writes:
            for p in t.writers + t.readers:
                if (not p.is_dma) and (not is_dma) and p.eng == e and e == "pe":
                    continue
                self._wait(e, p.sem, p.val)

    def _record(self, op, reads, writes):
        for t in reads:
            if not op.is_dma:
                t.readers = [r for r in t.readers if r.is_dma or r.eng != op.eng]
            t.readers.append(op)
        for t in writes:
            if t.readers:
                t.writers = [op]
                t.readers = []
            else:
                if not op.is_dma:
                    t.writers = [w for w in t.writers if w.is_dma or w.eng != op.eng]
                t.writers.append(op)

    def op(self, e, fn, reads=(), writes=()):
        self._deps(e, False, reads, writes)
        n = self.ecount[e]
        sem = self._esem(e, n // SEM_CHUNK)
        val = (n % SEM_CHUNK) + 1
        fn(self.eobj[e]).then_inc(sem, 1)
        self.ecount[e] = n + 1
        o = Op()
        o.eng, o.sem, o.val, o.is_dma = e, sem, val, False
        self._record(o, reads, writes)
        return o

    def dma(self, q, out, in_, reads=(), writes=(), **kw):
        if q not in self.dma_sems:
            self.dma_sems[q] = [[self.stack.enter_context(self.nc.semaphore(f"d_{q}_{i}")), 0]
                                for i in range(self.n_dma_sems)]
            self.dma_rr[q] = 0
        self._deps(q, True, reads, writes)
        i = self.dma_rr[q]
        self.dma_rr[q] = (i + 1) % self.n_dma_sems
        ent = self.dma_sems[q][i]
        sem, cur = ent
        if cur > 0:
            self._wait(q, sem, cur)
        val = cur + 16
        ent[1] = val
        self.eobj[q].dma_start(out=out, in_=in_, **kw).then_inc(sem, 16)
        o = Op()
        o.eng, o.sem, o.val, o.is_dma = q, sem, val, True
        self._record(o, reads, writes)
        return o

    def barrier(self):
        for e in self.ENGS:
            for f in self.ENGS:
                n = self.ecount[f]
                if f == e or n == 0:
                    continue
                self._wait(e, self.esems[f][(n - 1) // SEM_CHUNK], (n - 1) % SEM_CHUNK + 1)
            for q, lst in self.dma_sems.items():
                for sem, cur in lst:
                    if cur > 0:
                        self._wait(e, sem, cur)

    def wait_all(self, e, toks):
        for t in toks:
            for p in t.writers + t.readers:
                self._wait(e, p.sem, p.val)


def _rel_bucket_np(n):
    n = np.maximum(n, 0)
    exact = 16
    lr = np.log(np.maximum(n, 1).astype(np.float32) / np.float32(exact)) / np.float32(math.log(128 / exact))
    large = exact + (lr * np.float32(32 - exact)).astype(np.int32)
    return np.where(n < exact, n, np.minimum(large, 31))


def _constants():
    c = {}
    npr = np.arange(4096)
    n = npr - 2048
    bk = _rel_bucket_np(n)
    oh = np.zeros((33, 2, 4096), np.float32)
    okw = (n >= 0) & (n < 512)
    oks = (n >= 0)
    oh[bk[okw], 0, npr[okw]] = 1.0
    oh[32, 0, npr[~okw]] = 1.0
    oh[bk[oks], 1, npr[oks]] = 1.0
    oh[32, 1, npr[~oks]] = 1.0
    c["oh"] = oh
    e32 = (np.arange(2048)[None, :] // 64 == np.arange(32)[:, None]).astype(np.float32)
    e128 = np.zeros((128, 2048), np.float32)
    e128[0:32] = e32
    e128[64:96] = e32
    c["Esel"] = e128
    cs = np.arange(127) * 16
    ss = np.arange(32) * 64
    c["Cov"] = ((cs[:, None] < ss[None, :] + 64) & (cs[:, None] + 32 > ss[None, :])).astype(np.float32)
    t = np.arange(2048)
    cur = t // 64
    j = np.arange(32)
    forced = (j[None, :] == 0) | (j[None, :] == cur[:, None]) | (j[None, :] == cur[:, None] - 1)
    allowed = j[None, :] * 64 <= t[:, None]
    mul = (allowed & ~forced).astype(np.float32)
    add = np.where(forced, 1e4, np.where(allowed, 0.0, -1e30)).astype(np.float32)
    c["selmul"] = np.ascontiguousarray(mul.reshape(16, 128, 32).transpose(1, 0, 2))
    c["seladd"] = np.ascontiguousarray(add.reshape(16, 128, 32).transpose(1, 0, 2))
    c["identf"] = np.eye(128, dtype=np.float32)
    c["ones"] = np.ones((128, 128), np.float32)
    c["iota"] = np.tile(np.arange(128, dtype=np.float32)[None, :], (128, 1))
    sel = np.zeros((8, 128), np.float32)
    for h in range(8):
        sel[h, h * 16:(h + 1) * 16] = 1.0
    c["selh"] = sel
    return c


def _blockdiag2(w):
    out = np.zeros(w.shape[:-2] + (128, 128), np.float32)
    out[..., :64, :64] = w
    out[..., 64:, 64:] = w
    return out


def _prep(inp):
    sh = {}
    f = lambda a: np.ascontiguousarray(np.asarray(a, np.float32))
    sh["w_mod"] = f(inp["w_mod"][0])
    sh["bmodT"] = f(inp["b_mod"][0].reshape(48, 128).T)
    bm = inp["b_mod"][0]
    sh["bmod_bc"] = f(np.broadcast_to(np.stack([bm[2048:3072], bm[5120:6144]])[None], (128, 2, 1024)))
    sh["gmix"] = f(inp["ln_mix_g"][0].reshape(8, 128).T)
    sh["gffn"] = f(inp["ln_ffn_g"][0].reshape(8, 128).T)
    sh["gfin_bc"] = f(np.broadcast_to(inp["ln_final_g"][None, :], (128, 1024)))
    w_in = np.asarray(inp["w_in"][0], np.float32)
    sp = np.cumsum([0, 512, 128, 128, 128, 128, 128, 128, 24, 512, 512, 512])
    q, kc, vc, ks, vs, kw, vw, gt, cb, cc, ch = [w_in[:, sp[i]:sp[i + 1]] for i in range(11)]
    qh = q.reshape(1024, 8, 64)
    qperm = np.concatenate([np.concatenate([qh[:, j], qh[:, 4 + j]], axis=1) for j in range(4)], axis=1)
    cols = [qperm, kc, vc, ks, kw]
    for c4 in range(4):
        cols += [cb[:, c4 * 128:(c4 + 1) * 128], cc[:, c4 * 128:(c4 + 1) * 128], ch[:, c4 * 128:(c4 + 1) * 128]]
    cols += [vs, vw, gt]
    sh["w_in"] = f(np.concatenate(cols, axis=1))
    for nm, w1, w2, pe in (("k", "cmp_wk1", "cmp_wk2", "cmp_pe_k"), ("v", "cmp_wv1", "cmp_wv2", "cmp_pe_v")):
        a = np.asarray(inp[w1][0], np.float32).reshape(32, 64, 64)
        sh["bd1" + nm] = f(_blockdiag2(a).transpose(1, 0, 2))
        sh["bd2" + nm] = f(_blockdiag2(np.asarray(inp[w2][0], np.float32)))
        p = np.asarray(inp[pe][0], np.float32).T
        sh["pe2" + nm] = f(np.concatenate([p, p], axis=0))
    sh["convw"] = f(inp["conv_w"][0][:, 0, :].reshape(3, 4, 128).transpose(2, 1, 0))
    sh["gattn_bc"] = f(np.broadcast_to(inp["norm_attn_g"][0][None, :], (128, 512)))
    sh["gconv"] = f(inp["norm_conv_g"][0].reshape(4, 128).T)
    sh["w_out"] = f(inp["w_out"][0])
    sh["peer_wq"] = f(inp["peer_wq"][0])
    pk = np.asarray(inp["peer_keys"][0], np.float32)
    kb = np.zeros((128, 8, 256), np.float32)
    for h in range(8):
        for p in range(2):
            kb[p * 64:(p + 1) * 64, h, p * 128:(p + 1) * 128] = pk[h, p].T
    sh["peer_kb"] = kb
    u = np.asarray(inp["peer_u"][0], np.float32)
    sh["peer_ut"] = f(u.reshape(128, 128, 8, 128).transpose(0, 3, 2, 1).reshape(128 * 128, 1024))
    sh["peer_v"] = f(inp["peer_v"][0])
    rt = np.zeros((33, 8), np.float32)
    rt[:32] = inp["rel_table"]
    rt[32] = NEG
    sh["relt"] = rt
    sh.update(_constants())
    return sh


def build_nc(nseq=2, dbg=False, stop_after=99):
    nc = bass.Bass("TRN2", target_bir_lowering=False)
    Tok.registry = []
    T = nseq * S
    NTT = nseq * NT

    def din(name, shape, dt=F32):
        return nc.dram_tensor(name, list(shape), dt, kind="ExternalInput").ap()

    def dscr(name, shape, dt=F32):
        return nc.dram_tensor(name, list(shape), dt, kind="Internal").ap()

    def dout(name, shape, dt=F32):
        return nc.dram_tensor(name, list(shape), dt, kind="ExternalOutput").ap()

    x_d = din("x", [T, D])
    cT_d = din("cT", [128, 8, nseq])
    w_mod_d = din("w_mod", [1024, 6144])
    bmodT_d = din("bmodT", [128, 48])
    bmod_bc_d = din("bmod_bc", [128, 2, 1024])
    gmix_d = din("gmix", [128, 8])
    gffn_d = din("gffn", [128, 8])
    gfin_d = din("gfin_bc", [128, 1024])
    w_in_d = din("w_in", [1024, 2840])
    bd1_d = {n: din("bd1" + n, [128, 32, 128]) for n in "kv"}
    bd2_d = {n: din("bd2" + n, [128, 128]) for n in "kv"}
    pe2_d = {n: din("pe2" + n, [128, 32]) for n in "kv"}
    convw_d = din("convw", [128, 4, 3])
    gattn_d = din("gattn_bc", [128, 512])
    gconv_d = din("gconv", [128, 4])
    w_out_d = din("w_out", [1024, 1024])
    wq_d = din("peer_wq", [1024, 1024])
    kb_d = din("peer_kb", [128, 8, 256])
    ut_d = din("peer_ut", [128 * 128, 1024])
    pv_d = din("peer_v", [16384, 1024])
    relt_d = din("relt", [33, 8])
    oh_d = din("oh", [33, 2, 4096])
    esel_d = din("Esel", [128, 2048])
    cov_d = din("Cov", [127, 32])
    selmul_d = din("selmul", [128, 16, 32])
    seladd_d = din("seladd", [128, 16, 32])
    identf_d = din("identf", [128, 128])
    ones_d = din("ones", [128, 128])
    iota_d = din("iota", [128, 128])
    selh_d = din("selh", [8, 128])
    out_d = dout("out", [T, D])

    tb_d = dscr("tb_s", [2, 8, 4096])
    LW = 1024
    fw_d = dscr("fw_s", [2, 8, 128 * (LW + 1)])
    LC = 4096
    fc_d = dscr("fc_s", [8, 127 * (LC + 16)])
    gt_d = dscr("gt_s", [2, nseq, 128, 1024])
    if dbg:
        x1_d = dout("x1_s", [T, D])
        h2T_d = dout("h2T_s", [128, 8, T], BF16)
    else:
        x1_d = dscr("x1_s", [T, D])
        h2T_d = dscr("h2T_s", [128, 8, T], BF16)
    t_x1d = [Tok() for _ in range(NTT)]
    t_h2d = [Tok() for _ in range(NTT)]

    dbg_out = {}
    uniq = [0]

    with ExitStack() as st0:
        P = Prog(nc, st0)
        scopes = [st0]

        def sb(name, shape, dt=F32):
            uniq[0] += 1
            return scopes[-1].enter_context(nc.sbuf_tensor(f"sb{uniq[0]}_{name}", list(shape), dt))

        class Scope:
            def __enter__(self):
                self.st = ExitStack()
                scopes.append(self.st)
                return self

            def __exit__(self, *a):
                if a[0] is None:
                    P.barrier()
                scopes.pop()
                self.st.close()
                return False

        psg = [st0.enter_context(nc.psum_tensor(f"psum{i}", [128, 512], F32)) for i in range(3)]
        psO = st0.enter_context(nc.psum_tensor("psumO", [128, 4, 512], F32))
        ps = [psg[0][:, :], psg[1][:, :], psg[2][:, :]] + [psO[:, i, :] for i in range(4)]
        tps = [Tok() for _ in range(7)]
        psb = st0.enter_context(nc.psum_tensor("psumb", [128, 1024], BF16))
        tpsb = Tok()

        def load_const(name, src, shape, dt=F32, q="sp"):
            t = sb(name, shape, dt)
            k = Tok()
            P.dma(q, t[:], src, writes=[k])
            return t, k

        def dbgdump(name, shape, dt, src_ap, toks):
            if not dbg:
                return
            dbg_out[name] = dout("dbg_" + name, shape, dt)
            P.dma("sp", dbg_out[name], src_ap, reads=toks)

        identb, t_identb = load_const("identb", identf_d, [128, 128], BF16, "pool")
        onesb, t_onesb = load_const("onesb", ones_d, [128, 128], BF16, "pool")
        gmix, t_gmix = load_const("gmix", gmix_d, [128, 8])
        gffn, t_gffn = load_const("gffn", gffn_d, [128, 8])
        bmodT, t_bmodT = load_const("bmodT", bmodT_d, [128, 48])
        convw, t_convw = load_const("convw", convw_d, [128, 4, 3])
        gconv, t_gconv = load_const("gconv", gconv_d, [128, 4])
        relt, t_relt = load_const("relt", relt_d, [33, 8])
        modT = sb("modT", [128, 48, nseq]); t_modT = Tok()
        gs1 = sb("gs1", [128, 8, nseq]); gs2 = sb("gs2", [128, 8, nseq]); t_gs = Tok()
        t_gtd = Tok()
        t_fw = Tok(); t_fc = Tok()

        with Scope():
            cT = sb("cT", [128, 8, nseq]); t_cT = Tok()
            P.dma("sp", cT[:], cT_d, writes=[t_cT])
            c_act = sb("c_act", [128, 8, nseq]); t_cact = Tok()
            P.op("act", lambda e: e.activation(out=c_act[:], in_=cT[:], func=AF.Silu), reads=[t_cT], writes=[t_cact])
            c_rep = sb("c_rep", [128, 8, nseq, 128]); t_crep = Tok()
            P.op("dve", lambda e: e.tensor_copy(out=c_rep[:], in_=c_act[:].unsqueeze(3).broadcast_to([128, 8, nseq, 128])),
                 reads=[t_cact], writes=[t_crep])
            bmod_bc = sb("bmod_bc", [128, 2, 1024]); t_bmbc = Tok()
            P.dma("sp", bmod_bc[:], bmod_bc_d, writes=[t_bmbc])
            gstage = [sb(f"gstage{i}", [128, nseq, 128]) for i in range(2)]; t_gst = [Tok(), Tok()]
            wm_view = w_mod_d.rearrange("(k p) n -> p k n", p=128)
            wmp = [sb(f"wm{i}", [128, 8, 128]) for i in range(3)]
            t_wmp = [Tok() for _ in range(3)]
            for j in range(48):
                wt, tw = wmp[j % 3], t_wmp[j % 3]
                P.dma("sp", wt[:], wm_view[:, :, j * 128:(j + 1) * 128], writes=[tw])
                for k in range(8):
                    P.op("pe", lambda e, k=k, wt=wt: e.matmul(ps[0][:, j * nseq:(j + 1) * nseq], lhsT=wt[:, k, :],
                                                              rhs=c_act[:, k, :], start=(k == 0), stop=(k == 7)),
                         reads=[tw, t_cact], writes=[tps[0]])
                which = {2: 0, 5: 1}.get(j // 8)
                if which is not None:
                    jj = j % 8
                    bi = 1 + (j % 2)
                    for b in range(nseq):
                        for k in range(8):
                            P.op("pe", lambda e, k=k, b=b, wt=wt, bi=bi: e.matmul(
                                ps[bi][:, b * 128:(b + 1) * 128], lhsT=c_rep[:, k, b, :], rhs=wt[:, k, :],
                                start=(k == 0), stop=(k == 7)), reads=[tw, t_crep], writes=[tps[bi]])
                    gsb, tg = gstage[j % 2], t_gst[j % 2]
                    P.op("dve", lambda e, bi=bi, which=which, jj=jj, gsb=gsb: e.tensor_tensor(
                        out=gsb[:],
                        in0=ps[bi][:, 0:nseq * 128].rearrange("p (b n) -> p b n", b=nseq),
                        in1=bmod_bc[:, which, jj * 128:(jj + 1) * 128].unsqueeze(1).broadcast_to([128, nseq, 128]),
                        op=ALU.add), reads=[tps[bi], t_bmbc], writes=[tg])
                    P.dma("sp", gt_d[which].rearrange("b p n -> p b n")[:, :, jj * 128:(jj + 1) * 128], gsb[:],
                          reads=[tg], writes=[t_gtd])
            P.op("dve", lambda e: e.tensor_tensor(
                out=modT[:], in0=ps[0][:, 0:48 * nseq].rearrange("p (j b) -> p j b", b=nseq),
                in1=bmodT[:].unsqueeze(2).broadcast_to([128, 48, nseq]), op=ALU.add),
                reads=[tps[0], t_bmodT], writes=[t_modT])
            P.op("dve", lambda e: e.scalar_tensor_tensor(
                out=gs1[:], in0=modT[:, 8:16, :], scalar=1.0, in1=gmix[:].unsqueeze(2).broadcast_to([128, 8, nseq]),
                op0=ALU.add, op1=ALU.mult), reads=[t_modT, t_gmix], writes=[t_gs])
            P.op("dve", lambda e: e.scalar_tensor_tensor(
                out=gs2[:], in0=modT[:, 32:40, :], scalar=1.0, in1=gffn[:].unsqueeze(2).broadcast_to([128, 8, nseq]),
                op0=ALU.add, op1=ALU.mult), reads=[t_modT, t_gffn], writes=[t_gs])
            dbgdump("modT", [128, 48, nseq], F32, modT[:], [t_modT])

            ohp = [sb(f"ohp{i}", [33, 512]) for i in range(2)]
            t_ohp = [Tok(), Tok()]
            tbs = [sb(f"tbs{i}", [8, 512]) for i in range(2)]
            t_tbs = [Tok(), Tok()]
            t_tbd = Tok()
            for kind in range(2):
                for pc in range(8):
                    i = (kind * 8 + pc) % 2
                    P.dma("sp", ohp[i][:], oh_d[:, kind, pc * 512:(pc + 1) * 512], writes=[t_ohp[i]])
                    bi = 3 + i
                    P.op("pe", lambda e, i=i, bi=bi: e.matmul(ps[bi][0:8, :], lhsT=relt[:, :], rhs=ohp[i][:, :],
                                                               start=True, stop=True),
                         reads=[t_relt, t_ohp[i]], writes=[tps[bi]])
                    P.op("act", lambda e, i=i, bi=bi: e.copy(out=tbs[i][:], in_=ps[bi][0:8, :]),
                         reads=[tps[bi]], writes=[t_tbs[i]])
                    P.dma("sp", tb_d[kind, :, pc * 512:(pc + 1) * 512], tbs[i][:], reads=[t_tbs[i]], writes=[t_tbd])
            for kind in range(2):
                for h in range(8):
                    src = bass.AP(tb_d.tensor, (kind * 8 + h) * 4096 + (2048 - 128), [[0, 128], [1, LW]])
                    dst = bass.AP(fw_d.tensor, (kind * 8 + h) * 128 * (LW + 1), [[LW + 1, 128], [1, LW]])
                    P.dma("sp", dst, src, reads=[t_tbd], writes=[t_fw])
            for h in range(8):
                src = bass.AP(tb_d.tensor, (8 + h) * 4096, [[0, 127], [1, LC]])
                dst = bass.AP(fc_d.tensor, h * 127 * (LC + 16), [[LC + 16, 127], [1, LC]])
                P.dma("sp", dst, src, reads=[t_tbd], writes=[t_fc])

        with Scope():
            qT = sb("qT", [128, 4, S], BF16); t_qT = [Tok() for _ in range(4)]
            kT = {n: sb(n + "T", [128, S], BF16) for n in ("kc", "vc", "ks", "kw")}
            t_kT = {n: [Tok() for _ in range(4)] for n in kT}
            vs_aug = sb("vs_aug", [128, NT, 2, 65], BF16); vw_aug = sb("vw_aug", [128, NT, 2, 65], BF16)
            t_va = [Tok() for _ in range(NT)]
            gat = sb("gat", [128, NT, 24]); t_gat = [Tok() for _ in range(NT)]
            mixT = sb("mixT", [128, 8, S], BF16)
            t_mixc = [Tok() for _ in range(4)]
            t_mixa = [Tok() for _ in range(NT)]
            t_ones = Tok()
            P.op("pool", lambda e: e.memset(vs_aug[:, :, :, 64:65], 1.0), writes=[t_ones])
            P.op("pool", lambda e: e.memset(vw_aug[:, :, :, 64:65], 1.0), writes=[t_ones])
            ssq = sb("ssq", [128, NTT]); rstd = sb("rstd", [128, NTT]); t_st = [Tok() for _ in range(NTT)]
            ssq2 = sb("ssq2", [128, NTT]); rstd2 = sb("rstd2", [128, NTT]); t_st2 = [Tok() for _ in range(NTT)]
            ssqa = sb("ssqa", [128, NTT]); rstda = sb("rstda", [128, NTT]); t_sta = [Tok() for _ in range(NTT)]

            def rms_stats(src_ap, t_src, junk_, t_junk_, ssq_, rstd_, tk, col, width):
                P.op("act", lambda e: e.activation(out=junk_, in_=src_ap, func=AF.Square, accum_out=ssq_[:, col:col + 1]),
                     reads=[t_src], writes=[t_junk_, tk])
                P.op("dve", lambda e: e.tensor_scalar(out=rstd_[:, col:col + 1], in0=ssq_[:, col:col + 1], scalar1=1.0 / width,
                                                      scalar2=EPS, op0=ALU.mult, op1=ALU.add), reads=[tk], writes=[tk])
                P.op("act", lambda e: e.activation(out=rstd_[:, col:col + 1], in_=rstd_[:, col:col + 1], func=AF.Sqrt),
                     reads=[tk], writes=[tk])
                P.op("dve", lambda e: e.reciprocal(out=rstd_[:, col:col + 1], in_=rstd_[:, col:col + 1]),
                     reads=[tk], writes=[tk])

            def norm_transpose(xt, t_xt, gi, s, ssq_, rstd_, t_st_, gs_, sh_lo, dst, t_dst, col0, junk, t_junk, xs, t_xs):
                rms_stats(xt[:], t_xt, junk[:], t_junk, ssq_, rstd_, t_st_[gi], gi, D)
                P.op("dve", lambda e: e.tensor_scalar(out=xs[:], in0=xt[:], scalar1=rstd_[:, gi:gi + 1], scalar2=None,
                                                      op0=ALU.mult), reads=[t_xt, t_st_[gi]], writes=[t_xs])
                for c in range(8):
                    P.op("pe", lambda e, c=c: e.transpose(out=psb[:, c * 128:(c + 1) * 128], in_=xs[:, c * 128:(c + 1) * 128],
                                                          identity=identb[:]), reads=[t_xs, t_identb], writes=[tpsb])
                for c in range(8):
                    o_ap = dst[:, c, col0:col0 + 128]
                    i_ap = psb[:, c * 128:(c + 1) * 128]
                    sc_ap = gs_[:, c, s:s + 1]
                    bi_ap = modT[:, sh_lo + c, s:s + 1]
                    if gi % 2 == 0:
                        P.op("dve", lambda e, o_ap=o_ap, i_ap=i_ap, sc_ap=sc_ap, bi_ap=bi_ap: e.tensor_scalar(
                            out=o_ap, in0=i_ap, scalar1=sc_ap, scalar2=bi_ap, op0=ALU.mult, op1=ALU.add),
                            reads=[tpsb, t_gs, t_modT], writes=[t_dst])
                    else:
                        P.op("act", lambda e, o_ap=o_ap, i_ap=i_ap, sc_ap=sc_ap, bi_ap=bi_ap: e.activation(
                            out=o_ap, in_=i_ap, func=AF.Identity, scale=sc_ap, bias=bi_ap),
                            reads=[tpsb, t_gs, t_modT], writes=[t_dst])

            def evac(i, out_ap, in_ap, reads, writes):
                if i % 2:
                    P.op("act", lambda e: e.copy(out=out_ap, in_=in_ap), reads=reads, writes=writes)
                else:
                    P.op("dve", lambda e: e.tensor_copy(out=out_ap, in_=in_ap), reads=reads, writes=writes)

            for s in range(nseq):
                with Scope():
                    win = sb("win", [128, 8, 2840], BF16); t_win = Tok()
                    wiv = w_in_d.rearrange("(k p) n -> p k n", p=128)
                    for k in range(8):
                        P.dma("pool", win[:, k, :], wiv[:, k, :], writes=[t_win])
                    xpool = [sb(f"xt{i}", [128, 1024]) for i in range(2)]; t_xp = [Tok(), Tok()]
                    junk = sb("junk", [128, 1024], BF16); t_junk = Tok()
                    xs = sb("xs", [128, 1024], BF16); t_xs = Tok()
                    hTg = [sb(f"hTg{i}", [128, 8, 512], BF16) for i in range(2)]; t_hTg = [Tok(), Tok()]
                    cbs = sb("cbs", [128, 512]); ccs = sb("ccs", [128, 512]); t_cbs = Tok(); t_ccs = Tok()
                    zb = sb("zb", [128, 4, 514]); t_zb = [Tok() for _ in range(4)]
                    yb = sb("yb", [128, 512]); t_yb = Tok()
                    oc = sb("oc", [128, 4, 512]); t_oc = [Tok() for _ in range(4)]
                    osq = sb("osq", [128, 4, 512], BF16); t_osq = [Tok() for _ in range(4)]
                    rbc = sb("rbc", [128, 512]); t_rbc = Tok()
                    for grp in range(4):
                        hT, t_hT = hTg[grp % 2], t_hTg[grp % 2]
                        s0 = grp * 512
                        for tl in range(4):
                            ti = grp * 4 + tl
                            gi = s * NT + ti
                            xt, t_xt = xpool[gi % 2], t_xp[gi % 2]
                            P.dma("sp", xt[:], x_d[gi * 128:(gi + 1) * 128, :], writes=[t_xt])
                            norm_transpose(xt, t_xt, gi, s, ssq, rstd, t_st, gs1, 0, hT, t_hT, tl * 128, junk, t_junk, xs, t_xs)
                        if s == 0 and grp == 0:
                            dbgdump("hT0", [128, 8, 512], BF16, hT[:], [t_hT])

                        def proj_chunk(cc, bi):
                            for k in range(8):
                                P.op("pe", lambda e, k=k: e.matmul(ps[bi][:, :], lhsT=win[:, k, cc * 128:(cc + 1) * 128],
                                                                   rhs=hT[:, k, :], start=(k == 0), stop=(k == 7)),
                                     reads=[t_win, t_hT], writes=[tps[bi]])

                        nb = 0
                        for cc in range(4):
                            bi = nb % 3; nb += 1
                            proj_chunk(cc, bi)
                            evac(cc, qT[:, cc, s0:s0 + 512], ps[bi][:, :], [tps[bi]], [t_qT[grp]])
                        for idx, n in enumerate(("kc", "vc", "ks", "kw")):
                            bi = nb % 3; nb += 1
                            proj_chunk(4 + idx, bi)
                            evac(idx, kT[n][:, s0:s0 + 512], ps[bi][:, :], [tps[bi]], [t_kT[n][grp]])
                        for c4 in range(4):
                            b_cb = nb % 3; nb += 1
                            proj_chunk(8 + c4 * 3 + 0, b_cb)
                            P.op("act", lambda e, b=b_cb: e.copy(out=cbs[:], in_=ps[b][:, :]), reads=[tps[b_cb]], writes=[t_cbs])
                            b_cc = nb % 3; nb += 1
                            proj_chunk(8 + c4 * 3 + 1, b_cc)
                            P.op("act", lambda e, b=b_cc: e.copy(out=ccs[:], in_=ps[b][:, :]), reads=[tps[b_cc]], writes=[t_ccs])
                            b_ch = nb % 3; nb += 1
                            proj_chunk(8 + c4 * 3 + 2, b_ch)
                            if grp == 0:
                                P.op("dve", lambda e, c4=c4: e.memset(zb[:, c4, 0:2], 0.0), writes=[t_zb[c4]])
                            P.op("dve", lambda e, c4=c4, b=b_ch: e.tensor_tensor(out=zb[:, c4, 2:514], in0=ps[b][:, :], in1=ccs[:],
                                                                                 op=ALU.mult),
                                 reads=[tps[b_ch], t_ccs], writes=[t_zb[c4]])
                            P.op("dve", lambda e, c4=c4: e.tensor_scalar(out=yb[:], in0=zb[:, c4, 2:514], scalar1=convw[:, c4, 2:3],
                                                                         scalar2=None, op0=ALU.mult),
                                 reads=[t_zb[c4], t_convw], writes=[t_yb])
                            P.op("dve", lambda e, c4=c4: e.scalar_tensor_tensor(out=yb[:], in0=zb[:, c4, 1:513], scalar=convw[:, c4, 1:2],
                                                                                in1=yb[:], op0=ALU.mult, op1=ALU.add),
                                 reads=[t_zb[c4], t_convw, t_yb], writes=[t_yb])
                            P.op("dve", lambda e, c4=c4: e.scalar_tensor_tensor(out=yb[:], in0=zb[:, c4, 0:512], scalar=convw[:, c4, 0:1],
                                                                                in1=yb[:], op0=ALU.mult, op1=ALU.add),
                                 reads=[t_zb[c4], t_convw, t_yb], writes=[t_yb])
                            P.op("dve", lambda e, c4=c4: e.tensor_tensor(out=oc[:, c4, :], in0=yb[:], in1=cbs[:], op=ALU.mult),
                                 reads=[t_yb, t_cbs], writes=[t_oc[c4]])
                            P.op("dve", lambda e, c4=c4: e.tensor_copy(out=zb[:, c4, 0:2], in_=zb[:, c4, 512:514]),
                                 reads=[t_zb[c4]], writes=[t_zb[c4]])
                            P.op("act", lambda e, c4=c4: e.activation(out=osq[:, c4, :], in_=oc[:, c4, :], func=AF.Square),
                                 reads=[t_oc[c4]], writes=[t_osq[c4]])
                        for c4 in range(4):
                            P.op("pe", lambda e, c4=c4: e.matmul(ps[3][:, :], lhsT=onesb[:, :], rhs=osq[:, c4, :],
                                                                 start=(c4 == 0), stop=(c4 == 3)),
                                 reads=[t_onesb, t_osq[c4]], writes=[tps[3]])
                        P.op("dve", lambda e: e.tensor_scalar(out=rbc[:], in0=ps[3][:, :], scalar1=1.0 / 512, scalar2=EPS,
                                                              op0=ALU.mult, op1=ALU.add), reads=[tps[3]], writes=[t_rbc])
                        P.op("act", lambda e: e.activation(out=rbc[:], in_=rbc[:], func=AF.Sqrt), reads=[t_rbc], writes=[t_rbc])
                        P.op("dve", lambda e: e.reciprocal(out=rbc[:], in_=rbc[:]), reads=[t_rbc], writes=[t_rbc])
                        for c4 in range(4):
                            P.op("dve", lambda e, c4=c4: e.scalar_tensor_tensor(
                                out=mixT[:, 4 + c4, s0:s0 + 512], in0=oc[:, c4, :], scalar=gconv[:, c4:c4 + 1], in1=rbc[:],
                                op0=ALU.mult, op1=ALU.mult), reads=[t_oc[c4], t_gconv, t_rbc], writes=[t_mixc[grp]])
                        for tl in range(4):
                            ti = grp * 4 + tl
                            bi = 4 + (tl % 2)
                            for k in range(8):
                                P.op("pe", lambda e, k=k, tl=tl, bi=bi: e.matmul(
                                    ps[bi][:, 0:280], lhsT=hT[:, k, tl * 128:(tl + 1) * 128], rhs=win[:, k, 2560:2840],
                                    start=(k == 0), stop=(k == 7)), reads=[t_win, t_hT], writes=[tps[bi]])
                            P.op("act", lambda e, ti=ti, bi=bi: e.copy(
                                out=vs_aug[:, ti, :, 0:64], in_=ps[bi][:, 0:128].rearrange("p (g d) -> p g d", g=2)),
                                reads=[tps[bi]], writes=[t_va[ti]])
                            P.op("act", lambda e, ti=ti, bi=bi: e.copy(
                                out=vw_aug[:, ti, :, 0:64], in_=ps[bi][:, 128:256].rearrange("p (g d) -> p g d", g=2)),
                                reads=[tps[bi]], writes=[t_va[ti]])
                            P.op("act", lambda e, ti=ti, bi=bi: e.activation(out=gat[:, ti, :], in_=ps[bi][:, 256:280],
                                                                             func=AF.Sigmoid),
                                 reads=[tps[bi]], writes=[t_gat[ti]])
                    if s == 0:
                        dbgdump("qT", [128, 4, S], BF16, qT[:], t_qT)
                        dbgdump("gat", [128, NT, 24], F32, gat[:], t_gat)
                        dbgdump("vs", [128, NT, 2, 65], BF16, vs_aug[:], t_va + [t_ones])
                        if stop_after <= 1:
                            dbgdump("mixT", [128, 8, S], BF16, mixT[:], t_mixc + t_mixa)
                if stop_after <= 1:
                    continue

                with Scope():
                    bd1 = {}; bd2 = {}; pe2 = {}; t_cw = Tok()
                    for n in "kv":
                        bd1[n] = sb("bd1" + n, [128, 32, 128], BF16)
                        P.dma("pool", bd1[n][:], bd1_d[n], writes=[t_cw])
                        bd2[n] = sb("bd2" + n, [128, 128], BF16)
                        P.dma("pool", bd2[n][:], bd2_d[n], writes=[t_cw])
                        pe2[n] = sb("pe2" + n, [128, 32], BF16)
                        P.dma("pool", pe2[n][:], pe2_d[n], writes=[t_cw])
                    Bw = sb("Bw", [128, 5, 8, 128], BF16); t_Bw = Tok()
                    Bs = sb("Bs", [128, 3, 8, 128], BF16); t_Bs = Tok()
                    for dl in range(5):
                        src = bass.AP(fw_d.tensor, 128 + dl * 128, [[LW, 128], [128 * (LW + 1), 8], [1, 128]])
                        P.dma("pool", Bw[:, dl, :, :], src, reads=[t_fw], writes=[t_Bw])
                    for dl in range(3):
                        src = bass.AP(fw_d.tensor, 8 * 128 * (LW + 1) + 128 + dl * 128,
                                      [[LW, 128], [128 * (LW + 1), 8], [1, 128]])
                        P.dma("pool", Bs[:, dl, :, :], src, reads=[t_fw], writes=[t_Bs])
                    if s == 0:
                        dbgdump("Bw", [128, 5, 8, 128], BF16, Bw[:], [t_Bw])
                        dbgdump("Bs", [128, 3, 8, 128], BF16, Bs[:], [t_Bs])
                    esel, t_esel = load_const("esel", esel_d, [128, 2048], BF16, "pool")
                    selmul, t_selmul = load_const("selmul", selmul_d, [128, 16, 32])
                    seladd, t_seladd = load_const("seladd", seladd_d, [128, 16, 32])
                    gattn, t_gattn = load_const("gattn", gattn_d, [128, 512])
                    cbias = sb("cbias", [128, 2]); t_cbias = Tok()
                    for i, n in enumerate("kv"):
                        for l in range(32):
                            P.op("pe", lambda e, n=n, l=l, i=i: e.matmul(ps[2][:, i:i + 1], lhsT=bd1[n][:, l, :],
                                                                         rhs=pe2[n][:, l:l + 1], start=(l == 0), stop=(l == 31)),
                                 reads=[t_cw], writes=[tps[2]])
                    P.op("dve", lambda e: e.tensor_copy(out=cbias[:], in_=ps[2][:, 0:2]), reads=[tps[2]], writes=[t_cbias])
                    kcmpT = sb("kcmpT", [128, 128], BF16); t_kcmp = Tok()
                    vc_aug = sb("vc_aug", [128, 2, 97], BF16); t_vca = Tok()
                    for g in range(2):
                        P.dma("pool", vc_aug[0:127, g, 65:97], cov_d, writes=[t_vca])
                    P.op("dve", lambda e: e.memset(vc_aug[:, :, 64:65], 1.0), writes=[t_vca])
                    hid = {n: sb("hid" + n, [128, 128], BF16) for n in "kv"}; t_hid = Tok()
                    for i, (n, srcn) in enumerate((("k", "kc"), ("v", "vc"))):
                        src = kT[srcn]
                        for l in range(32):
                            P.op("pe", lambda e, n=n, l=l, i=i, src=src: e.matmul(
                                ps[i][:, 0:127], lhsT=bd1[n][:, l, :], rhs=src[:, l:l + 2017:16],
                                start=(l == 0), stop=(l == 31)), reads=[t_cw] + t_kT[srcn], writes=[tps[i]])
                        P.op("act", lambda e, n=n, i=i: e.activation(out=hid[n][:, 0:127], in_=ps[i][:, 0:127],
                                                                     func=AF.Gelu_apprx_tanh, bias=cbias[:, i:i + 1]),
                             reads=[tps[i], t_cbias], writes=[t_hid])
                    P.op("pe", lambda e: e.matmul(ps[0][:, 0:127], lhsT=bd2["k"][:, :], rhs=hid["k"][:, 0:127], start=True, stop=True),
                         reads=[t_cw, t_hid], writes=[tps[0]])
                    P.op("dve", lambda e: e.tensor_copy(out=kcmpT[:, 0:127], in_=ps[0][:, 0:127]), reads=[tps[0]], writes=[t_kcmp])
                    P.op("pe", lambda e: e.matmul(ps[1][0:127, 0:128], lhsT=hid["v"][:, 0:127], rhs=bd2["v"][:, :], start=True, stop=True),
                         reads=[t_cw, t_hid], writes=[tps[1]])
                    P.op("dve", lambda e: e.tensor_copy(out=vc_aug[0:127, :, 0:64],
                                                        in_=ps[1][0:127, 0:128].rearrange("p (g d) -> p g d", g=2)),
                         reads=[tps[1]], writes=[t_vca])
                    if s == 0:
                        dbgdump("kcmpT", [128, 128], BF16, kcmpT[:], [t_kcmp])
                        dbgdump("vc_aug", [128, 2, 97], BF16, vc_aug[:], [t_vca])

                    bcp = [sb(f"bcp{i}", [128, 8, 128], BF16) for i in range(2)]; t_bcp = [Tok(), Tok()]
                    tS = [sb(f"tS{i}", [128, 512]) for i in range(2)]; t_tS = [Tok(), Tok()]
                    pT = [sb(f"pT{i}", [128, 512], BF16) for i in range(2)]; t_pT = [Tok(), Tok()]
                    o_acc = [sb(f"oacc{i}", [128, 8, 64]) for i in range(2)]; t_oacc = [Tok(), Tok()]
                    rs4 = sb("rs4", [128, 4]); wg4 = sb("wg4", [128, 4]); t_rs = Tok()
                    otmp = sb("otmp", [128, 4, 64]); t_otmp = Tok()
                    itmp = sb("itmp", [128, 4, 32]); imp = sb("imp", [128, 32]); t_imp = Tok()
                    m8 = sb("m8", [128, 8]); t_m8 = Tok()
                    negsel = sb("negsel", [128, 128], BF16); t_negsel = Tok()
                    P.op("dve", lambda e: e.memset(negsel[:], 0.0), writes=[t_negsel])
                    nsT4 = sb("nsT4", [128, 4, 128], BF16); t_nsT = Tok()
                    junk2 = sb("junk2", [128, 512], BF16); t_junk2 = Tok()
                    on_b = sb("on_b", [128, 512], BF16); t_onb = Tok()
                    rr = [0]

                    def score_tile(g, lhs_list, bias_ap, t_bias, rows):
                        i = rr[0] % 2
                        rr[0] += 1
                        bank, tb = ps[i], tps[i]
                        nmm = len(lhs_list)
                        for m, (l_ap, r_ap, rd) in enumerate(lhs_list):
                            P.op("pe", lambda e, l_ap=l_ap, r_ap=r_ap, m=m: e.matmul(
                                bank[0:rows, :].rearrange("p (j q) -> p j q", j=4), lhsT=l_ap, rhs=r_ap,
                                start=(m == 0), stop=(m == nmm - 1)), reads=rd, writes=[tb])
                        P.op("dve", lambda e: e.scalar_tensor_tensor(
                            out=tS[i][0:rows, :].rearrange("p (j q) -> p j q", j=4),
                            in0=bank[0:rows, :].rearrange("p (j q) -> p j q", j=4), scalar=0.125,
                            in1=bias_ap, op0=ALU.mult, op1=ALU.add), reads=[tb, t_bias], writes=[t_tS[i]])
                        P.op("act", lambda e: e.activation(out=pT[i][0:rows, :], in_=tS[i][0:rows, :], func=AF.Exp),
                             reads=[t_tS[i]], writes=[t_pT[i]])
                        return pT[i], t_pT[i]

                    def evac_branch(g, br, qi, oa, t_oa, with_imp=False):
                        ncol = 97 if with_imp else 65
                        rd = tps[3:7]
                        P.op("dve", lambda e: e.tensor_scalar(out=rs4[:], in0=psO[:, :, 64], scalar1=1e-30, scalar2=None,
                                                              op0=ALU.max), reads=rd, writes=[t_rs])
                        P.op("dve", lambda e: e.reciprocal(out=rs4[:], in_=rs4[:]), reads=[t_rs], writes=[t_rs])
                        g0 = 12 * g + br
                        P.op("dve", lambda e: e.tensor_tensor(out=wg4[:], in0=rs4[:], in1=gat[:, qi, g0:g0 + 10:3], op=ALU.mult),
                             reads=[t_rs, t_gat[qi]], writes=[t_rs])
                        if br == 0:
                            P.op("dve", lambda e: e.tensor_tensor(
                                out=oa[:, 4 * g:4 * g + 4, :], in0=psO[:, :, 0:64],
                                in1=wg4[:].unsqueeze(2).broadcast_to([128, 4, 64]), op=ALU.mult),
                                reads=rd + [t_rs], writes=[t_oa])
                        else:
                            P.op("dve", lambda e: e.tensor_tensor(
                                out=otmp[:], in0=psO[:, :, 0:64],
                                in1=wg4[:].unsqueeze(2).broadcast_to([128, 4, 64]), op=ALU.mult),
                                reads=rd + [t_rs], writes=[t_otmp])
                            P.op("pool", lambda e: e.tensor_tensor(out=oa[:, 4 * g:4 * g + 4, :], in0=oa[:, 4 * g:4 * g + 4, :],
                                                                   in1=otmp[:], op=ALU.add),
                                 reads=[t_otmp, t_oa], writes=[t_oa])
                        if with_imp:
                            P.op("dve", lambda e: e.tensor_tensor(
                                out=itmp[:], in0=psO[:, :, 65:97], in1=rs4[:].unsqueeze(2).broadcast_to([128, 4, 32]),
                                op=ALU.mult), reads=rd + [t_rs], writes=[t_imp])
                            P.op("dve", lambda e: e.tensor_reduce(out=imp[:], in_=itmp[:].rearrange("p j n -> p n j"),
                                                                  axis=AX.X, op=ALU.add), reads=[t_imp], writes=[t_imp])

                    for qi in range(NT):
                        gi = s * NT + qi
                        bc, t_bc = bcp[qi % 2], t_bcp[qi % 2]
                        src = bass.AP(fc_d.tensor, 2048 + 128 * qi - 31, [[LC, 127], [127 * (LC + 16), 8], [1, 128]])
                        P.dma("pool", bc[0:127, :, :], src, reads=[t_fc], writes=[t_bc])
                        oa, t_oa = o_acc[qi % 2], t_oacc[qi % 2]
                        for g in range(2):
                            pr = slice(g * 64, (g + 1) * 64)
                            rhs_q = qT[pr, :, qi * 128:(qi + 1) * 128]
                            rd_q = [t_qT[qi // 4]]
                            p_, t_p = score_tile(g, [(kcmpT[pr, 0:127], rhs_q, rd_q + [t_kcmp])],
                                                 bc[0:127, 4 * g:4 * g + 4, :],
                                                 t_bc, 127)
                            for j in range(4):
                                P.op("pe", lambda e, j=j, p_=p_: e.matmul(psO[:, j, 0:97], lhsT=p_[0:127, j * 128:(j + 1) * 128],
                                                                          rhs=vc_aug[0:127, g, :], start=True, stop=True),
                                     reads=[t_p, t_vca], writes=[tps[3 + j]])
                            evac_branch(g, 0, qi, oa, t_oa, with_imp=True)
                            P.op("dve", lambda e: e.tensor_tensor(out=imp[:], in0=imp[:], in1=selmul[:, qi, :], op=ALU.mult),
                                 reads=[t_imp, t_selmul], writes=[t_imp])
                            P.op("dve", lambda e: e.tensor_tensor(out=imp[:], in0=imp[:], in1=seladd[:, qi, :], op=ALU.add),
                                 reads=[t_imp, t_seladd], writes=[t_imp])
                            P.op("dve", lambda e: e.max(out=m8[:], in_=imp[:]), reads=[t_imp], writes=[t_m8])
                            P.op("dve", lambda e: e.tensor_scalar(out=negsel[:, g * 64:g * 64 + 32], in0=imp[:], scalar1=m8[:, 7:8],
                                                                  scalar2=NEG, op0=ALU.is_lt, op1=ALU.mult),
                                 reads=[t_imp, t_m8], writes=[t_negsel])
                            P.op("pe", lambda e: e.transpose(out=psb[:, 0:128], in_=negsel[:, :], identity=identb[:]),
                                 reads=[t_negsel, t_identb], writes=[tpsb])
                            P.op("act", lambda e: e.copy(out=nsT4[:], in_=psb[:, 0:128].unsqueeze(1).broadcast_to([128, 4, 128])),
                                 reads=[tpsb], writes=[t_nsT])
                            if dbg and s == 0 and qi == 5 and g == 1:
                                dbgdump("negsel", [128, 128], BF16, negsel[:], [t_negsel])
                            for kj in range(qi + 1):
                                dl = min(qi - kj, 2)
                                p_, t_p = score_tile(
                                    g, [(kT["ks"][pr, kj * 128:(kj + 1) * 128], rhs_q, rd_q + [t_kT["ks"][kj // 4]]),
                                        (esel[pr, kj * 128:(kj + 1) * 128], nsT4[pr, :, :], [t_esel, t_nsT])],
                                    Bs[:, dl, 4 * g:4 * g + 4, :], t_Bs, 128)
                                for j in range(4):
                                    P.op("pe", lambda e, j=j, p_=p_, kj=kj: e.matmul(
                                        psO[:, j, 0:65], lhsT=p_[:, j * 128:(j + 1) * 128], rhs=vs_aug[:, kj, g, :],
                                        start=(kj == 0), stop=(kj == qi)), reads=[t_p, t_va[kj], t_ones], writes=[tps[3 + j]])
                            evac_branch(g, 1, qi, oa, t_oa)
                            k0 = max(0, qi - 4)
                            for kj in range(k0, qi + 1):
                                p_, t_p = score_tile(
                                    g, [(kT["kw"][pr, kj * 128:(kj + 1) * 128], rhs_q, rd_q + [t_kT["kw"][kj // 4]])],
                                    Bw[:, qi - kj, 4 * g:4 * g + 4, :], t_Bw, 128)
                                for j in range(4):
                                    P.op("pe", lambda e, j=j, p_=p_, kj=kj: e.matmul(
                                        psO[:, j, 0:65], lhsT=p_[:, j * 128:(j + 1) * 128], rhs=vw_aug[:, kj, g, :],
                                        start=(kj == k0), stop=(kj == qi)), reads=[t_p, t_va[kj], t_ones], writes=[tps[3 + j]])
                            evac_branch(g, 2, qi, oa, t_oa)
                        if dbg and s == 0:
                            if qi == 0:
                                dbg_out["oattn"] = dout("dbg_oattn", [NT, 128, 512], F32)
                            P.dma("sp", dbg_out["oattn"][qi], oa[:].rearrange("p h d -> p (h d)"), reads=[t_oa])
                        oaf = oa[:].rearrange("p h d -> p (h d)")
                        rms_stats(oaf, t_oa, junk2[:], t_junk2, ssqa, rstda, t_sta[gi], gi, 512)
                        P.op("dve", lambda e, oaf=oaf: e.scalar_tensor_tensor(
                            out=on_b[:], in0=oaf, scalar=rstda[:, gi:gi + 1], in1=gattn[:], op0=ALU.mult, op1=ALU.mult),
                            reads=[t_oa, t_sta[gi], t_gattn], writes=[t_onb])
                        for c in range(4):
                            P.op("pe", lambda e, c=c: e.transpose(out=psb[:, 512 + c * 128:512 + (c + 1) * 128],
                                                                  in_=on_b[:, c * 128:(c + 1) * 128], identity=identb[:]),
                                 reads=[t_onb, t_identb], writes=[tpsb])
                        for c in range(4):
                            evac(qi, mixT[:, c, qi * 128:(qi + 1) * 128], psb[:, 512 + c * 128:512 + (c + 1) * 128],
                                 [tpsb], [t_mixa[qi]])
                    if s == 0:
                        dbgdump("mixT", [128, 8, S], BF16, mixT[:], t_mixc + t_mixa)
                if stop_after <= 2:
                    continue

                with Scope():
                    wout = sb("wout", [128, 8, 1024], BF16); t_wout = Tok()
                    wov = w_out_d.rearrange("(k p) n -> p k n", p=128)
                    for k in range(8):
                        P.dma("pool", wout[:, k, :], wov[:, k, :], writes=[t_wout])
                    gt1 = sb("gt1", [128, 1024]); t_gt1 = Tok()
                    P.dma("sp", gt1[:], gt_d[0, s], reads=[t_gtd], writes=[t_gt1])
                    xpool = [sb(f"xt3{i}", [128, 1024]) for i in range(2)]; t_xp = [Tok(), Tok()]
                    x1p = [sb(f"x1t{i}", [128, 1024]) for i in range(2)]; t_x1p = [Tok(), Tok()]
                    junk = sb("junk3", [128, 1024], BF16); t_junk = Tok()
                    xs = sb("xs3", [128, 1024], BF16); t_xs = Tok()
                    h2st = [sb(f"h2st{i}", [128, 8, 128], BF16) for i in range(2)]; t_h2st = [Tok(), Tok()]
                    for ti in range(NT):
                        gi = s * NT + ti
                        xt, t_xt = xpool[ti % 2], t_xp[ti % 2]
                        x1t, t_x1t = x1p[ti % 2], t_x1p[ti % 2]
                        P.dma("sp", xt[:], x_d[gi * 128:(gi + 1) * 128, :], writes=[t_xt])
                        for half in range(2):
                            for k in range(8):
                                P.op("pe", lambda e, k=k, half=half: e.matmul(
                                    ps[half][:, :], lhsT=mixT[:, k, ti * 128:(ti + 1) * 128],
                                    rhs=wout[:, k, half * 512:(half + 1) * 512], start=(k == 0), stop=(k == 7)),
                                    reads=[t_wout, t_mixa[ti], t_mixc[ti // 4]], writes=[tps[half]])
                            hs = slice(half * 512, (half + 1) * 512)
                            P.op("dve", lambda e, half=half, hs=hs: e.tensor_tensor(out=x1t[:, hs], in0=ps[half][:, :], in1=gt1[:, hs],
                                                                                    op=ALU.mult),
                                 reads=[tps[half], t_gt1], writes=[t_x1t])
                        P.op("pool", lambda e: e.tensor_tensor(out=x1t[:], in0=x1t[:], in1=xt[:], op=ALU.add),
                             reads=[t_x1t, t_xt], writes=[t_x1t])
                        P.dma("sp", x1_d[gi * 128:(gi + 1) * 128, :], x1t[:], reads=[t_x1t], writes=[t_x1d[gi]])
                        hs_, t_hs = h2st[ti % 2], t_h2st[ti % 2]
                        norm_transpose(x1t, t_x1t, gi, s, ssq2, rstd2, t_st2, gs2, 24, hs_, t_hs, 0, junk, t_junk, xs, t_xs)
                        P.dma("sp", h2T_d[:, :, gi * 128:(gi + 1) * 128], hs_[:], reads=[t_hs], writes=[t_h2d[gi]])

        if stop_after <= 3:
            P.wait_all("sp", Tok.registry)
            return nc, dbg_out

        try:
            utb_d = dscr("utb_s", [16384, 1024], BF16)
            vb_d = dscr("vb_s", [16384, 1024], BF16)
            s2_d = dscr("s2_s", [8, T, 128])
            t_utb = [Tok() for _ in range(16)]
            t_vb = [Tok() for _ in range(16)]
            with Scope():
                stg = [sb(f"cst{i}", [128, 8, 1024], BF16) for i in range(2)]; t_stg = [Tok(), Tok()]
                n = 0
                for src_d, dst_d, tks in ((ut_d, utb_d, t_utb), (pv_d, vb_d, t_vb)):
                    for c in range(16):
                        i = n % 2; n += 1
                        sv = src_d[c * 1024:(c + 1) * 1024, :].rearrange("(c p) n -> p c n", p=128)
                        dv = dst_d[c * 1024:(c + 1) * 1024, :].rearrange("(c p) n -> p c n", p=128)
                        P.dma("pool", stg[i][:], sv, writes=[t_stg[i]])
                        P.dma("sp", dv, stg[i][:], reads=[t_stg[i]], writes=[tks[c]])
            if stop_after == 3.5:
                raise _Stop()

            with Scope():
                wq, t_wq = None, Tok()
                wq = sb("wq", [128, 8, 1024], BF16)
                wqv = wq_d.rearrange("(k p) n -> p k n", p=128)
                for k in range(8):
                    P.dma("pool", wq[:, k, :], wqv[:, k, :], writes=[t_wq])
                kb, t_kb = load_const("kb", kb_d, [128, 8, 256], BF16, "pool")
                identf, t_identf = load_const("identf", identf_d, [128, 128])
                iota, t_iota = load_const("iota", iota_d, [128, 128])
                selh, t_selh = load_const("selh", selh_d, [8, 128])
                gfin, t_gfin = load_const("gfin", gfin_d, [128, 1024])
                gt2 = sb("gt2", [128, 1024]); t_gt2 = Tok()
                Wbuf = sb("Wbuf", [128, 256, 128], BF16); t_W = [Tok() for _ in range(64)]
                h2g = sb("h2g", [128, 8, 256], BF16); t_h2g = Tok()
                qTp = sb("qTp", [128, 8, 256], BF16); t_qTp = Tok()
                s_sb = sb("s_sb", [128, 8, 256]); t_s = Tok()
                work = sb("work", [128, 8, 256]); t_work = Tok()
                v16 = sb("v16", [128, 8, 2, 16]); t_v16 = Tok()
                idx = sb("idx", [128, 8, 16], U32); t_idx = Tok()
                cand = sb("cand", [128, 8, 256]); t_cand = Tok()
                cwork = sb("cwork", [128, 8, 256]); t_cwork = Tok()
                ts16 = sb("ts16", [128, 8, 16]); t_ts = Tok()
                d16 = sb("d16", [128, 8, 16]); t_d16 = Tok()
                zz = sb("zz", [128, 8]); mu = sb("mu", [128, 8]); tauE = sb("tauE", [128, 8]); t_zz = Tok()
                tm3 = sb("tm3", [128, 3, 128]); t_tm3 = Tok()
                tT3 = sb("tT3", [128, 3, 128]); t_tT3 = Tok()
                s2hm = [sb(f"s2hm{i}", [8, 16 * 128]) for i in range(2)]; t_s2hm = [Tok(), Tok()]
                NR = 4
                ebuf = [sb(f"ebuf{i}", [128, 128], BF16) for i in range(NR)]; t_eb = [Tok() for _ in range(NR)]
                Rt = [sb(f"Rt{i}", [128, 128], BF16) for i in range(NR)]; t_Rt = [Tok() for _ in range(NR)]
                Lt = [sb(f"Lt{i}", [128, 128], BF16) for i in range(NR)]; t_Lt = [Tok() for _ in range(NR)]
                mk = [sb(f"mk{i}", [128, 128], BF16) for i in range(NR)]; t_mk = [Tok() for _ in range(NR)]
                NC = 4
                utc = [sb(f"utc{i}", [128, 8, 128], BF16) for i in range(NC)]; t_utc = [Tok() for _ in range(NC)]
                vch = [sb(f"vch{i}", [128, 1024], BF16) for i in range(NC)]; t_vch = [Tok() for _ in range(NC)]
                abuf = [sb(f"abuf{i}", [128, 256], BF16) for i in range(2)]; t_ab = [Tok(), Tok()]
                wab = [sb(f"wab{i}", [128, 256], BF16) for i in range(2)]; t_wab = [Tok(), Tok()]
                x1t = sb("x1f", [128, 1024]); t_x1t = Tok()
                yt = sb("yf", [128, 1024]); t_yt = Tok()
                ot = sb("of", [128, 1024]); t_ot = Tok()
                junk = sb("junkf", [128, 1024], BF16); t_junk = Tok()
                ssq3 = sb("ssq3", [128, NTT]); rstd3 = sb("rstd3", [128, NTT]); t_st3 = [Tok() for _ in range(NTT)]
                t_s2d = Tok()
                t_out = Tok()

                def top16(dst_lo, dst_hi, src, wrk, rd, wr_dst, wr_wrk):
                    P.op("dve", lambda e: e.max(out=dst_lo, in_=src), reads=rd, writes=[wr_dst])
                    P.op("dve", lambda e: e.match_replace(out=wrk, in_to_replace=dst_lo, in_values=src, imm_value=-1e30),
                         reads=rd + [wr_dst], writes=[wr_wrk])
                    P.op("dve", lambda e: e.max(out=dst_hi, in_=wrk), reads=[wr_wrk], writes=[wr_dst])

                ngroups = T // 256
                for gidx in range(ngroups):
                    g0 = gidx * 256
                    sq = g0 // S
                    if g0 % S == 0:
                        P.dma("sp", gt2[:], gt_d[1, sq], reads=[t_gtd], writes=[t_gt2])
                    P.dma("sp", h2g[:], h2T_d[:, :, g0:g0 + 256], reads=t_h2d[gidx * 2:gidx * 2 + 2], writes=[t_h2g])
                    for h in range(8):
                        bi = h % 3
                        for k in range(8):
                            P.op("pe", lambda e, k=k, h=h, bi=bi: e.matmul(ps[bi][:, 0:256], lhsT=wq[:, k, h * 128:(h + 1) * 128],
                                                                           rhs=h2g[:, k, :], start=(k == 0), stop=(k == 7)),
                                 reads=[t_wq, t_h2g], writes=[tps[bi]])
                        evac(h, qTp[:, h, :], ps[bi][:, 0:256], [tps[bi]], [t_qTp])
                    for tt in range(2):
                        gi = gidx * 2 + tt
                        tsl = slice(tt * 128, (tt + 1) * 128)
                        for h in range(8):
                            bi = 3 + h // 2
                            P.op("pe", lambda e, h=h, bi=bi: e.matmul(ps[bi][:, (h % 2) * 256:(h % 2 + 1) * 256], lhsT=qTp[:, h, tsl],
                                                                      rhs=kb[:, h, :], start=True, stop=True),
                                 reads=[t_qTp, t_kb], writes=[tps[bi]])
                        for hp in range(4):
                            evac(hp, s_sb[:, 2 * hp:2 * hp + 2, :], ps[3 + hp][:, :].rearrange("p (h n) -> p h n", h=2),
                                 [tps[3 + hp]], [t_s])
                        for h in range(8):
                            for p in range(2):
                                top16(v16[:, h, p, 0:8], v16[:, h, p, 8:16], s_sb[:, h, p * 128:(p + 1) * 128],
                                      work[:, h, p * 128:(p + 1) * 128], [t_s], t_v16, t_work)
                            P.op("dve", lambda e, h=h: e.max_index(out=idx[:, h, 0:8], in_max=v16[:, h, 0, 0:8],
                                                                   in_values=s_sb[:, h, 0:128]),
                                 reads=[t_s, t_v16], writes=[t_idx])
                            P.op("dve", lambda e, h=h: e.max_index(out=idx[:, h, 8:16], in_max=v16[:, h, 0, 8:16],
                                                                   in_values=work[:, h, 0:128]),
                                 reads=[t_work, t_v16], writes=[t_idx])
                        P.op("dve", lambda e: e.tensor_tensor(
                            out=cand[:].rearrange("p h (a b) -> p h a b", a=16),
                            in0=v16[:, :, 0, :].unsqueeze(3).broadcast_to([128, 8, 16, 16]),
                            in1=v16[:, :, 1, :].unsqueeze(2).broadcast_to([128, 8, 16, 16]), op=ALU.add),
                            reads=[t_v16], writes=[t_cand])
                        for h in range(8):
                            top16(ts16[:, h, 0:8], ts16[:, h, 8:16], cand[:, h, :], cwork[:, h, :], [t_cand], t_ts, t_cwork)
                        if stop_after == 3.6:
                            dbgdump("v16", [128, 8, 2, 16], F32, v16[:], [t_v16])
                            dbgdump("ts16", [128, 8, 16], F32, ts16[:], [t_ts])
                            dbgdump("idx", [128, 8, 16], U32, idx[:], [t_idx])
                            dbgdump("s_sb", [128, 8, 256], F32, s_sb[:], [t_s])
                            raise _Stop()
                        P.op("dve", lambda e: e.tensor_tensor(out=d16[:], in0=ts16[:], in1=ts16[:, :, 0:1].broadcast_to([128, 8, 16]),
                                                              op=ALU.subtract), reads=[t_ts], writes=[t_d16])
                        P.op("act", lambda e: e.activation(out=d16[:], in_=d16[:], func=AF.Exp), reads=[t_d16], writes=[t_d16])
                        P.op("dve", lambda e: e.tensor_reduce(out=zz[:], in_=d16[:], axis=AX.X, op=ALU.add),
                             reads=[t_d16], writes=[t_zz])
                        P.op("act", lambda e: e.activation(out=zz[:], in_=zz[:], func=AF.Ln), reads=[t_zz], writes=[t_zz])
                        P.op("dve", lambda e: e.tensor_tensor(out=mu[:], in0=zz[:], in1=ts16[:, :, 0], op=ALU.add),
                             reads=[t_zz, t_ts], writes=[t_zz])
                        P.op("dve", lambda e: e.tensor_scalar(out=tauE[:], in0=ts16[:, :, 15], scalar1=-1e-4, scalar2=None,
                                                              op0=ALU.add), reads=[t_ts], writes=[t_zz])
                        P.op("dve", lambda e: e.tensor_tensor(
                            out=tm3[:, 0, :].rearrange("p (h a) -> p h a", h=8),
                            in0=tauE[:].unsqueeze(2).broadcast_to([128, 8, 16]), in1=v16[:, :, 0, :], op=ALU.subtract),
                            reads=[t_zz, t_v16], writes=[t_tm3])
                        P.op("dve", lambda e: e.tensor_tensor(
                            out=tm3[:, 1, :].rearrange("p (h a) -> p h a", h=8),
                            in0=v16[:, :, 0, :], in1=mu[:].unsqueeze(2).broadcast_to([128, 8, 16]), op=ALU.subtract),
                            reads=[t_zz, t_v16], writes=[t_tm3])
                        P.op("dve", lambda e: e.tensor_copy(out=tm3[:, 2, :], in_=idx[:].rearrange("p h a -> p (h a)")),
                             reads=[t_idx], writes=[t_tm3])
                        for w3 in range(3):
                            P.op("pe", lambda e, w3=w3: e.transpose(out=ps[2][:, w3 * 128:(w3 + 1) * 128], in_=tm3[:, w3, :],
                                                                    identity=identf[:]),
                                 reads=[t_tm3, t_identf], writes=[tps[2]])
                        P.op("dve", lambda e: e.tensor_copy(out=tT3[:], in_=ps[2][:, 0:384].rearrange("p (w t) -> p w t", w=3)),
                             reads=[tps[2]], writes=[t_tT3])
                        if stop_after == 3.7:
                            dbgdump("tT3", [128, 3, 128], F32, tT3[:], [t_tT3])
                            dbgdump("tm3", [128, 3, 128], F32, tm3[:], [t_tm3])
                            raise _Stop()
                        P.dma("sp", s2_d[:, g0 + tt * 128:g0 + (tt + 1) * 128, :].rearrange("h t j -> t h j"), s_sb[:, :, 128:256],
                              reads=[t_s], writes=[t_s2d])
                        for sl in range(8):
                            i2 = sl % 2
                            t0 = g0 + tt * 128 + sl * 16
                            P.dma("sp", s2hm[i2][:, :], s2_d[:, t0:t0 + 16, :].rearrange("h t j -> h (t j)"),
                                  reads=[t_s2d], writes=[t_s2hm[i2]])
                            for q4 in range(4):
                                tb0 = tt * 128 + sl * 16 + q4 * 4
                                P.op("pe", lambda e, i2=i2, q4=q4: e.matmul(ps[0][:, :], lhsT=selh[:, :],
                                                                            rhs=s2hm[i2][:, q4 * 512:(q4 + 1) * 512],
                                                                            start=True, stop=True),
                                     reads=[t_selh, t_s2hm[i2]], writes=[tps[0]])
                                P.op("pe", lambda e, i2=i2, q4=q4: e.matmul(ps[3][:, :], lhsT=selh[:, :],
                                                                            rhs=s2hm[i2][:, q4 * 512:(q4 + 1) * 512],
                                                                            start=True, stop=True),
                                     reads=[t_selh, t_s2hm[i2]], writes=[tps[3]])
                                if stop_after == 3.75:
                                    dbg_out["s2rep"] = dout("dbg_s2rep", [128, 512], F32)
                                    s2c = sb("s2c", [128, 512])
                                    t_s2c = Tok()
                                    P.op("dve", lambda e: e.tensor_copy(out=s2c[:], in_=ps[0][:, :]), reads=[tps[0]], writes=[t_s2c])
                                    P.dma("sp", dbg_out["s2rep"], s2c[:], reads=[t_s2c])
                                    raise _Stop()
                                wb = 1 + ((sl * 4 + q4) % 2)
                                for t in range(4):
                                    tl = tb0 + t - tt * 128
                                    r = (tb0 + t) % NR
                                    sl_ps = ps[0][:, t * 128:(t + 1) * 128]
                                    sl_pd = ps[3][:, t * 128:(t + 1) * 128]
                                    if "A" in PDBG:
                                        P.op("dve", lambda e, r=r: e.memset(ebuf[r][:], 1.0), writes=[t_eb[r]])
                                    else:
                                        P.op("act", lambda e, r=r, sl_ps=sl_ps, tl=tl: e.activation(
                                            out=ebuf[r][:], in_=sl_ps, func=AF.Exp, bias=tT3[:, 1, tl:tl + 1]),
                                            reads=[tps[0], t_tT3], writes=[t_eb[r]])
                                    if "S" in PDBG:
                                        P.op("dve", lambda e, r=r: e.tensor_copy(out=Rt[r][:], in_=ebuf[r][:]), reads=[t_eb[r]], writes=[t_Rt[r]])
                                    else:
                                        P.op("dve", lambda e, r=r, sl_pd=sl_pd, tl=tl: e.tensor_scalar(
                                            out=mk[r][:], in0=sl_pd, scalar1=tT3[:, 0, tl:tl + 1], scalar2=None,
                                            op0=ALU.is_ge), reads=[tps[3], t_tT3], writes=[t_mk[r]])
                                        P.op("pool", lambda e, r=r: e.tensor_tensor(out=Rt[r][:], in0=mk[r][:], in1=ebuf[r][:],
                                                                                    op=ALU.mult),
                                             reads=[t_mk[r], t_eb[r]], writes=[t_Rt[r]])
                                    P.op(LENG, lambda e, r=r, tl=tl: e.tensor_scalar(
                                        out=Lt[r][:], in0=iota[:], scalar1=tT3[:, 2, tl:tl + 1], scalar2=None, op0=ALU.is_equal),
                                        reads=[t_iota, t_tT3], writes=[t_Lt[r]])
                                    if "M" not in PDBG:
                                        P.op("pe", lambda e, r=r, t=t, wb=wb: e.matmul(ps[wb][:, t * 128:(t + 1) * 128], lhsT=Rt[r][:],
                                                                                       rhs=Lt[r][:], start=True, stop=True),
                                             reads=[t_Rt[r], t_Lt[r]], writes=[tps[wb]])
                                if stop_after == 3.76:
                                    dbgdump("Rt", [128, 128], BF16, Rt[3][:], [t_Rt[3]])
                                    dbgdump("Lt", [128, 128], BF16, Lt[3][:], [t_Lt[3]])
                                    dbgdump("eb", [128, 128], BF16, ebuf[3][:], [t_eb[3]])
                                    raise _Stop()
                                evac(q4, Wbuf[:, tb0:tb0 + 4, :], ps[wb][:, :].rearrange("p (t i) -> p t i", t=4),
                                     [tps[wb]], [t_W[tb0 // 4]])
                    if dbg and gidx == 0:
                        dbgdump("Wbuf", [128, 256, 128], BF16, Wbuf[:], t_W)
                        if stop_after == 3.8:
                            raise _Stop()
                    for i in range(128):
                        c = i % NC
                        P.dma("sp", utc[c][:], utb_d[i * 128:(i + 1) * 128, :].rearrange("p (k j) -> p k j", k=8),
                              reads=[t_utb[i // 8]], writes=[t_utc[c]])
                        P.dma("sp", vch[c][:], vb_d[i * 128:(i + 1) * 128, :], reads=[t_vb[i // 8]], writes=[t_vch[c]])
                        ba = i % 2
                        for k in range(8):
                            P.op("pe", lambda e, k=k, c=c, ba=ba: e.matmul(ps[ba][:, 0:256], lhsT=utc[c][:, k, :], rhs=h2g[:, k, :],
                                                                           start=(k == 0), stop=(k == 7)),
                                 reads=[t_utc[c], t_h2g], writes=[tps[ba]])
                        P.op("act", lambda e, ba=ba: e.activation(out=abuf[ba][:], in_=ps[ba][:, 0:256], func=AF.Gelu_apprx_tanh),
                             reads=[tps[ba]], writes=[t_ab[ba]])
                        weng = "dve" if i % 2 == 0 else "pool"
                        P.op(weng, lambda e, ba=ba, i=i: e.tensor_tensor(out=wab[ba][:], in0=abuf[ba][:], in1=Wbuf[:, :, i],
                                                                         op=ALU.mult),
                             reads=[t_ab[ba]] + t_W, writes=[t_wab[ba]])
                        for tt in range(2):
                            for half in range(2):
                                P.op("pe", lambda e, tt=tt, half=half, ba=ba, c=c, i=i: e.matmul(
                                    psO[:, tt * 2 + half, :], lhsT=wab[ba][:, tt * 128:(tt + 1) * 128],
                                    rhs=vch[c][:, half * 512:(half + 1) * 512], start=(i == 0), stop=(i == 127)),
                                    reads=[t_wab[ba], t_vch[c]], writes=[tps[3 + tt * 2 + half]])
                    for tt in range(2):
                        gi = gidx * 2 + tt
                        P.dma("sp", x1t[:], x1_d[gi * 128:(gi + 1) * 128, :], reads=[t_x1d[gi]], writes=[t_x1t])
                        for half in range(2):
                            hs = slice(half * 512, (half + 1) * 512)
                            P.op("dve", lambda e, tt=tt, half=half, hs=hs: e.tensor_tensor(
                                out=yt[:, hs], in0=psO[:, tt * 2 + half, :], in1=gt2[:, hs], op=ALU.mult),
                                reads=[tps[3 + tt * 2 + half], t_gt2], writes=[t_yt])
                        if dbg and gidx == 0 and tt == 0:
                            dbgdump("peer0", [128, 1024], F32, yt[:], [t_yt])
                        P.op("pool", lambda e: e.tensor_tensor(out=yt[:], in0=yt[:], in1=x1t[:], op=ALU.add),
                             reads=[t_yt, t_x1t], writes=[t_yt])
                        rms_stats(yt[:], t_yt, junk[:], t_junk, ssq3, rstd3, t_st3[gi], gi, D)
                        P.op("dve", lambda e, gi=gi: e.scalar_tensor_tensor(out=ot[:], in0=yt[:], scalar=rstd3[:, gi:gi + 1], in1=gfin[:],
                                                                            op0=ALU.mult, op1=ALU.mult),
                             reads=[t_yt, t_st3[gi], t_gfin], writes=[t_ot])
                        P.dma("sp", out_d[gi * 128:(gi + 1) * 128, :], ot[:], reads=[t_ot], writes=[t_out])
                    if stop_after == 4 and gidx == 0:
                        raise _Stop()

        except _Stop:
            pass
        P.wait_all("sp", Tok.registry)
        print("ops per engine", P.ecount, "waits", P.nwaits)
    return nc, dbg_out


_NC_CACHE = {}


def kernel(**inputs):
    inp = {k: np.asarray(v) for k, v in inputs.items()}
    sh = _prep(inp)
    if "nc" not in _NC_CACHE:
        _NC_CACHE["nc"] = build_nc(nseq=2)[0]
    nc = _NC_CACHE["nc"]
    x = np.asarray(inp["x"], np.float32)
    c = np.asarray(inp["c"], np.float32)
    in_maps = []
    for core in range(8):
        m = dict(sh)
        m["x"] = np.ascontiguousarray(x[2 * core:2 * core + 2].reshape(2 * S, D))
        m["cT"] = np.ascontiguousarray(c[2 * core:2 * core + 2].T.reshape(8, 128, 2).transpose(1, 0, 2))
        in_maps.append(m)
    res = run_bass_kernel_spmd(nc, in_maps, core_ids=list(range(8)))
    out = np.concatenate([np.asarray(r["out"]).reshape(2, S, D) for r in res.results], axis=0)
    return out.astype(np.float32)
```

```python
import math
import numpy as np
from contextlib import ExitStack
import concourse.bass as bass
import concourse.mybir as mybir
from concourse.bass_utils import run_bass_kernel_spmd

F32 = mybir.dt.float32
BF16 = mybir.dt.bfloat16
U32 = mybir.dt.uint32
AF = mybir.ActivationFunctionType
ALU = mybir.AluOpType
AX = mybir.AxisListType

S = 2048
D = 1024
NT = 16
EPS = 1e-6
NEG = -30000.0
LENG = "dve"
import os
PDBG = os.environ.get("PDBG", "")
SEM_CHUNK = 30000


class _Stop(Exception):
    pass


class Tok:
    __slots__ = ("writers", "readers")
    registry = []

    def __init__(self):
        self.writers = []
        self.readers = []
        Tok.registry.append(self)


class Op:
    __slots__ = ("eng", "sem", "val", "is_dma")


class Prog:
    ENGS = ("pe", "dve", "act", "pool", "sp")

    def __init__(self, nc, stack, n_dma_sems=8):
        self.nc = nc
        self.stack = stack
        self.eobj = {"pe": nc.tensor, "dve": nc.vector, "act": nc.scalar,
                     "pool": nc.gpsimd, "sp": nc.sync}
        self.esems = {e: [] for e in self.ENGS}
        self.ecount = {e: 0 for e in self.ENGS}
        self.waited = {e: {} for e in self.ENGS}
        self.dma_sems = {}
        self.dma_rr = {}
        self.n_dma_sems = n_dma_sems
        self.nwaits = 0

    def _esem(self, e, chunk):
        lst = self.esems[e]
        while len(lst) <= chunk:
            lst.append(self.stack.enter_context(self.nc.semaphore(f"s_{e}_{len(lst)}")))
        return lst[chunk]

    def _wait(self, e, sem, val):
        w = self.waited[e]
        k = id(sem)
        if w.get(k, 0) >= val:
            return
        w[k] = val
        self.eobj[e].wait_ge(sem, val)
        self.nwaits += 1

    def _deps(self, e, is_dma, reads, writes):
        for t in reads:
            for p in t.writers:
                if (not p.is_dma) and (not is_dma) and p.eng == e and e == "pe":
                    continue
                self._wait(e, p.sem, p.val)
        for t in writes:
            for p in t.writers + t.readers:
                if (not p.is_dma) and (not is_dma) and p.eng == e and e == "pe":
                    continue
                self._wait(e, p.sem, p.val)

    def _record(self, op, reads, writes):
        for t in reads:
            if not op.is_dma:
                t.readers = [r for r in t.readers if r.is_dma or r.eng != op.eng]
            t.readers.append(op)
        for t in writes:
            if t.readers:
                t.writers = [op]
                t.readers = []
            else:
                if not op.is_dma:
                    t.writers = [w for w in t.writers if w.is_dma or w.eng != op.eng]
                t.writers.append(op)

    def op(self, e, fn, reads=(), writes=()):
        self._deps(e, False, reads, writes)
        n = self.ecount[e]
        sem = self._esem(e, n // SEM_CHUNK)
        val = (n % SEM_CHUNK) + 1
        fn(self.eobj[e]).then_inc(sem, 1)
        self.ecount[e] = n + 1
        o = Op()
        o.eng, o.sem, o.val, o.is_dma = e, sem, val, False
        self._record(o, reads, writes)
        return o

    def dma(self, q, out, in_, reads=(), writes=(), **kw):
        if q not in self.dma_sems:
            self.dma_sems[q] = [[self.stack.enter_context(self.nc.semaphore(f"d_{q}_{i}")), 0]
                                for i in range(self.n_dma_sems)]
            self.dma_rr[q] = 0
        self._deps(q, True, reads, writes)
        i = self.dma_rr[q]
        self.dma_rr[q] = (i + 1) % self.n_dma_sems
        ent = self.dma_sems[q][i]
        sem, cur = ent
        if cur > 0:
            self._wait(q, sem, cur)
        val = cur + 16
        ent[1] = val
        self.eobj[q].dma_start(out=out, in_=in_, **kw).then_inc(sem, 16)
        o = Op()
        o.eng, o.sem, o.val, o.is_dma = q, sem, val, True
        self._record(o, reads, writes)
        return o

    def barrier(self):
        for e in self.ENGS:
            for f in self.ENGS:
                n = self.ecount[f]
                if f == e or n == 0:
                    continue
                self._wait(e, self.esems[f][(n - 1) // SEM_CHUNK], (n - 1) % SEM_CHUNK + 1)
            for q, lst in self.dma_sems.items():
                for sem, cur in lst:
                    if cur > 0:
                        self._wait(e, sem, cur)

    def wait_all(self, e, toks):
        for t in toks:
            for p in t.writers + t.readers:
                self._wait(e, p.sem, p.val)


def _rel_bucket_np(n):
    n = np.maximum(n, 0)
    exact = 16
    lr = np.log(np.maximum(n, 1).astype(np.float32) / np.float32(exact)) / np.float32(math.log(128 / exact))
    large = exact + (lr * np.float32(32 - exact)).astype(np.int32)
    return np.where(n < exact, n, np.minimum(large, 31))


def _constants():
    c = {}
    npr = np.arange(4096)
    n = npr - 2048
    bk = _rel_bucket_np(n)
    oh = np.zeros((33, 2, 4096), np.float32)
    okw = (n >= 0) & (n < 512)
    oks = (n >= 0)
    oh[bk[okw], 0, npr[okw]] = 1.0
    oh[32, 0, npr[~okw]] = 1.0
    oh[bk[oks], 1, npr[oks]] = 1.0
    oh[32, 1, npr[~oks]] = 1.0
    c["oh"] = oh
    e32 = (np.arange(2048)[None, :] // 64 == np.arange(32)[:, None]).astype(np.float32)
    e128 = np.zeros((128, 2048), np.float32)
    e128[0:32] = e32
    e128[64:96] = e32
    c["Esel"] = e128
    cs = np.arange(127) * 16
    ss = np.arange(32) * 64
    c["Cov"] = ((cs[:, None] < ss[None, :] + 64) & (cs[:, None] + 32 > ss[None, :])).astype(np.float32)
    t = np.arange(2048)
    cur = t // 64
    j = np.arange(32)
    forced = (j[None, :] == 0) | (j[None, :] == cur[:, None]) | (j[None, :] == cur[:, None] - 1)
    allowed = j[None, :] * 64 <= t[:, None]
    mul = (allowed & ~forced).astype(np.float32)
    add = np.where(forced, 1e4, np.where(allowed, 0.0, -1e30)).astype(np.float32)
    c["selmul"] = np.ascontiguousarray(mul.reshape(16, 128, 32).transpose(1, 0, 2))
    c["seladd"] = np.ascontiguousarray(add.reshape(16, 128, 32).transpose(1, 0, 2))
    c["identf"] = np.eye(128, dtype=np.float32)
    c["ones"] = np.ones((128, 128), np.float32)
    c["iota"] = np.tile(np.arange(128, dtype=np.float32)[None, :], (128, 1))
    sel = np.zeros((8, 128), np.float32)
    for h in range(8):
        sel[h, h * 16:(h + 1) * 16] = 1.0
    c["selh"] = np.concatenate([sel, sel], axis=0)
    return c


def _blockdiag2(w):
    out = np.zeros(w.shape[:-2] + (128, 128), np.float32)
    out[..., :64, :64] = w
    out[..., 64:, 64:] = w
    return out


def _prep(inp):
    sh = {}
    f = lambda a: np.ascontiguousarray(np.asarray(a, np.float32))
    sh["w_mod"] = f(inp["w_mod"][0])
    sh["bmodT"] = f(inp["b_mod"][0].reshape(48, 128).T)
    bm = inp["b_mod"][0]
    sh["bmod_bc"] = f(np.broadcast_to(np.stack([bm[2048:3072], bm[5120:6144]])[None], (128, 2, 1024)))
    sh["gmix"] = f(inp["ln_mix_g"][0].reshape(8, 128).T)
    sh["gffn"] = f(inp["ln_ffn_g"][0].reshape(8, 128).T)
    sh["gfin_bc"] = f(np.broadcast_to(inp["ln_final_g"][None, :], (128, 1024)))
    w_in = np.asarray(inp["w_in"][0], np.float32)
    sp = np.cumsum([0, 512, 128, 128, 128, 128, 128, 128, 24, 512, 512, 512])
    q, kc, vc, ks, vs, kw, vw, gt, cb, cc, ch = [w_in[:, sp[i]:sp[i + 1]] for i in range(11)]
    qh = q.reshape(1024, 8, 64)
    qperm = np.concatenate([np.concatenate([qh[:, j], qh[:, 4 + j]], axis=1) for j in range(4)], axis=1)
    cols = [qperm, kc, vc, ks, kw]
    for c4 in range(4):
        cols += [cb[:, c4 * 128:(c4 + 1) * 128], cc[:, c4 * 128:(c4 + 1) * 128], ch[:, c4 * 128:(c4 + 1) * 128]]
    cols += [vs, vw, gt]
    sh["w_in"] = f(np.concatenate(cols, axis=1))
    for nm, w1, w2, pe in (("k", "cmp_wk1", "cmp_wk2", "cmp_pe_k"), ("v", "cmp_wv1", "cmp_wv2", "cmp_pe_v")):
        a = np.asarray(inp[w1][0], np.float32).reshape(32, 64, 64)
        sh["bd1" + nm] = f(_blockdiag2(a).transpose(1, 0, 2))
        sh["bd2" + nm] = f(_blockdiag2(np.asarray(inp[w2][0], np.float32)))
        p = np.asarray(inp[pe][0], np.float32).T
        sh["pe2" + nm] = f(np.concatenate([p, p], axis=0))
    sh["convw"] = f(inp["conv_w"][0][:, 0, :].reshape(3, 4, 128).transpose(2, 1, 0))
    sh["gattn_bc"] = f(np.broadcast_to(inp["norm_attn_g"][0][None, :], (128, 512)))
    sh["gconv"] = f(inp["norm_conv_g"][0].reshape(4, 128).T)
    sh["w_out"] = f(inp["w_out"][0])
    sh["peer_wq"] = f(inp["peer_wq"][0])
    pk = np.asarray(inp["peer_keys"][0], np.float32)
    kb = np.zeros((128, 8, 256), np.float32)
    for h in range(8):
        for p in range(2):
            kb[p * 64:(p + 1) * 64, h, p * 128:(p + 1) * 128] = pk[h, p].T
    sh["peer_kb"] = kb
    u = np.asarray(inp["peer_u"][0], np.float32)
    sh["peer_ut"] = f(u.reshape(128, 128, 8, 128).transpose(0, 3, 2, 1).reshape(128 * 128, 1024))
    sh["peer_v"] = f(inp["peer_v"][0])
    rt = np.zeros((33, 8), np.float32)
    rt[:32] = inp["rel_table"]
    rt[32] = NEG
    sh["relt"] = rt
    sh.update(_constants())
    return sh


def build_nc(nseq=2, dbg=False, stop_after=99):
    nc = bass.Bass("TRN2", target_bir_lowering=False)
    Tok.registry = []
    T = nseq * S
    NTT = nseq * NT

    def din(name, shape, dt=F32):
        return nc.dram_tensor(name, list(shape), dt, kind="ExternalInput").ap()

    def dscr(name, shape, dt=F32):
        return nc.dram_tensor(name, list(shape), dt, kind="Internal").ap()

    def dout(name, shape, dt=F32):
        return nc.dram_tensor(name, list(shape), dt, kind="ExternalOutput").ap()

    x_d = din("x", [T, D])
    cT_d = din("cT", [128, 8, nseq])
    w_mod_d = din("w_mod", [1024, 6144])
    bmodT_d = din("bmodT", [128, 48])
    bmod_bc_d = din("bmod_bc", [128, 2, 1024])
    gmix_d = din("gmix", [128, 8])
    gffn_d = din("gffn", [128, 8])
    gfin_d = din("gfin_bc", [128, 1024])
    w_in_d = din("w_in", [1024, 2840])
    bd1_d = {n: din("bd1" + n, [128, 32, 128]) for n in "kv"}
    bd2_d = {n: din("bd2" + n, [128, 128]) for n in "kv"}
    pe2_d = {n: din("pe2" + n, [128, 32]) for n in "kv"}
    convw_d = din("convw", [128, 4, 3])
    gattn_d = din("gattn_bc", [128, 512])
    gconv_d = din("gconv", [128, 4])
    w_out_d = din("w_out", [1024, 1024])
    wq_d = din("peer_wq", [1024, 1024])
    kb_d = din("peer_kb", [128, 8, 256])
    ut_d = din("peer_ut", [128 * 128, 1024])
    pv_d = din("peer_v", [16384, 1024])
    relt_d = din("relt", [33, 8])
    oh_d = din("oh", [33, 2, 4096])
    esel_d = din("Esel", [128, 2048])
    cov_d = din("Cov", [127, 32])
    selmul_d = din("selmul", [128, 16, 32])
    seladd_d = din("seladd", [128, 16, 32])
    identf_d = din("identf", [128, 128])
    ones_d = din("ones", [128, 128])
    iota_d = din("iota", [128, 128])
    selh_d = din("selh", [16, 128])
    out_d = dout("out", [T, D])

    tb_d = dscr("tb_s", [2, 8, 4096])
    LW = 1024
    fw_d = dscr("fw_s", [2, 8, 128 * (LW + 1)])
    LC = 4096
    fc_d = dscr("fc_s", [8, 127 * (LC + 16)])
    gt_d = dscr("gt_s", [2, nseq, 128, 1024])
    if dbg:
        x1_d = dout("x1_s", [T, D])
        h2T_d = dout("h2T_s", [128, 8, T], BF16)
    else:
        x1_d = dscr("x1_s", [T, D])
        h2T_d = dscr("h2T_s", [128, 8, T], BF16)
    t_x1d = [Tok() for _ in range(NTT)]
    t_h2d = [Tok() for _ in range(NTT)]

    dbg_out = {}
    uniq = [0]

    with ExitStack() as st0:
        P = Prog(nc, st0)
        scopes = [st0]

        def sb(name, shape, dt=F32):
            uniq[0] += 1
            return scopes[-1].enter_context(nc.sbuf_tensor(f"sb{uniq[0]}_{name}", list(shape), dt))

        class Scope:
            def __enter__(self):
                self.st = ExitStack()
                scopes.append(self.st)
                return self

            def __exit__(self, *a):
                if a[0] is None:
                    P.barrier()
                scopes.pop()
                self.st.close()
                return False

        psg = [st0.enter_context(nc.psum_tensor(f"psum{i}", [128, 512], F32)) for i in range(3)]
        psO = st0.enter_context(nc.psum_tensor("psumO", [128, 4, 512], F32))
        ps = [psg[0][:, :], psg[1][:, :], psg[2][:, :]] + [psO[:, i, :] for i in range(4)]
        tps = [Tok() for _ in range(7)]
        psb = st0.enter_context(nc.psum_tensor("psumb", [128, 1024], BF16))
        tpsb = Tok()

        def load_const(name, src, shape, dt=F32, q="sp"):
            t = sb(name, shape, dt)
            k = Tok()
            P.dma(q, t[:], src, writes=[k])
            return t, k

        def dbgdump(name, shape, dt, src_ap, toks):
            if not dbg:
                return
            dbg_out[name] = dout("dbg_" + name, shape, dt)
            P.dma("sp", dbg_out[name], src_ap, reads=toks)

        identb, t_identb = load_const("identb", identf_d, [128, 128], BF16, "pool")
        onesb, t_onesb = load_const("onesb", ones_d, [128, 128], BF16, "pool")
        gmix, t_gmix = load_const("gmix", gmix_d, [128, 8])
        gffn, t_gffn = load_const("gffn", gffn_d, [128, 8])
        bmodT, t_bmodT = load_const("bmodT", bmodT_d, [128, 48])
        convw, t_convw = load_const("convw", convw_d, [128, 4, 3])
        gconv, t_gconv = load_const("gconv", gconv_d, [128, 4])
        relt, t_relt = load_const("relt", relt_d, [33, 8])
        modT = sb("modT", [128, 48, nseq]); t_modT = Tok()
        gs1 = sb("gs1", [128, 8, nseq]); gs2 = sb("gs2", [128, 8, nseq]); t_gs = Tok()
        t_gtd = Tok()
        t_fw = Tok(); t_fc = Tok()

        with Scope():
            cT = sb("cT", [128, 8, nseq]); t_cT = Tok()
            P.dma("sp", cT[:], cT_d, writes=[t_cT])
            c_act = sb("c_act", [128, 8, nseq]); t_cact = Tok()
            P.op("act", lambda e: e.activation(out=c_act[:], in_=cT[:], func=AF.Silu), reads=[t_cT], writes=[t_cact])
            c_rep = sb("c_rep", [128, 8, nseq, 128]); t_crep = Tok()
            P.op("dve", lambda e: e.tensor_copy(out=c_rep[:], in_=c_act[:].unsqueeze(3).broadcast_to([128, 8, nseq, 128])),
                 reads=[t_cact], writes=[t_crep])
            bmod_bc = sb("bmod_bc", [128, 2, 1024]); t_bmbc = Tok()
            P.dma("sp", bmod_bc[:], bmod_bc_d, writes=[t_bmbc])
            gstage = [sb(f"gstage{i}", [128, nseq, 128]) for i in range(2)]; t_gst = [Tok(), Tok()]
            wm_view = w_mod_d.rearrange("(k p) n -> p k n", p=128)
            wmp = [sb(f"wm{i}", [128, 8, 128]) for i in range(3)]
            t_wmp = [Tok() for _ in range(3)]
            for j in range(48):
                wt, tw = wmp[j % 3], t_wmp[j % 3]
                P.dma("sp", wt[:], wm_view[:, :, j * 128:(j + 1) * 128], writes=[tw])
                for k in range(8):
                    P.op("pe", lambda e, k=k, wt=wt: e.matmul(ps[0][:, j * nseq:(j + 1) * nseq], lhsT=wt[:, k, :],
                                                              rhs=c_act[:, k, :], start=(k == 0), stop=(k == 7)),
                         reads=[tw, t_cact], writes=[tps[0]])
                which = {2: 0, 5: 1}.get(j // 8)
                if which is not None:
                    jj = j % 8
                    bi = 1 + (j % 2)
                    for b in range(nseq):
                        for k in range(8):
                            P.op("pe", lambda e, k=k, b=b, wt=wt, bi=bi: e.matmul(
                                ps[bi][:, b * 128:(b + 1) * 128], lhsT=c_rep[:, k, b, :], rhs=wt[:, k, :],
                                start=(k == 0), stop=(k == 7)), reads=[tw, t_crep], writes=[tps[bi]])
                    gsb, tg = gstage[j % 2], t_gst[j % 2]
                    P.op("dve", lambda e, bi=bi, which=which, jj=jj, gsb=gsb: e.tensor_tensor(
                        out=gsb[:],
                        in0=ps[bi][:, 0:nseq * 128].rearrange("p (b n) -> p b n", b=nseq),
                        in1=bmod_bc[:, which, jj * 128:(jj + 1) * 128].unsqueeze(1).broadcast_to([128, nseq, 128]),
                        op=ALU.add), reads=[tps[bi], t_bmbc], writes=[tg])
                    P.dma("sp", gt_d[which].rearrange("b p n -> p b n")[:, :, jj * 128:(jj + 1) * 128], gsb[:],
                          reads=[tg], writes=[t_gtd])
            P.op("dve", lambda e: e.tensor_tensor(
                out=modT[:], in0=ps[0][:, 0:48 * nseq].rearrange("p (j b) -> p j b", b=nseq),
                in1=bmodT[:].unsqueeze(2).broadcast_to([128, 48, nseq]), op=ALU.add),
                reads=[tps[0], t_bmodT], writes=[t_modT])
            P.op("dve", lambda e: e.scalar_tensor_tensor(
                out=gs1[:], in0=modT[:, 8:16, :], scalar=1.0, in1=gmix[:].unsqueeze(2).broadcast_to([128, 8, nseq]),
                op0=ALU.add, op1=ALU.mult), reads=[t_modT, t_gmix], writes=[t_gs])
            P.op("dve", lambda e: e.scalar_tensor_tensor(
                out=gs2[:], in0=modT[:, 32:40, :], scalar=1.0, in1=gffn[:].unsqueeze(2).broadcast_to([128, 8, nseq]),
                op0=ALU.add, op1=ALU.mult), reads=[t_modT, t_gffn], writes=[t_gs])
            dbgdump("modT", [128, 48, nseq], F32, modT[:], [t_modT])

            ohp = [sb(f"ohp{i}", [33, 512]) for i in range(2)]
            t_ohp = [Tok(), Tok()]
            tbs = [sb(f"tbs{i}", [8, 512]) for i in range(2)]
            t_tbs = [Tok(), Tok()]
            t_tbd = Tok()
            for kind in range(2):
                for pc in range(8):
                    i = (kind * 8 + pc) % 2
                    P.dma("sp", ohp[i][:], oh_d[:, kind, pc * 512:(pc + 1) * 512], writes=[t_ohp[i]])
                    bi = 3 + i
                    P.op("pe", lambda e, i=i, bi=bi: e.matmul(ps[bi][0:8, :], lhsT=relt[:, :], rhs=ohp[i][:, :],
                                                               start=True, stop=True),
                         reads=[t_relt, t_ohp[i]], writes=[tps[bi]])
                    P.op("act", lambda e, i=i, bi=bi: e.copy(out=tbs[i][:], in_=ps[bi][0:8, :]),
                         reads=[tps[bi]], writes=[t_tbs[i]])
                    P.dma("sp", tb_d[kind, :, pc * 512:(pc + 1) * 512], tbs[i][:], reads=[t_tbs[i]], writes=[t_tbd])
            for kind in range(2):
                for h in range(8):
                    src = bass.AP(tb_d.tensor, (kind * 8 + h) * 4096 + (2048 - 128), [[0, 128], [1, LW]])
                    dst = bass.AP(fw_d.tensor, (kind * 8 + h) * 128 * (LW + 1), [[LW + 1, 128], [1, LW]])
                    P.dma("sp", dst, src, reads=[t_tbd], writes=[t_fw])
            for h in range(8):
                src = bass.AP(tb_d.tensor, (8 + h) * 4096, [[0, 127], [1, LC]])
                dst = bass.AP(fc_d.tensor, h * 127 * (LC + 16), [[LC + 16, 127], [1, LC]])
                P.dma("sp", dst, src, reads=[t_tbd], writes=[t_fc])

        with Scope():
            qT = sb("qT", [128, 4, S], BF16); t_qT = [Tok() for _ in range(4)]
            kT = {n: sb(n + "T", [128, S], BF16) for n in ("kc", "vc", "ks", "kw")}
            t_kT = {n: [Tok() for _ in range(4)] for n in kT}
            vs_aug = sb("vs_aug", [128, NT, 2, 65], BF16); vw_aug = sb("vw_aug", [128, NT, 2, 65], BF16)
            t_va = [Tok() for _ in range(NT)]
            gat = sb("gat", [128, NT, 24]); t_gat = [Tok() for _ in range(NT)]
            mixT = sb("mixT", [128, 8, S], BF16)
            t_mixc = [Tok() for _ in range(4)]
            t_mixa = [Tok() for _ in range(NT)]
            t_ones = Tok()
            P.op("pool", lambda e: e.memset(vs_aug[:, :, :, 64:65], 1.0), writes=[t_ones])
            P.op("pool", lambda e: e.memset(vw_aug[:, :, :, 64:65], 1.0), writes=[t_ones])
            ssq = sb("ssq", [128, NTT]); rstd = sb("rstd", [128, NTT]); t_st = [Tok() for _ in range(NTT)]
            ssq2 = sb("ssq2", [128, NTT]); rstd2 = sb("rstd2", [128, NTT]); t_st2 = [Tok() for _ in range(NTT)]
            ssqa = sb("ssqa", [128, NTT]); rstda = sb("rstda", [128, NTT]); t_sta = [Tok() for _ in range(NTT)]

            def rms_stats(src_ap, t_src, junk_, t_junk_, ssq_, rstd_, tk, col, width):
                P.op("act", lambda e: e.activation(out=junk_, in_=src_ap, func=AF.Square, accum_out=ssq_[:, col:col + 1]),
                     reads=[t_src], writes=[t_junk_, tk])
                P.op("dve", lambda e: e.tensor_scalar(out=rstd_[:, col:col + 1], in0=ssq_[:, col:col + 1], scalar1=1.0 / width,
                                                      scalar2=EPS, op0=ALU.mult, op1=ALU.add), reads=[tk], writes=[tk])
                P.op("act", lambda e: e.activation(out=rstd_[:, col:col + 1], in_=rstd_[:, col:col + 1], func=AF.Sqrt),
                     reads=[tk], writes=[tk])
                P.op("dve", lambda e: e.reciprocal(out=rstd_[:, col:col + 1], in_=rstd_[:, col:col + 1]),
                     reads=[tk], writes=[tk])

            def norm_transpose(xt, t_xt, gi, s, ssq_, rstd_, t_st_, gs_, sh_lo, dst, t_dst, col0, junk, t_junk, xs, t_xs):
                rms_stats(xt[:], t_xt, junk[:], t_junk, ssq_, rstd_, t_st_[gi], gi, D)
                P.op("dve", lambda e: e.tensor_scalar(out=xs[:], in0=xt[:], scalar1=rstd_[:, gi:gi + 1], scalar2=None,
                                                      op0=ALU.mult), reads=[t_xt, t_st_[gi]], writes=[t_xs])
                for c in range(8):
                    P.op("pe", lambda e, c=c: e.transpose(out=psb[:, c * 128:(c + 1) * 128], in_=xs[:, c * 128:(c + 1) * 128],
                                                          identity=identb[:]), reads=[t_xs, t_identb], writes=[tpsb])
                for c in range(8):
                    o_ap = dst[:, c, col0:col0 + 128]
                    i_ap = psb[:, c * 128:(c + 1) * 128]
                    sc_ap = gs_[:, c, s:s + 1]
                    bi_ap = modT[:, sh_lo + c, s:s + 1]
                    if gi % 2 == 0:
                        P.op("dve", lambda e, o_ap=o_ap, i_ap=i_ap, sc_ap=sc_ap, bi_ap=bi_ap: e.tensor_scalar(
                            out=o_ap, in0=i_ap, scalar1=sc_ap, scalar2=bi_ap, op0=ALU.mult, op1=ALU.add),
                            reads=[tpsb, t_gs, t_modT], writes=[t_dst])
                    else:
                        P.op("act", lambda e, o_ap=o_ap, i_ap=i_ap, sc_ap=sc_ap, bi_ap=bi_ap: e.activation(
                            out=o_ap, in_=i_ap, func=AF.Identity, scale=sc_ap, bias=bi_ap),
                            reads=[tpsb, t_gs, t_modT], writes=[t_dst])

            def evac(i, out_ap, in_ap, reads, writes):
                if i % 2:
                    P.op("act", lambda e: e.copy(out=out_ap, in_=in_ap), reads=reads, writes=writes)
                else:
                    P.op("dve", lambda e: e.tensor_copy(out=out_ap, in_=in_ap), reads=reads, writes=writes)

            for s in range(nseq):
                with Scope():
                    win = sb("win", [128, 8, 2840], BF16); t_win = Tok()
                    wiv = w_in_d.rearrange("(k p) n -> p k n", p=128)
                    for k in range(8):
                        P.dma("pool", win[:, k, :], wiv[:, k, :], writes=[t_win])
                    xpool = [sb(f"xt{i}", [128, 1024]) for i in range(2)]; t_xp = [Tok(), Tok()]
                    junk = sb("junk", [128, 1024], BF16); t_junk = Tok()
                    xs = sb("xs", [128, 1024], BF16); t_xs = Tok()
                    hTg = [sb(f"hTg{i}", [128, 8, 512], BF16) for i in range(2)]; t_hTg = [Tok(), Tok()]
                    cbs = sb("cbs", [128, 512]); ccs = sb("ccs", [128, 512]); t_cbs = Tok(); t_ccs = Tok()
                    zb = sb("zb", [128, 4, 514]); t_zb = [Tok() for _ in range(4)]
                    yb = sb("yb", [128, 512]); t_yb = Tok()
                    oc = sb("oc", [128, 4, 512]); t_oc = [Tok() for _ in range(4)]
                    osq = sb("osq", [128, 4, 512], BF16); t_osq = [Tok() for _ in range(4)]
                    rbc = sb("rbc", [128, 512]); t_rbc = Tok()
                    for grp in range(4):
                        hT, t_hT = hTg[grp % 2], t_hTg[grp % 2]
                        s0 = grp * 512
                        for tl in range(4):
                            ti = grp * 4 + tl
                            gi = s * NT + ti
                            xt, t_xt = xpool[gi % 2], t_xp[gi % 2]
                            P.dma("sp", xt[:], x_d[gi * 128:(gi + 1) * 128, :], writes=[t_xt])
                            norm_transpose(xt, t_xt, gi, s, ssq, rstd, t_st, gs1, 0, hT, t_hT, tl * 128, junk, t_junk, xs, t_xs)
                        if s == 0 and grp == 0:
                            dbgdump("hT0", [128, 8, 512], BF16, hT[:], [t_hT])

                        def proj_chunk(cc, bi):
                            for k in range(8):
                                P.op("pe", lambda e, k=k: e.matmul(ps[bi][:, :], lhsT=win[:, k, cc * 128:(cc + 1) * 128],
                                                                   rhs=hT[:, k, :], start=(k == 0), stop=(k == 7)),
                                     reads=[t_win, t_hT], writes=[tps[bi]])

                        nb = 0
                        for cc in range(4):
                            bi = nb % 3; nb += 1
                            proj_chunk(cc, bi)
                            evac(cc, qT[:, cc, s0:s0 + 512], ps[bi][:, :], [tps[bi]], [t_qT[grp]])
                        for idx, n in enumerate(("kc", "vc", "ks", "kw")):
                            bi = nb % 3; nb += 1
                            proj_chunk(4 + idx, bi)
                            evac(idx, kT[n][:, s0:s0 + 512], ps[bi][:, :], [tps[bi]], [t_kT[n][grp]])
                        for c4 in range(4):
                            b_cb = nb % 3; nb += 1
                            proj_chunk(8 + c4 * 3 + 0, b_cb)
                            P.op("act", lambda e, b=b_cb: e.copy(out=cbs[:], in_=ps[b][:, :]), reads=[tps[b_cb]], writes=[t_cbs])
                            b_cc = nb % 3; nb += 1
                            proj_chunk(8 + c4 * 3 + 1, b_cc)
                            P.op("act", lambda e, b=b_cc: e.copy(out=ccs[:], in_=ps[b][:, :]), reads=[tps[b_cc]], writes=[t_ccs])
                            b_ch = nb % 3; nb += 1
                            proj_chunk(8 + c4 * 3 + 2, b_ch)
                            if grp == 0:
                                P.op("dve", lambda e, c4=c4: e.memset(zb[:, c4, 0:2], 0.0), writes=[t_zb[c4]])
                            P.op("dve", lambda e, c4=c4, b=b_ch: e.tensor_tensor(out=zb[:, c4, 2:514], in0=ps[b][:, :], in1=ccs[:],
                                                                                 op=ALU.mult),
                                 reads=[tps[b_ch], t_ccs], writes=[t_zb[c4]])
                            P.op("dve", lambda e, c4=c4: e.tensor_scalar(out=yb[:], in0=zb[:, c4, 2:514], scalar1=convw[:, c4, 2:3],
                                                                         scalar2=None, op0=ALU.mult),
                                 reads=[t_zb[c4], t_convw], writes=[t_yb])
                            P.op("dve", lambda e, c4=c4: e.scalar_tensor_tensor(out=yb[:], in0=zb[:, c4, 1:513], scalar=convw[:, c4, 1:2],
                                                                                in1=yb[:], op0=ALU.mult, op1=ALU.add),
                                 reads=[t_zb[c4], t_convw, t_yb], writes=[t_yb])
                            P.op("dve", lambda e, c4=c4: e.scalar_tensor_tensor(out=yb[:], in0=zb[:, c4, 0:512], scalar=convw[:, c4, 0:1],
                                                                                in1=yb[:], op0=ALU.mult, op1=ALU.add),
                                 reads=[t_zb[c4], t_convw, t_yb], writes=[t_yb])
                            P.op("dve", lambda e, c4=c4: e.tensor_tensor(out=oc[:, c4, :], in0=yb[:], in1=cbs[:], op=ALU.mult),
                                 reads=[t_yb, t_cbs], writes=[t_oc[c4]])
                            P.op("dve", lambda e, c4=c4: e.tensor_copy(out=zb[:, c4, 0:2], in_=zb[:, c4, 512:514]),
                                 reads=[t_zb[c4]], writes=[t_zb[c4]])
                            P.op("act", lambda e, c4=c4: e.activation(out=osq[:, c4, :], in_=oc[:, c4, :], func=AF.Square),
                                 reads=[t_oc[c4]], writes=[t_osq[c4]])
                        for c4 in range(4):
                            P.op("pe", lambda e, c4=c4: e.matmul(ps[3][:, :], lhsT=onesb[:, :], rhs=osq[:, c4, :],
                                                                 start=(c4 == 0), stop=(c4 == 3)),
                                 reads=[t_onesb, t_osq[c4]], writes=[tps[3]])
                        P.op("dve", lambda e: e.tensor_scalar(out=rbc[:], in0=ps[3][:, :], scalar1=1.0 / 512, scalar2=EPS,
                                                              op0=ALU.mult, op1=ALU.add), reads=[tps[3]], writes=[t_rbc])
                        P.op("act", lambda e: e.activation(out=rbc[:], in_=rbc[:], func=AF.Sqrt), reads=[t_rbc], writes=[t_rbc])
                        P.op("dve", lambda e: e.reciprocal(out=rbc[:], in_=rbc[:]), reads=[t_rbc], writes=[t_rbc])
                        for c4 in range(4):
                            P.op("dve", lambda e, c4=c4: e.scalar_tensor_tensor(
                                out=mixT[:, 4 + c4, s0:s0 + 512], in0=oc[:, c4, :], scalar=gconv[:, c4:c4 + 1], in1=rbc[:],
                                op0=ALU.mult, op1=ALU.mult), reads=[t_oc[c4], t_gconv, t_rbc], writes=[t_mixc[grp]])
                        for tl in range(4):
                            ti = grp * 4 + tl
                            bi = 4 + (tl % 2)
                            for k in range(8):
                                P.op("pe", lambda e, k=k, tl=tl, bi=bi: e.matmul(
                                    ps[bi][:, 0:280], lhsT=hT[:, k, tl * 128:(tl + 1) * 128], rhs=win[:, k, 2560:2840],
                                    start=(k == 0), stop=(k == 7)), reads=[t_win, t_hT], writes=[tps[bi]])
                            P.op("act", lambda e, ti=ti, bi=bi: e.copy(
                                out=vs_aug[:, ti, :, 0:64], in_=ps[bi][:, 0:128].rearrange("p (g d) -> p g d", g=2)),
                                reads=[tps[bi]], writes=[t_va[ti]])
                            P.op("act", lambda e, ti=ti, bi=bi: e.copy(
                                out=vw_aug[:, ti, :, 0:64], in_=ps[bi][:, 128:256].rearrange("p (g d) -> p g d", g=2)),
                                reads=[tps[bi]], writes=[t_va[ti]])
                            P.op("act", lambda e, ti=ti, bi=bi: e.activation(out=gat[:, ti, :], in_=ps[bi][:, 256:280],
                                                                             func=AF.Sigmoid),
                                 reads=[tps[bi]], writes=[t_gat[ti]])
                    if s == 0:
                        dbgdump("qT", [128, 4, S], BF16, qT[:], t_qT)
                        dbgdump("gat", [128, NT, 24], F32, gat[:], t_gat)
                        dbgdump("vs", [128, NT, 2, 65], BF16, vs_aug[:], t_va + [t_ones])
                        if stop_after <= 1:
                            dbgdump("mixT", [128, 8, S], BF16, mixT[:], t_mixc + t_mixa)
                if stop_after <= 1:
                    continue

                with Scope():
                    bd1 = {}; bd2 = {}; pe2 = {}; t_cw = Tok()
                    for n in "kv":
                        bd1[n] = sb("bd1" + n, [128, 32, 128], BF16)
                        P.dma("pool", bd1[n][:], bd1_d[n], writes=[t_cw])
                        bd2[n] = sb("bd2" + n, [128, 128], BF16)
                        P.dma("pool", bd2[n][:], bd2_d[n], writes=[t_cw])
                        pe2[n] = sb("pe2" + n, [128, 32], BF16)
                        P.dma("pool", pe2[n][:], pe2_d[n], writes=[t_cw])
                    Bw = sb("Bw", [128, 5, 8, 128], BF16); t_Bw = Tok()
                    Bs = sb("Bs", [128, 3, 8, 128], BF16); t_Bs = Tok()
                    for dl in range(5):
                        src = bass.AP(fw_d.tensor, 128 + dl * 128, [[LW, 128], [128 * (LW + 1), 8], [1, 128]])
                        P.dma("pool", Bw[:, dl, :, :], src, reads=[t_fw], writes=[t_Bw])
                    for dl in range(3):
                        src = bass.AP(fw_d.tensor, 8 * 128 * (LW + 1) + 128 + dl * 128,
                                      [[LW, 128], [128 * (LW + 1), 8], [1, 128]])
                        P.dma("pool", Bs[:, dl, :, :], src, reads=[t_fw], writes=[t_Bs])
                    if s == 0:
                        dbgdump("Bw", [128, 5, 8, 128], BF16, Bw[:], [t_Bw])
                        dbgdump("Bs", [128, 3, 8, 128], BF16, Bs[:], [t_Bs])
                    esel, t_esel = load_const("esel", esel_d, [128, 2048], BF16, "pool")
                    selmul, t_selmul = load_const("selmul", selmul_d, [128, 16, 32])
                    seladd, t_seladd = load_const("seladd", seladd_d, [128, 16, 32])
                    gattn, t_gattn = load_const("gattn", gattn_d, [128, 512])
                    cbias = sb("cbias", [128, 2]); t_cbias = Tok()
                    for i, n in enumerate("kv"):
                        for l in range(32):
                            P.op("pe", lambda e, n=n, l=l, i=i: e.matmul(ps[2][:, i:i + 1], lhsT=bd1[n][:, l, :],
                                                                         rhs=pe2[n][:, l:l + 1], start=(l == 0), stop=(l == 31)),
                                 reads=[t_cw], writes=[tps[2]])
                    P.op("dve", lambda e: e.tensor_copy(out=cbias[:], in_=ps[2][:, 0:2]), reads=[tps[2]], writes=[t_cbias])
                    kcmpT = sb("kcmpT", [128, 128], BF16); t_kcmp = Tok()
                    vc_aug = sb("vc_aug", [128, 2, 97], BF16); t_vca = Tok()
                    for g in range(2):
                        P.dma("pool", vc_aug[0:127, g, 65:97], cov_d, writes=[t_vca])
                    P.op("dve", lambda e: e.memset(vc_aug[:, :, 64:65], 1.0), writes=[t_vca])
                    hid = {n: sb("hid" + n, [128, 128], BF16) for n in "kv"}; t_hid = Tok()
                    for i, (n, srcn) in enumerate((("k", "kc"), ("v", "vc"))):
                        src = kT[srcn]
                        for l in range(32):
                            P.op("pe", lambda e, n=n, l=l, i=i, src=src: e.matmul(
                                ps[i][:, 0:127], lhsT=bd1[n][:, l, :], rhs=src[:, l:l + 2017:16],
                                start=(l == 0), stop=(l == 31)), reads=[t_cw] + t_kT[srcn], writes=[tps[i]])
                        P.op("act", lambda e, n=n, i=i: e.activation(out=hid[n][:, 0:127], in_=ps[i][:, 0:127],
                                                                     func=AF.Gelu_apprx_tanh, bias=cbias[:, i:i + 1]),
                             reads=[tps[i], t_cbias], writes=[t_hid])
                    P.op("pe", lambda e: e.matmul(ps[0][:, 0:127], lhsT=bd2["k"][:, :], rhs=hid["k"][:, 0:127], start=True, stop=True),
                         reads=[t_cw, t_hid], writes=[tps[0]])
                    P.op("dve", lambda e: e.tensor_copy(out=kcmpT[:, 0:127], in_=ps[0][:, 0:127]), reads=[tps[0]], writes=[t_kcmp])
                    P.op("pe", lambda e: e.matmul(ps[1][0:127, 0:128], lhsT=hid["v"][:, 0:127], rhs=bd2["v"][:, :], start=True, stop=True),
                         reads=[t_cw, t_hid], writes=[tps[1]])
                    P.op("dve", lambda e: e.tensor_copy(out=vc_aug[0:127, :, 0:64],
                                                        in_=ps[1][0:127, 0:128].rearrange("p (g d) -> p g d", g=2)),
                         reads=[tps[1]], writes=[t_vca])
                    if s == 0:
                        dbgdump("kcmpT", [128, 128], BF16, kcmpT[:], [t_kcmp])
                        dbgdump("vc_aug", [128, 2, 97], BF16, vc_aug[:], [t_vca])

                    bcp = [sb(f"bcp{i}", [128, 8, 128], BF16) for i in range(2)]; t_bcp = [Tok(), Tok()]
                    tS = [sb(f"tS{i}", [128, 512]) for i in range(2)]; t_tS = [Tok(), Tok()]
                    pT = [sb(f"pT{i}", [128, 512], BF16) for i in range(2)]; t_pT = [Tok(), Tok()]
                    o_acc = [sb(f"oacc{i}", [128, 8, 64]) for i in range(2)]; t_oacc = [Tok(), Tok()]
                    rs4 = sb("rs4", [128, 4]); wg4 = sb("wg4", [128, 4]); t_rs = Tok()
                    otmp = sb("otmp", [128, 4, 64]); t_otmp = Tok()
                    itmp = sb("itmp", [128, 4, 32]); imp = sb("imp", [128, 32]); t_imp = Tok()
                    m8 = sb("m8", [128, 8]); t_m8 = Tok()
                    negsel = sb("negsel", [128, 128], BF16); t_negsel = Tok()
                    P.op("dve", lambda e: e.memset(negsel[:], 0.0), writes=[t_negsel])
                    nsT4 = sb("nsT4", [128, 4, 128], BF16); t_nsT = Tok()
                    junk2 = sb("junk2", [128, 512], BF16); t_junk2 = Tok()
                    on_b = sb("on_b", [128, 512], BF16); t_onb = Tok()
                    rr = [0]

                    def score_tile(g, lhs_list, bias_ap, t_bias, rows):
                        i = rr[0] % 2
                        rr[0] += 1
                        bank, tb = ps[i], tps[i]
                        nmm = len(lhs_list)
                        for m, (l_ap, r_ap, rd) in enumerate(lhs_list):
                            P.op("pe", lambda e, l_ap=l_ap, r_ap=r_ap, m=m: e.matmul(
                                bank[0:rows, :].rearrange("p (j q) -> p j q", j=4), lhsT=l_ap, rhs=r_ap,
                                start=(m == 0), stop=(m == nmm - 1)), reads=rd, writes=[tb])
                        P.op("dve", lambda e: e.scalar_tensor_tensor(
                            out=tS[i][0:rows, :].rearrange("p (j q) -> p j q", j=4),
                            in0=bank[0:rows, :].rearrange("p (j q) -> p j q", j=4), scalar=0.125,
                            in1=bias_ap, op0=ALU.mult, op1=ALU.add), reads=[tb, t_bias], writes=[t_tS[i]])
                        P.op("act", lambda e: e.activation(out=pT[i][0:rows, :], in_=tS[i][0:rows, :], func=AF.Exp),
                             reads=[t_tS[i]], writes=[t_pT[i]])
                        return pT[i], t_pT[i]

                    def evac_branch(g, br, qi, oa, t_oa, with_imp=False):
                        ncol = 97 if with_imp else 65
                        rd = tps[3:7]
                        P.op("dve", lambda e: e.tensor_scalar(out=rs4[:], in0=psO[:, :, 64], scalar1=1e-30, scalar2=None,
                                                              op0=ALU.max), reads=rd, writes=[t_rs])
                        P.op("dve", lambda e: e.reciprocal(out=rs4[:], in_=rs4[:]), reads=[t_rs], writes=[t_rs])
                        g0 = 12 * g + br
                        P.op("dve", lambda e: e.tensor_tensor(out=wg4[:], in0=rs4[:], in1=gat[:, qi, g0:g0 + 10:3], op=ALU.mult),
                             reads=[t_rs, t_gat[qi]], writes=[t_rs])
                        if br == 0:
                            P.op("dve", lambda e: e.tensor_tensor(
                                out=oa[:, 4 * g:4 * g + 4, :], in0=psO[:, :, 0:64],
                                in1=wg4[:].unsqueeze(2).broadcast_to([128, 4, 64]), op=ALU.mult),
                                reads=rd + [t_rs], writes=[t_oa])
                        else:
                            P.op("dve", lambda e: e.tensor_tensor(
                                out=otmp[:], in0=psO[:, :, 0:64],
                                in1=wg4[:].unsqueeze(2).broadcast_to([128, 4, 64]), op=ALU.mult),
                                reads=rd + [t_rs], writes=[t_otmp])
                            P.op("pool", lambda e: e.tensor_tensor(out=oa[:, 4 * g:4 * g + 4, :], in0=oa[:, 4 * g:4 * g + 4, :],
                                                                   in1=otmp[:], op=ALU.add),
                                 reads=[t_otmp, t_oa], writes=[t_oa])
                        if with_imp:
                            P.op("dve", lambda e: e.tensor_tensor(
                                out=itmp[:], in0=psO[:, :, 65:97], in1=rs4[:].unsqueeze(2).broadcast_to([128, 4, 32]),
                                op=ALU.mult), reads=rd + [t_rs], writes=[t_imp])
                            P.op("dve", lambda e: e.tensor_reduce(out=imp[:], in_=itmp[:].rearrange("p j n -> p n j"),
                                                                  axis=AX.X, op=ALU.add), reads=[t_imp], writes=[t_imp])

                    for qi in range(NT):
                        gi = s * NT + qi
                        bc, t_bc = bcp[qi % 2], t_bcp[qi % 2]
                        src = bass.AP(fc_d.tensor, 2048 + 128 * qi - 31, [[LC, 127], [127 * (LC + 16), 8], [1, 128]])
                        P.dma("pool", bc[0:127, :, :], src, reads=[t_fc], writes=[t_bc])
                        oa, t_oa = o_acc[qi % 2], t_oacc[qi % 2]
                        for g in range(2):
                            pr = slice(g * 64, (g + 1) * 64)
                            rhs_q = qT[pr, :, qi * 128:(qi + 1) * 128]
                            rd_q = [t_qT[qi // 4]]
                            p_, t_p = score_tile(g, [(kcmpT[pr, 0:127], rhs_q, rd_q + [t_kcmp])],
                                                 bc[0:127, 4 * g:4 * g + 4, :],
                                                 t_bc, 127)
                            for j in range(4):
                                P.op("pe", lambda e, j=j, p_=p_: e.matmul(psO[:, j, 0:97], lhsT=p_[0:127, j * 128:(j + 1) * 128],
                                                                          rhs=vc_aug[0:127, g, :], start=True, stop=True),
                                     reads=[t_p, t_vca], writes=[tps[3 + j]])
                            evac_branch(g, 0, qi, oa, t_oa, with_imp=True)
                            P.op("dve", lambda e: e.tensor_tensor(out=imp[:], in0=imp[:], in1=selmul[:, qi, :], op=ALU.mult),
                                 reads=[t_imp, t_selmul], writes=[t_imp])
                            P.op("dve", lambda e: e.tensor_tensor(out=imp[:], in0=imp[:], in1=seladd[:, qi, :], op=ALU.add),
                                 reads=[t_imp, t_seladd], writes=[t_imp])
                            P.op("dve", lambda e: e.max(out=m8[:], in_=imp[:]), reads=[t_imp], writes=[t_m8])
                            P.op("dve", lambda e: e.tensor_scalar(out=negsel[:, g * 64:g * 64 + 32], in0=imp[:], scalar1=m8[:, 7:8],
                                                                  scalar2=NEG, op0=ALU.is_lt, op1=ALU.mult),
                                 reads=[t_imp, t_m8], writes=[t_negsel])
                            P.op("pe", lambda e: e.transpose(out=psb[:, 0:128], in_=negsel[:, :], identity=identb[:]),
                                 reads=[t_negsel, t_identb], writes=[tpsb])
                            P.op("act", lambda e: e.copy(out=nsT4[:], in_=psb[:, 0:128].unsqueeze(1).broadcast_to([128, 4, 128])),
                                 reads=[tpsb], writes=[t_nsT])
                            if dbg and s == 0 and qi == 5 and g == 1:
                                dbgdump("negsel", [128, 128], BF16, negsel[:], [t_negsel])
                            for kj in range(qi + 1):
                                dl = min(qi - kj, 2)
                                p_, t_p = score_tile(
                                    g, [(kT["ks"][pr, kj * 128:(kj + 1) * 128], rhs_q, rd_q + [t_kT["ks"][kj // 4]]),
                                        (esel[pr, kj * 128:(kj + 1) * 128], nsT4[pr, :, :], [t_esel, t_nsT])],
                                    Bs[:, dl, 4 * g:4 * g + 4, :], t_Bs, 128)
                                for j in range(4):
                                    P.op("pe", lambda e, j=j, p_=p_, kj=kj: e.matmul(
                                        psO[:, j, 0:65], lhsT=p_[:, j * 128:(j + 1) * 128], rhs=vs_aug[:, kj, g, :],
                                        start=(kj == 0), stop=(kj == qi)), reads=[t_p, t_va[kj], t_ones], writes=[tps[3 + j]])
                            evac_branch(g, 1, qi, oa, t_oa)
                            k0 = max(0, qi - 4)
                            for kj in range(k0, qi + 1):
                                p_, t_p = score_tile(
                                    g, [(kT["kw"][pr, kj * 128:(kj + 1) * 128], rhs_q, rd_q + [t_kT["kw"][kj // 4]])],
                                    Bw[:, qi - kj, 4 * g:4 * g + 4, :], t_Bw, 128)
                                for j in range(4):
                                    P.op("pe", lambda e, j=j, p_=p_, kj=kj: e.matmul(
                                        psO[:, j, 0:65], lhsT=p_[:, j * 128:(j + 1) * 128], rhs=vw_aug[:, kj, g, :],
                                        start=(kj == k0), stop=(kj == qi)), reads=[t_p, t_va[kj], t_ones], writes=[tps[3 + j]])
                            evac_branch(g, 2, qi, oa, t_oa)
                        if dbg and s == 0:
                            if qi == 0:
                                dbg_out["oattn"] = dout("dbg_oattn", [NT, 128, 512], F32)
                            P.dma("sp", dbg_out["oattn"][qi], oa[:].rearrange("p h d -> p (h d)"), reads=[t_oa])
                        oaf = oa[:].rearrange("p h d -> p (h d)")
                        rms_stats(oaf, t_oa, junk2[:], t_junk2, ssqa, rstda, t_sta[gi], gi, 512)
                        P.op("dve", lambda e, oaf=oaf: e.scalar_tensor_tensor(
                            out=on_b[:], in0=oaf, scalar=rstda[:, gi:gi + 1], in1=gattn[:], op0=ALU.mult, op1=ALU.mult),
                            reads=[t_oa, t_sta[gi], t_gattn], writes=[t_onb])
                        for c in range(4):
                            P.op("pe", lambda e, c=c: e.transpose(out=psb[:, 512 + c * 128:512 + (c + 1) * 128],
                                                                  in_=on_b[:, c * 128:(c + 1) * 128], identity=identb[:]),
                                 reads=[t_onb, t_identb], writes=[tpsb])
                        for c in range(4):
                            evac(qi, mixT[:, c, qi * 128:(qi + 1) * 128], psb[:, 512 + c * 128:512 + (c + 1) * 128],
                                 [tpsb], [t_mixa[qi]])
                    if s == 0:
                        dbgdump("mixT", [128, 8, S], BF16, mixT[:], t_mixc + t_mixa)
                if stop_after <= 2:
                    continue

                with Scope():
                    wout = sb("wout", [128, 8, 1024], BF16); t_wout = Tok()
                    wov = w_out_d.rearrange("(k p) n -> p k n", p=128)
                    for k in range(8):
                        P.dma("pool", wout[:, k, :], wov[:, k, :], writes=[t_wout])
                    gt1 = sb("gt1", [128, 1024]); t_gt1 = Tok()
                    P.dma("sp", gt1[:], gt_d[0, s], reads=[t_gtd], writes=[t_gt1])
                    xpool = [sb(f"xt3{i}", [128, 1024]) for i in range(2)]; t_xp = [Tok(), Tok()]
                    x1p = [sb(f"x1t{i}", [128, 1024]) for i in range(2)]; t_x1p = [Tok(), Tok()]
                    junk = sb("junk3", [128, 1024], BF16); t_junk = Tok()
                    xs = sb("xs3", [128, 1024], BF16); t_xs = Tok()
                    h2st = [sb(f"h2st{i}", [128, 8, 128], BF16) for i in range(2)]; t_h2st = [Tok(), Tok()]
                    for ti in range(NT):
                        gi = s * NT + ti
                        xt, t_xt = xpool[ti % 2], t_xp[ti % 2]
                        x1t, t_x1t = x1p[ti % 2], t_x1p[ti % 2]
                        P.dma("sp", xt[:], x_d[gi * 128:(gi + 1) * 128, :], writes=[t_xt])
                        for half in range(2):
                            for k in range(8):
                                P.op("pe", lambda e, k=k, half=half: e.matmul(
                                    ps[half][:, :], lhsT=mixT[:, k, ti * 128:(ti + 1) * 128],
                                    rhs=wout[:, k, half * 512:(half + 1) * 512], start=(k == 0), stop=(k == 7)),
                                    reads=[t_wout, t_mixa[ti], t_mixc[ti // 4]], writes=[tps[half]])
                            hs = slice(half * 512, (half + 1) * 512)
                            P.op("dve", lambda e, half=half, hs=hs: e.tensor_tensor(out=x1t[:, hs], in0=ps[half][:, :], in1=gt1[:, hs],
                                                                                    op=ALU.mult),
                                 reads=[tps[half], t_gt1], writes=[t_x1t])
                        P.op("pool", lambda e: e.tensor_tensor(out=x1t[:], in0=x1t[:], in1=xt[:], op=ALU.add),
                             reads=[t_x1t, t_xt], writes=[t_x1t])
                        P.dma("sp", x1_d[gi * 128:(gi + 1) * 128, :], x1t[:], reads=[t_x1t], writes=[t_x1d[gi]])
                        hs_, t_hs = h2st[ti % 2], t_h2st[ti % 2]
                        norm_transpose(x1t, t_x1t, gi, s, ssq2, rstd2, t_st2, gs2, 24, hs_, t_hs, 0, junk, t_junk, xs, t_xs)
                        P.dma("sp", h2T_d[:, :, gi * 128:(gi + 1) * 128], hs_[:], reads=[t_hs], writes=[t_h2d[gi]])

        if stop_after <= 3:
            P.wait_all("sp", Tok.registry)
            return nc, dbg_out

        try:
            utb_d = dscr("utb_s", [16384, 1024], BF16)
            vb_d = dscr("vb_s", [16384, 1024], BF16)
            s2_d = dscr("s2_s", [16, T, 128], BF16)
            t_utb = [Tok() for _ in range(16)]
            t_vb = [Tok() for _ in range(16)]
            with Scope():
                stg = [sb(f"cst{i}", [128, 8, 1024], BF16) for i in range(2)]; t_stg = [Tok(), Tok()]
                n = 0
                for src_d, dst_d, tks in ((ut_d, utb_d, t_utb), (pv_d, vb_d, t_vb)):
                    for c in range(16):
                        i = n % 2; n += 1
                        sv = src_d[c * 1024:(c + 1) * 1024, :].rearrange("(c p) n -> p c n", p=128)
                        dv = dst_d[c * 1024:(c + 1) * 1024, :].rearrange("(c p) n -> p c n", p=128)
                        P.dma("pool", stg[i][:], sv, writes=[t_stg[i]])
                        P.dma("sp", dv, stg[i][:], reads=[t_stg[i]], writes=[tks[c]])
            if stop_after == 3.5:
                raise _Stop()

            with Scope():
                wq, t_wq = None, Tok()
                wq = sb("wq", [128, 8, 1024], BF16)
                wqv = wq_d.rearrange("(k p) n -> p k n", p=128)
                for k in range(8):
                    P.dma("pool", wq[:, k, :], wqv[:, k, :], writes=[t_wq])
                kb, t_kb = load_const("kb", kb_d, [128, 8, 256], BF16, "pool")
                identf, t_identf = load_const("identf", identf_d, [128, 128])
                iota, t_iota = load_const("iota", iota_d, [128, 128])
                selh, t_selh = load_const("selh", selh_d, [16, 128], BF16, "pool")
                gfin, t_gfin = load_const("gfin", gfin_d, [128, 1024])
                gt2 = sb("gt2", [128, 1024]); t_gt2 = Tok()
                Wbuf = sb("Wbuf", [128, 256, 128], BF16); t_W = [Tok() for _ in range(64)]
                h2g = sb("h2g", [128, 8, 256], BF16); t_h2g = Tok()
                qTp = sb("qTp", [128, 8, 256], BF16); t_qTp = Tok()
                s_sb = sb("s_sb", [128, 8, 256]); t_s = Tok()
                work = sb("work", [128, 8, 256]); t_work = Tok()
                v16 = sb("v16", [128, 8, 2, 16]); t_v16 = Tok()
                idx = sb("idx", [128, 8, 16], U32); t_idx = Tok()
                cand = sb("cand", [128, 8, 256]); t_cand = Tok()
                cwork = sb("cwork", [128, 8, 256]); t_cwork = Tok()
                ts16 = sb("ts16", [128, 8, 16]); t_ts = Tok()
                d16 = sb("d16", [128, 8, 16]); t_d16 = Tok()
                zz = sb("zz", [128, 8]); mu = sb("mu", [128, 8]); tauE = sb("tauE", [128, 8]); t_zz = Tok()
                tm3 = sb("tm3", [128, 3, 128]); t_tm3 = Tok()
                tT3 = sb("tT3", [128, 3, 128]); t_tT3 = Tok()
                s2hm = [sb(f"s2hm{i}", [16, 16 * 128], BF16) for i in range(2)]; t_s2hm = [Tok(), Tok()]
                s2hb = sb("s2hb", [128, 2, 8, 128], BF16); t_s2hb = Tok()
                NR = 4
                ebuf = [sb(f"ebuf{i}", [128, 128], BF16) for i in range(NR)]; t_eb = [Tok() for _ in range(NR)]
                Rt = [sb(f"Rt{i}", [128, 128], BF16) for i in range(NR)]; t_Rt = [Tok() for _ in range(NR)]
                Lt = [sb(f"Lt{i}", [128, 128], BF16) for i in range(NR)]; t_Lt = [Tok() for _ in range(NR)]
                mk = [sb(f"mk{i}", [128, 128], BF16) for i in range(NR)]; t_mk = [Tok() for _ in range(NR)]
                NC = 6
                utc = [sb(f"utc{i}", [128, 8, 128], BF16) for i in range(NC)]; t_utc = [Tok() for _ in range(NC)]
                vch = [sb(f"vch{i}", [128, 1024], BF16) for i in range(NC)]; t_vch = [Tok() for _ in range(NC)]
                abuf = [sb(f"abuf{i}", [128, 256], BF16) for i in range(2)]; t_ab = [Tok(), Tok()]
                wab = [sb(f"wab{i}", [128, 256], BF16) for i in range(2)]; t_wab = [Tok(), Tok()]
                x1t = sb("x1f", [128, 1024]); t_x1t = Tok()
                yt = sb("yf", [128, 1024]); t_yt = Tok()
                ot = sb("of", [128, 1024]); t_ot = Tok()
                junk = sb("junkf", [128, 1024], BF16); t_junk = Tok()
                ssq3 = sb("ssq3", [128, NTT]); rstd3 = sb("rstd3", [128, NTT]); t_st3 = [Tok() for _ in range(NTT)]
                t_s2d = Tok()
                t_out = Tok()

                def top16(dst_lo, dst_hi, src, wrk, rd, wr_dst, wr_wrk):
                    P.op("dve", lambda e: e.max(out=dst_lo, in_=src), reads=rd, writes=[wr_dst])
                    P.op("dve", lambda e: e.match_replace(out=wrk, in_to_replace=dst_lo, in_values=src, imm_value=-1e30),
                         reads=rd + [wr_dst], writes=[wr_wrk])
                    P.op("dve", lambda e: e.max(out=dst_hi, in_=wrk), reads=[wr_wrk], writes=[wr_dst])

                ngroups = T // 256
                for gidx in range(ngroups):
                    g0 = gidx * 256
                    sq = g0 // S
                    if g0 % S == 0:
                        P.dma("sp", gt2[:], gt_d[1, sq], reads=[t_gtd], writes=[t_gt2])
                    P.dma("sp", h2g[:], h2T_d[:, :, g0:g0 + 256], reads=t_h2d[gidx * 2:gidx * 2 + 2], writes=[t_h2g])
                    for h in range(8):
                        bi = h % 3
                        for k in range(8):
                            P.op("pe", lambda e, k=k, h=h, bi=bi: e.matmul(ps[bi][:, 0:256], lhsT=wq[:, k, h * 128:(h + 1) * 128],
                                                                           rhs=h2g[:, k, :], start=(k == 0), stop=(k == 7)),
                                 reads=[t_wq, t_h2g], writes=[tps[bi]])
                        evac(h, qTp[:, h, :], ps[bi][:, 0:256], [tps[bi]], [t_qTp])
                    for tt in range(2):
                        gi = gidx * 2 + tt
                        tsl = slice(tt * 128, (tt + 1) * 128)
                        for h in range(8):
                            bi = 3 + h // 2
                            P.op("pe", lambda e, h=h, bi=bi: e.matmul(ps[bi][:, (h % 2) * 256:(h % 2 + 1) * 256], lhsT=qTp[:, h, tsl],
                                                                      rhs=kb[:, h, :], start=True, stop=True),
                                 reads=[t_qTp, t_kb], writes=[tps[bi]])
                        for hp in range(4):
                            evac(hp, s_sb[:, 2 * hp:2 * hp + 2, :], ps[3 + hp][:, :].rearrange("p (h n) -> p h n", h=2),
                                 [tps[3 + hp]], [t_s])
                        for h in range(8):
                            for p in range(2):
                                top16(v16[:, h, p, 0:8], v16[:, h, p, 8:16], s_sb[:, h, p * 128:(p + 1) * 128],
                                      work[:, h, p * 128:(p + 1) * 128], [t_s], t_v16, t_work)
                            P.op("dve", lambda e, h=h: e.max_index(out=idx[:, h, 0:8], in_max=v16[:, h, 0, 0:8],
                                                                   in_values=s_sb[:, h, 0:128]),
                                 reads=[t_s, t_v16], writes=[t_idx])
                            P.op("dve", lambda e, h=h: e.max_index(out=idx[:, h, 8:16], in_max=v16[:, h, 0, 8:16],
                                                                   in_values=work[:, h, 0:128]),
                                 reads=[t_work, t_v16], writes=[t_idx])
                        P.op("dve", lambda e: e.tensor_tensor(
                            out=cand[:].rearrange("p h (a b) -> p h a b", a=16),
                            in0=v16[:, :, 0, :].unsqueeze(3).broadcast_to([128, 8, 16, 16]),
                            in1=v16[:, :, 1, :].unsqueeze(2).broadcast_to([128, 8, 16, 16]), op=ALU.add),
                            reads=[t_v16], writes=[t_cand])
                        for h in range(8):
                            top16(ts16[:, h, 0:8], ts16[:, h, 8:16], cand[:, h, :], cwork[:, h, :], [t_cand], t_ts, t_cwork)
                        if stop_after == 3.6:
                            dbgdump("v16", [128, 8, 2, 16], F32, v16[:], [t_v16])
                            dbgdump("ts16", [128, 8, 16], F32, ts16[:], [t_ts])
                            dbgdump("idx", [128, 8, 16], U32, idx[:], [t_idx])
                            dbgdump("s_sb", [128, 8, 256], F32, s_sb[:], [t_s])
                            raise _Stop()
                        P.op("dve", lambda e: e.tensor_tensor(out=d16[:], in0=ts16[:], in1=ts16[:, :, 0:1].broadcast_to([128, 8, 16]),
                                                              op=ALU.subtract), reads=[t_ts], writes=[t_d16])
                        P.op("act", lambda e: e.activation(out=d16[:], in_=d16[:], func=AF.Exp), reads=[t_d16], writes=[t_d16])
                        P.op("dve", lambda e: e.tensor_reduce(out=zz[:], in_=d16[:], axis=AX.X, op=ALU.add),
                             reads=[t_d16], writes=[t_zz])
                        P.op("act", lambda e: e.activation(out=zz[:], in_=zz[:], func=AF.Ln), reads=[t_zz], writes=[t_zz])
                        P.op("dve", lambda e: e.tensor_tensor(out=mu[:], in0=zz[:], in1=ts16[:, :, 0], op=ALU.add),
                             reads=[t_zz, t_ts], writes=[t_zz])
                        P.op("dve", lambda e: e.tensor_scalar(out=tauE[:], in0=ts16[:, :, 15], scalar1=-1e-4, scalar2=None,
                                                              op0=ALU.add), reads=[t_ts], writes=[t_zz])
                        P.op("dve", lambda e: e.tensor_tensor(
                            out=tm3[:, 0, :].rearrange("p (h a) -> p h a", h=8),
                            in0=tauE[:].unsqueeze(2).broadcast_to([128, 8, 16]), in1=v16[:, :, 0, :], op=ALU.subtract),
                            reads=[t_zz, t_v16], writes=[t_tm3])
                        P.op("dve", lambda e: e.tensor_tensor(
                            out=tm3[:, 1, :].rearrange("p (h a) -> p h a", h=8),
                            in0=v16[:, :, 0, :], in1=mu[:].unsqueeze(2).broadcast_to([128, 8, 16]), op=ALU.subtract),
                            reads=[t_zz, t_v16], writes=[t_tm3])
                        P.op("dve", lambda e: e.tensor_copy(out=tm3[:, 2, :], in_=idx[:].rearrange("p h a -> p (h a)")),
                             reads=[t_idx], writes=[t_tm3])
                        for w3 in range(3):
                            P.op("pe", lambda e, w3=w3: e.transpose(out=ps[2][:, w3 * 128:(w3 + 1) * 128], in_=tm3[:, w3, :],
                                                                    identity=identf[:]),
                                 reads=[t_tm3, t_identf], writes=[tps[2]])
                        P.op("dve", lambda e: e.tensor_copy(out=tT3[:], in_=ps[2][:, 0:384].rearrange("p (w t) -> p w t", w=3)),
                             reads=[tps[2]], writes=[t_tT3])
                        if stop_after == 3.7:
                            dbgdump("tT3", [128, 3, 128], F32, tT3[:], [t_tT3])
                            dbgdump("tm3", [128, 3, 128], F32, tm3[:], [t_tm3])
                            raise _Stop()
                        P.op("dve", lambda e: e.tensor_copy(out=s2hb[:, 0, :, :], in_=s_sb[:, :, 128:256]),
                             reads=[t_s], writes=[t_s2hb])
                        P.op("dve", lambda e: e.tensor_tensor(out=s2hb[:, 1, :, :], in0=s_sb[:, :, 128:256], in1=s2hb[:, 0, :, :],
                                                              op=ALU.subtract), reads=[t_s, t_s2hb], writes=[t_s2hb])
                        P.dma("sp", s2_d[:, g0 + tt * 128:g0 + (tt + 1) * 128, :].rearrange("(w h) t j -> t w h j", w=2), s2hb[:],
                              reads=[t_s2hb], writes=[t_s2d])
                        for sl in range(8):
                            i2 = sl % 2
                            t0 = g0 + tt * 128 + sl * 16
                            P.dma("sp", s2hm[i2][:, :], s2_d[:, t0:t0 + 16, :].rearrange("h t j -> h (t j)"),
                                  reads=[t_s2d], writes=[t_s2hm[i2]])
                            for q4 in range(4):
                                tb0 = tt * 128 + sl * 16 + q4 * 4
                                qpar = (sl * 4 + q4) % 2
                                bA = 0 if qpar == 0 else 4
                                bD = 3 if qpar == 0 else 5
                                for bb in (bA, bD):
                                    P.op("pe", lambda e, i2=i2, q4=q4, bb=bb: e.matmul(
                                        ps[bb][:, :], lhsT=selh[:, :], rhs=s2hm[i2][:, q4 * 512:(q4 + 1) * 512],
                                        start=True, stop=True), reads=[t_selh, t_s2hm[i2]], writes=[tps[bb]])
                                wb = 1 + ((sl * 4 + q4) % 2)
                                for t in range(4):
                                    tl = tb0 + t - tt * 128
                                    r = (tb0 + t) % NR
                                    sl_ps = ps[bA][:, t * 128:(t + 1) * 128]
                                    sl_pd = ps[bD][:, t * 128:(t + 1) * 128]
                                    if "A" in PDBG:
                                        P.op("dve", lambda e, r=r: e.memset(ebuf[r][:], 1.0), writes=[t_eb[r]])
                                    else:
                                        P.op("act", lambda e, r=r, sl_ps=sl_ps, tl=tl: e.activation(
                                            out=ebuf[r][:], in_=sl_ps, func=AF.Exp, bias=tT3[:, 1, tl:tl + 1]),
                                            reads=[tps[bA], t_tT3], writes=[t_eb[r]])
                                    if "S" in PDBG:
                                        P.op("dve", lambda e, r=r: e.tensor_copy(out=Rt[r][:], in_=ebuf[r][:]), reads=[t_eb[r]], writes=[t_Rt[r]])
                                    else:
                                        P.op("dve", lambda e, r=r, sl_pd=sl_pd, tl=tl: e.tensor_scalar(
                                            out=mk[r][:], in0=sl_pd, scalar1=tT3[:, 0, tl:tl + 1], scalar2=None,
                                            op0=ALU.is_ge), reads=[tps[bD], t_tT3], writes=[t_mk[r]])
                                        P.op("pool", lambda e, r=r: e.tensor_tensor(out=Rt[r][:], in0=mk[r][:], in1=ebuf[r][:],
                                                                                    op=ALU.mult),
                                             reads=[t_mk[r], t_eb[r]], writes=[t_Rt[r]])
                                    P.op(LENG, lambda e, r=r, tl=tl: e.tensor_scalar(
                                        out=Lt[r][:], in0=iota[:], scalar1=tT3[:, 2, tl:tl + 1], scalar2=None, op0=ALU.is_equal),
                                        reads=[t_iota, t_tT3], writes=[t_Lt[r]])
                                    if "M" not in PDBG:
                                        P.op("pe", lambda e, r=r, t=t, wb=wb: e.matmul(ps[wb][:, t * 128:(t + 1) * 128], lhsT=Rt[r][:],
                                                                                       rhs=Lt[r][:], start=True, stop=True),
                                             reads=[t_Rt[r], t_Lt[r]], writes=[tps[wb]])
                                if stop_after == 3.76:
                                    dbgdump("Rt", [128, 128], BF16, Rt[3][:], [t_Rt[3]])
                                    dbgdump("Lt", [128, 128], BF16, Lt[3][:], [t_Lt[3]])
                                    dbgdump("eb", [128, 128], BF16, ebuf[3][:], [t_eb[3]])
                                    raise _Stop()
                                evac(q4, Wbuf[:, tb0:tb0 + 4, :], ps[wb][:, :].rearrange("p (t i) -> p t i", t=4),
                                     [tps[wb]], [t_W[tb0 // 4]])
                    if dbg and gidx == 0:
                        dbgdump("Wbuf", [128, 256, 128], BF16, Wbuf[:], t_W)
                    if stop_after == 3.8:
                        raise _Stop()
                    def ld(i):
                        c = i % NC
                        P.dma("sp", utc[c][:], utb_d[i * 128:(i + 1) * 128, :].rearrange("p (k j) -> p k j", k=8),
                              reads=[t_utb[i // 8]], writes=[t_utc[c]])
                        P.dma("sp", vch[c][:], vb_d[i * 128:(i + 1) * 128, :], reads=[t_vb[i // 8]], writes=[t_vch[c]])

                    def emU(i):
                        c = i % NC
                        ba = i % 2
                        for k in range(8):
                            P.op("pe", lambda e, k=k: e.matmul(ps[ba][:, 0:256], lhsT=utc[c][:, k, :], rhs=h2g[:, k, :],
                                                               start=(k == 0), stop=(k == 7)),
                                 reads=[t_utc[c], t_h2g], writes=[tps[ba]])
                        P.op("act", lambda e: e.activation(out=abuf[ba][:], in_=ps[ba][:, 0:256], func=AF.Gelu_apprx_tanh),
                             reads=[tps[ba]], writes=[t_ab[ba]])
                        weng = "dve" if i % 2 == 0 else "pool"
                        P.op(weng, lambda e: e.tensor_tensor(out=wab[ba][:], in0=abuf[ba][:], in1=Wbuf[:, :, i], op=ALU.mult),
                             reads=[t_ab[ba]] + t_W, writes=[t_wab[ba]])

                    def emV(i):
                        c = i % NC
                        ba = i % 2
                        for tt in range(2):
                            for half in range(2):
                                P.op("pe", lambda e, tt=tt, half=half: e.matmul(
                                    psO[:, tt * 2 + half, :], lhsT=wab[ba][:, tt * 128:(tt + 1) * 128],
                                    rhs=vch[c][:, half * 512:(half + 1) * 512], start=(i == 0), stop=(i == 127)),
                                    reads=[t_wab[ba], t_vch[c]], writes=[tps[3 + tt * 2 + half]])

                    PF = NC - 2
                    for i in range(PF):
                        ld(i)
                    emU(0)
                    for i in range(128):
                        if i + PF < 128:
                            ld(i + PF)
                        if i + 1 < 128:
                            emU(i + 1)
                        emV(i)
                    for tt in range(2):
                        gi = gidx * 2 + tt
                        P.dma("sp", x1t[:], x1_d[gi * 128:(gi + 1) * 128, :], reads=[t_x1d[gi]], writes=[t_x1t])
                        for half in range(2):
                            hs = slice(half * 512, (half + 1) * 512)
                            P.op("dve", lambda e, tt=tt, half=half, hs=hs: e.tensor_tensor(
                                out=yt[:, hs], in0=psO[:, tt * 2 + half, :], in1=gt2[:, hs], op=ALU.mult),
                                reads=[tps[3 + tt * 2 + half], t_gt2], writes=[t_yt])
                        if dbg and gidx == 0 and tt == 0:
                            dbgdump("peer0", [128, 1024], F32, yt[:], [t_yt])
                        P.op("pool", lambda e: e.tensor_tensor(out=yt[:], in0=yt[:], in1=x1t[:], op=ALU.add),
                             reads=[t_yt, t_x1t], writes=[t_yt])
                        rms_stats(yt[:], t_yt, junk[:], t_junk, ssq3, rstd3, t_st3[gi], gi, D)
                        P.op("dve", lambda e, gi=gi: e.scalar_tensor_tensor(out=ot[:], in0=yt[:], scalar=rstd3[:, gi:gi + 1], in1=gfin[:],
                                                                            op0=ALU.mult, op1=ALU.mult),
                             reads=[t_yt, t_st3[gi], t_gfin], writes=[t_ot])
                        P.dma("sp", out_d[gi * 128:(gi + 1) * 128, :], ot[:], reads=[t_ot], writes=[t_out])
                    if stop_after == 4 and gidx == 0:
                        raise _Stop()

        except _Stop:
            pass
        P.wait_all("sp", Tok.registry)
        print("ops per engine", P.ecount, "waits", P.nwaits)
    return nc, dbg_out


_NC_CACHE = {}


def kernel(**inputs):
    inp = {k: np.asarray(v) for k, v in inputs.items()}
    sh = _prep(inp)
    if "nc" not in _NC_CACHE:
        _NC_CACHE["nc"] = build_nc(nseq=2)[0]
    nc = _NC_CACHE["nc"]
    x = np.asarray(inp["x"], np.float32)
    c = np.asarray(inp["c"], np.float32)
    in_maps = []
    for core in range(8):
        m = dict(sh)
        m["x"] = np.ascontiguousarray(x[2 * core:2 * core + 2].reshape(2 * S, D))
        m["cT"] = np.ascontiguousarray(c[2 * core:2 * core + 2].T.reshape(8, 128, 2).transpose(1, 0, 2))
        in_maps.append(m)
    res = run_bass_kernel_spmd(nc, in_maps, core_ids=list(range(8)))
    out = np.concatenate([np.asarray(r["out"]).reshape(2, S, D) for r in res.results], axis=0)
    return out.astype(np.float32)
```

```python
import math
import numpy as np
from contextlib import ExitStack
import concourse.bass as bass
import concourse.mybir as mybir
from concourse.bass_utils import run_bass_kernel_spmd

F32 = mybir.dt.float32
BF16 = mybir.dt.bfloat16
U32 = mybir.dt.uint32
AF = mybir.ActivationFunctionType
ALU = mybir.AluOpType
AX = mybir.AxisListType

S = 2048
D = 1024
NT = 16
EPS = 1e-6
NEG = -30000.0
LENG = "dve"
import os
PDBG = os.environ.get("PDBG", "")
SEM_CHUNK = 30000


class _Stop(Exception):
    pass


class Tok:
    __slots__ = ("writers", "readers")
    registry = []

    def __init__(self):
        self.writers = []
        self.readers = []
        Tok.registry.append(self)


class Op:
    __slots__ = ("eng", "sem", "val", "is_dma")


class Prog:
    ENGS = ("pe", "dve", "act", "pool", "sp")

    def __init__(self, nc, stack, n_dma_sems=8):
        self.nc = nc
        self.stack = stack
        self.eobj = {"pe": nc.tensor, "dve": nc.vector, "act": nc.scalar,
                     "pool": nc.gpsimd, "sp": nc.sync}
        self.esems = {e: [] for e in self.ENGS}
        self.ecount = {e: 0 for e in self.ENGS}
        self.waited = {e: {} for e in self.ENGS}
        self.dma_sems = {}
        self.dma_rr = {}
        self.n_dma_sems = n_dma_sems
        self.nwaits = 0

    def _esem(self, e, chunk):
        lst = self.esems[e]
        while len(lst) <= chunk:
            lst.append(self.stack.enter_context(self.nc.semaphore(f"s_{e}_{len(lst)}")))
        return lst[chunk]

    def _wait(self, e, sem, val):
        w = self.waited[e]
        k = id(sem)
        if w.get(k, 0) >= val:
            return
        w[k] = val
        self.eobj[e].wait_ge(sem, val)
        self.nwaits += 1

    def _deps(self, e, is_dma, reads, writes):
        for t in reads:
            for p in t.writers:
                if (not p.is_dma) and (not is_dma) and p.eng == e and e == "pe":
                    continue
                self._wait(e, p.sem, p.val)
        for t in writes:
            for p in t.writers + t.readers:
                if (not p.is_dma) and (not is_dma) and p.eng == e and e == "pe":
                    continue
                self._wait(e, p.sem, p.val)

    def _record(self, op, reads, writes):
        for t in reads:
            if not op.is_dma:
                t.readers = [r for r in t.readers if r.is_dma or r.eng != op.eng]
            t.readers.append(op)
        for t in writes:
            if t.readers:
                t.writers = [op]
                t.readers = []
            else:
                if not op.is_dma:
                    t.writers = [w for w in t.writers if w.is_dma or w.eng != op.eng]
                t.writers.append(op)

    def op(self, e, fn, reads=(), writes=()):
        self._deps(e, False, reads, writes)
        n = self.ecount[e]
        sem = self._esem(e, n // SEM_CHUNK)
        val = (n % SEM_CHUNK) + 1
        fn(self.eobj[e]).then_inc(sem, 1)
        self.ecount[e] = n + 1
        o = Op()
        o.eng, o.sem, o.val, o.is_dma = e, sem, val, False
        self._record(o, reads, writes)
        return o

    def dma(self, q, out, in_, reads=(), writes=(), **kw):
        if q not in self.dma_sems:
            self.dma_sems[q] = [[self.stack.enter_context(self.nc.semaphore(f"d_{q}_{i}")), 0]
                                for i in range(self.n_dma_sems)]
            self.dma_rr[q] = 0
        self._deps(q, True, reads, writes)
        i = self.dma_rr[q]
        self.dma_rr[q] = (i + 1) % self.n_dma_sems
        ent = self.dma_sems[q][i]
        sem, cur = ent
        if cur > 0:
            self._wait(q, sem, cur)
        val = cur + 16
        ent[1] = val
        self.eobj[q].dma_start(out=out, in_=in_, **kw).then_inc(sem, 16)
        o = Op()
        o.eng, o.sem, o.val, o.is_dma = q, sem, val, True
        self._record(o, reads, writes)
        return o

    def barrier(self):
        for e in self.ENGS:
            for f in self.ENGS:
                n = self.ecount[f]
                if f == e or n == 0:
                    continue
                self._wait(e, self.esems[f][(n - 1) // SEM_CHUNK], (n - 1) % SEM_CHUNK + 1)
            for q, lst in self.dma_sems.items():
                for sem, cur in lst:
                    if cur > 0:
                        self._wait(e, sem, cur)

    def wait_all(self, e, toks):
        for t in toks:
            for p in t.writers + t.readers:
                self._wait(e, p.sem, p.val)


def _rel_bucket_np(n):
    n = np.maximum(n, 0)
    exact = 16
    lr = np.log(np.maximum(n, 1).astype(np.float32) / np.float32(exact)) / np.float32(math.log(128 / exact))
    large = exact + (lr * np.float32(32 - exact)).astype(np.int32)
    return np.where(n < exact, n, np.minimum(large, 31))


def _constants():
    c = {}
    npr = np.arange(4096)
    n = npr - 2048
    bk = _rel_bucket_np(n)
    oh = np.zeros((33, 2, 4096), np.float32)
    okw = (n >= 0) & (n < 512)
    oks = (n >= 0)
    oh[bk[okw], 0, npr[okw]] = 1.0
    oh[32, 0, npr[~okw]] = 1.0
    oh[bk[oks], 1, npr[oks]] = 1.0
    oh[32, 1, npr[~oks]] = 1.0
    c["oh"] = oh
    e32 = (np.arange(2048)[None, :] // 64 == np.arange(32)[:, None]).astype(np.float32)
    e128 = np.zeros((128, 2048), np.float32)
    e128[0:32] = e32
    e128[64:96] = e32
    c["Esel"] = e128
    cs = np.arange(127) * 16
    ss = np.arange(32) * 64
    c["Cov"] = ((cs[:, None] < ss[None, :] + 64) & (cs[:, None] + 32 > ss[None, :])).astype(np.float32)
    t = np.arange(2048)
    cur = t // 64
    j = np.arange(32)
    forced = (j[None, :] == 0) | (j[None, :] == cur[:, None]) | (j[None, :] == cur[:, None] - 1)
    allowed = j[None, :] * 64 <= t[:, None]
    mul = (allowed & ~forced).astype(np.float32)
    add = np.where(forced, 1e4, np.where(allowed, 0.0, -1e30)).astype(np.float32)
    c["selmul"] = np.ascontiguousarray(mul.reshape(16, 128, 32).transpose(1, 0, 2))
    c["seladd"] = np.ascontiguousarray(add.reshape(16, 128, 32).transpose(1, 0, 2))
    c["identf"] = np.eye(128, dtype=np.float32)
    c["ones"] = np.ones((128, 128), np.float32)
    c["iota"] = np.tile(np.arange(128, dtype=np.float32)[None, :], (128, 1))
    sel = np.zeros((8, 128), np.float32)
    for h in range(8):
        sel[h, h * 16:(h + 1) * 16] = 1.0
    c["selh"] = np.concatenate([sel, sel], axis=0)
    return c


def _blockdiag2(w):
    out = np.zeros(w.shape[:-2] + (128, 128), np.float32)
    out[..., :64, :64] = w
    out[..., 64:, 64:] = w
    return out


def _prep(inp):
    sh = {}
    f = lambda a: np.ascontiguousarray(np.asarray(a, np.float32))
    sh["w_mod"] = f(inp["w_mod"][0])
    sh["bmodT"] = f(inp["b_mod"][0].reshape(48, 128).T)
    bm = inp["b_mod"][0]
    sh["bmod_bc"] = f(np.broadcast_to(np.stack([bm[2048:3072], bm[5120:6144]])[None], (128, 2, 1024)))
    sh["gmix"] = f(inp["ln_mix_g"][0].reshape(8, 128).T)
    sh["gffn"] = f(inp["ln_ffn_g"][0].reshape(8, 128).T)
    sh["gfin_bc"] = f(np.broadcast_to(inp["ln_final_g"][None, :], (128, 1024)))
    w_in = np.asarray(inp["w_in"][0], np.float32)
    sp = np.cumsum([0, 512, 128, 128, 128, 128, 128, 128, 24, 512, 512, 512])
    q, kc, vc, ks, vs, kw, vw, gt, cb, cc, ch = [w_in[:, sp[i]:sp[i + 1]] for i in range(11)]
    qh = q.reshape(1024, 8, 64)
    qperm = np.concatenate([np.concatenate([qh[:, j], qh[:, 4 + j]], axis=1) for j in range(4)], axis=1)
    cols = [qperm, kc, vc, ks, kw]
    for c4 in range(4):
        cols += [cb[:, c4 * 128:(c4 + 1) * 128], cc[:, c4 * 128:(c4 + 1) * 128], ch[:, c4 * 128:(c4 + 1) * 128]]
    cols += [vs, vw, gt]
    sh["w_in"] = f(np.concatenate(cols, axis=1))
    for nm, w1, w2, pe in (("k", "cmp_wk1", "cmp_wk2", "cmp_pe_k"), ("v", "cmp_wv1", "cmp_wv2", "cmp_pe_v")):
        a = np.asarray(inp[w1][0], np.float32).reshape(32, 64, 64)
        sh["bd1" + nm] = f(_blockdiag2(a).transpose(1, 0, 2))
        sh["bd2" + nm] = f(_blockdiag2(np.asarray(inp[w2][0], np.float32)))
        p = np.asarray(inp[pe][0], np.float32).T
        sh["pe2" + nm] = f(np.concatenate([p, p], axis=0))
    sh["convw"] = f(inp["conv_w"][0][:, 0, :].reshape(3, 4, 128).transpose(2, 1, 0))
    sh["gattn_bc"] = f(np.broadcast_to(inp["norm_attn_g"][0][None, :], (128, 512)))
    sh["gconv"] = f(inp["norm_conv_g"][0].reshape(4, 128).T)
    sh["w_out"] = f(inp["w_out"][0])
    sh["peer_wq"] = f(inp["peer_wq"][0])
    pk = np.asarray(inp["peer_keys"][0], np.float32)
    kb = np.zeros((128, 8, 256), np.float32)
    for h in range(8):
        for p in range(2):
            kb[p * 64:(p + 1) * 64, h, p * 128:(p + 1) * 128] = pk[h, p].T
    sh["peer_kb"] = kb
    u = np.asarray(inp["peer_u"][0], np.float32)
    sh["peer_ut"] = f(u.reshape(128, 128, 8, 128).transpose(0, 3, 2, 1).reshape(128 * 128, 1024))
    sh["peer_v"] = f(inp["peer_v"][0])
    rt = np.zeros((33, 8), np.float32)
    rt[:32] = inp["rel_table"]
    rt[32] = NEG
    sh["relt"] = rt
    sh.update(_constants())
    return sh


def build_nc(nseq=2, dbg=False, stop_after=99):
    nc = bass.Bass("TRN2", target_bir_lowering=False)
    Tok.registry = []
    T = nseq * S
    NTT = nseq * NT

    def din(name, shape, dt=F32):
        return nc.dram_tensor(name, list(shape), dt, kind="ExternalInput").ap()

    def dscr(name, shape, dt=F32):
        return nc.dram_tensor(name, list(shape), dt, kind="Internal").ap()

    def dout(name, shape, dt=F32):
        return nc.dram_tensor(name, list(shape), dt, kind="ExternalOutput").ap()

    x_d = din("x", [T, D])
    cT_d = din("cT", [128, 8, nseq])
    w_mod_d = din("w_mod", [1024, 6144])
    bmodT_d = din("bmodT", [128, 48])
    bmod_bc_d = din("bmod_bc", [128, 2, 1024])
    gmix_d = din("gmix", [128, 8])
    gffn_d = din("gffn", [128, 8])
    gfin_d = din("gfin_bc", [128, 1024])
    w_in_d = din("w_in", [1024, 2840])
    bd1_d = {n: din("bd1" + n, [128, 32, 128]) for n in "kv"}
    bd2_d = {n: din("bd2" + n, [128, 128]) for n in "kv"}
    pe2_d = {n: din("pe2" + n, [128, 32]) for n in "kv"}
    convw_d = din("convw", [128, 4, 3])
    gattn_d = din("gattn_bc", [128, 512])
    gconv_d = din("gconv", [128, 4])
    w_out_d = din("w_out", [1024, 1024])
    wq_d = din("peer_wq", [1024, 1024])
    kb_d = din("peer_kb", [128, 8, 256])
    ut_d = din("peer_ut", [128 * 128, 1024])
    pv_d = din("peer_v", [16384, 1024])
    relt_d = din("relt", [33, 8])
    oh_d = din("oh", [33, 2, 4096])
    esel_d = din("Esel", [128, 2048])
    cov_d = din("Cov", [127, 32])
    selmul_d = din("selmul", [128, 16, 32])
    seladd_d = din("seladd", [128, 16, 32])
    identf_d = din("identf", [128, 128])
    ones_d = din("ones", [128, 128])
    iota_d = din("iota", [128, 128])
    selh_d = din("selh", [16, 128])
    out_d = dout("out", [T, D])

    tb_d = dscr("tb_s", [2, 8, 4096])
    LW = 1024
    fw_d = dscr("fw_s", [2, 8, 128 * (LW + 1)])
    LC = 4096
    fc_d = dscr("fc_s", [8, 127 * (LC + 16)])
    gt_d = dscr("gt_s", [2, nseq, 128, 1024])
    if dbg:
        x1_d = dout("x1_s", [T, D])
        h2T_d = dout("h2T_s", [128, 8, T], BF16)
    else:
        x1_d = dscr("x1_s", [T, D])
        h2T_d = dscr("h2T_s", [128, 8, T], BF16)
    t_x1d = [Tok() for _ in range(NTT)]
    t_h2d = [Tok() for _ in range(NTT)]

    dbg_out = {}
    uniq = [0]

    with ExitStack() as st0:
        P = Prog(nc, st0)
        scopes = [st0]

        def sb(name, shape, dt=F32):
            uniq[0] += 1
            return scopes[-1].enter_context(nc.sbuf_tensor(f"sb{uniq[0]}_{name}", list(shape), dt))

        class Scope:
            def __enter__(self):
                self.st = ExitStack()
                scopes.append(self.st)
                return self

            def __exit__(self, *a):
                if a[0] is None:
                    P.barrier()
                scopes.pop()
                self.st.close()
                return False

        psg = [st0.enter_context(nc.psum_tensor(f"psum{i}", [128, 512], F32)) for i in range(3)]
        psO = st0.enter_context(nc.psum_tensor("psumO", [128, 4, 512], F32))
        ps = [psg[0][:, :], psg[1][:, :], psg[2][:, :]] + [psO[:, i, :] for i in range(4)]
        tps = [Tok() for _ in range(7)]
        psb = st0.enter_context(nc.psum_tensor("psumb", [128, 1024], BF16))
        tpsb = Tok()

        def load_const(name, src, shape, dt=F32, q="sp"):
            t = sb(name, shape, dt)
            k = Tok()
            P.dma(q, t[:], src, writes=[k])
            return t, k

        def dbgdump(name, shape, dt, src_ap, toks):
            if not dbg:
                return
            dbg_out[name] = dout("dbg_" + name, shape, dt)
            P.dma("sp", dbg_out[name], src_ap, reads=toks)

        identb, t_identb = load_const("identb", identf_d, [128, 128], BF16, "pool")
        onesb, t_onesb = load_const("onesb", ones_d, [128, 128], BF16, "pool")
        gmix, t_gmix = load_const("gmix", gmix_d, [128, 8])
        gffn, t_gffn = load_const("gffn", gffn_d, [128, 8])
        bmodT, t_bmodT = load_const("bmodT", bmodT_d, [128, 48])
        convw, t_convw = load_const("convw", convw_d, [128, 4, 3])
        gconv, t_gconv = load_const("gconv", gconv_d, [128, 4])
        relt, t_relt = load_const("relt", relt_d, [33, 8])
        modT = sb("modT", [128, 48, nseq]); t_modT = Tok()
        gs1 = sb("gs1", [128, 8, nseq]); gs2 = sb("gs2", [128, 8, nseq]); t_gs = Tok()
        t_gtd = Tok()
        t_fw = Tok(); t_fc = Tok()

        with Scope():
            cT = sb("cT", [128, 8, nseq]); t_cT = Tok()
            P.dma("sp", cT[:], cT_d, writes=[t_cT])
            c_act = sb("c_act", [128, 8, nseq]); t_cact = Tok()
            P.op("act", lambda e: e.activation(out=c_act[:], in_=cT[:], func=AF.Silu), reads=[t_cT], writes=[t_cact])
            c_rep = sb("c_rep", [128, 8, nseq, 128]); t_crep = Tok()
            P.op("dve", lambda e: e.tensor_copy(out=c_rep[:], in_=c_act[:].unsqueeze(3).broadcast_to([128, 8, nseq, 128])),
                 reads=[t_cact], writes=[t_crep])
            bmod_bc = sb("bmod_bc", [128, 2, 1024]); t_bmbc = Tok()
            P.dma("sp", bmod_bc[:], bmod_bc_d, writes=[t_bmbc])
            gstage = [sb(f"gstage{i}", [128, nseq, 128]) for i in range(2)]; t_gst = [Tok(), Tok()]
            wm_view = w_mod_d.rearrange("(k p) n -> p k n", p=128)
            wmp = [sb(f"wm{i}", [128, 8, 128]) for i in range(3)]
            t_wmp = [Tok() for _ in range(3)]
            for j in range(48):
                wt, tw = wmp[j % 3], t_wmp[j % 3]
                P.dma("sp", wt[:], wm_view[:, :, j * 128:(j + 1) * 128], writes=[tw])
                for k in range(8):
                    P.op("pe", lambda e, k=k, wt=wt: e.matmul(ps[0][:, j * nseq:(j + 1) * nseq], lhsT=wt[:, k, :],
                                                              rhs=c_act[:, k, :], start=(k == 0), stop=(k == 7)),
                         reads=[tw, t_cact], writes=[tps[0]])
                which = {2: 0, 5: 1}.get(j // 8)
                if which is not None:
                    jj = j % 8
                    bi = 1 + (j % 2)
                    for b in range(nseq):
                        for k in range(8):
                            P.op("pe", lambda e, k=k, b=b, wt=wt, bi=bi: e.matmul(
                                ps[bi][:, b * 128:(b + 1) * 128], lhsT=c_rep[:, k, b, :], rhs=wt[:, k, :],
                                start=(k == 0), stop=(k == 7)), reads=[tw, t_crep], writes=[tps[bi]])
                    gsb, tg = gstage[j % 2], t_gst[j % 2]
                    P.op("dve", lambda e, bi=bi, which=which, jj=jj, gsb=gsb: e.tensor_tensor(
                        out=gsb[:],
                        in0=ps[bi][:, 0:nseq * 128].rearrange("p (b n) -> p b n", b=nseq),
                        in1=bmod_bc[:, which, jj * 128:(jj + 1) * 128].unsqueeze(1).broadcast_to([128, nseq, 128]),
                        op=ALU.add), reads=[tps[bi], t_bmbc], writes=[tg])
                    P.dma("sp", gt_d[which].rearrange("b p n -> p b n")[:, :, jj * 128:(jj + 1) * 128], gsb[:],
                          reads=[tg], writes=[t_gtd])
            P.op("dve", lambda e: e.tensor_tensor(
                out=modT[:], in0=ps[0][:, 0:48 * nseq].rearrange("p (j b) -> p j b", b=nseq),
                in1=bmodT[:].unsqueeze(2).broadcast_to([128, 48, nseq]), op=ALU.add),
                reads=[tps[0], t_bmodT], writes=[t_modT])
            P.op("dve", lambda e: e.scalar_tensor_tensor(
                out=gs1[:], in0=modT[:, 8:16, :], scalar=1.0, in1=gmix[:].unsqueeze(2).broadcast_to([128, 8, nseq]),
                op0=ALU.add, op1=ALU.mult), reads=[t_modT, t_gmix], writes=[t_gs])
            P.op("dve", lambda e: e.scalar_tensor_tensor(
                out=gs2[:], in0=modT[:, 32:40, :], scalar=1.0, in1=gffn[:].unsqueeze(2).broadcast_to([128, 8, nseq]),
                op0=ALU.add, op1=ALU.mult), reads=[t_modT, t_gffn], writes=[t_gs])
            dbgdump("modT", [128, 48, nseq], F32, modT[:], [t_modT])

            ohp = [sb(f"ohp{i}", [33, 512]) for i in range(2)]
            t_ohp = [Tok(), Tok()]
            tbs = [sb(f"tbs{i}", [8, 512]) for i in range(2)]
            t_tbs = [Tok(), Tok()]
            t_tbd = Tok()
            for kind in range(2):
                for pc in range(8):
                    i = (kind * 8 + pc) % 2
                    P.dma("sp", ohp[i][:], oh_d[:, kind, pc * 512:(pc + 1) * 512], writes=[t_ohp[i]])
                    bi = 3 + i
                    P.op("pe", lambda e, i=i, bi=bi: e.matmul(ps[bi][0:8, :], lhsT=relt[:, :], rhs=ohp[i][:, :],
                                                               start=True, stop=True),
                         reads=[t_relt, t_ohp[i]], writes=[tps[bi]])
                    P.op("act", lambda e, i=i, bi=bi: e.copy(out=tbs[i][:], in_=ps[bi][0:8, :]),
                         reads=[tps[bi]], writes=[t_tbs[i]])
                    P.dma("sp", tb_d[kind, :, pc * 512:(pc + 1) * 512], tbs[i][:], reads=[t_tbs[i]], writes=[t_tbd])
            for kind in range(2):
                for h in range(8):
                    src = bass.AP(tb_d.tensor, (kind * 8 + h) * 4096 + (2048 - 128), [[0, 128], [1, LW]])
                    dst = bass.AP(fw_d.tensor, (kind * 8 + h) * 128 * (LW + 1), [[LW + 1, 128], [1, LW]])
                    P.dma("sp", dst, src, reads=[t_tbd], writes=[t_fw])
            for h in range(8):
                src = bass.AP(tb_d.tensor, (8 + h) * 4096, [[0, 127], [1, LC]])
                dst = bass.AP(fc_d.tensor, h * 127 * (LC + 16), [[LC + 16, 127], [1, LC]])
                P.dma("sp", dst, src, reads=[t_tbd], writes=[t_fc])

        with Scope():
            qT = sb("qT", [128, 4, S], BF16); t_qT = [Tok() for _ in range(4)]
            kT = {n: sb(n + "T", [128, S], BF16) for n in ("kc", "vc", "ks", "kw")}
            t_kT = {n: [Tok() for _ in range(4)] for n in kT}
            vs_aug = sb("vs_aug", [128, NT, 2, 65], BF16); vw_aug = sb("vw_aug", [128, NT, 2, 65], BF16)
            t_va = [Tok() for _ in range(NT)]
            gat = sb("gat", [128, NT, 24]); t_gat = [Tok() for _ in range(NT)]
            mixT = sb("mixT", [128, 8, S], BF16)
            t_mixc = [Tok() for _ in range(4)]
            t_mixa = [Tok() for _ in range(NT)]
            t_ones = Tok()
            P.op("pool", lambda e: e.memset(vs_aug[:, :, :, 64:65], 1.0), writes=[t_ones])
            P.op("pool", lambda e: e.memset(vw_aug[:, :, :, 64:65], 1.0), writes=[t_ones])
            ssq = sb("ssq", [128, NTT]); rstd = sb("rstd", [128, NTT]); t_st = [Tok() for _ in range(NTT)]
            ssq2 = sb("ssq2", [128, NTT]); rstd2 = sb("rstd2", [128, NTT]); t_st2 = [Tok() for _ in range(NTT)]
            ssqa = sb("ssqa", [128, NTT]); rstda = sb("rstda", [128, NTT]); t_sta = [Tok() for _ in range(NTT)]

            def rms_stats(src_ap, t_src, junk_, t_junk_, ssq_, rstd_, tk, col, width):
                P.op("act", lambda e: e.activation(out=junk_, in_=src_ap, func=AF.Square, accum_out=ssq_[:, col:col + 1]),
                     reads=[t_src], writes=[t_junk_, tk])
                P.op("dve", lambda e: e.tensor_scalar(out=rstd_[:, col:col + 1], in0=ssq_[:, col:col + 1], scalar1=1.0 / width,
                                                      scalar2=EPS, op0=ALU.mult, op1=ALU.add), reads=[tk], writes=[tk])
                P.op("act", lambda e: e.activation(out=rstd_[:, col:col + 1], in_=rstd_[:, col:col + 1], func=AF.Sqrt),
                     reads=[tk], writes=[tk])
                P.op("dve", lambda e: e.reciprocal(out=rstd_[:, col:col + 1], in_=rstd_[:, col:col + 1]),
                     reads=[tk], writes=[tk])

            def norm_transpose(xt, t_xt, gi, s, ssq_, rstd_, t_st_, gs_, sh_lo, dst, t_dst, col0, junk, t_junk, xs, t_xs):
                rms_stats(xt[:], t_xt, junk[:], t_junk, ssq_, rstd_, t_st_[gi], gi, D)
                P.op("dve", lambda e: e.tensor_scalar(out=xs[:], in0=xt[:], scalar1=rstd_[:, gi:gi + 1], scalar2=None,
                                                      op0=ALU.mult), reads=[t_xt, t_st_[gi]], writes=[t_xs])
                for c in range(8):
                    P.op("pe", lambda e, c=c: e.transpose(out=psb[:, c * 128:(c + 1) * 128], in_=xs[:, c * 128:(c + 1) * 128],
                                                          identity=identb[:]), reads=[t_xs, t_identb], writes=[tpsb])
                for c in range(8):
                    o_ap = dst[:, c, col0:col0 + 128]
                    i_ap = psb[:, c * 128:(c + 1) * 128]
                    sc_ap = gs_[:, c, s:s + 1]
                    bi_ap = modT[:, sh_lo + c, s:s + 1]
                    if gi % 2 == 0:
                        P.op("dve", lambda e, o_ap=o_ap, i_ap=i_ap, sc_ap=sc_ap, bi_ap=bi_ap: e.tensor_scalar(
                            out=o_ap, in0=i_ap, scalar1=sc_ap, scalar2=bi_ap, op0=ALU.mult, op1=ALU.add),
                            reads=[tpsb, t_gs, t_modT], writes=[t_dst])
                    else:
                        P.op("act", lambda e, o_ap=o_ap, i_ap=i_ap, sc_ap=sc_ap, bi_ap=bi_ap: e.activation(
                            out=o_ap, in_=i_ap, func=AF.Identity, scale=sc_ap, bias=bi_ap),
                            reads=[tpsb, t_gs, t_modT], writes=[t_dst])

            def evac(i, out_ap, in_ap, reads, writes):
                if i % 2:
                    P.op("act", lambda e: e.copy(out=out_ap, in_=in_ap), reads=reads, writes=writes)
                else:
                    P.op("dve", lambda e: e.tensor_copy(out=out_ap, in_=in_ap), reads=reads, writes=writes)

            for s in range(nseq):
                with Scope():
                    win = sb("win", [128, 8, 2840], BF16); t_win = Tok()
                    wiv = w_in_d.rearrange("(k p) n -> p k n", p=128)
                    for k in range(8):
                        P.dma("pool", win[:, k, :], wiv[:, k, :], writes=[t_win])
                    xpool = [sb(f"xt{i}", [128, 1024]) for i in range(2)]; t_xp = [Tok(), Tok()]
                    junk = sb("junk", [128, 1024], BF16); t_junk = Tok()
                    xs = sb("xs", [128, 1024], BF16); t_xs = Tok()
                    hTg = [sb(f"hTg{i}", [128, 8, 512], BF16) for i in range(2)]; t_hTg = [Tok(), Tok()]
                    cbs = sb("cbs", [128, 512]); ccs = sb("ccs", [128, 512]); t_cbs = Tok(); t_ccs = Tok()
                    zb = sb("zb", [128, 4, 514]); t_zb = [Tok() for _ in range(4)]
                    yb = sb("yb", [128, 512]); t_yb = Tok()
                    oc = sb("oc", [128, 4, 512]); t_oc = [Tok() for _ in range(4)]
                    osq = sb("osq", [128, 4, 512], BF16); t_osq = [Tok() for _ in range(4)]
                    rbc = sb("rbc", [128, 512]); t_rbc = Tok()
                    for grp in range(4):
                        hT, t_hT = hTg[grp % 2], t_hTg[grp % 2]
                        s0 = grp * 512
                        for tl in range(4):
                            ti = grp * 4 + tl
                            gi = s * NT + ti
                            xt, t_xt = xpool[gi % 2], t_xp[gi % 2]
                            P.dma("sp", xt[:], x_d[gi * 128:(gi + 1) * 128, :], writes=[t_xt])
                            norm_transpose(xt, t_xt, gi, s, ssq, rstd, t_st, gs1, 0, hT, t_hT, tl * 128, junk, t_junk, xs, t_xs)
                        if s == 0 and grp == 0:
                            dbgdump("hT0", [128, 8, 512], BF16, hT[:], [t_hT])

                        def proj_chunk(cc, bi):
                            for k in range(8):
                                P.op("pe", lambda e, k=k: e.matmul(ps[bi][:, :], lhsT=win[:, k, cc * 128:(cc + 1) * 128],
                                                                   rhs=hT[:, k, :], start=(k == 0), stop=(k == 7)),
                                     reads=[t_win, t_hT], writes=[tps[bi]])

                        nb = 0
                        for cc in range(4):
                            bi = nb % 3; nb += 1
                            proj_chunk(cc, bi)
                            evac(cc, qT[:, cc, s0:s0 + 512], ps[bi][:, :], [tps[bi]], [t_qT[grp]])
                        for idx, n in enumerate(("kc", "vc", "ks", "kw")):
                            bi = nb % 3; nb += 1
                            proj_chunk(4 + idx, bi)
                            evac(idx, kT[n][:, s0:s0 + 512], ps[bi][:, :], [tps[bi]], [t_kT[n][grp]])
                        for c4 in range(4):
                            b_cb = nb % 3; nb += 1
                            proj_chunk(8 + c4 * 3 + 0, b_cb)
                            P.op("act", lambda e, b=b_cb: e.copy(out=cbs[:], in_=ps[b][:, :]), reads=[tps[b_cb]], writes=[t_cbs])
                            b_cc = nb % 3; nb += 1
                            proj_chunk(8 + c4 * 3 + 1, b_cc)
                            P.op("act", lambda e, b=b_cc: e.copy(out=ccs[:], in_=ps[b][:, :]), reads=[tps[b_cc]], writes=[t_ccs])
                            b_ch = nb % 3; nb += 1
                            proj_chunk(8 + c4 * 3 + 2, b_ch)
                            if grp == 0:
                                P.op("dve", lambda e, c4=c4: e.memset(zb[:, c4, 0:2], 0.0), writes=[t_zb[c4]])
                            P.op("dve", lambda e, c4=c4, b=b_ch: e.tensor_tensor(out=zb[:, c4, 2:514], in0=ps[b][:, :], in1=ccs[:],
                                                                                 op=ALU.mult),
                                 reads=[tps[b_ch], t_ccs], writes=[t_zb[c4]])
                            P.op("dve", lambda e, c4=c4: e.tensor_scalar(out=yb[:], in0=zb[:, c4, 2:514], scalar1=convw[:, c4, 2:3],
                                                                         scalar2=None, op0=ALU.mult),
                                 reads=[t_zb[c4], t_convw], writes=[t_yb])
                            P.op("dve", lambda e, c4=c4: e.scalar_tensor_tensor(out=yb[:], in0=zb[:, c4, 1:513], scalar=convw[:, c4, 1:2],
                                                                                in1=yb[:], op0=ALU.mult, op1=ALU.add),
                                 reads=[t_zb[c4], t_convw, t_yb], writes=[t_yb])
                            P.op("dve", lambda e, c4=c4: e.scalar_tensor_tensor(out=yb[:], in0=zb[:, c4, 0:512], scalar=convw[:, c4, 0:1],
                                                                                in1=yb[:], op0=ALU.mult, op1=ALU.add),
                                 reads=[t_zb[c4], t_convw, t_yb], writes=[t_yb])
                            P.op("dve", lambda e, c4=c4: e.tensor_tensor(out=oc[:, c4, :], in0=yb[:], in1=cbs[:], op=ALU.mult),
                                 reads=[t_yb, t_cbs], writes=[t_oc[c4]])
                            P.op("dve", lambda e, c4=c4: e.tensor_copy(out=zb[:, c4, 0:2], in_=zb[:, c4, 512:514]),
                                 reads=[t_zb[c4]], writes=[t_zb[c4]])
                            P.op("act", lambda e, c4=c4: e.activation(out=osq[:, c4, :], in_=oc[:, c4, :], func=AF.Square),
                                 reads=[t_oc[c4]], writes=[t_osq[c4]])
                        for c4 in range(4):
                            P.op("pe", lambda e, c4=c4: e.matmul(ps[3][:, :], lhsT=onesb[:, :], rhs=osq[:, c4, :],
                                                                 start=(c4 == 0), stop=(c4 == 3)),
                                 reads=[t_onesb, t_osq[c4]], writes=[tps[3]])
                        P.op("dve", lambda e: e.tensor_scalar(out=rbc[:], in0=ps[3][:, :], scalar1=1.0 / 512, scalar2=EPS,
                                                              op0=ALU.mult, op1=ALU.add), reads=[tps[3]], writes=[t_rbc])
                        P.op("act", lambda e: e.activation(out=rbc[:], in_=rbc[:], func=AF.Sqrt), reads=[t_rbc], writes=[t_rbc])
                        P.op("dve", lambda e: e.reciprocal(out=rbc[:], in_=rbc[:]), reads=[t_rbc], writes=[t_rbc])
                        for c4 in range(4):
                            P.op("dve", lambda e, c4=c4: e.scalar_tensor_tensor(
                                out=mixT[:, 4 + c4, s0:s0 + 512], in0=oc[:, c4, :], scalar=gconv[:, c4:c4 + 1], in1=rbc[:],
                                op0=ALU.mult, op1=ALU.mult), reads=[t_oc[c4], t_gconv, t_rbc], writes=[t_mixc[grp]])
                        for tl in range(4):
                            ti = grp * 4 + tl
                            bi = 4 + (tl % 2)
                            for k in range(8):
                                P.op("pe", lambda e, k=k, tl=tl, bi=bi: e.matmul(
                                    ps[bi][:, 0:280], lhsT=hT[:, k, tl * 128:(tl + 1) * 128], rhs=win[:, k, 2560:2840],
                                    start=(k == 0), stop=(k == 7)), reads=[t_win, t_hT], writes=[tps[bi]])
                            P.op("act", lambda e, ti=ti, bi=bi: e.copy(
                                out=vs_aug[:, ti, :, 0:64], in_=ps[bi][:, 0:128].rearrange("p (g d) -> p g d", g=2)),
                                reads=[tps[bi]], writes=[t_va[ti]])
                            P.op("act", lambda e, ti=ti, bi=bi: e.copy(
                                out=vw_aug[:, ti, :, 0:64], in_=ps[bi][:, 128:256].rearrange("p (g d) -> p g d", g=2)),
                                reads=[tps[bi]], writes=[t_va[ti]])
                            P.op("act", lambda e, ti=ti, bi=bi: e.activation(out=gat[:, ti, :], in_=ps[bi][:, 256:280],
                                                                             func=AF.Sigmoid),
                                 reads=[tps[bi]], writes=[t_gat[ti]])
                    if s == 0:
                        dbgdump("qT", [128, 4, S], BF16, qT[:], t_qT)
                        dbgdump("gat", [128, NT, 24], F32, gat[:], t_gat)
                        dbgdump("vs", [128, NT, 2, 65], BF16, vs_aug[:], t_va + [t_ones])
                        if stop_after <= 1:
                            dbgdump("mixT", [128, 8, S], BF16, mixT[:], t_mixc + t_mixa)
                if stop_after <= 1:
                    continue

                with Scope():
                    bd1 = {}; bd2 = {}; pe2 = {}; t_cw = Tok()
                    for n in "kv":
                        bd1[n] = sb("bd1" + n, [128, 32, 128], BF16)
                        P.dma("pool", bd1[n][:], bd1_d[n], writes=[t_cw])
                        bd2[n] = sb("bd2" + n, [128, 128], BF16)
                        P.dma("pool", bd2[n][:], bd2_d[n], writes=[t_cw])
                        pe2[n] = sb("pe2" + n, [128, 32], BF16)
                        P.dma("pool", pe2[n][:], pe2_d[n], writes=[t_cw])
                    Bw = sb("Bw", [128, 5, 8, 128], BF16); t_Bw = Tok()
                    Bs = sb("Bs", [128, 3, 8, 128], BF16); t_Bs = Tok()
                    for dl in range(5):
                        src = bass.AP(fw_d.tensor, 128 + dl * 128, [[LW, 128], [128 * (LW + 1), 8], [1, 128]])
                        P.dma("pool", Bw[:, dl, :, :], src, reads=[t_fw], writes=[t_Bw])
                    for dl in range(3):
                        src = bass.AP(fw_d.tensor, 8 * 128 * (LW + 1) + 128 + dl * 128,
                                      [[LW, 128], [128 * (LW + 1), 8], [1, 128]])
                        P.dma("pool", Bs[:, dl, :, :], src, reads=[t_fw], writes=[t_Bs])
                    if s == 0:
                        dbgdump("Bw", [128, 5, 8, 128], BF16, Bw[:], [t_Bw])
                        dbgdump("Bs", [128, 3, 8, 128], BF16, Bs[:], [t_Bs])
                    esel, t_esel = load_const("esel", esel_d, [128, 2048], BF16, "pool")
                    selmul, t_selmul = load_const("selmul", selmul_d, [128, 16, 32])
                    seladd, t_seladd = load_const("seladd", seladd_d, [128, 16, 32])
                    gattn, t_gattn = load_const("gattn", gattn_d, [128, 512])
                    cbias = sb("cbias", [128, 2]); t_cbias = Tok()
                    for i, n in enumerate("kv"):
                        for l in range(32):
                            P.op("pe", lambda e, n=n, l=l, i=i: e.matmul(ps[2][:, i:i + 1], lhsT=bd1[n][:, l, :],
                                                                         rhs=pe2[n][:, l:l + 1], start=(l == 0), stop=(l == 31)),
                                 reads=[t_cw], writes=[tps[2]])
                    P.op("dve", lambda e: e.tensor_copy(out=cbias[:], in_=ps[2][:, 0:2]), reads=[tps[2]], writes=[t_cbias])
                    kcmpT = sb("kcmpT", [128, 128], BF16); t_kcmp = Tok()
                    vc_aug = sb("vc_aug", [128, 2, 97], BF16); t_vca = Tok()
                    for g in range(2):
                        P.dma("pool", vc_aug[0:127, g, 65:97], cov_d, writes=[t_vca])
                    P.op("dve", lambda e: e.memset(vc_aug[:, :, 64:65], 1.0), writes=[t_vca])
                    hid = {n: sb("hid" + n, [128, 128], BF16) for n in "kv"}; t_hid = Tok()
                    for i, (n, srcn) in enumerate((("k", "kc"), ("v", "vc"))):
                        src = kT[srcn]
                        for l in range(32):
                            P.op("pe", lambda e, n=n, l=l, i=i, src=src: e.matmul(
                                ps[i][:, 0:127], lhsT=bd1[n][:, l, :], rhs=src[:, l:l + 2017:16],
                                start=(l == 0), stop=(l == 31)), reads=[t_cw] + t_kT[srcn], writes=[tps[i]])
                        P.op("act", lambda e, n=n, i=i: e.activation(out=hid[n][:, 0:127], in_=ps[i][:, 0:127],
                                                                     func=AF.Gelu_apprx_tanh, bias=cbias[:, i:i + 1]),
                             reads=[tps[i], t_cbias], writes=[t_hid])
                    P.op("pe", lambda e: e.matmul(ps[0][:, 0:127], lhsT=bd2["k"][:, :], rhs=hid["k"][:, 0:127], start=True, stop=True),
                         reads=[t_cw, t_hid], writes=[tps[0]])
                    P.op("dve", lambda e: e.tensor_copy(out=kcmpT[:, 0:127], in_=ps[0][:, 0:127]), reads=[tps[0]], writes=[t_kcmp])
                    P.op("pe", lambda e: e.matmul(ps[1][0:127, 0:128], lhsT=hid["v"][:, 0:127], rhs=bd2["v"][:, :], start=True, stop=True),
                         reads=[t_cw, t_hid], writes=[tps[1]])
                    P.op("dve", lambda e: e.tensor_copy(out=vc_aug[0:127, :, 0:64],
                                                        in_=ps[1][0:127, 0:128].rearrange("p (g d) -> p g d", g=2)),
                         reads=[tps[1]], writes=[t_vca])
                    if s == 0:
                        dbgdump("kcmpT", [128, 128], BF16, kcmpT[:], [t_kcmp])
                        dbgdump("vc_aug", [128, 2, 97], BF16, vc_aug[:], [t_vca])

                    bcp = [sb(f"bcp{i}", [128, 8, 128], BF16) for i in range(2)]; t_bcp = [Tok(), Tok()]
                    NSB = 4
                    sbank = [0, 1, 2, 6]
                    tS = [sb(f"tS{i}", [128, 512]) for i in range(NSB)]; t_tS = [Tok() for _ in range(NSB)]
                    pT = [sb(f"pT{i}", [128, 512], BF16) for i in range(NSB)]; t_pT = [Tok() for _ in range(NSB)]
                    o_acc = [sb(f"oacc{i}", [128, 8, 64]) for i in range(2)]; t_oacc = [Tok(), Tok()]
                    rs4 = sb("rs4", [128, 4]); wg4 = sb("wg4", [128, 4]); t_rs = Tok()
                    otmp = sb("otmp", [128, 4, 64]); t_otmp = Tok()
                    itmp = sb("itmp", [128, 4, 32]); imp = sb("imp", [128, 32]); t_imp = Tok()
                    m8 = sb("m8", [128, 8]); t_m8 = Tok()
                    negsel = [sb(f"negsel{i}", [128, 128], BF16) for i in range(2)]; t_negsel = [Tok(), Tok()]
                    for i in range(2):
                        P.op("dve", lambda e, i=i: e.memset(negsel[i][:], 0.0), writes=[t_negsel[i]])
                    nsT4 = [sb(f"nsT4{i}", [128, 4, 128], BF16) for i in range(2)]; t_nsT = [Tok(), Tok()]
                    junk2 = sb("junk2", [128, 512], BF16); t_junk2 = Tok()
                    on_b = sb("on_b", [128, 512], BF16); t_onb = Tok()
                    tpsb_a = tpsb
                    BK = {0: 0, 2: 1, 1: 2}
                    tpo = {br: [Tok()] for br in range(3)}

                    def evac_branch(g, br, qi, oa, t_oa):
                        c0 = 0
                        rd = tpo[br]
                        pob = psO[:, BK[br], :].rearrange("p (j c) -> p j c", j=4)
                        if br == 0:
                            P.op("dve", lambda e: e.tensor_scalar(out=rs4[:], in0=pob[:, :, 64], scalar1=1e-30, scalar2=None,
                                                                  op0=ALU.max), reads=rd, writes=[t_rs])
                            P.op("dve", lambda e: e.reciprocal(out=rs4[:], in_=rs4[:]), reads=[t_rs], writes=[t_rs])
                        else:
                            P.op("dve", lambda e: e.reciprocal(out=rs4[:], in_=pob[:, :, 64]), reads=rd, writes=[t_rs])
                        g0_ = 12 * g + br
                        P.op("dve", lambda e: e.tensor_tensor(out=wg4[:], in0=rs4[:], in1=gat[:, qi, g0_:g0_ + 10:3], op=ALU.mult),
                             reads=[t_rs, t_gat[qi]], writes=[t_rs])
                        if br == 0:
                            P.op("dve", lambda e: e.tensor_tensor(
                                out=oa[:, 4 * g:4 * g + 4, :], in0=pob[:, :, 0:64],
                                in1=wg4[:].unsqueeze(2).broadcast_to([128, 4, 64]), op=ALU.mult),
                                reads=rd + [t_rs], writes=[t_oa])
                            P.op("dve", lambda e: e.tensor_tensor(
                                out=itmp[:], in0=pob[:, :, 65:97], in1=rs4[:].unsqueeze(2).broadcast_to([128, 4, 32]),
                                op=ALU.mult), reads=rd + [t_rs], writes=[t_imp])
                            P.op("dve", lambda e: e.tensor_reduce(out=imp[:], in_=itmp[:].rearrange("p j n -> p n j"),
                                                                  axis=AX.X, op=ALU.add), reads=[t_imp], writes=[t_imp])
                        else:
                            P.op("dve", lambda e: e.tensor_tensor(
                                out=otmp[:], in0=pob[:, :, 0:64],
                                in1=wg4[:].unsqueeze(2).broadcast_to([128, 4, 64]), op=ALU.mult),
                                reads=rd + [t_rs], writes=[t_otmp])
                            P.op("pool", lambda e: e.tensor_tensor(out=oa[:, 4 * g:4 * g + 4, :], in0=oa[:, 4 * g:4 * g + 4, :],
                                                                   in1=otmp[:], op=ALU.add),
                                 reads=[t_otmp, t_oa], writes=[t_oa])

                    def selection(g, qi, it):
                        ns, t_ns = negsel[it % 2], t_negsel[it % 2]
                        nT, t_nT = nsT4[it % 2], t_nsT[it % 2]
                        P.op("dve", lambda e: e.tensor_tensor(out=imp[:], in0=imp[:], in1=selmul[:, qi, :], op=ALU.mult),
                             reads=[t_imp, t_selmul], writes=[t_imp])
                        P.op("dve", lambda e: e.tensor_tensor(out=imp[:], in0=imp[:], in1=seladd[:, qi, :], op=ALU.add),
                             reads=[t_imp, t_seladd], writes=[t_imp])
                        P.op("dve", lambda e: e.max(out=m8[:], in_=imp[:]), reads=[t_imp], writes=[t_m8])
                        P.op("dve", lambda e: e.tensor_scalar(out=ns[:, g * 64:g * 64 + 32], in0=imp[:], scalar1=m8[:, 7:8],
                                                              scalar2=NEG, op0=ALU.is_lt, op1=ALU.mult),
                             reads=[t_imp, t_m8], writes=[t_ns])

                        def later():
                            P.op("pe", lambda e: e.transpose(out=psb[:, 0:128], in_=ns[:, :], identity=identb[:]),
                                 reads=[t_ns, t_identb], writes=[tpsb_a])
                            P.op("act", lambda e: e.copy(out=nT[:], in_=psb[:, 0:128].unsqueeze(1).broadcast_to([128, 4, 128])),
                                 reads=[tpsb_a], writes=[t_nT])
                            sel_ready.add(it)
                        deferred.append([cur_n[0] + 2, later])

                    def finish_qi(qi, oa, t_oa):
                        gi = s * NT + qi
                        if dbg and s == 0:
                            if qi == 0:
                                dbg_out["oattn"] = dout("dbg_oattn", [NT, 128, 512], F32)
                            P.dma("sp", dbg_out["oattn"][qi], oa[:].rearrange("p h d -> p (h d)"), reads=[t_oa])
                        oaf = oa[:].rearrange("p h d -> p (h d)")
                        rms_stats(oaf, t_oa, junk2[:], t_junk2, ssqa, rstda, t_sta[gi], gi, 512)
                        P.op("dve", lambda e: e.scalar_tensor_tensor(
                            out=on_b[:], in0=oaf, scalar=rstda[:, gi:gi + 1], in1=gattn[:], op0=ALU.mult, op1=ALU.mult),
                            reads=[t_oa, t_sta[gi], t_gattn], writes=[t_onb])

                        def later():
                            for c in range(4):
                                P.op("pe", lambda e, c=c: e.transpose(out=psb[:, 512 + c * 128:512 + (c + 1) * 128],
                                                                      in_=on_b[:, c * 128:(c + 1) * 128], identity=identb[:]),
                                     reads=[t_onb, t_identb], writes=[tpsb])
                            for c in range(4):
                                evac(1, mixT[:, c, qi * 128:(qi + 1) * 128], psb[:, 512 + c * 128:512 + (c + 1) * 128],
                                     [tpsb], [t_mixa[qi]])
                        deferred.append([cur_n[0] + 4, later])

                    deferred = []
                    cur_n = [0]
                    sel_ready = set()
                    tiles = []
                    it = 0
                    for qi in range(NT):
                        oa, t_oa = o_acc[qi % 2], t_oacc[qi % 2]
                        for g in range(2):
                            pr = slice(g * 64, (g + 1) * 64)
                            rhs_q = qT[pr, :, qi * 128:(qi + 1) * 128]
                            rd_q = [t_qT[qi // 4]]
                            hs4 = slice(4 * g, 4 * g + 4)
                            bc, t_bc = bcp[qi % 2], t_bcp[qi % 2]
                            pre = None
                            if g == 0:
                                def pre(qi=qi, bc=bc, t_bc=t_bc):
                                    src = bass.AP(fc_d.tensor, 2048 + 128 * qi - 31, [[LC, 127], [127 * (LC + 16), 8], [1, 128]])
                                    P.dma("pool", bc[0:127, :, :], src, reads=[t_fc], writes=[t_bc])
                            tiles.append(dict(
                                pre=pre, rows=127, br=0, first=True, last=True, ncol=97,
                                mm=[(kcmpT[pr, 0:127], rhs_q, rd_q + [t_kcmp])],
                                bias=bc[0:127, hs4, :], t_bias=t_bc, v=vc_aug[0:127, g, :], v_rd=[t_vca],
                                post=(lambda g=g, qi=qi, oa=oa, t_oa=t_oa, it=it: (evac_branch(g, 0, qi, oa, t_oa),
                                                                                   selection(g, qi, it)))))
                            k0 = max(0, qi - 4)
                            for kj in range(k0, qi + 1):
                                tiles.append(dict(
                                    pre=None, rows=128, br=2, first=(kj == k0), last=(kj == qi), ncol=65,
                                    mm=[(kT["kw"][pr, kj * 128:(kj + 1) * 128], rhs_q, rd_q + [t_kT["kw"][kj // 4]])],
                                    bias=Bw[:, qi - kj, hs4, :], t_bias=t_Bw, v=vw_aug[:, kj, g, :], v_rd=[t_va[kj], t_ones],
                                    post=(lambda g=g, qi=qi, oa=oa, t_oa=t_oa: evac_branch(g, 2, qi, oa, t_oa)) if kj == qi else None))
                            nT, t_nT = nsT4[it % 2], t_nsT[it % 2]
                            for kj in range(qi + 1):
                                post = None
                                if kj == qi:
                                    if g == 1:
                                        post = (lambda g=g, qi=qi, oa=oa, t_oa=t_oa: (evac_branch(g, 1, qi, oa, t_oa),
                                                                                      finish_qi(qi, oa, t_oa)))
                                    else:
                                        post = (lambda g=g, qi=qi, oa=oa, t_oa=t_oa: evac_branch(g, 1, qi, oa, t_oa))
                                tiles.append(dict(
                                    need=it,
                                    pre=None, rows=128, br=1, first=(kj == 0), last=(kj == qi), ncol=65,
                                    mm=[(kT["ks"][pr, kj * 128:(kj + 1) * 128], rhs_q, rd_q + [t_kT["ks"][kj // 4]]),
                                        (esel[pr, kj * 128:(kj + 1) * 128], nT[pr, :, :], [t_esel, t_nT])],
                                    bias=Bs[:, min(qi - kj, 2), hs4, :], t_bias=t_Bs, v=vs_aug[:, kj, g, :],
                                    v_rd=[t_va[kj], t_ones], post=post))
                            it += 1

                    def emit_S(tl, sl):
                        if tl["pre"] is not None:
                            tl["pre"]()
                        rows = tl["rows"]
                        nmm = len(tl["mm"])
                        for m, (l_ap, r_ap, rd) in enumerate(tl["mm"]):
                            P.op("pe", lambda e, l_ap=l_ap, r_ap=r_ap, m=m: e.matmul(
                                ps[sbank[sl]][0:rows, :].rearrange("p (j q) -> p j q", j=4), lhsT=l_ap, rhs=r_ap,
                                start=(m == 0), stop=(m == nmm - 1)), reads=rd, writes=[tps[sbank[sl]]])
                        P.op("dve", lambda e: e.scalar_tensor_tensor(
                            out=tS[sl][0:rows, :].rearrange("p (j q) -> p j q", j=4),
                            in0=ps[sbank[sl]][0:rows, :].rearrange("p (j q) -> p j q", j=4), scalar=0.125,
                            in1=tl["bias"], op0=ALU.mult, op1=ALU.add), reads=[tps[sbank[sl]], tl["t_bias"]], writes=[t_tS[sl]])
                        P.op("act", lambda e: e.activation(out=pT[sl][0:rows, :], in_=tS[sl][0:rows, :], func=AF.Exp),
                             reads=[t_tS[sl]], writes=[t_pT[sl]])

                    def emit_PV(tl, sl):
                        rows = tl["rows"]
                        bk = BK[tl["br"]]
                        for j in range(4):
                            P.op("pe", lambda e, j=j: e.matmul(psO[:, bk, j * 128:j * 128 + tl["ncol"]],
                                                               lhsT=pT[sl][0:rows, j * 128:(j + 1) * 128], rhs=tl["v"],
                                                               start=(tl["first"] and j == 0), stop=(tl["last"] and j == 3),
                                                               skip_group_check=True),
                                 reads=[t_pT[sl]] + tl["v_rd"], writes=tpo[tl["br"]])
                        if tl["post"] is not None:
                            tl["post"]()

                    ntl = len(tiles)
                    LA = 2
                    nxt = 0
                    for n_ in range(ntl):
                        cur_n[0] = n_
                        while True:
                            for d_ in [d for d in deferred if d[0] <= n_]:
                                deferred.remove(d_)
                                d_[1]()
                            progressed = False
                            while nxt < ntl and nxt <= n_ + LA and (tiles[nxt].get("need") is None
                                                                   or tiles[nxt]["need"] in sel_ready):
                                emit_S(tiles[nxt], nxt % NSB)
                                nxt += 1
                                progressed = True
                            if nxt > n_:
                                break
                            d_ = min(deferred, key=lambda d: d[0])
                            deferred.remove(d_)
                            d_[1]()
                        emit_PV(tiles[n_], n_ % NSB)
                    for d_ in sorted(deferred, key=lambda d: d[0]):
                        d_[1]()
                    deferred.clear()

                    if s == 0:
                        dbgdump("mixT", [128, 8, S], BF16, mixT[:], t_mixc + t_mixa)
                if stop_after <= 2:
                    continue

                with Scope():
                    wout = sb("wout", [128, 8, 1024], BF16); t_wout = Tok()
                    wov = w_out_d.rearrange("(k p) n -> p k n", p=128)
                    for k in range(8):
                        P.dma("pool", wout[:, k, :], wov[:, k, :], writes=[t_wout])
                    gt1 = sb("gt1", [128, 1024]); t_gt1 = Tok()
                    P.dma("sp", gt1[:], gt_d[0, s], reads=[t_gtd], writes=[t_gt1])
                    xpool = [sb(f"xt3{i}", [128, 1024]) for i in range(2)]; t_xp = [Tok(), Tok()]
                    x1p = [sb(f"x1t{i}", [128, 1024]) for i in range(2)]; t_x1p = [Tok(), Tok()]
                    junk = sb("junk3", [128, 1024], BF16); t_junk = Tok()
                    xs = sb("xs3", [128, 1024], BF16); t_xs = Tok()
                    h2st = [sb(f"h2st{i}", [128, 8, 128], BF16) for i in range(2)]; t_h2st = [Tok(), Tok()]
                    for ti in range(NT):
                        gi = s * NT + ti
                        xt, t_xt = xpool[ti % 2], t_xp[ti % 2]
                        x1t, t_x1t = x1p[ti % 2], t_x1p[ti % 2]
                        P.dma("sp", xt[:], x_d[gi * 128:(gi + 1) * 128, :], writes=[t_xt])
                        for half in range(2):
                            for k in range(8):
                                P.op("pe", lambda e, k=k, half=half: e.matmul(
                                    ps[half][:, :], lhsT=mixT[:, k, ti * 128:(ti + 1) * 128],
                                    rhs=wout[:, k, half * 512:(half + 1) * 512], start=(k == 0), stop=(k == 7)),
                                    reads=[t_wout, t_mixa[ti], t_mixc[ti // 4]], writes=[tps[half]])
                            hs = slice(half * 512, (half + 1) * 512)
                            P.op("dve", lambda e, half=half, hs=hs: e.tensor_tensor(out=x1t[:, hs], in0=ps[half][:, :], in1=gt1[:, hs],
                                                                                    op=ALU.mult),
                                 reads=[tps[half], t_gt1], writes=[t_x1t])
                        P.op("pool", lambda e: e.tensor_tensor(out=x1t[:], in0=x1t[:], in1=xt[:], op=ALU.add),
                             reads=[t_x1t, t_xt], writes=[t_x1t])
                        P.dma("sp", x1_d[gi * 128:(gi + 1) * 128, :], x1t[:], reads=[t_x1t], writes=[t_x1d[gi]])
                        hs_, t_hs = h2st[ti % 2], t_h2st[ti % 2]
                        norm_transpose(x1t, t_x1t, gi, s, ssq2, rstd2, t_st2, gs2, 24, hs_, t_hs, 0, junk, t_junk, xs, t_xs)
                        P.dma("sp", h2T_d[:, :, gi * 128:(gi + 1) * 128], hs_[:], reads=[t_hs], writes=[t_h2d[gi]])

        if stop_after <= 3:
            P.wait_all("sp", Tok.registry)
            return nc, dbg_out

        try:
            utb_d = dscr("utb_s", [16384, 1024], BF16)
            vb_d = dscr("vb_s", [16384, 1024], BF16)
            s2_d = dscr("s2_s", [16, T, 128], BF16)
            t_utb = [Tok() for _ in range(16)]
            t_vb = [Tok() for _ in range(16)]
            with Scope():
                stg = [sb(f"cst{i}", [128, 8, 1024], BF16) for i in range(2)]; t_stg = [Tok(), Tok()]
                n = 0
                for src_d, dst_d, tks in ((ut_d, utb_d, t_utb), (pv_d, vb_d, t_vb)):
                    for c in range(16):
                        i = n % 2; n += 1
                        sv = src_d[c * 1024:(c + 1) * 1024, :].rearrange("(c p) n -> p c n", p=128)
                        dv = dst_d[c * 1024:(c + 1) * 1024, :].rearrange("(c p) n -> p c n", p=128)
                        P.dma("pool", stg[i][:], sv, writes=[t_stg[i]])
                        P.dma("sp", dv, stg[i][:], reads=[t_stg[i]], writes=[tks[c]])
            if stop_after == 3.5:
                raise _Stop()

            with Scope():
                wq, t_wq = None, Tok()
                wq = sb("wq", [128, 8, 1024], BF16)
                wqv = wq_d.rearrange("(k p) n -> p k n", p=128)
                for k in range(8):
                    P.dma("pool", wq[:, k, :], wqv[:, k, :], writes=[t_wq])
                kb, t_kb = load_const("kb", kb_d, [128, 8, 256], BF16, "pool")
                identf, t_identf = load_const("identf", identf_d, [128, 128])
                iota, t_iota = load_const("iota", iota_d, [128, 128])
                selh, t_selh = load_const("selh", selh_d, [16, 128], BF16, "pool")
                gfin, t_gfin = load_const("gfin", gfin_d, [128, 1024])
                gt2 = sb("gt2", [128, 1024]); t_gt2 = Tok()
                Wbuf = sb("Wbuf", [128, 256, 128], BF16); t_W = [Tok() for _ in range(64)]
                h2g = sb("h2g", [128, 8, 256], BF16); t_h2g = Tok()
                qTp = sb("qTp", [128, 8, 256], BF16); t_qTp = Tok()
                s_sb = sb("s_sb", [128, 8, 256]); t_s = Tok()
                work = sb("work", [128, 8, 256]); t_work = Tok()
                v16 = sb("v16", [128, 8, 2, 16]); t_v16 = Tok()
                idx = sb("idx", [128, 8, 16], U32); t_idx = Tok()
                cand = sb("cand", [128, 8, 256]); t_cand = Tok()
                cwork = sb("cwork", [128, 8, 256]); t_cwork = Tok()
                ts16 = sb("ts16", [128, 8, 16]); t_ts = Tok()
                d16 = sb("d16", [128, 8, 16]); t_d16 = Tok()
                zz = sb("zz", [128, 8]); mu = sb("mu", [128, 8]); tauE = sb("tauE", [128, 8]); t_zz = Tok()
                tm3 = sb("tm3", [128, 3, 128]); t_tm3 = Tok()
                tT3 = sb("tT3", [128, 3, 128]); t_tT3 = Tok()
                s2hm = [sb(f"s2hm{i}", [16, 16 * 128], BF16) for i in range(2)]; t_s2hm = [Tok(), Tok()]
                s2hb = sb("s2hb", [128, 2, 8, 128], BF16); t_s2hb = Tok()
                NRQ = 3
                e4 = [sb(f"e4{i}", [128, 4, 128], BF16) for i in range(NRQ)]; t_e4 = [Tok() for _ in range(NRQ)]
                Rt4 = [sb(f"Rt4{i}", [128, 4, 128], BF16) for i in range(NRQ)]; t_Rt4 = [Tok() for _ in range(NRQ)]
                Lt4 = [sb(f"Lt4{i}", [128, 4, 128], BF16) for i in range(NRQ)]; t_Lt4 = [Tok() for _ in range(NRQ)]
                mk4 = [sb(f"mk4{i}", [128, 4, 128], BF16) for i in range(NRQ)]; t_mk4 = [Tok() for _ in range(NRQ)]
                NC = 6
                utc = [sb(f"utc{i}", [128, 8, 128], BF16) for i in range(NC)]; t_utc = [Tok() for _ in range(NC)]
                vch = [sb(f"vch{i}", [128, 1024], BF16) for i in range(NC)]; t_vch = [Tok() for _ in range(NC)]
                abuf = [sb(f"abuf{i}", [128, 256], BF16) for i in range(2)]; t_ab = [Tok(), Tok()]
                wab = [sb(f"wab{i}", [128, 256], BF16) for i in range(2)]; t_wab = [Tok(), Tok()]
                x1t = sb("x1f", [128, 1024]); t_x1t = Tok()
                yt = sb("yf", [128, 1024]); t_yt = Tok()
                ot = sb("of", [128, 1024]); t_ot = Tok()
                junk = sb("junkf", [128, 1024], BF16); t_junk = Tok()
                ssq3 = sb("ssq3", [128, NTT]); rstd3 = sb("rstd3", [128, NTT]); t_st3 = [Tok() for _ in range(NTT)]
                t_s2d = Tok()
                t_out = Tok()

                def top16(dst_lo, dst_hi, src, wrk, rd, wr_dst, wr_wrk):
                    P.op("dve", lambda e: e.max(out=dst_lo, in_=src), reads=rd, writes=[wr_dst])
                    P.op("dve", lambda e: e.match_replace(out=wrk, in_to_replace=dst_lo, in_values=src, imm_value=-1e30),
                         reads=rd + [wr_dst], writes=[wr_wrk])
                    P.op("dve", lambda e: e.max(out=dst_hi, in_=wrk), reads=[wr_wrk], writes=[wr_dst])

                ngroups = T // 256
                for gidx in range(ngroups):
                    g0 = gidx * 256
                    sq = g0 // S
                    if g0 % S == 0:
                        P.dma("sp", gt2[:], gt_d[1, sq], reads=[t_gtd], writes=[t_gt2])
                    P.dma("sp", h2g[:], h2T_d[:, :, g0:g0 + 256], reads=t_h2d[gidx * 2:gidx * 2 + 2], writes=[t_h2g])
                    for h in range(8):
                        bi = h % 3
                        for k in range(8):
                            P.op("pe", lambda e, k=k, h=h, bi=bi: e.matmul(ps[bi][:, 0:256], lhsT=wq[:, k, h * 128:(h + 1) * 128],
                                                                           rhs=h2g[:, k, :], start=(k == 0), stop=(k == 7)),
                                 reads=[t_wq, t_h2g], writes=[tps[bi]])
                        evac(h, qTp[:, h, :], ps[bi][:, 0:256], [tps[bi]], [t_qTp])
                    for tt in range(2):
                        gi = gidx * 2 + tt
                        tsl = slice(tt * 128, (tt + 1) * 128)
                        for h in range(8):
                            bi = 3 + h // 2
                            P.op("pe", lambda e, h=h, bi=bi: e.matmul(ps[bi][:, (h % 2) * 256:(h % 2 + 1) * 256], lhsT=qTp[:, h, tsl],
                                                                      rhs=kb[:, h, :], start=True, stop=True),
                                 reads=[t_qTp, t_kb], writes=[tps[bi]])
                        for hp in range(4):
                            evac(hp, s_sb[:, 2 * hp:2 * hp + 2, :], ps[3 + hp][:, :].rearrange("p (h n) -> p h n", h=2),
                                 [tps[3 + hp]], [t_s])
                        for h in range(8):
                            for p in range(2):
                                top16(v16[:, h, p, 0:8], v16[:, h, p, 8:16], s_sb[:, h, p * 128:(p + 1) * 128],
                                      work[:, h, p * 128:(p + 1) * 128], [t_s], t_v16, t_work)
                            P.op("dve", lambda e, h=h: e.max_index(out=idx[:, h, 0:8], in_max=v16[:, h, 0, 0:8],
                                                                   in_values=s_sb[:, h, 0:128]),
                                 reads=[t_s, t_v16], writes=[t_idx])
                            P.op("dve", lambda e, h=h: e.max_index(out=idx[:, h, 8:16], in_max=v16[:, h, 0, 8:16],
                                                                   in_values=work[:, h, 0:128]),
                                 reads=[t_work, t_v16], writes=[t_idx])
                        P.op("dve", lambda e: e.tensor_tensor(
                            out=cand[:].rearrange("p h (a b) -> p h a b", a=16),
                            in0=v16[:, :, 0, :].unsqueeze(3).broadcast_to([128, 8, 16, 16]),
                            in1=v16[:, :, 1, :].unsqueeze(2).broadcast_to([128, 8, 16, 16]), op=ALU.add),
                            reads=[t_v16], writes=[t_cand])
                        for h in range(8):
                            top16(ts16[:, h, 0:8], ts16[:, h, 8:16], cand[:, h, :], cwork[:, h, :], [t_cand], t_ts, t_cwork)
                        if stop_after == 3.6:
                            dbgdump("v16", [128, 8, 2, 16], F32, v16[:], [t_v16])
                            dbgdump("ts16", [128, 8, 16], F32, ts16[:], [t_ts])
                            dbgdump("idx", [128, 8, 16], U32, idx[:], [t_idx])
                            dbgdump("s_sb", [128, 8, 256], F32, s_sb[:], [t_s])
                            raise _Stop()
                        P.op("dve", lambda e: e.tensor_tensor(out=d16[:], in0=ts16[:], in1=ts16[:, :, 0:1].broadcast_to([128, 8, 16]),
                                                              op=ALU.subtract), reads=[t_ts], writes=[t_d16])
                        P.op("act", lambda e: e.activation(out=d16[:], in_=d16[:], func=AF.Exp), reads=[t_d16], writes=[t_d16])
                        P.op("dve", lambda e: e.tensor_reduce(out=zz[:], in_=d16[:], axis=AX.X, op=ALU.add),
                             reads=[t_d16], writes=[t_zz])
                        P.op("act", lambda e: e.activation(out=zz[:], in_=zz[:], func=AF.Ln), reads=[t_zz], writes=[t_zz])
                        P.op("dve", lambda e: e.tensor_tensor(out=mu[:], in0=zz[:], in1=ts16[:, :, 0], op=ALU.add),
                             reads=[t_zz, t_ts], writes=[t_zz])
                        P.op("dve", lambda e: e.tensor_scalar(out=tauE[:], in0=ts16[:, :, 15], scalar1=-1e-4, scalar2=None,
                                                              op0=ALU.add), reads=[t_ts], writes=[t_zz])
                        P.op("dve", lambda e: e.tensor_tensor(
                            out=tm3[:, 0, :].rearrange("p (h a) -> p h a", h=8),
                            in0=tauE[:].unsqueeze(2).broadcast_to([128, 8, 16]), in1=v16[:, :, 0, :], op=ALU.subtract),
                            reads=[t_zz, t_v16], writes=[t_tm3])
                        P.op("dve", lambda e: e.tensor_tensor(
                            out=tm3[:, 1, :].rearrange("p (h a) -> p h a", h=8),
                            in0=v16[:, :, 0, :], in1=mu[:].unsqueeze(2).broadcast_to([128, 8, 16]), op=ALU.subtract),
                            reads=[t_zz, t_v16], writes=[t_tm3])
                        P.op("dve", lambda e: e.tensor_copy(out=tm3[:, 2, :], in_=idx[:].rearrange("p h a -> p (h a)")),
                             reads=[t_idx], writes=[t_tm3])
                        for w3 in range(3):
                            P.op("pe", lambda e, w3=w3: e.transpose(out=ps[2][:, w3 * 128:(w3 + 1) * 128], in_=tm3[:, w3, :],
                                                                    identity=identf[:]),
                                 reads=[t_tm3, t_identf], writes=[tps[2]])
                        P.op("dve", lambda e: e.tensor_copy(out=tT3[:], in_=ps[2][:, 0:384].rearrange("p (w t) -> p w t", w=3)),
                             reads=[tps[2]], writes=[t_tT3])
                        if stop_after == 3.7:
                            dbgdump("tT3", [128, 3, 128], F32, tT3[:], [t_tT3])
                            dbgdump("tm3", [128, 3, 128], F32, tm3[:], [t_tm3])
                            raise _Stop()
                        P.op("dve", lambda e: e.tensor_copy(out=s2hb[:, 0, :, :], in_=s_sb[:, :, 128:256]),
                             reads=[t_s], writes=[t_s2hb])
                        P.op("dve", lambda e: e.tensor_tensor(out=s2hb[:, 1, :, :], in0=s_sb[:, :, 128:256], in1=s2hb[:, 0, :, :],
                                                              op=ALU.subtract), reads=[t_s, t_s2hb], writes=[t_s2hb])
                        P.dma("sp", s2_d[:, g0 + tt * 128:g0 + (tt + 1) * 128, :].rearrange("(w h) t j -> t w h j", w=2), s2hb[:],
                              reads=[t_s2hb], writes=[t_s2d])
                        quads = []
                        for sl in range(8):
                            for q4 in range(4):
                                quads.append((sl, q4))

                        def load_slab(sl):
                            i2 = sl % 2
                            t0 = g0 + tt * 128 + sl * 16
                            P.dma("sp", s2hm[i2][:, :], s2_d[:, t0:t0 + 16, :].rearrange("h t j -> h (t j)"),
                                  reads=[t_s2d], writes=[t_s2hm[i2]])

                        def emit_sel(qn):
                            sl, q4 = quads[qn]
                            i2 = sl % 2
                            if q4 == 0:
                                load_slab(sl)
                            bA = 0 if qn % 2 == 0 else 4
                            bD = 3 if qn % 2 == 0 else 5
                            for bb in (bA, bD):
                                P.op("pe", lambda e, bb=bb: e.matmul(ps[bb][:, :], lhsT=selh[:, :],
                                                                     rhs=s2hm[i2][:, q4 * 512:(q4 + 1) * 512], start=True, stop=True),
                                     reads=[t_selh, t_s2hm[i2]], writes=[tps[bb]])
                            r = qn % NRQ
                            tl0 = sl * 16 + q4 * 4
                            for t in range(4):
                                P.op("act", lambda e, t=t: e.activation(out=e4[r][:, t, :], in_=ps[bA][:, t * 128:(t + 1) * 128],
                                                                        func=AF.Exp, bias=tT3[:, 1, tl0 + t:tl0 + t + 1]),
                                     reads=[tps[bA], t_tT3], writes=[t_e4[r]])
                            P.op("dve", lambda e: e.tensor_tensor(
                                out=mk4[r][:], in0=ps[bD][:, :].rearrange("p (t j) -> p t j", t=4),
                                in1=tT3[:, 0, tl0:tl0 + 4].unsqueeze(2).broadcast_to([128, 4, 128]), op=ALU.is_ge),
                                reads=[tps[bD], t_tT3], writes=[t_mk4[r]])
                            P.op("dve", lambda e: e.tensor_tensor(
                                out=Lt4[r][:], in0=iota[:].unsqueeze(1).broadcast_to([128, 4, 128]),
                                in1=tT3[:, 2, tl0:tl0 + 4].unsqueeze(2).broadcast_to([128, 4, 128]), op=ALU.is_equal),
                                reads=[t_iota, t_tT3], writes=[t_Lt4[r]])
                            P.op("dve", lambda e: e.tensor_tensor(out=Rt4[r][:], in0=mk4[r][:], in1=e4[r][:], op=ALU.mult),
                                 reads=[t_mk4[r], t_e4[r]], writes=[t_Rt4[r]])

                        def emit_w(qn):
                            sl, q4 = quads[qn]
                            r = qn % NRQ
                            wb = 1 + (qn % 2)
                            tb0 = tt * 128 + sl * 16 + q4 * 4
                            for t in range(4):
                                P.op("pe", lambda e, t=t: e.matmul(ps[wb][:, t * 128:(t + 1) * 128], lhsT=Rt4[r][:, t, :],
                                                                   rhs=Lt4[r][:, t, :], start=True, stop=True),
                                     reads=[t_Rt4[r], t_Lt4[r]], writes=[tps[wb]])
                            evac(qn, Wbuf[:, tb0:tb0 + 4, :], ps[wb][:, :].rearrange("p (t i) -> p t i", t=4),
                                 [tps[wb]], [t_W[tb0 // 4]])

                        emit_sel(0)
                        for qn in range(len(quads)):
                            if qn + 1 < len(quads):
                                emit_sel(qn + 1)
                            emit_w(qn)

                    if dbg and gidx == 0:
                        dbgdump("Wbuf", [128, 256, 128], BF16, Wbuf[:], t_W)
                    if stop_after == 3.8:
                        raise _Stop()
                    def ld(i):
                        c = i % NC
                        P.dma("sp", utc[c][:], utb_d[i * 128:(i + 1) * 128, :].rearrange("p (k j) -> p k j", k=8),
                              reads=[t_utb[i // 8]], writes=[t_utc[c]])
                        P.dma("sp", vch[c][:], vb_d[i * 128:(i + 1) * 128, :], reads=[t_vb[i // 8]], writes=[t_vch[c]])

                    def emU(i):
                        c = i % NC
                        ba = i % 2
                        for k in range(8):
                            P.op("pe", lambda e, k=k: e.matmul(ps[ba][:, 0:256], lhsT=utc[c][:, k, :], rhs=h2g[:, k, :],
                                                               start=(k == 0), stop=(k == 7)),
                                 reads=[t_utc[c], t_h2g], writes=[tps[ba]])
                        P.op("act", lambda e: e.activation(out=abuf[ba][:], in_=ps[ba][:, 0:256], func=AF.Gelu_apprx_tanh),
                             reads=[tps[ba]], writes=[t_ab[ba]])
                        weng = "dve" if i % 2 == 0 else "pool"
                        P.op(weng, lambda e: e.tensor_tensor(out=wab[ba][:], in0=abuf[ba][:], in1=Wbuf[:, :, i], op=ALU.mult),
                             reads=[t_ab[ba]] + t_W, writes=[t_wab[ba]])

                    def emV(i):
                        c = i % NC
                        ba = i % 2
                        for tt in range(2):
                            for half in range(2):
                                P.op("pe", lambda e, tt=tt, half=half: e.matmul(
                                    psO[:, tt * 2 + half, :], lhsT=wab[ba][:, tt * 128:(tt + 1) * 128],
                                    rhs=vch[c][:, half * 512:(half + 1) * 512], start=(i == 0), stop=(i == 127)),
                                    reads=[t_wab[ba], t_vch[c]], writes=[tps[3 + tt * 2 + half]])

                    PF = NC - 2
                    for i in range(PF):
                        ld(i)
                    emU(0)
                    for i in range(128):
                        if i + PF < 128:
                            ld(i + PF)
                        if i + 1 < 128:
                            emU(i + 1)
                        emV(i)
                    for tt in range(2):
                        gi = gidx * 2 + tt
                        P.dma("sp", x1t[:], x1_d[gi * 128:(gi + 1) * 128, :], reads=[t_x1d[gi]], writes=[t_x1t])
                        for half in range(2):
                            hs = slice(half * 512, (half + 1) * 512)
                            P.op("dve", lambda e, tt=tt, half=half, hs=hs: e.tensor_tensor(
                                out=yt[:, hs], in0=psO[:, tt * 2 + half, :], in1=gt2[:, hs], op=ALU.mult),
                                reads=[tps[3 + tt * 2 + half], t_gt2], writes=[t_yt])
                        if dbg and gidx == 0 and tt == 0:
                            dbgdump("peer0", [128, 1024], F32, yt[:], [t_yt])
                        P.op("pool", lambda e: e.tensor_tensor(out=yt[:], in0=yt[:], in1=x1t[:], op=ALU.add),
                             reads=[t_yt, t_x1t], writes=[t_yt])
                        rms_stats(yt[:], t_yt, junk[:], t_junk, ssq3, rstd3, t_st3[gi], gi, D)
                        P.op("dve", lambda e, gi=gi: e.scalar_tensor_tensor(out=ot[:], in0=yt[:], scalar=rstd3[:, gi:gi + 1], in1=gfin[:],
                                                                            op0=ALU.mult, op1=ALU.mult),
                             reads=[t_yt, t_st3[gi], t_gfin], writes=[t_ot])
                        P.dma("sp", out_d[gi * 128:(gi + 1) * 128, :], ot[:], reads=[t_ot], writes=[t_out])
                    if stop_after == 4 and gidx == 0:
                        raise _Stop()

        except _Stop:
            pass
        P.wait_all("sp", Tok.registry)
        print("ops per engine", P.ecount, "waits", P.nwaits)
    return nc, dbg_out


_NC_CACHE = {}


def kernel(**inputs):
    inp = {k: np.asarray(v) for k, v in inputs.items()}
    sh = _prep(inp)
    if "nc" not in _NC_CACHE:
        _NC_CACHE["nc"] = build_nc(nseq=2)[0]
    nc = _NC_CACHE["nc"]
    x = np.asarray(inp["x"], np.float32)
    c = np.asarray(inp["c"], np.float32)
    in_maps = []
    for core in range(8):
        m = dict(sh)
        m["x"] = np.ascontiguousarray(x[2 * core:2 * core + 2].reshape(2 * S, D))
        m["cT"] = np.ascontiguousarray(c[2 * core:2 * core + 2].T.reshape(8, 128, 2).transpose(1, 0, 2))
        in_maps.append(m)
    res = run_bass_kernel_spmd(nc, in_maps, core_ids=list(range(8)))
    out = np.concatenate([np.asarray(r["out"]).reshape(2, S, D) for r in res.results], axis=0)
    return out.astype(np.float32)
```

```python
import math
import numpy as np
from contextlib import ExitStack
import concourse.bass as bass
import concourse.mybir as mybir
from concourse.bass_utils import run_bass_kernel_spmd

F32 = mybir.dt.float32
BF16 = mybir.dt.bfloat16
U32 = mybir.dt.uint32
AF = mybir.ActivationFunctionType
ALU = mybir.AluOpType
AX = mybir.AxisListType

S = 2048
D = 1024
NT = 16
EPS = 1e-6
NEG = -30000.0
LENG = "dve"
import os
PDBG = os.environ.get("PDBG", "")
SEM_CHUNK = 30000


class _Stop(Exception):
    pass


class Tok:
    __slots__ = ("writers", "readers")
    registry = []

    def __init__(self):
        self.writers = []
        self.readers = []
        Tok.registry.append(self)


class Op:
    __slots__ = ("eng", "sem", "val", "is_dma")


class Prog:
    ENGS = ("pe", "dve", "act", "pool", "sp")

    def __init__(self, nc, stack, n_dma_sems=8):
        self.nc = nc
        self.stack = stack
        self.eobj = {"pe": nc.tensor, "dve": nc.vector, "act": nc.scalar,
                     "pool": nc.gpsimd, "sp": nc.sync}
        self.esems = {e: [] for e in self.ENGS}
        self.ecount = {e: 0 for e in self.ENGS}
        self.waited = {e: {} for e in self.ENGS}
        self.dma_sems = {}
        self.dma_rr = {}
        self.n_dma_sems = n_dma_sems
        self.nwaits = 0

    def _esem(self, e, chunk):
        lst = self.esems[e]
        while len(lst) <= chunk:
            lst.append(self.stack.enter_context(self.nc.semaphore(f"s_{e}_{len(lst)}")))
        return lst[chunk]

    def _wait(self, e, sem, val):
        w = self.waited[e]
        k = id(sem)
        if w.get(k, 0) >= val:
            return
        w[k] = val
        self.eobj[e].wait_ge(sem, val)
        self.nwaits += 1

    def _deps(self, e, is_dma, reads, writes):
        for t in reads:
            for p in t.writers:
                if (not p.is_dma) and (not is_dma) and p.eng == e and e == "pe":
                    continue
                self._wait(e, p.sem, p.val)
        for t in writes:
            for p in t.writers + t.readers:
                if (not p.is_dma) and (not is_dma) and p.eng == e and e == "pe":
                    continue
                self._wait(e, p.sem, p.val)

    def _record(self, op, reads, writes):
        for t in reads:
            if not op.is_dma:
                t.readers = [r for r in t.readers if r.is_dma or r.eng != op.eng]
            t.readers.append(op)
        for t in writes:
            if t.readers:
                t.writers = [op]
                t.readers = []
            else:
                if not op.is_dma:
                    t.writers = [w for w in t.writers if w.is_dma or w.eng != op.eng]
                t.writers.append(op)

    def op(self, e, fn, reads=(), writes=()):
        self._deps(e, False, reads, writes)
        n = self.ecount[e]
        sem = self._esem(e, n // SEM_CHUNK)
        val = (n % SEM_CHUNK) + 1
        fn(self.eobj[e]).then_inc(sem, 1)
        self.ecount[e] = n + 1
        o = Op()
        o.eng, o.sem, o.val, o.is_dma = e, sem, val, False
        self._record(o, reads, writes)
        return o

    def dma(self, q, out, in_, reads=(), writes=(), **kw):
        if q not in self.dma_sems:
            self.dma_sems[q] = [[self.stack.enter_context(self.nc.semaphore(f"d_{q}_{i}")), 0]
                                for i in range(self.n_dma_sems)]
            self.dma_rr[q] = 0
        self._deps(q, True, reads, writes)
        i = self.dma_rr[q]
        self.dma_rr[q] = (i + 1) % self.n_dma_sems
        ent = self.dma_sems[q][i]
        sem, cur = ent
        if cur > 0:
            self._wait(q, sem, cur)
        val = cur + 16
        ent[1] = val
        self.eobj[q].dma_start(out=out, in_=in_, **kw).then_inc(sem, 16)
        o = Op()
        o.eng, o.sem, o.val, o.is_dma = q, sem, val, True
        self._record(o, reads, writes)
        return o

    def barrier(self):
        for e in self.ENGS:
            for f in self.ENGS:
                n = self.ecount[f]
                if f == e or n == 0:
                    continue
                self._wait(e, self.esems[f][(n - 1) // SEM_CHUNK], (n - 1) % SEM_CHUNK + 1)
            for q, lst in self.dma_sems.items():
                for sem, cur in lst:
                    if cur > 0:
                        self._wait(e, sem, cur)

    def wait_all(self, e, toks):
        for t in toks:
            for p in t.writers + t.readers:
                self._wait(e, p.sem, p.val)


def _rel_bucket_np(n):
    n = np.maximum(n, 0)
    exact = 16
    lr = np.log(np.maximum(n, 1).astype(np.float32) / np.float32(exact)) / np.float32(math.log(128 / exact))
    large = exact + (lr * np.float32(32 - exact)).astype(np.int32)
    return np.where(n < exact, n, np.minimum(large, 31))


def _constants():
    c = {}
    npr = np.arange(4096)
    n = npr - 2048
    bk = _rel_bucket_np(n)
    oh = np.zeros((33, 2, 4096), np.float32)
    okw = (n >= 0) & (n < 512)
    oks = (n >= 0)
    oh[bk[okw], 0, npr[okw]] = 1.0
    oh[32, 0, npr[~okw]] = 1.0
    oh[bk[oks], 1, npr[oks]] = 1.0
    oh[32, 1, npr[~oks]] = 1.0
    c["oh"] = oh
    e32 = (np.arange(2048)[None, :] // 64 == np.arange(32)[:, None]).astype(np.float32)
    e128 = np.zeros((128, 2048), np.float32)
    e128[0:32] = e32
    e128[64:96] = e32
    c["Esel"] = e128
    cs = np.arange(127) * 16
    ss = np.arange(32) * 64
    c["Cov"] = ((cs[:, None] < ss[None, :] + 64) & (cs[:, None] + 32 > ss[None, :])).astype(np.float32)
    t = np.arange(2048)
    cur = t // 64
    j = np.arange(32)
    forced = (j[None, :] == 0) | (j[None, :] == cur[:, None]) | (j[None, :] == cur[:, None] - 1)
    allowed = j[None, :] * 64 <= t[:, None]
    mul = (allowed & ~forced).astype(np.float32)
    add = np.where(forced, 1e4, np.where(allowed, 0.0, -1e30)).astype(np.float32)
    c["selmul"] = np.ascontiguousarray(mul.reshape(16, 128, 32).transpose(1, 0, 2))
    c["seladd"] = np.ascontiguousarray(add.reshape(16, 128, 32).transpose(1, 0, 2))
    c["identf"] = np.eye(128, dtype=np.float32)
    c["ones"] = np.ones((128, 128), np.float32)
    c["iota"] = np.tile(np.arange(128, dtype=np.float32)[None, :], (128, 1))
    sel = np.zeros((8, 128), np.float32)
    for h in range(8):
        sel[h, h * 16:(h + 1) * 16] = 1.0
    c["selh"] = np.concatenate([sel, sel], axis=0)
    return c


def _blockdiag2(w):
    out = np.zeros(w.shape[:-2] + (128, 128), np.float32)
    out[..., :64, :64] = w
    out[..., 64:, 64:] = w
    return out


def _prep(inp):
    sh = {}
    f = lambda a: np.ascontiguousarray(np.asarray(a, np.float32))
    sh["w_mod"] = f(inp["w_mod"][0])
    sh["bmodT"] = f(inp["b_mod"][0].reshape(48, 128).T)
    bm = inp["b_mod"][0]
    sh["bmod_bc"] = f(np.broadcast_to(np.stack([bm[2048:3072], bm[5120:6144]])[None], (128, 2, 1024)))
    sh["gmix"] = f(inp["ln_mix_g"][0].reshape(8, 128).T)
    sh["gffn"] = f(inp["ln_ffn_g"][0].reshape(8, 128).T)
    sh["gfin_bc"] = f(np.broadcast_to(inp["ln_final_g"][None, :], (128, 1024)))
    w_in = np.asarray(inp["w_in"][0], np.float32)
    sp = np.cumsum([0, 512, 128, 128, 128, 128, 128, 128, 24, 512, 512, 512])
    q, kc, vc, ks, vs, kw, vw, gt, cb, cc, ch = [w_in[:, sp[i]:sp[i + 1]] for i in range(11)]
    qh = q.reshape(1024, 8, 64)
    qperm = np.concatenate([np.concatenate([qh[:, j], qh[:, 4 + j]], axis=1) for j in range(4)], axis=1)
    cols = [qperm, kc, vc, ks, kw]
    for c4 in range(4):
        cols += [cb[:, c4 * 128:(c4 + 1) * 128], cc[:, c4 * 128:(c4 + 1) * 128], ch[:, c4 * 128:(c4 + 1) * 128]]
    cols += [vs, vw, gt]
    sh["w_in"] = f(np.concatenate(cols, axis=1))
    for nm, w1, w2, pe in (("k", "cmp_wk1", "cmp_wk2", "cmp_pe_k"), ("v", "cmp_wv1", "cmp_wv2", "cmp_pe_v")):
        a = np.asarray(inp[w1][0], np.float32).reshape(32, 64, 64)
        sh["bd1" + nm] = f(_blockdiag2(a).transpose(1, 0, 2))
        sh["bd2" + nm] = f(_blockdiag2(np.asarray(inp[w2][0], np.float32)))
        p = np.asarray(inp[pe][0], np.float32).T
        sh["pe2" + nm] = f(np.concatenate([p, p], axis=0))
    sh["convw"] = f(inp["conv_w"][0][:, 0, :].reshape(3, 4, 128).transpose(2, 1, 0))
    sh["gattn_bc"] = f(np.broadcast_to(inp["norm_attn_g"][0][None, :], (128, 512)))
    sh["gconv"] = f(inp["norm_conv_g"][0].reshape(4, 128).T)
    sh["w_out"] = f(inp["w_out"][0])
    sh["peer_wq"] = f(inp["peer_wq"][0])
    pk = np.asarray(inp["peer_keys"][0], np.float32)
    kb = np.zeros((128, 8, 256), np.float32)
    for h in range(8):
        for p in range(2):
            kb[p * 64:(p + 1) * 64, h, p * 128:(p + 1) * 128] = pk[h, p].T
    sh["peer_kb"] = kb
    u = np.asarray(inp["peer_u"][0], np.float32)
    sh["peer_ut"] = f(u.reshape(128, 128, 8, 128).transpose(0, 3, 2, 1).reshape(128 * 128, 1024))
    sh["peer_v"] = f(inp["peer_v"][0])
    rt = np.zeros((33, 8), np.float32)
    rt[:32] = inp["rel_table"]
    rt[32] = NEG
    sh["relt"] = rt
    sh.update(_constants())
    return sh


def build_nc(nseq=2, dbg=False, stop_after=99):
    nc = bass.Bass("TRN2", target_bir_lowering=False)
    Tok.registry = []
    T = nseq * S
    NTT = nseq * NT

    def din(name, shape, dt=F32):
        return nc.dram_tensor(name, list(shape), dt, kind="ExternalInput").ap()

    def dscr(name, shape, dt=F32):
        return nc.dram_tensor(name, list(shape), dt, kind="Internal").ap()

    def dout(name, shape, dt=F32):
        return nc.dram_tensor(name, list(shape), dt, kind="ExternalOutput").ap()

    x_d = din("x", [T, D])
    cT_d = din("cT", [128, 8, nseq])
    w_mod_d = din("w_mod", [1024, 6144])
    bmodT_d = din("bmodT", [128, 48])
    bmod_bc_d = din("bmod_bc", [128, 2, 1024])
    gmix_d = din("gmix", [128, 8])
    gffn_d = din("gffn", [128, 8])
    gfin_d = din("gfin_bc", [128, 1024])
    w_in_d = din("w_in", [1024, 2840])
    bd1_d = {n: din("bd1" + n, [128, 32, 128]) for n in "kv"}
    bd2_d = {n: din("bd2" + n, [128, 128]) for n in "kv"}
    pe2_d = {n: din("pe2" + n, [128, 32]) for n in "kv"}
    convw_d = din("convw", [128, 4, 3])
    gattn_d = din("gattn_bc", [128, 512])
    gconv_d = din("gconv", [128, 4])
    w_out_d = din("w_out", [1024, 1024])
    wq_d = din("peer_wq", [1024, 1024])
    kb_d = din("peer_kb", [128, 8, 256])
    ut_d = din("peer_ut", [128 * 128, 1024])
    pv_d = din("peer_v", [16384, 1024])
    relt_d = din("relt", [33, 8])
    oh_d = din("oh", [33, 2, 4096])
    esel_d = din("Esel", [128, 2048])
    cov_d = din("Cov", [127, 32])
    selmul_d = din("selmul", [128, 16, 32])
    seladd_d = din("seladd", [128, 16, 32])
    identf_d = din("identf", [128, 128])
    ones_d = din("ones", [128, 128])
    iota_d = din("iota", [128, 128])
    selh_d = din("selh", [16, 128])
    out_d = dout("out", [T, D])

    tb_d = dscr("tb_s", [2, 8, 4096])
    LW = 1024
    fw_d = dscr("fw_s", [2, 8, 128 * (LW + 1)])
    LC = 4096
    fc_d = dscr("fc_s", [8, 127 * (LC + 16)])
    gt_d = dscr("gt_s", [2, nseq, 128, 1024])
    if dbg:
        x1_d = dout("x1_s", [T, D])
        h2T_d = dout("h2T_s", [128, 8, T], BF16)
    else:
        x1_d = dscr("x1_s", [T, D])
        h2T_d = dscr("h2T_s", [128, 8, T], BF16)
    t_x1d = [Tok() for _ in range(NTT)]
    t_h2d = [Tok() for _ in range(NTT)]

    dbg_out = {}
    uniq = [0]

    with ExitStack() as st0:
        P = Prog(nc, st0)
        scopes = [st0]

        def sb(name, shape, dt=F32):
            uniq[0] += 1
            return scopes[-1].enter_context(nc.sbuf_tensor(f"sb{uniq[0]}_{name}", list(shape), dt))

        class Scope:
            def __enter__(self):
                self.st = ExitStack()
                scopes.append(self.st)
                return self

            def __exit__(self, *a):
                if a[0] is None:
                    P.barrier()
                scopes.pop()
                self.st.close()
                return False

        psg = [st0.enter_context(nc.psum_tensor(f"psum{i}", [128, 512], F32)) for i in range(3)]
        psO = st0.enter_context(nc.psum_tensor("psumO", [128, 4, 512], F32))
        ps = [psg[0][:, :], psg[1][:, :], psg[2][:, :]] + [psO[:, i, :] for i in range(4)]
        tps = [Tok() for _ in range(7)]
        psb = st0.enter_context(nc.psum_tensor("psumb", [128, 1024], BF16))
        tpsb = Tok()

        def load_const(name, src, shape, dt=F32, q="sp"):
            t = sb(name, shape, dt)
            k = Tok()
            P.dma(q, t[:], src, writes=[k])
            return t, k

        def dbgdump(name, shape, dt, src_ap, toks):
            if not dbg:
                return
            dbg_out[name] = dout("dbg_" + name, shape, dt)
            P.dma("sp", dbg_out[name], src_ap, reads=toks)

        identb, t_identb = load_const("identb", identf_d, [128, 128], BF16, "pool")
        onesb, t_onesb = load_const("onesb", ones_d, [128, 128], BF16, "pool")
        gmix, t_gmix = load_const("gmix", gmix_d, [128, 8])
        gffn, t_gffn = load_const("gffn", gffn_d, [128, 8])
        bmodT, t_bmodT = load_const("bmodT", bmodT_d, [128, 48])
        convw, t_convw = load_const("convw", convw_d, [128, 4, 3])
        gconv, t_gconv = load_const("gconv", gconv_d, [128, 4])
        relt, t_relt = load_const("relt", relt_d, [33, 8])
        modT = sb("modT", [128, 48, nseq]); t_modT = Tok()
        gs1 = sb("gs1", [128, 8, nseq]); gs2 = sb("gs2", [128, 8, nseq]); t_gs = Tok()
        t_gtd = Tok()
        t_fw = Tok(); t_fc = Tok()

        with Scope():
            cT = sb("cT", [128, 8, nseq]); t_cT = Tok()
            P.dma("sp", cT[:], cT_d, writes=[t_cT])
            c_act = sb("c_act", [128, 8, nseq]); t_cact = Tok()
            P.op("act", lambda e: e.activation(out=c_act[:], in_=cT[:], func=AF.Silu), reads=[t_cT], writes=[t_cact])
            c_rep = sb("c_rep", [128, 8, nseq, 128]); t_crep = Tok()
            P.op("dve", lambda e: e.tensor_copy(out=c_rep[:], in_=c_act[:].unsqueeze(3).broadcast_to([128, 8, nseq, 128])),
                 reads=[t_cact], writes=[t_crep])
            bmod_bc = sb("bmod_bc", [128, 2, 1024]); t_bmbc = Tok()
            P.dma("sp", bmod_bc[:], bmod_bc_d, writes=[t_bmbc])
            gstage = [sb(f"gstage{i}", [128, nseq, 128]) for i in range(2)]; t_gst = [Tok(), Tok()]
            wm_view = w_mod_d.rearrange("(k p) n -> p k n", p=128)
            wmp = [sb(f"wm{i}", [128, 8, 128]) for i in range(3)]
            t_wmp = [Tok() for _ in range(3)]
            for j in range(48):
                wt, tw = wmp[j % 3], t_wmp[j % 3]
                P.dma("sp", wt[:], wm_view[:, :, j * 128:(j + 1) * 128], writes=[tw])
                for k in range(8):
                    P.op("pe", lambda e, k=k, wt=wt: e.matmul(ps[0][:, j * nseq:(j + 1) * nseq], lhsT=wt[:, k, :],
                                                              rhs=c_act[:, k, :], start=(k == 0), stop=(k == 7)),
                         reads=[tw, t_cact], writes=[tps[0]])
                which = {2: 0, 5: 1}.get(j // 8)
                if which is not None:
                    jj = j % 8
                    bi = 1 + (j % 2)
                    for b in range(nseq):
                        for k in range(8):
                            P.op("pe", lambda e, k=k, b=b, wt=wt, bi=bi: e.matmul(
                                ps[bi][:, b * 128:(b + 1) * 128], lhsT=c_rep[:, k, b, :], rhs=wt[:, k, :],
                                start=(k == 0), stop=(k == 7)), reads=[tw, t_crep], writes=[tps[bi]])
                    gsb, tg = gstage[j % 2], t_gst[j % 2]
                    P.op("dve", lambda e, bi=bi, which=which, jj=jj, gsb=gsb: e.tensor_tensor(
                        out=gsb[:],
                        in0=ps[bi][:, 0:nseq * 128].rearrange("p (b n) -> p b n", b=nseq),
                        in1=bmod_bc[:, which, jj * 128:(jj + 1) * 128].unsqueeze(1).broadcast_to([128, nseq, 128]),
                        op=ALU.add), reads=[tps[bi], t_bmbc], writes=[tg])
                    P.dma("sp", gt_d[which].rearrange("b p n -> p b n")[:, :, jj * 128:(jj + 1) * 128], gsb[:],
                          reads=[tg], writes=[t_gtd])
            P.op("dve", lambda e: e.tensor_tensor(
                out=modT[:], in0=ps[0][:, 0:48 * nseq].rearrange("p (j b) -> p j b", b=nseq),
                in1=bmodT[:].unsqueeze(2).broadcast_to([128, 48, nseq]), op=ALU.add),
                reads=[tps[0], t_bmodT], writes=[t_modT])
            P.op("dve", lambda e: e.scalar_tensor_tensor(
                out=gs1[:], in0=modT[:, 8:16, :], scalar=1.0, in1=gmix[:].unsqueeze(2).broadcast_to([128, 8, nseq]),
                op0=ALU.add, op1=ALU.mult), reads=[t_modT, t_gmix], writes=[t_gs])
            P.op("dve", lambda e: e.scalar_tensor_tensor(
                out=gs2[:], in0=modT[:, 32:40, :], scalar=1.0, in1=gffn[:].unsqueeze(2).broadcast_to([128, 8, nseq]),
                op0=ALU.add, op1=ALU.mult), reads=[t_modT, t_gffn], writes=[t_gs])
            dbgdump("modT", [128, 48, nseq], F32, modT[:], [t_modT])

            ohp = [sb(f"ohp{i}", [33, 512]) for i in range(2)]
            t_ohp = [Tok(), Tok()]
            tbs = [sb(f"tbs{i}", [8, 512]) for i in range(2)]
            t_tbs = [Tok(), Tok()]
            t_tbd = Tok()
            for kind in range(2):
                for pc in range(8):
                    i = (kind * 8 + pc) % 2
                    P.dma("sp", ohp[i][:], oh_d[:, kind, pc * 512:(pc + 1) * 512], writes=[t_ohp[i]])
                    bi = 3 + i
                    P.op("pe", lambda e, i=i, bi=bi: e.matmul(ps[bi][0:8, :], lhsT=relt[:, :], rhs=ohp[i][:, :],
                                                               start=True, stop=True),
                         reads=[t_relt, t_ohp[i]], writes=[tps[bi]])
                    P.op("act", lambda e, i=i, bi=bi: e.copy(out=tbs[i][:], in_=ps[bi][0:8, :]),
                         reads=[tps[bi]], writes=[t_tbs[i]])
                    P.dma("sp", tb_d[kind, :, pc * 512:(pc + 1) * 512], tbs[i][:], reads=[t_tbs[i]], writes=[t_tbd])
            for kind in range(2):
                for h in range(8):
                    src = bass.AP(tb_d.tensor, (kind * 8 + h) * 4096 + (2048 - 128), [[0, 128], [1, LW]])
                    dst = bass.AP(fw_d.tensor, (kind * 8 + h) * 128 * (LW + 1), [[LW + 1, 128], [1, LW]])
                    P.dma("sp", dst, src, reads=[t_tbd], writes=[t_fw])
            for h in range(8):
                src = bass.AP(tb_d.tensor, (8 + h) * 4096, [[0, 127], [1, LC]])
                dst = bass.AP(fc_d.tensor, h * 127 * (LC + 16), [[LC + 16, 127], [1, LC]])
                P.dma("sp", dst, src, reads=[t_tbd], writes=[t_fc])

        with Scope():
            qT = sb("qT", [128, 4, S], BF16); t_qT = [Tok() for _ in range(4)]
            kT = {n: sb(n + "T", [128, S], BF16) for n in ("kc", "vc", "ks", "kw")}
            t_kT = {n: [Tok() for _ in range(4)] for n in kT}
            vs_aug = sb("vs_aug", [128, NT, 2, 65], BF16); vw_aug = sb("vw_aug", [128, NT, 2, 65], BF16)
            t_va = [Tok() for _ in range(NT)]
            gat = sb("gat", [128, NT, 24]); t_gat = [Tok() for _ in range(NT)]
            mixT = sb("mixT", [128, 8, S], BF16)
            t_mixc = [Tok() for _ in range(4)]
            t_mixa = [Tok() for _ in range(NT)]
            t_ones = Tok()
            P.op("pool", lambda e: e.memset(vs_aug[:, :, :, 64:65], 1.0), writes=[t_ones])
            P.op("pool", lambda e: e.memset(vw_aug[:, :, :, 64:65], 1.0), writes=[t_ones])
            ssq = sb("ssq", [128, NTT]); rstd = sb("rstd", [128, NTT]); t_st = [Tok() for _ in range(NTT)]
            ssq2 = sb("ssq2", [128, NTT]); rstd2 = sb("rstd2", [128, NTT]); t_st2 = [Tok() for _ in range(NTT)]
            ssqa = sb("ssqa", [128, NTT]); rstda = sb("rstda", [128, NTT]); t_sta = [Tok() for _ in range(NTT)]

            def rms_stats(src_ap, t_src, junk_, t_junk_, ssq_, rstd_, tk, col, width):
                P.op("act", lambda e: e.activation(out=junk_, in_=src_ap, func=AF.Square, accum_out=ssq_[:, col:col + 1]),
                     reads=[t_src], writes=[t_junk_, tk])
                P.op("dve", lambda e: e.tensor_scalar(out=rstd_[:, col:col + 1], in0=ssq_[:, col:col + 1], scalar1=1.0 / width,
                                                      scalar2=EPS, op0=ALU.mult, op1=ALU.add), reads=[tk], writes=[tk])
                P.op("act", lambda e: e.activation(out=rstd_[:, col:col + 1], in_=rstd_[:, col:col + 1], func=AF.Sqrt),
                     reads=[tk], writes=[tk])
                P.op("dve", lambda e: e.reciprocal(out=rstd_[:, col:col + 1], in_=rstd_[:, col:col + 1]),
                     reads=[tk], writes=[tk])

            def norm_transpose(xt, t_xt, gi, s, ssq_, rstd_, t_st_, gs_, sh_lo, dst, t_dst, col0, junk, t_junk, xs, t_xs):
                rms_stats(xt[:], t_xt, junk[:], t_junk, ssq_, rstd_, t_st_[gi], gi, D)
                P.op("dve", lambda e: e.tensor_scalar(out=xs[:], in0=xt[:], scalar1=rstd_[:, gi:gi + 1], scalar2=None,
                                                      op0=ALU.mult), reads=[t_xt, t_st_[gi]], writes=[t_xs])
                for c in range(8):
                    P.op("pe", lambda e, c=c: e.transpose(out=psb[:, c * 128:(c + 1) * 128], in_=xs[:, c * 128:(c + 1) * 128],
                                                          identity=identb[:]), reads=[t_xs, t_identb], writes=[tpsb])
                for c in range(8):
                    o_ap = dst[:, c, col0:col0 + 128]
                    i_ap = psb[:, c * 128:(c + 1) * 128]
                    sc_ap = gs_[:, c, s:s + 1]
                    bi_ap = modT[:, sh_lo + c, s:s + 1]
                    if gi % 2 == 0:
                        P.op("dve", lambda e, o_ap=o_ap, i_ap=i_ap, sc_ap=sc_ap, bi_ap=bi_ap: e.tensor_scalar(
                            out=o_ap, in0=i_ap, scalar1=sc_ap, scalar2=bi_ap, op0=ALU.mult, op1=ALU.add),
                            reads=[tpsb, t_gs, t_modT], writes=[t_dst])
                    else:
                        P.op("act", lambda e, o_ap=o_ap, i_ap=i_ap, sc_ap=sc_ap, bi_ap=bi_ap: e.activation(
                            out=o_ap, in_=i_ap, func=AF.Identity, scale=sc_ap, bias=bi_ap),
                            reads=[tpsb, t_gs, t_modT], writes=[t_dst])

            def evac(i, out_ap, in_ap, reads, writes):
                if i % 2:
                    P.op("act", lambda e: e.copy(out=out_ap, in_=in_ap), reads=reads, writes=writes)
                else:
                    P.op("dve", lambda e: e.tensor_copy(out=out_ap, in_=in_ap), reads=reads, writes=writes)

            for s in range(nseq):
                with Scope():
                    win = sb("win", [128, 8, 2840], BF16); t_win = Tok()
                    wiv = w_in_d.rearrange("(k p) n -> p k n", p=128)
                    for k in range(8):
                        P.dma("pool", win[:, k, :], wiv[:, k, :], writes=[t_win])
                    xpool = [sb(f"xt{i}", [128, 1024]) for i in range(2)]; t_xp = [Tok(), Tok()]
                    junk = sb("junk", [128, 1024], BF16); t_junk = Tok()
                    xs = sb("xs", [128, 1024], BF16); t_xs = Tok()
                    hTg = [sb(f"hTg{i}", [128, 8, 512], BF16) for i in range(2)]; t_hTg = [Tok(), Tok()]
                    cbs = sb("cbs", [128, 512]); ccs = sb("ccs", [128, 512]); t_cbs = Tok(); t_ccs = Tok()
                    zb = sb("zb", [128, 4, 514]); t_zb = [Tok() for _ in range(4)]
                    yb = sb("yb", [128, 512]); t_yb = Tok()
                    oc = sb("oc", [128, 4, 512]); t_oc = [Tok() for _ in range(4)]
                    osq = sb("osq", [128, 4, 512], BF16); t_osq = [Tok() for _ in range(4)]
                    rbc = sb("rbc", [128, 512]); t_rbc = Tok()
                    for grp in range(4):
                        hT, t_hT = hTg[grp % 2], t_hTg[grp % 2]
                        s0 = grp * 512
                        for tl in range(4):
                            ti = grp * 4 + tl
                            gi = s * NT + ti
                            xt, t_xt = xpool[gi % 2], t_xp[gi % 2]
                            P.dma("sp", xt[:], x_d[gi * 128:(gi + 1) * 128, :], writes=[t_xt])
                            norm_transpose(xt, t_xt, gi, s, ssq, rstd, t_st, gs1, 0, hT, t_hT, tl * 128, junk, t_junk, xs, t_xs)
                        if s == 0 and grp == 0:
                            dbgdump("hT0", [128, 8, 512], BF16, hT[:], [t_hT])

                        def proj_chunk(cc, bi):
                            for k in range(8):
                                P.op("pe", lambda e, k=k: e.matmul(ps[bi][:, :], lhsT=win[:, k, cc * 128:(cc + 1) * 128],
                                                                   rhs=hT[:, k, :], start=(k == 0), stop=(k == 7)),
                                     reads=[t_win, t_hT], writes=[tps[bi]])

                        nb = 0
                        for cc in range(4):
                            bi = nb % 3; nb += 1
                            proj_chunk(cc, bi)
                            evac(cc, qT[:, cc, s0:s0 + 512], ps[bi][:, :], [tps[bi]], [t_qT[grp]])
                        for idx, n in enumerate(("kc", "vc", "ks", "kw")):
                            bi = nb % 3; nb += 1
                            proj_chunk(4 + idx, bi)
                            evac(idx, kT[n][:, s0:s0 + 512], ps[bi][:, :], [tps[bi]], [t_kT[n][grp]])
                        for c4 in range(4):
                            b_cb = nb % 3; nb += 1
                            proj_chunk(8 + c4 * 3 + 0, b_cb)
                            P.op("act", lambda e, b=b_cb: e.copy(out=cbs[:], in_=ps[b][:, :]), reads=[tps[b_cb]], writes=[t_cbs])
                            b_cc = nb % 3; nb += 1
                            proj_chunk(8 + c4 * 3 + 1, b_cc)
                            P.op("act", lambda e, b=b_cc: e.copy(out=ccs[:], in_=ps[b][:, :]), reads=[tps[b_cc]], writes=[t_ccs])
                            b_ch = nb % 3; nb += 1
                            proj_chunk(8 + c4 * 3 + 2, b_ch)
                            if grp == 0:
                                P.op("dve", lambda e, c4=c4: e.memset(zb[:, c4, 0:2], 0.0), writes=[t_zb[c4]])
                            P.op("dve", lambda e, c4=c4, b=b_ch: e.tensor_tensor(out=zb[:, c4, 2:514], in0=ps[b][:, :], in1=ccs[:],
                                                                                 op=ALU.mult),
                                 reads=[tps[b_ch], t_ccs], writes=[t_zb[c4]])
                            P.op("dve", lambda e, c4=c4: e.tensor_scalar(out=yb[:], in0=zb[:, c4, 2:514], scalar1=convw[:, c4, 2:3],
                                                                         scalar2=None, op0=ALU.mult),
                                 reads=[t_zb[c4], t_convw], writes=[t_yb])
                            P.op("dve", lambda e, c4=c4: e.scalar_tensor_tensor(out=yb[:], in0=zb[:, c4, 1:513], scalar=convw[:, c4, 1:2],
                                                                                in1=yb[:], op0=ALU.mult, op1=ALU.add),
                                 reads=[t_zb[c4], t_convw, t_yb], writes=[t_yb])
                            P.op("dve", lambda e, c4=c4: e.scalar_tensor_tensor(out=yb[:], in0=zb[:, c4, 0:512], scalar=convw[:, c4, 0:1],
                                                                                in1=yb[:], op0=ALU.mult, op1=ALU.add),
                                 reads=[t_zb[c4], t_convw, t_yb], writes=[t_yb])
                            P.op("dve", lambda e, c4=c4: e.tensor_tensor(out=oc[:, c4, :], in0=yb[:], in1=cbs[:], op=ALU.mult),
                                 reads=[t_yb, t_cbs], writes=[t_oc[c4]])
                            P.op("dve", lambda e, c4=c4: e.tensor_copy(out=zb[:, c4, 0:2], in_=zb[:, c4, 512:514]),
                                 reads=[t_zb[c4]], writes=[t_zb[c4]])
                            P.op("act", lambda e, c4=c4: e.activation(out=osq[:, c4, :], in_=oc[:, c4, :], func=AF.Square),
                                 reads=[t_oc[c4]], writes=[t_osq[c4]])
                        for c4 in range(4):
                            P.op("pe", lambda e, c4=c4: e.matmul(ps[3][:, :], lhsT=onesb[:, :], rhs=osq[:, c4, :],
                                                                 start=(c4 == 0), stop=(c4 == 3)),
                                 reads=[t_onesb, t_osq[c4]], writes=[tps[3]])
                        P.op("dve", lambda e: e.tensor_scalar(out=rbc[:], in0=ps[3][:, :], scalar1=1.0 / 512, scalar2=EPS,
                                                              op0=ALU.mult, op1=ALU.add), reads=[tps[3]], writes=[t_rbc])
                        P.op("act", lambda e: e.activation(out=rbc[:], in_=rbc[:], func=AF.Sqrt), reads=[t_rbc], writes=[t_rbc])
                        P.op("dve", lambda e: e.reciprocal(out=rbc[:], in_=rbc[:]), reads=[t_rbc], writes=[t_rbc])
                        for c4 in range(4):
                            P.op("dve", lambda e, c4=c4: e.scalar_tensor_tensor(
                                out=mixT[:, 4 + c4, s0:s0 + 512], in0=oc[:, c4, :], scalar=gconv[:, c4:c4 + 1], in1=rbc[:],
                                op0=ALU.mult, op1=ALU.mult), reads=[t_oc[c4], t_gconv, t_rbc], writes=[t_mixc[grp]])
                        for tl in range(4):
                            ti = grp * 4 + tl
                            bi = 4 + (tl % 2)
                            for k in range(8):
                                P.op("pe", lambda e, k=k, tl=tl, bi=bi: e.matmul(
                                    ps[bi][:, 0:280], lhsT=hT[:, k, tl * 128:(tl + 1) * 128], rhs=win[:, k, 2560:2840],
                                    start=(k == 0), stop=(k == 7)), reads=[t_win, t_hT], writes=[tps[bi]])
                            P.op("act", lambda e, ti=ti, bi=bi: e.copy(
                                out=vs_aug[:, ti, :, 0:64], in_=ps[bi][:, 0:128].rearrange("p (g d) -> p g d", g=2)),
                                reads=[tps[bi]], writes=[t_va[ti]])
                            P.op("act", lambda e, ti=ti, bi=bi: e.copy(
                                out=vw_aug[:, ti, :, 0:64], in_=ps[bi][:, 128:256].rearrange("p (g d) -> p g d", g=2)),
                                reads=[tps[bi]], writes=[t_va[ti]])
                            P.op("act", lambda e, ti=ti, bi=bi: e.activation(out=gat[:, ti, :], in_=ps[bi][:, 256:280],
                                                                             func=AF.Sigmoid),
                                 reads=[tps[bi]], writes=[t_gat[ti]])
                    if s == 0:
                        dbgdump("qT", [128, 4, S], BF16, qT[:], t_qT)
                        dbgdump("gat", [128, NT, 24], F32, gat[:], t_gat)
                        dbgdump("vs", [128, NT, 2, 65], BF16, vs_aug[:], t_va + [t_ones])
                        if stop_after <= 1:
                            dbgdump("mixT", [128, 8, S], BF16, mixT[:], t_mixc + t_mixa)
                if stop_after <= 1:
                    continue

                with Scope():
                    bd1 = {}; bd2 = {}; pe2 = {}; t_cw = Tok()
                    for n in "kv":
                        bd1[n] = sb("bd1" + n, [128, 32, 128], BF16)
                        P.dma("pool", bd1[n][:], bd1_d[n], writes=[t_cw])
                        bd2[n] = sb("bd2" + n, [128, 128], BF16)
                        P.dma("pool", bd2[n][:], bd2_d[n], writes=[t_cw])
                        pe2[n] = sb("pe2" + n, [128, 32], BF16)
                        P.dma("pool", pe2[n][:], pe2_d[n], writes=[t_cw])
                    Bw = sb("Bw", [128, 5, 8, 128], BF16); t_Bw = Tok()
                    Bs = sb("Bs", [128, 3, 8, 128], BF16); t_Bs = Tok()
                    for dl in range(5):
                        src = bass.AP(fw_d.tensor, 128 + dl * 128, [[LW, 128], [128 * (LW + 1), 8], [1, 128]])
                        P.dma("pool", Bw[:, dl, :, :], src, reads=[t_fw], writes=[t_Bw])
                    for dl in range(3):
                        src = bass.AP(fw_d.tensor, 8 * 128 * (LW + 1) + 128 + dl * 128,
                                      [[LW, 128], [128 * (LW + 1), 8], [1, 128]])
                        P.dma("pool", Bs[:, dl, :, :], src, reads=[t_fw], writes=[t_Bs])
                    if s == 0:
                        dbgdump("Bw", [128, 5, 8, 128], BF16, Bw[:], [t_Bw])
                        dbgdump("Bs", [128, 3, 8, 128], BF16, Bs[:], [t_Bs])
                    esel, t_esel = load_const("esel", esel_d, [128, 2048], BF16, "pool")
                    selmul, t_selmul = load_const("selmul", selmul_d, [128, 16, 32])
                    seladd, t_seladd = load_const("seladd", seladd_d, [128, 16, 32])
                    gattn, t_gattn = load_const("gattn", gattn_d, [128, 512])
                    cbias = sb("cbias", [128, 2]); t_cbias = Tok()
                    for i, n in enumerate("kv"):
                        for l in range(32):
                            P.op("pe", lambda e, n=n, l=l, i=i: e.matmul(ps[2][:, i:i + 1], lhsT=bd1[n][:, l, :],
                                                                         rhs=pe2[n][:, l:l + 1], start=(l == 0), stop=(l == 31)),
                                 reads=[t_cw], writes=[tps[2]])
                    P.op("dve", lambda e: e.tensor_copy(out=cbias[:], in_=ps[2][:, 0:2]), reads=[tps[2]], writes=[t_cbias])
                    kcmpT = sb("kcmpT", [128, 128], BF16); t_kcmp = Tok()
                    vc_aug = sb("vc_aug", [128, 2, 97], BF16); t_vca = Tok()
                    for g in range(2):
                        P.dma("pool", vc_aug[0:127, g, 65:97], cov_d, writes=[t_vca])
                    P.op("dve", lambda e: e.memset(vc_aug[:, :, 64:65], 1.0), writes=[t_vca])
                    hid = {n: sb("hid" + n, [128, 128], BF16) for n in "kv"}; t_hid = Tok()
                    for i, (n, srcn) in enumerate((("k", "kc"), ("v", "vc"))):
                        src = kT[srcn]
                        for l in range(32):
                            P.op("pe", lambda e, n=n, l=l, i=i, src=src: e.matmul(
                                ps[i][:, 0:127], lhsT=bd1[n][:, l, :], rhs=src[:, l:l + 2017:16],
                                start=(l == 0), stop=(l == 31)), reads=[t_cw] + t_kT[srcn], writes=[tps[i]])
                        P.op("act", lambda e, n=n, i=i: e.activation(out=hid[n][:, 0:127], in_=ps[i][:, 0:127],
                                                                     func=AF.Gelu_apprx_tanh, bias=cbias[:, i:i + 1]),
                             reads=[tps[i], t_cbias], writes=[t_hid])
                    P.op("pe", lambda e: e.matmul(ps[0][:, 0:127], lhsT=bd2["k"][:, :], rhs=hid["k"][:, 0:127], start=True, stop=True),
                         reads=[t_cw, t_hid], writes=[tps[0]])
                    P.op("dve", lambda e: e.tensor_copy(out=kcmpT[:, 0:127], in_=ps[0][:, 0:127]), reads=[tps[0]], writes=[t_kcmp])
                    P.op("pe", lambda e: e.matmul(ps[1][0:127, 0:128], lhsT=hid["v"][:, 0:127], rhs=bd2["v"][:, :], start=True, stop=True),
                         reads=[t_cw, t_hid], writes=[tps[1]])
                    P.op("dve", lambda e: e.tensor_copy(out=vc_aug[0:127, :, 0:64],
                                                        in_=ps[1][0:127, 0:128].rearrange("p (g d) -> p g d", g=2)),
                         reads=[tps[1]], writes=[t_vca])
                    if s == 0:
                        dbgdump("kcmpT", [128, 128], BF16, kcmpT[:], [t_kcmp])
                        dbgdump("vc_aug", [128, 2, 97], BF16, vc_aug[:], [t_vca])

                    bcp = [sb(f"bcp{i}", [128, 8, 128], BF16) for i in range(2)]; t_bcp = [Tok(), Tok()]
                    NSB = 4
                    sbank = [0, 1, 2, 6]
                    tS = [sb(f"tS{i}", [128, 512]) for i in range(NSB)]; t_tS = [Tok() for _ in range(NSB)]
                    pT = [sb(f"pT{i}", [128, 512], BF16) for i in range(NSB)]; t_pT = [Tok() for _ in range(NSB)]
                    o_acc = [sb(f"oacc{i}", [128, 8, 64]) for i in range(2)]; t_oacc = [Tok(), Tok()]
                    rs4 = sb("rs4", [128, 4]); wg4 = sb("wg4", [128, 4]); t_rs = Tok()
                    otmp = sb("otmp", [128, 4, 64]); t_otmp = Tok()
                    itmp = sb("itmp", [128, 4, 32]); imp = sb("imp", [128, 32]); t_imp = Tok()
                    m8 = sb("m8", [128, 8]); t_m8 = Tok()
                    negsel = [sb(f"negsel{i}", [128, 128], BF16) for i in range(2)]; t_negsel = [Tok(), Tok()]
                    for i in range(2):
                        P.op("dve", lambda e, i=i: e.memset(negsel[i][:], 0.0), writes=[t_negsel[i]])
                    nsT4 = [sb(f"nsT4{i}", [128, 4, 128], BF16) for i in range(2)]; t_nsT = [Tok(), Tok()]
                    junk2 = sb("junk2", [128, 512], BF16); t_junk2 = Tok()
                    on_b = sb("on_b", [128, 512], BF16); t_onb = Tok()
                    tpsb_a = tpsb
                    BK = {0: 0, 2: 1, 1: 2}
                    tpo = {br: [Tok()] for br in range(3)}

                    def evac_branch(g, br, qi, oa, t_oa):
                        c0 = 0
                        rd = tpo[br]
                        pob = psO[:, BK[br], :].rearrange("p (j c) -> p j c", j=4)
                        if br == 0:
                            P.op("dve", lambda e: e.tensor_scalar(out=rs4[:], in0=pob[:, :, 64], scalar1=1e-30, scalar2=None,
                                                                  op0=ALU.max), reads=rd, writes=[t_rs])
                            P.op("dve", lambda e: e.reciprocal(out=rs4[:], in_=rs4[:]), reads=[t_rs], writes=[t_rs])
                        else:
                            P.op("dve", lambda e: e.reciprocal(out=rs4[:], in_=pob[:, :, 64]), reads=rd, writes=[t_rs])
                        g0_ = 12 * g + br
                        P.op("dve", lambda e: e.tensor_tensor(out=wg4[:], in0=rs4[:], in1=gat[:, qi, g0_:g0_ + 10:3], op=ALU.mult),
                             reads=[t_rs, t_gat[qi]], writes=[t_rs])
                        if br == 0:
                            P.op("dve", lambda e: e.tensor_tensor(
                                out=oa[:, 4 * g:4 * g + 4, :], in0=pob[:, :, 0:64],
                                in1=wg4[:].unsqueeze(2).broadcast_to([128, 4, 64]), op=ALU.mult),
                                reads=rd + [t_rs], writes=[t_oa])
                            P.op("dve", lambda e: e.tensor_tensor(
                                out=itmp[:], in0=pob[:, :, 65:97], in1=rs4[:].unsqueeze(2).broadcast_to([128, 4, 32]),
                                op=ALU.mult), reads=rd + [t_rs], writes=[t_imp])
                            P.op("dve", lambda e: e.tensor_reduce(out=imp[:], in_=itmp[:].rearrange("p j n -> p n j"),
                                                                  axis=AX.X, op=ALU.add), reads=[t_imp], writes=[t_imp])
                        else:
                            P.op("dve", lambda e: e.tensor_tensor(
                                out=otmp[:], in0=pob[:, :, 0:64],
                                in1=wg4[:].unsqueeze(2).broadcast_to([128, 4, 64]), op=ALU.mult),
                                reads=rd + [t_rs], writes=[t_otmp])
                            P.op("pool", lambda e: e.tensor_tensor(out=oa[:, 4 * g:4 * g + 4, :], in0=oa[:, 4 * g:4 * g + 4, :],
                                                                   in1=otmp[:], op=ALU.add),
                                 reads=[t_otmp, t_oa], writes=[t_oa])

                    def selection(g, qi, it):
                        ns, t_ns = negsel[it % 2], t_negsel[it % 2]
                        nT, t_nT = nsT4[it % 2], t_nsT[it % 2]
                        P.op("dve", lambda e: e.tensor_tensor(out=imp[:], in0=imp[:], in1=selmul[:, qi, :], op=ALU.mult),
                             reads=[t_imp, t_selmul], writes=[t_imp])
                        P.op("dve", lambda e: e.tensor_tensor(out=imp[:], in0=imp[:], in1=seladd[:, qi, :], op=ALU.add),
                             reads=[t_imp, t_seladd], writes=[t_imp])
                        P.op("dve", lambda e: e.max(out=m8[:], in_=imp[:]), reads=[t_imp], writes=[t_m8])
                        P.op("dve", lambda e: e.tensor_scalar(out=ns[:, g * 64:g * 64 + 32], in0=imp[:], scalar1=m8[:, 7:8],
                                                              scalar2=NEG, op0=ALU.is_lt, op1=ALU.mult),
                             reads=[t_imp, t_m8], writes=[t_ns])

                        def later():
                            P.op("pe", lambda e: e.transpose(out=psb[:, 0:128], in_=ns[:, :], identity=identb[:]),
                                 reads=[t_ns, t_identb], writes=[tpsb_a])
                            P.op("act", lambda e: e.copy(out=nT[:], in_=psb[:, 0:128].unsqueeze(1).broadcast_to([128, 4, 128])),
                                 reads=[tpsb_a], writes=[t_nT])
                            sel_ready.add(it)
                        deferred.append([cur_n[0] + 2, later])

                    def finish_qi(qi, oa, t_oa):
                        gi = s * NT + qi
                        if dbg and s == 0:
                            if qi == 0:
                                dbg_out["oattn"] = dout("dbg_oattn", [NT, 128, 512], F32)
                            P.dma("sp", dbg_out["oattn"][qi], oa[:].rearrange("p h d -> p (h d)"), reads=[t_oa])
                        oaf = oa[:].rearrange("p h d -> p (h d)")
                        rms_stats(oaf, t_oa, junk2[:], t_junk2, ssqa, rstda, t_sta[gi], gi, 512)
                        P.op("dve", lambda e: e.scalar_tensor_tensor(
                            out=on_b[:], in0=oaf, scalar=rstda[:, gi:gi + 1], in1=gattn[:], op0=ALU.mult, op1=ALU.mult),
                            reads=[t_oa, t_sta[gi], t_gattn], writes=[t_onb])

                        def later():
                            for c in range(4):
                                P.op("pe", lambda e, c=c: e.transpose(out=psb[:, 512 + c * 128:512 + (c + 1) * 128],
                                                                      in_=on_b[:, c * 128:(c + 1) * 128], identity=identb[:]),
                                     reads=[t_onb, t_identb], writes=[tpsb])
                            for c in range(4):
                                evac(1, mixT[:, c, qi * 128:(qi + 1) * 128], psb[:, 512 + c * 128:512 + (c + 1) * 128],
                                     [tpsb], [t_mixa[qi]])
                        deferred.append([cur_n[0] + 4, later])

                    deferred = []
                    cur_n = [0]
                    sel_ready = set()
                    tiles = []
                    it = 0
                    for qi in range(NT):
                        oa, t_oa = o_acc[qi % 2], t_oacc[qi % 2]
                        for g in range(2):
                            pr = slice(g * 64, (g + 1) * 64)
                            rhs_q = qT[pr, :, qi * 128:(qi + 1) * 128]
                            rd_q = [t_qT[qi // 4]]
                            hs4 = slice(4 * g, 4 * g + 4)
                            bc, t_bc = bcp[qi % 2], t_bcp[qi % 2]
                            pre = None
                            if g == 0:
                                def pre(qi=qi, bc=bc, t_bc=t_bc):
                                    src = bass.AP(fc_d.tensor, 2048 + 128 * qi - 31, [[LC, 127], [127 * (LC + 16), 8], [1, 128]])
                                    P.dma("pool", bc[0:127, :, :], src, reads=[t_fc], writes=[t_bc])
                            tiles.append(dict(
                                pre=pre, rows=127, br=0, first=True, last=True, ncol=97,
                                mm=[(kcmpT[pr, 0:127], rhs_q, rd_q + [t_kcmp])],
                                bias=bc[0:127, hs4, :], t_bias=t_bc, v=vc_aug[0:127, g, :], v_rd=[t_vca],
                                post=(lambda g=g, qi=qi, oa=oa, t_oa=t_oa, it=it: (evac_branch(g, 0, qi, oa, t_oa),
                                                                                   selection(g, qi, it)))))
                            k0 = max(0, qi - 4)
                            for kj in range(k0, qi + 1):
                                tiles.append(dict(
                                    pre=None, rows=128, br=2, first=(kj == k0), last=(kj == qi), ncol=65,
                                    mm=[(kT["kw"][pr, kj * 128:(kj + 1) * 128], rhs_q, rd_q + [t_kT["kw"][kj // 4]])],
                                    bias=Bw[:, qi - kj, hs4, :], t_bias=t_Bw, v=vw_aug[:, kj, g, :], v_rd=[t_va[kj], t_ones],
                                    post=(lambda g=g, qi=qi, oa=oa, t_oa=t_oa: evac_branch(g, 2, qi, oa, t_oa)) if kj == qi else None))
                            nT, t_nT = nsT4[it % 2], t_nsT[it % 2]
                            for kj in range(qi + 1):
                                post = None
                                if kj == qi:
                                    if g == 1:
                                        post = (lambda g=g, qi=qi, oa=oa, t_oa=t_oa: (evac_branch(g, 1, qi, oa, t_oa),
                                                                                      finish_qi(qi, oa, t_oa)))
                                    else:
                                        post = (lambda g=g, qi=qi, oa=oa, t_oa=t_oa: evac_branch(g, 1, qi, oa, t_oa))
                                tiles.append(dict(
                                    need=it,
                                    pre=None, rows=128, br=1, first=(kj == 0), last=(kj == qi), ncol=65,
                                    mm=[(kT["ks"][pr, kj * 128:(kj + 1) * 128], rhs_q, rd_q + [t_kT["ks"][kj // 4]]),
                                        (esel[pr, kj * 128:(kj + 1) * 128], nT[pr, :, :], [t_esel, t_nT])],
                                    bias=Bs[:, min(qi - kj, 2), hs4, :], t_bias=t_Bs, v=vs_aug[:, kj, g, :],
                                    v_rd=[t_va[kj], t_ones], post=post))
                            it += 1

                    def emit_S(tl, sl):
                        if tl["pre"] is not None:
                            tl["pre"]()
                        rows = tl["rows"]
                        nmm = len(tl["mm"])
                        for m, (l_ap, r_ap, rd) in enumerate(tl["mm"]):
                            P.op("pe", lambda e, l_ap=l_ap, r_ap=r_ap, m=m: e.matmul(
                                ps[sbank[sl]][0:rows, :].rearrange("p (j q) -> p j q", j=4), lhsT=l_ap, rhs=r_ap,
                                start=(m == 0), stop=(m == nmm - 1)), reads=rd, writes=[tps[sbank[sl]]])
                        P.op("dve", lambda e: e.scalar_tensor_tensor(
                            out=tS[sl][0:rows, :].rearrange("p (j q) -> p j q", j=4),
                            in0=ps[sbank[sl]][0:rows, :].rearrange("p (j q) -> p j q", j=4), scalar=0.125,
                            in1=tl["bias"], op0=ALU.mult, op1=ALU.add), reads=[tps[sbank[sl]], tl["t_bias"]], writes=[t_tS[sl]])
                        P.op("act", lambda e: e.activation(out=pT[sl][0:rows, :], in_=tS[sl][0:rows, :], func=AF.Exp),
                             reads=[t_tS[sl]], writes=[t_pT[sl]])

                    def emit_PV(tl, sl):
                        rows = tl["rows"]
                        bk = BK[tl["br"]]
                        for j in range(4):
                            P.op("pe", lambda e, j=j: e.matmul(psO[:, bk, j * 128:j * 128 + tl["ncol"]],
                                                               lhsT=pT[sl][0:rows, j * 128:(j + 1) * 128], rhs=tl["v"],
                                                               start=(tl["first"] and j == 0), stop=(tl["last"] and j == 3),
                                                               skip_group_check=True),
                                 reads=[t_pT[sl]] + tl["v_rd"], writes=tpo[tl["br"]])
                        if tl["post"] is not None:
                            tl["post"]()

                    ntl = len(tiles)
                    LA = 2
                    nxt = 0
                    for n_ in range(ntl):
                        cur_n[0] = n_
                        while True:
                            for d_ in [d for d in deferred if d[0] <= n_]:
                                deferred.remove(d_)
                                d_[1]()
                            progressed = False
                            while nxt < ntl and nxt <= n_ + LA and (tiles[nxt].get("need") is None
                                                                   or tiles[nxt]["need"] in sel_ready):
                                emit_S(tiles[nxt], nxt % NSB)
                                nxt += 1
                                progressed = True
                            if nxt > n_:
                                break
                            d_ = min(deferred, key=lambda d: d[0])
                            deferred.remove(d_)
                            d_[1]()
                        emit_PV(tiles[n_], n_ % NSB)
                    for d_ in sorted(deferred, key=lambda d: d[0]):
                        d_[1]()
                    deferred.clear()

                    if s == 0:
                        dbgdump("mixT", [128, 8, S], BF16, mixT[:], t_mixc + t_mixa)
                if stop_after <= 2:
                    continue

                with Scope():
                    wout = sb("wout", [128, 8, 1024], BF16); t_wout = Tok()
                    wov = w_out_d.rearrange("(k p) n -> p k n", p=128)
                    for k in range(8):
                        P.dma("pool", wout[:, k, :], wov[:, k, :], writes=[t_wout])
                    gt1 = sb("gt1", [128, 1024]); t_gt1 = Tok()
                    P.dma("sp", gt1[:], gt_d[0, s], reads=[t_gtd], writes=[t_gt1])
                    xpool = [sb(f"xt3{i}", [128, 1024]) for i in range(2)]; t_xp = [Tok(), Tok()]
                    x1p = [sb(f"x1t{i}", [128, 1024]) for i in range(2)]; t_x1p = [Tok(), Tok()]
                    junk = sb("junk3", [128, 1024], BF16); t_junk = Tok()
                    xs = sb("xs3", [128, 1024], BF16); t_xs = Tok()
                    h2st = [sb(f"h2st{i}", [128, 8, 128], BF16) for i in range(2)]; t_h2st = [Tok(), Tok()]
                    for ti in range(NT):
                        gi = s * NT + ti
                        xt, t_xt = xpool[ti % 2], t_xp[ti % 2]
                        x1t, t_x1t = x1p[ti % 2], t_x1p[ti % 2]
                        P.dma("sp", xt[:], x_d[gi * 128:(gi + 1) * 128, :], writes=[t_xt])
                        for half in range(2):
                            for k in range(8):
                                P.op("pe", lambda e, k=k, half=half: e.matmul(
                                    ps[half][:, :], lhsT=mixT[:, k, ti * 128:(ti + 1) * 128],
                                    rhs=wout[:, k, half * 512:(half + 1) * 512], start=(k == 0), stop=(k == 7)),
                                    reads=[t_wout, t_mixa[ti], t_mixc[ti // 4]], writes=[tps[half]])
                            hs = slice(half * 512, (half + 1) * 512)
                            P.op("dve", lambda e, half=half, hs=hs: e.tensor_tensor(out=x1t[:, hs], in0=ps[half][:, :], in1=gt1[:, hs],
                                                                                    op=ALU.mult),
                                 reads=[tps[half], t_gt1], writes=[t_x1t])
                        P.op("pool", lambda e: e.tensor_tensor(out=x1t[:], in0=x1t[:], in1=xt[:], op=ALU.add),
                             reads=[t_x1t, t_xt], writes=[t_x1t])
                        P.dma("sp", x1_d[gi * 128:(gi + 1) * 128, :], x1t[:], reads=[t_x1t], writes=[t_x1d[gi]])
                        hs_, t_hs = h2st[ti % 2], t_h2st[ti % 2]
                        norm_transpose(x1t, t_x1t, gi, s, ssq2, rstd2, t_st2, gs2, 24, hs_, t_hs, 0, junk, t_junk, xs, t_xs)
                        P.dma("sp", h2T_d[:, :, gi * 128:(gi + 1) * 128], hs_[:], reads=[t_hs], writes=[t_h2d[gi]])

        if stop_after <= 3:
            P.wait_all("sp", Tok.registry)
            return nc, dbg_out

        try:
            utb_d = dscr("utb_s", [16384, 1024], BF16)
            vb_d = dscr("vb_s", [16384, 1024], BF16)
            s2_d = dscr("s2_s", [16, T, 128], BF16)
            t_utb = [Tok() for _ in range(16)]
            t_vb = [Tok() for _ in range(16)]
            with Scope():
                stg = [sb(f"cst{i}", [128, 8, 1024], BF16) for i in range(2)]; t_stg = [Tok(), Tok()]
                n = 0
                for src_d, dst_d, tks in ((ut_d, utb_d, t_utb), (pv_d, vb_d, t_vb)):
                    for c in range(16):
                        i = n % 2; n += 1
                        sv = src_d[c * 1024:(c + 1) * 1024, :].rearrange("(c p) n -> p c n", p=128)
                        dv = dst_d[c * 1024:(c + 1) * 1024, :].rearrange("(c p) n -> p c n", p=128)
                        P.dma("pool", stg[i][:], sv, writes=[t_stg[i]])
                        P.dma("sp", dv, stg[i][:], reads=[t_stg[i]], writes=[tks[c]])
            if stop_after == 3.5:
                raise _Stop()

            with Scope():
                wq, t_wq = None, Tok()
                wq = sb("wq", [128, 8, 1024], BF16)
                wqv = wq_d.rearrange("(k p) n -> p k n", p=128)
                for k in range(8):
                    P.dma("pool", wq[:, k, :], wqv[:, k, :], writes=[t_wq])
                kb, t_kb = load_const("kb", kb_d, [128, 8, 256], BF16, "pool")
                identf, t_identf = load_const("identf", identf_d, [128, 128])
                iota, t_iota = load_const("iota", iota_d, [128, 128])
                selh, t_selh = load_const("selh", selh_d, [16, 128], BF16, "pool")
                gfin, t_gfin = load_const("gfin", gfin_d, [128, 1024])
                gt2 = sb("gt2", [128, 1024]); t_gt2 = Tok()
                Wbuf = sb("Wbuf", [128, 256, 128], BF16); t_W = [Tok() for _ in range(64)]
                h2g = sb("h2g", [128, 8, 256], BF16); t_h2g = Tok()
                qTp = sb("qTp", [128, 8, 256], BF16); t_qTp = Tok()
                s_sb = sb("s_sb", [128, 8, 256]); t_s = Tok()
                work = sb("work", [128, 8, 256]); t_work = Tok()
                v16 = sb("v16", [128, 8, 2, 16]); t_v16 = Tok()
                idx = sb("idx", [128, 8, 16], U32); t_idx = Tok()
                cand = sb("cand", [128, 8, 256]); t_cand = Tok()
                cwork = sb("cwork", [128, 256]); t_cwork = Tok()
                ts16 = sb("ts16", [128, 8, 16]); t_ts = Tok()
                d16 = sb("d16", [128, 8, 16]); t_d16 = Tok()
                zz = sb("zz", [128, 8]); mu = sb("mu", [128, 8]); tauE = sb("tauE", [128, 8]); t_zz = Tok()
                tm3 = sb("tm3", [128, 3, 128]); t_tm3 = Tok()
                s2hm = [sb(f"s2hm{i}", [16, 16 * 128], BF16) for i in range(2)]; t_s2hm = [Tok(), Tok()]
                s2hb = sb("s2hb", [128, 2, 8, 128], BF16); t_s2hb = Tok()
                NRQ = 3
                e4 = [sb(f"e4{i}", [128, 4, 128], BF16) for i in range(NRQ)]; t_e4 = [Tok() for _ in range(NRQ)]
                Rt4 = [sb(f"Rt4{i}", [128, 4, 128], BF16) for i in range(NRQ)]; t_Rt4 = [Tok() for _ in range(NRQ)]
                Lt4 = [sb(f"Lt4{i}", [128, 4, 128], BF16) for i in range(NRQ)]; t_Lt4 = [Tok() for _ in range(NRQ)]
                mk4 = [sb(f"mk4{i}", [128, 4, 128], BF16) for i in range(NRQ)]; t_mk4 = [Tok() for _ in range(NRQ)]
                NC = 5
                utc = [sb(f"utc{i}", [128, 8, 128], BF16) for i in range(NC)]; t_utc = [Tok() for _ in range(NC)]
                vch = [sb(f"vch{i}", [128, 1024], BF16) for i in range(NC)]; t_vch = [Tok() for _ in range(NC)]
                abuf = [sb(f"abuf{i}", [128, 256], BF16) for i in range(2)]; t_ab = [Tok(), Tok()]
                wab = [sb(f"wab{i}", [128, 256], BF16) for i in range(2)]; t_wab = [Tok(), Tok()]
                x1t = sb("x1f", [128, 1024]); t_x1t = Tok()
                yt = sb("yf", [128, 1024]); t_yt = Tok()
                ot = sb("of", [128, 1024]); t_ot = Tok()
                junk = sb("junkf", [128, 1024], BF16); t_junk = Tok()
                ssq3 = sb("ssq3", [128, NTT]); rstd3 = sb("rstd3", [128, NTT]); t_st3 = [Tok() for _ in range(NTT)]
                t_s2d = Tok()
                t_out = Tok()

                def top16(dst_lo, dst_hi, src, wrk, rd, wr_dst, wr_wrk):
                    P.op("dve", lambda e: e.max(out=dst_lo, in_=src), reads=rd, writes=[wr_dst])
                    P.op("dve", lambda e: e.match_replace(out=wrk, in_to_replace=dst_lo, in_values=src, imm_value=-1e30),
                         reads=rd + [wr_dst], writes=[wr_wrk])
                    P.op("dve", lambda e: e.max(out=dst_hi, in_=wrk), reads=[wr_wrk], writes=[wr_dst])

                ngroups = T // 256
                psX = psb[:, :].bitcast(F32)
                PB = [(ps[2], tps[2]), (psX, tpsb)]
                h2gb = [h2g, sb("h2g2", [128, 8, 256], BF16)]; t_h2gb = [t_h2g, Tok()]
                tT3b = [[sb(f"tT3_{i}_{j}", [128, 3, 128]) for j in range(2)] for i in range(2)]
                t_tT3b = [[Tok() for j in range(2)] for i in range(2)]

                def prep_steps(gidx):
                    g0 = gidx * 256
                    hb, t_hb = h2gb[gidx % 2], t_h2gb[gidx % 2]
                    steps = []

                    def s_q(hh):
                        def f():
                            if hh == 0:
                                P.dma("sp", hb[:], h2T_d[:, :, g0:g0 + 256], reads=t_h2d[gidx * 2:gidx * 2 + 2], writes=[t_hb])
                            for h in (hh, hh + 1):
                                bk, tb = PB[h % 2]
                                for k in range(8):
                                    P.op("pe", lambda e, k=k, h=h, bk=bk: e.matmul(bk[:, 0:256], lhsT=wq[:, k, h * 128:(h + 1) * 128],
                                                                                   rhs=hb[:, k, :], start=(k == 0), stop=(k == 7)),
                                         reads=[t_wq, t_hb], writes=[tb])
                                P.op("dve", lambda e, h=h, bk=bk: e.tensor_copy(out=qTp[:, h, :], in_=bk[:, 0:256]),
                                     reads=[tb], writes=[t_qTp])
                        return f
                    for hh in (0, 2, 4, 6):
                        steps.append(("q", s_q(hh)))

                    for tt in range(2):
                        tsl = slice(tt * 128, (tt + 1) * 128)
                        tT3, t_tT3 = tT3b[gidx % 2][tt], t_tT3b[gidx % 2][tt]

                        def sA(tsl=tsl):
                            for hp in range(4):
                                bk, tb = PB[hp % 2]
                                for h in (2 * hp, 2 * hp + 1):
                                    P.op("pe", lambda e, h=h, bk=bk: e.matmul(bk[:, (h % 2) * 256:(h % 2 + 1) * 256], lhsT=qTp[:, h, tsl],
                                                                              rhs=kb[:, h, :], start=True, stop=True),
                                         reads=[t_qTp, t_kb], writes=[tb])
                                P.op("dve", lambda e, hp=hp, bk=bk: e.tensor_copy(
                                    out=s_sb[:, 2 * hp:2 * hp + 2, :], in_=bk[:, :].rearrange("p (h n) -> p h n", h=2)),
                                    reads=[tb], writes=[t_s])

                        def sB():
                            for h in range(8):
                                for p in range(2):
                                    top16(v16[:, h, p, 0:8], v16[:, h, p, 8:16], s_sb[:, h, p * 128:(p + 1) * 128],
                                          work[:, h, p * 128:(p + 1) * 128], [t_s], t_v16, t_work)
                                P.op("dve", lambda e, h=h: e.max_index(out=idx[:, h, 0:8], in_max=v16[:, h, 0, 0:8],
                                                                       in_values=s_sb[:, h, 0:128]),
                                     reads=[t_s, t_v16], writes=[t_idx])
                                P.op("dve", lambda e, h=h: e.max_index(out=idx[:, h, 8:16], in_max=v16[:, h, 0, 8:16],
                                                                       in_values=work[:, h, 0:128]),
                                     reads=[t_work, t_v16], writes=[t_idx])
                            P.op("dve", lambda e: e.tensor_tensor(
                                out=cand[:].rearrange("p h (a b) -> p h a b", a=16),
                                in0=v16[:, :, 0, :].unsqueeze(3).broadcast_to([128, 8, 16, 16]),
                                in1=v16[:, :, 1, :].unsqueeze(2).broadcast_to([128, 8, 16, 16]), op=ALU.add),
                                reads=[t_v16], writes=[t_cand])
                            for h in range(8):
                                top16(ts16[:, h, 0:8], ts16[:, h, 8:16], cand[:, h, :], cwork[:, :], [t_cand], t_ts, t_cwork)
                            P.op("dve", lambda e: e.tensor_tensor(out=d16[:], in0=ts16[:], in1=ts16[:, :, 0:1].broadcast_to([128, 8, 16]),
                                                                  op=ALU.subtract), reads=[t_ts], writes=[t_d16])
                            P.op("act", lambda e: e.activation(out=d16[:], in_=d16[:], func=AF.Exp), reads=[t_d16], writes=[t_d16])
                            P.op("dve", lambda e: e.tensor_reduce(out=zz[:], in_=d16[:], axis=AX.X, op=ALU.add),
                                 reads=[t_d16], writes=[t_zz])
                            P.op("act", lambda e: e.activation(out=zz[:], in_=zz[:], func=AF.Ln), reads=[t_zz], writes=[t_zz])
                            P.op("dve", lambda e: e.tensor_tensor(out=mu[:], in0=zz[:], in1=ts16[:, :, 0], op=ALU.add),
                                 reads=[t_zz, t_ts], writes=[t_zz])
                            P.op("dve", lambda e: e.tensor_scalar(out=tauE[:], in0=ts16[:, :, 15], scalar1=-1e-4, scalar2=None,
                                                                  op0=ALU.add), reads=[t_ts], writes=[t_zz])
                            P.op("dve", lambda e: e.tensor_tensor(
                                out=tm3[:, 0, :].rearrange("p (h a) -> p h a", h=8),
                                in0=tauE[:].unsqueeze(2).broadcast_to([128, 8, 16]), in1=v16[:, :, 0, :], op=ALU.subtract),
                                reads=[t_zz, t_v16], writes=[t_tm3])
                            P.op("dve", lambda e: e.tensor_tensor(
                                out=tm3[:, 1, :].rearrange("p (h a) -> p h a", h=8),
                                in0=v16[:, :, 0, :], in1=mu[:].unsqueeze(2).broadcast_to([128, 8, 16]), op=ALU.subtract),
                                reads=[t_zz, t_v16], writes=[t_tm3])
                            P.op("dve", lambda e: e.tensor_copy(out=tm3[:, 2, :], in_=idx[:].rearrange("p h a -> p (h a)")),
                                 reads=[t_idx], writes=[t_tm3])
                            P.op("dve", lambda e: e.tensor_copy(out=s2hb[:, 0, :, :], in_=s_sb[:, :, 128:256]),
                                 reads=[t_s], writes=[t_s2hb])
                            P.op("dve", lambda e: e.tensor_tensor(out=s2hb[:, 1, :, :], in0=s_sb[:, :, 128:256], in1=s2hb[:, 0, :, :],
                                                                  op=ALU.subtract), reads=[t_s, t_s2hb], writes=[t_s2hb])

                        def sC(tt=tt, tT3=tT3, t_tT3=t_tT3):
                            bk, tb = PB[0]
                            for w3 in range(3):
                                P.op("pe", lambda e, w3=w3: e.transpose(out=bk[:, w3 * 128:(w3 + 1) * 128], in_=tm3[:, w3, :],
                                                                        identity=identf[:]),
                                     reads=[t_tm3, t_identf], writes=[tb])
                            P.op("dve", lambda e: e.tensor_copy(out=tT3[:], in_=bk[:, 0:384].rearrange("p (w t) -> p w t", w=3)),
                                 reads=[tb], writes=[t_tT3])
                            P.dma("sp", s2_d[:, g0 + tt * 128:g0 + (tt + 1) * 128, :].rearrange("(w h) t j -> t w h j", w=2), s2hb[:],
                                  reads=[t_s2hb], writes=[t_s2d])
                        steps.append(("A", sA))
                        steps.append(("B", sB))
                        steps.append(("C", sC))
                    return steps

                SCHED = [1, 3, 5, 7, 12, 14, 58, 62, 64, 112]

                for gidx in range(ngroups):
                    g0 = gidx * 256
                    sq = g0 // S
                    if g0 % S == 0:
                        P.dma("sp", gt2[:], gt_d[1, sq], reads=[t_gtd], writes=[t_gt2])
                    if gidx == 0:
                        for _, st_ in prep_steps(0):
                            st_()
                    h2g, t_h2g = h2gb[gidx % 2], t_h2gb[gidx % 2]
                    nxt_steps = prep_steps(gidx + 1) if gidx + 1 < ngroups else []
                    for tt in range(2):
                        gi = gidx * 2 + tt
                        tT3, t_tT3 = tT3b[gidx % 2][tt], t_tT3b[gidx % 2][tt]
                        quads = []
                        for sl in range(8):
                            for q4 in range(4):
                                quads.append((sl, q4))

                        def load_slab(sl):
                            i2 = sl % 2
                            t0 = g0 + tt * 128 + sl * 16
                            P.dma("sp", s2hm[i2][:, :], s2_d[:, t0:t0 + 16, :].rearrange("h t j -> h (t j)"),
                                  reads=[t_s2d], writes=[t_s2hm[i2]])

                        def emit_sel(qn):
                            sl, q4 = quads[qn]
                            i2 = sl % 2
                            if q4 == 0:
                                load_slab(sl)
                            bA = 0 if qn % 2 == 0 else 4
                            bD = 3 if qn % 2 == 0 else 5
                            for bb in (bA, bD):
                                P.op("pe", lambda e, bb=bb: e.matmul(ps[bb][:, :], lhsT=selh[:, :],
                                                                     rhs=s2hm[i2][:, q4 * 512:(q4 + 1) * 512], start=True, stop=True),
                                     reads=[t_selh, t_s2hm[i2]], writes=[tps[bb]])
                            r = qn % NRQ
                            tl0 = sl * 16 + q4 * 4
                            for t in range(4):
                                P.op("act", lambda e, t=t: e.activation(out=e4[r][:, t, :], in_=ps[bA][:, t * 128:(t + 1) * 128],
                                                                        func=AF.Exp, bias=tT3[:, 1, tl0 + t:tl0 + t + 1]),
                                     reads=[tps[bA], t_tT3], writes=[t_e4[r]])
                            P.op("dve", lambda e: e.tensor_tensor(
                                out=mk4[r][:], in0=ps[bD][:, :].rearrange("p (t j) -> p t j", t=4),
                                in1=tT3[:, 0, tl0:tl0 + 4].unsqueeze(2).broadcast_to([128, 4, 128]), op=ALU.is_ge),
                                reads=[tps[bD], t_tT3], writes=[t_mk4[r]])
                            P.op("dve", lambda e: e.tensor_tensor(
                                out=Lt4[r][:], in0=iota[:].unsqueeze(1).broadcast_to([128, 4, 128]),
                                in1=tT3[:, 2, tl0:tl0 + 4].unsqueeze(2).broadcast_to([128, 4, 128]), op=ALU.is_equal),
                                reads=[t_iota, t_tT3], writes=[t_Lt4[r]])
                            P.op("dve", lambda e: e.tensor_tensor(out=Rt4[r][:], in0=mk4[r][:], in1=e4[r][:], op=ALU.mult),
                                 reads=[t_mk4[r], t_e4[r]], writes=[t_Rt4[r]])

                        def emit_w(qn):
                            sl, q4 = quads[qn]
                            r = qn % NRQ
                            wb = 1 + (qn % 2)
                            tb0 = tt * 128 + sl * 16 + q4 * 4
                            for t in range(4):
                                P.op("pe", lambda e, t=t: e.matmul(ps[wb][:, t * 128:(t + 1) * 128], lhsT=Rt4[r][:, t, :],
                                                                   rhs=Lt4[r][:, t, :], start=True, stop=True),
                                     reads=[t_Rt4[r], t_Lt4[r]], writes=[tps[wb]])
                            evac(qn, Wbuf[:, tb0:tb0 + 4, :], ps[wb][:, :].rearrange("p (t i) -> p t i", t=4),
                                 [tps[wb]], [t_W[tb0 // 4]])

                        emit_sel(0)
                        for qn in range(len(quads)):
                            if qn + 1 < len(quads):
                                emit_sel(qn + 1)
                            emit_w(qn)

                    if dbg and gidx == 0:
                        dbgdump("Wbuf", [128, 256, 128], BF16, Wbuf[:], t_W)
                    if stop_after == 3.8:
                        raise _Stop()
                    def ld(i):
                        c = i % NC
                        P.dma("sp", utc[c][:], utb_d[i * 128:(i + 1) * 128, :].rearrange("p (k j) -> p k j", k=8),
                              reads=[t_utb[i // 8]], writes=[t_utc[c]])
                        P.dma("sp", vch[c][:], vb_d[i * 128:(i + 1) * 128, :], reads=[t_vb[i // 8]], writes=[t_vch[c]])

                    def emU(i):
                        c = i % NC
                        ba = i % 2
                        for k in range(8):
                            P.op("pe", lambda e, k=k: e.matmul(ps[ba][:, 0:256], lhsT=utc[c][:, k, :], rhs=h2g[:, k, :],
                                                               start=(k == 0), stop=(k == 7)),
                                 reads=[t_utc[c], t_h2g], writes=[tps[ba]])
                        P.op("act", lambda e: e.activation(out=abuf[ba][:], in_=ps[ba][:, 0:256], func=AF.Gelu_apprx_tanh),
                             reads=[tps[ba]], writes=[t_ab[ba]])
                        P.op("pool", lambda e: e.tensor_tensor(out=wab[ba][:], in0=abuf[ba][:], in1=Wbuf[:, :, i], op=ALU.mult),
                             reads=[t_ab[ba]] + t_W, writes=[t_wab[ba]])

                    def emV(i):
                        c = i % NC
                        ba = i % 2
                        for tt in range(2):
                            for half in range(2):
                                P.op("pe", lambda e, tt=tt, half=half: e.matmul(
                                    psO[:, tt * 2 + half, :], lhsT=wab[ba][:, tt * 128:(tt + 1) * 128],
                                    rhs=vch[c][:, half * 512:(half + 1) * 512], start=(i == 0), stop=(i == 127)),
                                    reads=[t_wab[ba], t_vch[c]], writes=[tps[3 + tt * 2 + half]])

                    PF = NC - 2
                    for i in range(PF):
                        ld(i)
                    emU(0)
                    for i in range(128):
                        if i + PF < 128:
                            ld(i + PF)
                        if i + 1 < 128:
                            emU(i + 1)
                        emV(i)
                        for k_, it_ in enumerate(SCHED):
                            if it_ == i and k_ < len(nxt_steps):
                                nxt_steps[k_][1]()
                    for tt in range(2):
                        gi = gidx * 2 + tt
                        P.dma("sp", x1t[:], x1_d[gi * 128:(gi + 1) * 128, :], reads=[t_x1d[gi]], writes=[t_x1t])
                        for half in range(2):
                            hs = slice(half * 512, (half + 1) * 512)
                            P.op("dve", lambda e, tt=tt, half=half, hs=hs: e.tensor_tensor(
                                out=yt[:, hs], in0=psO[:, tt * 2 + half, :], in1=gt2[:, hs], op=ALU.mult),
                                reads=[tps[3 + tt * 2 + half], t_gt2], writes=[t_yt])
                        if dbg and gidx == 0 and tt == 0:
                            dbgdump("peer0", [128, 1024], F32, yt[:], [t_yt])
                        P.op("pool", lambda e: e.tensor_tensor(out=yt[:], in0=yt[:], in1=x1t[:], op=ALU.add),
                             reads=[t_yt, t_x1t], writes=[t_yt])
                        rms_stats(yt[:], t_yt, junk[:], t_junk, ssq3, rstd3, t_st3[gi], gi, D)
                        P.op("dve", lambda e, gi=gi: e.scalar_tensor_tensor(out=ot[:], in0=yt[:], scalar=rstd3[:, gi:gi + 1], in1=gfin[:],
                                                                            op0=ALU.mult, op1=ALU.mult),
                             reads=[t_yt, t_st3[gi], t_gfin], writes=[t_ot])
                        P.dma("sp", out_d[gi * 128:(gi + 1) * 128, :], ot[:], reads=[t_ot], writes=[t_out])
                    if stop_after == 4 and gidx == 0:
                        raise _Stop()

        except _Stop:
            pass
        P.wait_all("sp", Tok.registry)
        print("ops per engine", P.ecount, "waits", P.nwaits)
    return nc, dbg_out


_NC_CACHE = {}


def kernel(**inputs):
    inp = {k: np.asarray(v) for k, v in inputs.items()}
    sh = _prep(inp)
    if "nc" not in _NC_CACHE:
        _NC_CACHE["nc"] = build_nc(nseq=2)[0]
    nc = _NC_CACHE["nc"]
    x = np.asarray(inp["x"], np.float32)
    c = np.asarray(inp["c"], np.float32)
    in_maps = []
    for core in range(8):
        m = dict(sh)
        m["x"] = np.ascontiguousarray(x[2 * core:2 * core + 2].reshape(2 * S, D))
        m["cT"] = np.ascontiguousarray(c[2 * core:2 * core + 2].T.reshape(8, 128, 2).transpose(1, 0, 2))
        in_maps.append(m)
    res = run_bass_kernel_spmd(nc, in_maps, core_ids=list(range(8)))
    out = np.concatenate([np.asarray(r["out"]).reshape(2, S, D) for r in res.results], axis=0)
    return out.astype(np.float32)
```

```python
import math
import numpy as np
from contextlib import ExitStack
import concourse.bass as bass
import concourse.mybir as mybir
from concourse.bass_utils import run_bass_kernel_spmd

F32 = mybir.dt.float32
BF16 = mybir.dt.bfloat16
U32 = mybir.dt.uint32
AF = mybir.ActivationFunctionType
ALU = mybir.AluOpType
AX = mybir.AxisListType

S = 2048
D = 1024
NT = 16
EPS = 1e-6
NEG = -30000.0
LENG = "dve"
import os
PDBG = os.environ.get("PDBG", "")
SEM_CHUNK = 30000


class _Stop(Exception):
    pass


class Tok:
    __slots__ = ("writers", "readers")
    registry = []

    def __init__(self):
        self.writers = []
        self.readers = []
        Tok.registry.append(self)


class Op:
    __slots__ = ("eng", "sem", "val", "is_dma")


class Prog:
    ENGS = ("pe", "dve", "act", "pool", "sp")

    def __init__(self, nc, stack, n_dma_sems=8):
        self.nc = nc
        self.stack = stack
        self.eobj = {"pe": nc.tensor, "dve": nc.vector, "act": nc.scalar,
                     "pool": nc.gpsimd, "sp": nc.sync}
        self.esems = {e: [] for e in self.ENGS}
        self.ecount = {e: 0 for e in self.ENGS}
        self.waited = {e: {} for e in self.ENGS}
        self.dma_sems = {}
        self.dma_rr = {}
        self.n_dma_sems = n_dma_sems
        self.nwaits = 0

    def _esem(self, e, chunk):
        lst = self.esems[e]
        while len(lst) <= chunk:
            lst.append(self.stack.enter_context(self.nc.semaphore(f"s_{e}_{len(lst)}")))
        return lst[chunk]

    def _wait(self, e, sem, val):
        w = self.waited[e]
        k = id(sem)
        if w.get(k, 0) >= val:
            return
        w[k] = val
        self.eobj[e].wait_ge(sem, val)
        self.nwaits += 1

    def _deps(self, e, is_dma, reads, writes):
        for t in reads:
            for p in t.writers:
                if (not p.is_dma) and (not is_dma) and p.eng == e and e == "pe":
                    continue
                self._wait(e, p.sem, p.val)
        for t in writes:
            for p in t.writers + t.readers:
                if (not p.is_dma) and (not is_dma) and p.eng == e and e == "pe":
                    continue
                self._wait(e, p.sem, p.val)

    def _record(self, op, reads, writes):
        for t in reads:
            if not op.is_dma:
                t.readers = [r for r in t.readers if r.is_dma or r.eng != op.eng]
            t.readers.append(op)
        for t in writes:
            if t.readers:
                t.writers = [op]
                t.readers = []
            else:
                if not op.is_dma:
                    t.writers = [w for w in t.writers if w.is_dma or w.eng != op.eng]
                t.writers.append(op)

    def op(self, e, fn, reads=(), writes=()):
        self._deps(e, False, reads, writes)
        n = self.ecount[e]
        sem = self._esem(e, n // SEM_CHUNK)
        val = (n % SEM_CHUNK) + 1
        fn(self.eobj[e]).then_inc(sem, 1)
        self.ecount[e] = n + 1
        o = Op()
        o.eng, o.sem, o.val, o.is_dma = e, sem, val, False
        self._record(o, reads, writes)
        return o

    def dma(self, q, out, in_, reads=(), writes=(), **kw):
        if q not in self.dma_sems:
            self.dma_sems[q] = [[self.stack.enter_context(self.nc.semaphore(f"d_{q}_{i}")), 0]
                                for i in range(self.n_dma_sems)]
            self.dma_rr[q] = 0
        self._deps(q, True, reads, writes)
        i = self.dma_rr[q]
        self.dma_rr[q] = (i + 1) % self.n_dma_sems
        ent = self.dma_sems[q][i]
        sem, cur = ent
        if cur > 0:
            self._wait(q, sem, cur)
        val = cur + 16
        ent[1] = val
        self.eobj[q].dma_start(out=out, in_=in_, **kw).then_inc(sem, 16)
        o = Op()
        o.eng, o.sem, o.val, o.is_dma = q, sem, val, True
        self._record(o, reads, writes)
        return o

    def barrier(self):
        for e in self.ENGS:
            for f in self.ENGS:
                n = self.ecount[f]
                if f == e or n == 0:
                    continue
                self._wait(e, self.esems[f][(n - 1) // SEM_CHUNK], (n - 1) % SEM_CHUNK + 1)
            for q, lst in self.dma_sems.items():
                for sem, cur in lst:
                    if cur > 0:
                        self._wait(e, sem, cur)

    def wait_all(self, e, toks):
        for t in toks:
            for p in t.writers + t.readers:
                self._wait(e, p.sem, p.val)


def _rel_bucket_np(n):
    n = np.maximum(n, 0)
    exact = 16
    lr = np.log(np.maximum(n, 1).astype(np.float32) / np.float32(exact)) / np.float32(math.log(128 / exact))
    large = exact + (lr * np.float32(32 - exact)).astype(np.int32)
    return np.where(n < exact, n, np.minimum(large, 31))


def _constants():
    c = {}
    npr = np.arange(4096)
    n = npr - 2048
    bk = _rel_bucket_np(n)
    oh = np.zeros((33, 2, 4096), np.float32)
    okw = (n >= 0) & (n < 512)
    oks = (n >= 0)
    oh[bk[okw], 0, npr[okw]] = 1.0
    oh[32, 0, npr[~okw]] = 1.0
    oh[bk[oks], 1, npr[oks]] = 1.0
    oh[32, 1, npr[~oks]] = 1.0
    c["oh"] = oh
    e32 = (np.arange(2048)[None, :] // 64 == np.arange(32)[:, None]).astype(np.float32)
    e128 = np.zeros((128, 2048), np.float32)
    e128[0:32] = e32
    e128[64:96] = e32
    c["Esel"] = e128
    cs = np.arange(127) * 16
    ss = np.arange(32) * 64
    c["Cov"] = ((cs[:, None] < ss[None, :] + 64) & (cs[:, None] + 32 > ss[None, :])).astype(np.float32)
    t = np.arange(2048)
    cur = t // 64
    j = np.arange(32)
    forced = (j[None, :] == 0) | (j[None, :] == cur[:, None]) | (j[None, :] == cur[:, None] - 1)
    allowed = j[None, :] * 64 <= t[:, None]
    mul = (allowed & ~forced).astype(np.float32)
    add = np.where(forced, 1e4, np.where(allowed, 0.0, -1e30)).astype(np.float32)
    c["selmul"] = np.ascontiguousarray(mul.reshape(16, 128, 32).transpose(1, 0, 2))
    c["seladd"] = np.ascontiguousarray(add.reshape(16, 128, 32).transpose(1, 0, 2))
    c["identf"] = np.eye(128, dtype=np.float32)
    c["ones"] = np.ones((128, 128), np.float32)
    c["iota"] = np.tile(np.arange(128, dtype=np.float32)[None, :], (128, 1))
    sel = np.zeros((8, 128), np.float32)
    for h in range(8):
        sel[h, h * 16:(h + 1) * 16] = 1.0
    c["selh"] = np.concatenate([sel, sel], axis=0)
    return c


def _blockdiag2(w):
    out = np.zeros(w.shape[:-2] + (128, 128), np.float32)
    out[..., :64, :64] = w
    out[..., 64:, 64:] = w
    return out


def _prep(inp):
    sh = {}
    f = lambda a: np.ascontiguousarray(np.asarray(a, np.float32))
    sh["w_mod"] = f(inp["w_mod"][0])
    sh["bmodT"] = f(inp["b_mod"][0].reshape(48, 128).T)
    bm = inp["b_mod"][0]
    sh["bmod_bc"] = f(np.broadcast_to(np.stack([bm[2048:3072], bm[5120:6144]])[None], (128, 2, 1024)))
    sh["gmix"] = f(inp["ln_mix_g"][0].reshape(8, 128).T)
    sh["gffn"] = f(inp["ln_ffn_g"][0].reshape(8, 128).T)
    sh["gfin_bc"] = f(np.broadcast_to(inp["ln_final_g"][None, :], (128, 1024)))
    w_in = np.asarray(inp["w_in"][0], np.float32)
    sp = np.cumsum([0, 512, 128, 128, 128, 128, 128, 128, 24, 512, 512, 512])
    q, kc, vc, ks, vs, kw, vw, gt, cb, cc, ch = [w_in[:, sp[i]:sp[i + 1]] for i in range(11)]
    qh = q.reshape(1024, 8, 64)
    qperm = np.concatenate([np.concatenate([qh[:, j], qh[:, 4 + j]], axis=1) for j in range(4)], axis=1)
    cols = [qperm, kc, vc, ks, kw]
    for c4 in range(4):
        cols += [cb[:, c4 * 128:(c4 + 1) * 128], cc[:, c4 * 128:(c4 + 1) * 128], ch[:, c4 * 128:(c4 + 1) * 128]]
    cols += [vs, vw, gt]
    sh["w_in"] = f(np.concatenate(cols, axis=1))
    for nm, w1, w2, pe in (("k", "cmp_wk1", "cmp_wk2", "cmp_pe_k"), ("v", "cmp_wv1", "cmp_wv2", "cmp_pe_v")):
        a = np.asarray(inp[w1][0], np.float32).reshape(32, 64, 64)
        sh["bd1" + nm] = f(_blockdiag2(a).transpose(1, 0, 2))
        sh["bd2" + nm] = f(_blockdiag2(np.asarray(inp[w2][0], np.float32)))
        p = np.asarray(inp[pe][0], np.float32).T
        sh["pe2" + nm] = f(np.concatenate([p, p], axis=0))
    sh["convw"] = f(inp["conv_w"][0][:, 0, :].reshape(3, 4, 128).transpose(2, 1, 0))
    sh["gattn_bc"] = f(np.broadcast_to(inp["norm_attn_g"][0][None, :], (128, 512)))
    sh["gconv"] = f(inp["norm_conv_g"][0].reshape(4, 128).T)
    sh["w_out"] = f(inp["w_out"][0])
    sh["peer_wq"] = f(inp["peer_wq"][0])
    pk = np.asarray(inp["peer_keys"][0], np.float32)
    kb = np.zeros((128, 8, 256), np.float32)
    for h in range(8):
        for p in range(2):
            kb[p * 64:(p + 1) * 64, h, p * 128:(p + 1) * 128] = pk[h, p].T
    sh["peer_kb"] = kb
    u = np.asarray(inp["peer_u"][0], np.float32)
    sh["peer_ut"] = f(u.reshape(128, 128, 8, 128).transpose(0, 3, 2, 1).reshape(128 * 128, 1024))
    sh["peer_v"] = f(inp["peer_v"][0])
    rt = np.zeros((33, 8), np.float32)
    rt[:32] = inp["rel_table"]
    rt[32] = NEG
    sh["relt"] = rt
    sh.update(_constants())
    return sh


def build_nc(nseq=2, dbg=False, stop_after=99):
    nc = bass.Bass("TRN2", target_bir_lowering=False)
    Tok.registry = []
    T = nseq * S
    NTT = nseq * NT

    def din(name, shape, dt=F32):
        return nc.dram_tensor(name, list(shape), dt, kind="ExternalInput").ap()

    def dscr(name, shape, dt=F32):
        return nc.dram_tensor(name, list(shape), dt, kind="Internal").ap()

    def dout(name, shape, dt=F32):
        return nc.dram_tensor(name, list(shape), dt, kind="ExternalOutput").ap()

    x_d = din("x", [T, D])
    cT_d = din("cT", [128, 8, nseq])
    w_mod_d = din("w_mod", [1024, 6144])
    bmodT_d = din("bmodT", [128, 48])
    bmod_bc_d = din("bmod_bc", [128, 2, 1024])
    gmix_d = din("gmix", [128, 8])
    gffn_d = din("gffn", [128, 8])
    gfin_d = din("gfin_bc", [128, 1024])
    w_in_d = din("w_in", [1024, 2840])
    bd1_d = {n: din("bd1" + n, [128, 32, 128]) for n in "kv"}
    bd2_d = {n: din("bd2" + n, [128, 128]) for n in "kv"}
    pe2_d = {n: din("pe2" + n, [128, 32]) for n in "kv"}
    convw_d = din("convw", [128, 4, 3])
    gattn_d = din("gattn_bc", [128, 512])
    gconv_d = din("gconv", [128, 4])
    w_out_d = din("w_out", [1024, 1024])
    wq_d = din("peer_wq", [1024, 1024])
    kb_d = din("peer_kb", [128, 8, 256])
    ut_d = din("peer_ut", [128 * 128, 1024])
    pv_d = din("peer_v", [16384, 1024])
    relt_d = din("relt", [33, 8])
    oh_d = din("oh", [33, 2, 4096])
    esel_d = din("Esel", [128, 2048])
    cov_d = din("Cov", [127, 32])
    selmul_d = din("selmul", [128, 16, 32])
    seladd_d = din("seladd", [128, 16, 32])
    identf_d = din("identf", [128, 128])
    ones_d = din("ones", [128, 128])
    iota_d = din("iota", [128, 128])
    selh_d = din("selh", [16, 128])
    out_d = dout("out", [T, D])

    tb_d = dscr("tb_s", [2, 8, 4096])
    LW = 1024
    fw_d = dscr("fw_s", [2, 8, 128 * (LW + 1)])
    LC = 4096
    fc_d = dscr("fc_s", [8, 127 * (LC + 16)])
    gt_d = dscr("gt_s", [2, nseq, 128, 1024])
    if dbg:
        x1_d = dout("x1_s", [T, D])
        h2T_d = dout("h2T_s", [128, 8, T], BF16)
    else:
        x1_d = dscr("x1_s", [T, D])
        h2T_d = dscr("h2T_s", [128, 8, T], BF16)
    t_x1d = [Tok() for _ in range(NTT)]
    t_h2d = [Tok() for _ in range(NTT)]

    dbg_out = {}
    uniq = [0]

    with ExitStack() as st0:
        P = Prog(nc, st0)
        scopes = [st0]

        def sb(name, shape, dt=F32):
            uniq[0] += 1
            return scopes[-1].enter_context(nc.sbuf_tensor(f"sb{uniq[0]}_{name}", list(shape), dt))

        class Scope:
            def __enter__(self):
                self.st = ExitStack()
                scopes.append(self.st)
                return self

            def __exit__(self, *a):
                if a[0] is None:
                    P.barrier()
                scopes.pop()
                self.st.close()
                return False

        psg = [st0.enter_context(nc.psum_tensor(f"psum{i}", [128, 512], F32)) for i in range(3)]
        psO = st0.enter_context(nc.psum_tensor("psumO", [128, 4, 512], F32))
        ps = [psg[0][:, :], psg[1][:, :], psg[2][:, :]] + [psO[:, i, :] for i in range(4)]
        tps = [Tok() for _ in range(7)]
        psb = st0.enter_context(nc.psum_tensor("psumb", [128, 1024], BF16))
        tpsb = Tok()

        def load_const(name, src, shape, dt=F32, q="sp"):
            t = sb(name, shape, dt)
            k = Tok()
            P.dma(q, t[:], src, writes=[k])
            return t, k

        def dbgdump(name, shape, dt, src_ap, toks):
            if not dbg:
                return
            dbg_out[name] = dout("dbg_" + name, shape, dt)
            P.dma("sp", dbg_out[name], src_ap, reads=toks)

        identb, t_identb = load_const("identb", identf_d, [128, 128], BF16, "pool")
        onesb, t_onesb = load_const("onesb", ones_d, [128, 128], BF16, "pool")
        gmix, t_gmix = load_const("gmix", gmix_d, [128, 8])
        gffn, t_gffn = load_const("gffn", gffn_d, [128, 8])
        bmodT, t_bmodT = load_const("bmodT", bmodT_d, [128, 48])
        convw, t_convw = load_const("convw", convw_d, [128, 4, 3])
        gconv, t_gconv = load_const("gconv", gconv_d, [128, 4])
        relt, t_relt = load_const("relt", relt_d, [33, 8])
        modT = sb("modT", [128, 48, nseq]); t_modT = Tok()
        gs1 = sb("gs1", [128, 8, nseq]); gs2 = sb("gs2", [128, 8, nseq]); t_gs = Tok()
        t_gtd = Tok()
        t_fw = Tok(); t_fc = Tok()

        with Scope():
            cT = sb("cT", [128, 8, nseq]); t_cT = Tok()
            P.dma("sp", cT[:], cT_d, writes=[t_cT])
            c_act = sb("c_act", [128, 8, nseq]); t_cact = Tok()
            P.op("act", lambda e: e.activation(out=c_act[:], in_=cT[:], func=AF.Silu), reads=[t_cT], writes=[t_cact])
            c_rep = sb("c_rep", [128, 8, nseq, 128]); t_crep = Tok()
            P.op("dve", lambda e: e.tensor_copy(out=c_rep[:], in_=c_act[:].unsqueeze(3).broadcast_to([128, 8, nseq, 128])),
                 reads=[t_cact], writes=[t_crep])
            bmod_bc = sb("bmod_bc", [128, 2, 1024]); t_bmbc = Tok()
            P.dma("sp", bmod_bc[:], bmod_bc_d, writes=[t_bmbc])
            gstage = [sb(f"gstage{i}", [128, nseq, 128]) for i in range(2)]; t_gst = [Tok(), Tok()]
            wm_view = w_mod_d.rearrange("(k p) n -> p k n", p=128)
            wmp = [sb(f"wm{i}", [128, 8, 128]) for i in range(3)]
            t_wmp = [Tok() for _ in range(3)]
            for j in range(48):
                wt, tw = wmp[j % 3], t_wmp[j % 3]
                P.dma("sp", wt[:], wm_view[:, :, j * 128:(j + 1) * 128], writes=[tw])
                for k in range(8):
                    P.op("pe", lambda e, k=k, wt=wt: e.matmul(ps[0][:, j * nseq:(j + 1) * nseq], lhsT=wt[:, k, :],
                                                              rhs=c_act[:, k, :], start=(k == 0), stop=(k == 7)),
                         reads=[tw, t_cact], writes=[tps[0]])
                which = {2: 0, 5: 1}.get(j // 8)
                if which is not None:
                    jj = j % 8
                    bi = 1 + (j % 2)
                    for b in range(nseq):
                        for k in range(8):
                            P.op("pe", lambda e, k=k, b=b, wt=wt, bi=bi: e.matmul(
                                ps[bi][:, b * 128:(b + 1) * 128], lhsT=c_rep[:, k, b, :], rhs=wt[:, k, :],
                                start=(k == 0), stop=(k == 7)), reads=[tw, t_crep], writes=[tps[bi]])
                    gsb, tg = gstage[j % 2], t_gst[j % 2]
                    P.op("dve", lambda e, bi=bi, which=which, jj=jj, gsb=gsb: e.tensor_tensor(
                        out=gsb[:],
                        in0=ps[bi][:, 0:nseq * 128].rearrange("p (b n) -> p b n", b=nseq),
                        in1=bmod_bc[:, which, jj * 128:(jj + 1) * 128].unsqueeze(1).broadcast_to([128, nseq, 128]),
                        op=ALU.add), reads=[tps[bi], t_bmbc], writes=[tg])
                    P.dma("sp", gt_d[which].rearrange("b p n -> p b n")[:, :, jj * 128:(jj + 1) * 128], gsb[:],
                          reads=[tg], writes=[t_gtd])
            P.op("dve", lambda e: e.tensor_tensor(
                out=modT[:], in0=ps[0][:, 0:48 * nseq].rearrange("p (j b) -> p j b", b=nseq),
                in1=bmodT[:].unsqueeze(2).broadcast_to([128, 48, nseq]), op=ALU.add),
                reads=[tps[0], t_bmodT], writes=[t_modT])
            P.op("dve", lambda e: e.scalar_tensor_tensor(
                out=gs1[:], in0=modT[:, 8:16, :], scalar=1.0, in1=gmix[:].unsqueeze(2).broadcast_to([128, 8, nseq]),
                op0=ALU.add, op1=ALU.mult), reads=[t_modT, t_gmix], writes=[t_gs])
            P.op("dve", lambda e: e.scalar_tensor_tensor(
                out=gs2[:], in0=modT[:, 32:40, :], scalar=1.0, in1=gffn[:].unsqueeze(2).broadcast_to([128, 8, nseq]),
                op0=ALU.add, op1=ALU.mult), reads=[t_modT, t_gffn], writes=[t_gs])
            dbgdump("modT", [128, 48, nseq], F32, modT[:], [t_modT])

            ohp = [sb(f"ohp{i}", [33, 512]) for i in range(2)]
            t_ohp = [Tok(), Tok()]
            tbs = [sb(f"tbs{i}", [8, 512]) for i in range(2)]
            t_tbs = [Tok(), Tok()]
            t_tbd = Tok()
            for kind in range(2):
                for pc in range(8):
                    i = (kind * 8 + pc) % 2
                    P.dma("sp", ohp[i][:], oh_d[:, kind, pc * 512:(pc + 1) * 512], writes=[t_ohp[i]])
                    bi = 3 + i
                    P.op("pe", lambda e, i=i, bi=bi: e.matmul(ps[bi][0:8, :], lhsT=relt[:, :], rhs=ohp[i][:, :],
                                                               start=True, stop=True),
                         reads=[t_relt, t_ohp[i]], writes=[tps[bi]])
                    P.op("act", lambda e, i=i, bi=bi: e.copy(out=tbs[i][:], in_=ps[bi][0:8, :]),
                         reads=[tps[bi]], writes=[t_tbs[i]])
                    P.dma("sp", tb_d[kind, :, pc * 512:(pc + 1) * 512], tbs[i][:], reads=[t_tbs[i]], writes=[t_tbd])
            for kind in range(2):
                for h in range(8):
                    src = bass.AP(tb_d.tensor, (kind * 8 + h) * 4096 + (2048 - 128), [[0, 128], [1, LW]])
                    dst = bass.AP(fw_d.tensor, (kind * 8 + h) * 128 * (LW + 1), [[LW + 1, 128], [1, LW]])
                    P.dma("sp", dst, src, reads=[t_tbd], writes=[t_fw])
            for h in range(8):
                src = bass.AP(tb_d.tensor, (8 + h) * 4096, [[0, 127], [1, LC]])
                dst = bass.AP(fc_d.tensor, h * 127 * (LC + 16), [[LC + 16, 127], [1, LC]])
                P.dma("sp", dst, src, reads=[t_tbd], writes=[t_fc])

        with Scope():
            qT = sb("qT", [128, 4, S], BF16); t_qT = [Tok() for _ in range(4)]
            kT = {n: sb(n + "T", [128, S], BF16) for n in ("kc", "vc", "ks", "kw")}
            t_kT = {n: [Tok() for _ in range(4)] for n in kT}
            vs_aug = sb("vs_aug", [128, NT, 2, 65], BF16); vw_aug = sb("vw_aug", [128, NT, 2, 65], BF16)
            t_va = [Tok() for _ in range(NT)]
            gat = sb("gat", [128, NT, 24]); t_gat = [Tok() for _ in range(NT)]
            mixT = sb("mixT", [128, 8, S], BF16)
            t_mixc = [Tok() for _ in range(4)]
            t_mixa = [Tok() for _ in range(NT)]
            t_ones = Tok()
            P.op("pool", lambda e: e.memset(vs_aug[:, :, :, 64:65], 1.0), writes=[t_ones])
            P.op("pool", lambda e: e.memset(vw_aug[:, :, :, 64:65], 1.0), writes=[t_ones])
            ssq = sb("ssq", [128, NTT]); rstd = sb("rstd", [128, NTT]); t_st = [Tok() for _ in range(NTT)]
            ssq2 = sb("ssq2", [128, NTT]); rstd2 = sb("rstd2", [128, NTT]); t_st2 = [Tok() for _ in range(NTT)]
            ssqa = sb("ssqa", [128, NTT]); rstda = sb("rstda", [128, NTT]); t_sta = [Tok() for _ in range(NTT)]

            def rms_stats(src_ap, t_src, junk_, t_junk_, ssq_, rstd_, tk, col, width):
                P.op("act", lambda e: e.activation(out=junk_, in_=src_ap, func=AF.Square, accum_out=ssq_[:, col:col + 1]),
                     reads=[t_src], writes=[t_junk_, tk])
                P.op("dve", lambda e: e.tensor_scalar(out=rstd_[:, col:col + 1], in0=ssq_[:, col:col + 1], scalar1=1.0 / width,
                                                      scalar2=EPS, op0=ALU.mult, op1=ALU.add), reads=[tk], writes=[tk])
                P.op("act", lambda e: e.activation(out=rstd_[:, col:col + 1], in_=rstd_[:, col:col + 1], func=AF.Sqrt),
                     reads=[tk], writes=[tk])
                P.op("dve", lambda e: e.reciprocal(out=rstd_[:, col:col + 1], in_=rstd_[:, col:col + 1]),
                     reads=[tk], writes=[tk])

            def norm_transpose(xt, t_xt, gi, s, ssq_, rstd_, t_st_, gs_, sh_lo, dst, t_dst, col0, junk, t_junk, xs, t_xs):
                rms_stats(xt[:], t_xt, junk[:], t_junk, ssq_, rstd_, t_st_[gi], gi, D)
                P.op("dve", lambda e: e.tensor_scalar(out=xs[:], in0=xt[:], scalar1=rstd_[:, gi:gi + 1], scalar2=None,
                                                      op0=ALU.mult), reads=[t_xt, t_st_[gi]], writes=[t_xs])
                for c in range(8):
                    P.op("pe", lambda e, c=c: e.transpose(out=psb[:, c * 128:(c + 1) * 128], in_=xs[:, c * 128:(c + 1) * 128],
                                                          identity=identb[:]), reads=[t_xs, t_identb], writes=[tpsb])
                for c in range(8):
                    o_ap = dst[:, c, col0:col0 + 128]
                    i_ap = psb[:, c * 128:(c + 1) * 128]
                    sc_ap = gs_[:, c, s:s + 1]
                    bi_ap = modT[:, sh_lo + c, s:s + 1]
                    if gi % 2 == 0:
                        P.op("dve", lambda e, o_ap=o_ap, i_ap=i_ap, sc_ap=sc_ap, bi_ap=bi_ap: e.tensor_scalar(
                            out=o_ap, in0=i_ap, scalar1=sc_ap, scalar2=bi_ap, op0=ALU.mult, op1=ALU.add),
                            reads=[tpsb, t_gs, t_modT], writes=[t_dst])
                    else:
                        P.op("act", lambda e, o_ap=o_ap, i_ap=i_ap, sc_ap=sc_ap, bi_ap=bi_ap: e.activation(
                            out=o_ap, in_=i_ap, func=AF.Identity, scale=sc_ap, bias=bi_ap),
                            reads=[tpsb, t_gs, t_modT], writes=[t_dst])

            def evac(i, out_ap, in_ap, reads, writes):
                if i % 2:
                    P.op("act", lambda e: e.copy(out=out_ap, in_=in_ap), reads=reads, writes=writes)
                else:
                    P.op("dve", lambda e: e.tensor_copy(out=out_ap, in_=in_ap), reads=reads, writes=writes)

            for s in range(nseq):
                with Scope():
                    win = sb("win", [128, 8, 2840], BF16); t_win = Tok()
                    wiv = w_in_d.rearrange("(k p) n -> p k n", p=128)
                    for k in range(8):
                        P.dma("pool", win[:, k, :], wiv[:, k, :], writes=[t_win])
                    xpool = [sb(f"xt{i}", [128, 1024]) for i in range(2)]; t_xp = [Tok(), Tok()]
                    junk = sb("junk", [128, 1024], BF16); t_junk = Tok()
                    xs = sb("xs", [128, 1024], BF16); t_xs = Tok()
                    hTg = [sb(f"hTg{i}", [128, 8, 512], BF16) for i in range(2)]; t_hTg = [Tok(), Tok()]
                    cbs = sb("cbs", [128, 512]); ccs = sb("ccs", [128, 512]); t_cbs = Tok(); t_ccs = Tok()
                    zb = sb("zb", [128, 4, 514]); t_zb = [Tok() for _ in range(4)]
                    yb = sb("yb", [128, 512]); t_yb = Tok()
                    oc = sb("oc", [128, 4, 512]); t_oc = [Tok() for _ in range(4)]
                    osq = sb("osq", [128, 4, 512], BF16); t_osq = [Tok() for _ in range(4)]
                    rbc = sb("rbc", [128, 512]); t_rbc = Tok()
                    for grp in range(4):
                        hT, t_hT = hTg[grp % 2], t_hTg[grp % 2]
                        s0 = grp * 512
                        for tl in range(4):
                            ti = grp * 4 + tl
                            gi = s * NT + ti
                            xt, t_xt = xpool[gi % 2], t_xp[gi % 2]
                            P.dma("sp", xt[:], x_d[gi * 128:(gi + 1) * 128, :], writes=[t_xt])
                            norm_transpose(xt, t_xt, gi, s, ssq, rstd, t_st, gs1, 0, hT, t_hT, tl * 128, junk, t_junk, xs, t_xs)
                        if s == 0 and grp == 0:
                            dbgdump("hT0", [128, 8, 512], BF16, hT[:], [t_hT])

                        def proj_chunk(cc, bi):
                            for k in range(8):
                                P.op("pe", lambda e, k=k: e.matmul(ps[bi][:, :], lhsT=win[:, k, cc * 128:(cc + 1) * 128],
                                                                   rhs=hT[:, k, :], start=(k == 0), stop=(k == 7)),
                                     reads=[t_win, t_hT], writes=[tps[bi]])

                        nb = 0
                        for cc in range(4):
                            bi = nb % 3; nb += 1
                            proj_chunk(cc, bi)
                            evac(cc, qT[:, cc, s0:s0 + 512], ps[bi][:, :], [tps[bi]], [t_qT[grp]])
                        for idx, n in enumerate(("kc", "vc", "ks", "kw")):
                            bi = nb % 3; nb += 1
                            proj_chunk(4 + idx, bi)
                            evac(idx, kT[n][:, s0:s0 + 512], ps[bi][:, :], [tps[bi]], [t_kT[n][grp]])
                        for c4 in range(4):
                            b_cb = nb % 3; nb += 1
                            proj_chunk(8 + c4 * 3 + 0, b_cb)
                            P.op("act", lambda e, b=b_cb: e.copy(out=cbs[:], in_=ps[b][:, :]), reads=[tps[b_cb]], writes=[t_cbs])
                            b_cc = nb % 3; nb += 1
                            proj_chunk(8 + c4 * 3 + 1, b_cc)
                            P.op("act", lambda e, b=b_cc: e.copy(out=ccs[:], in_=ps[b][:, :]), reads=[tps[b_cc]], writes=[t_ccs])
                            b_ch = nb % 3; nb += 1
                            proj_chunk(8 + c4 * 3 + 2, b_ch)
                            if grp == 0:
                                P.op("dve", lambda e, c4=c4: e.memset(zb[:, c4, 0:2], 0.0), writes=[t_zb[c4]])
                            P.op("dve", lambda e, c4=c4, b=b_ch: e.tensor_tensor(out=zb[:, c4, 2:514], in0=ps[b][:, :], in1=ccs[:],
                                                                                 op=ALU.mult),
                                 reads=[tps[b_ch], t_ccs], writes=[t_zb[c4]])
                            P.op("dve", lambda e, c4=c4: e.tensor_scalar(out=yb[:], in0=zb[:, c4, 2:514], scalar1=convw[:, c4, 2:3],
                                                                         scalar2=None, op0=ALU.mult),
                                 reads=[t_zb[c4], t_convw], writes=[t_yb])
                            P.op("dve", lambda e, c4=c4: e.scalar_tensor_tensor(out=yb[:], in0=zb[:, c4, 1:513], scalar=convw[:, c4, 1:2],
                                                                                in1=yb[:], op0=ALU.mult, op1=ALU.add),
                                 reads=[t_zb[c4], t_convw, t_yb], writes=[t_yb])
                            P.op("dve", lambda e, c4=c4: e.scalar_tensor_tensor(out=yb[:], in0=zb[:, c4, 0:512], scalar=convw[:, c4, 0:1],
                                                                                in1=yb[:], op0=ALU.mult, op1=ALU.add),
                                 reads=[t_zb[c4], t_convw, t_yb], writes=[t_yb])
                            P.op("dve", lambda e, c4=c4: e.tensor_tensor(out=oc[:, c4, :], in0=yb[:], in1=cbs[:], op=ALU.mult),
                                 reads=[t_yb, t_cbs], writes=[t_oc[c4]])
                            P.op("dve", lambda e, c4=c4: e.tensor_copy(out=zb[:, c4, 0:2], in_=zb[:, c4, 512:514]),
                                 reads=[t_zb[c4]], writes=[t_zb[c4]])
                            P.op("act", lambda e, c4=c4: e.activation(out=osq[:, c4, :], in_=oc[:, c4, :], func=AF.Square),
                                 reads=[t_oc[c4]], writes=[t_osq[c4]])
                        for c4 in range(4):
                            P.op("pe", lambda e, c4=c4: e.matmul(ps[3][:, :], lhsT=onesb[:, :], rhs=osq[:, c4, :],
                                                                 start=(c4 == 0), stop=(c4 == 3)),
                                 reads=[t_onesb, t_osq[c4]], writes=[tps[3]])
                        P.op("dve", lambda e: e.tensor_scalar(out=rbc[:], in0=ps[3][:, :], scalar1=1.0 / 512, scalar2=EPS,
                                                              op0=ALU.mult, op1=ALU.add), reads=[tps[3]], writes=[t_rbc])
                        P.op("act", lambda e: e.activation(out=rbc[:], in_=rbc[:], func=AF.Sqrt), reads=[t_rbc], writes=[t_rbc])
                        P.op("dve", lambda e: e.reciprocal(out=rbc[:], in_=rbc[:]), reads=[t_rbc], writes=[t_rbc])
                        for c4 in range(4):
                            P.op("dve", lambda e, c4=c4: e.scalar_tensor_tensor(
                                out=mixT[:, 4 + c4, s0:s0 + 512], in0=oc[:, c4, :], scalar=gconv[:, c4:c4 + 1], in1=rbc[:],
                                op0=ALU.mult, op1=ALU.mult), reads=[t_oc[c4], t_gconv, t_rbc], writes=[t_mixc[grp]])
                        for tl in range(4):
                            ti = grp * 4 + tl
                            bi = 4 + (tl % 2)
                            for k in range(8):
                                P.op("pe", lambda e, k=k, tl=tl, bi=bi: e.matmul(
                                    ps[bi][:, 0:280], lhsT=hT[:, k, tl * 128:(tl + 1) * 128], rhs=win[:, k, 2560:2840],
                                    start=(k == 0), stop=(k == 7)), reads=[t_win, t_hT], writes=[tps[bi]])
                            P.op("act", lambda e, ti=ti, bi=bi: e.copy(
                                out=vs_aug[:, ti, :, 0:64], in_=ps[bi][:, 0:128].rearrange("p (g d) -> p g d", g=2)),
                                reads=[tps[bi]], writes=[t_va[ti]])
                            P.op("act", lambda e, ti=ti, bi=bi: e.copy(
                                out=vw_aug[:, ti, :, 0:64], in_=ps[bi][:, 128:256].rearrange("p (g d) -> p g d", g=2)),
                                reads=[tps[bi]], writes=[t_va[ti]])
                            P.op("act", lambda e, ti=ti, bi=bi: e.activation(out=gat[:, ti, :], in_=ps[bi][:, 256:280],
                                                                             func=AF.Sigmoid),
                                 reads=[tps[bi]], writes=[t_gat[ti]])
                    if s == 0:
                        dbgdump("qT", [128, 4, S], BF16, qT[:], t_qT)
                        dbgdump("gat", [128, NT, 24], F32, gat[:], t_gat)
                        dbgdump("vs", [128, NT, 2, 65], BF16, vs_aug[:], t_va + [t_ones])
                        if stop_after <= 1:
                            dbgdump("mixT", [128, 8, S], BF16, mixT[:], t_mixc + t_mixa)
                if stop_after <= 1:
                    continue

                with Scope():
                    bd1 = {}; bd2 = {}; pe2 = {}; t_cw = Tok()
                    for n in "kv":
                        bd1[n] = sb("bd1" + n, [128, 32, 128], BF16)
                        P.dma("pool", bd1[n][:], bd1_d[n], writes=[t_cw])
                        bd2[n] = sb("bd2" + n, [128, 128], BF16)
                        P.dma("pool", bd2[n][:], bd2_d[n], writes=[t_cw])
                        pe2[n] = sb("pe2" + n, [128, 32], BF16)
                        P.dma("pool", pe2[n][:], pe2_d[n], writes=[t_cw])
                    Bw = sb("Bw", [128, 5, 8, 128], BF16); t_Bw = Tok()
                    Bs = sb("Bs", [128, 3, 8, 128], BF16); t_Bs = Tok()
                    for dl in range(5):
                        src = bass.AP(fw_d.tensor, 128 + dl * 128, [[LW, 128], [128 * (LW + 1), 8], [1, 128]])
                        P.dma("pool", Bw[:, dl, :, :], src, reads=[t_fw], writes=[t_Bw])
                    for dl in range(3):
                        src = bass.AP(fw_d.tensor, 8 * 128 * (LW + 1) + 128 + dl * 128,
                                      [[LW, 128], [128 * (LW + 1), 8], [1, 128]])
                        P.dma("pool", Bs[:, dl, :, :], src, reads=[t_fw], writes=[t_Bs])
                    if s == 0:
                        dbgdump("Bw", [128, 5, 8, 128], BF16, Bw[:], [t_Bw])
                        dbgdump("Bs", [128, 3, 8, 128], BF16, Bs[:], [t_Bs])
                    esel, t_esel = load_const("esel", esel_d, [128, 2048], BF16, "pool")
                    selmul, t_selmul = load_const("selmul", selmul_d, [128, 16, 32])
                    seladd, t_seladd = load_const("seladd", seladd_d, [128, 16, 32])
                    gattn, t_gattn = load_const("gattn", gattn_d, [128, 512])
                    cbias = sb("cbias", [128, 2]); t_cbias = Tok()
                    for i, n in enumerate("kv"):
                        for l in range(32):
                            P.op("pe", lambda e, n=n, l=l, i=i: e.matmul(ps[2][:, i:i + 1], lhsT=bd1[n][:, l, :],
                                                                         rhs=pe2[n][:, l:l + 1], start=(l == 0), stop=(l == 31)),
                                 reads=[t_cw], writes=[tps[2]])
                    P.op("dve", lambda e: e.tensor_copy(out=cbias[:], in_=ps[2][:, 0:2]), reads=[tps[2]], writes=[t_cbias])
                    kcmpT = sb("kcmpT", [128, 128], BF16); t_kcmp = Tok()
                    vc_aug = sb("vc_aug", [128, 2, 97], BF16); t_vca = Tok()
                    for g in range(2):
                        P.dma("pool", vc_aug[0:127, g, 65:97], cov_d, writes=[t_vca])
                    P.op("dve", lambda e: e.memset(vc_aug[:, :, 64:65], 1.0), writes=[t_vca])
                    hid = {n: sb("hid" + n, [128, 128], BF16) for n in "kv"}; t_hid = Tok()
                    for i, (n, srcn) in enumerate((("k", "kc"), ("v", "vc"))):
                        src = kT[srcn]
                        for l in range(32):
                            P.op("pe", lambda e, n=n, l=l, i=i, src=src: e.matmul(
                                ps[i][:, 0:127], lhsT=bd1[n][:, l, :], rhs=src[:, l:l + 2017:16],
                                start=(l == 0), stop=(l == 31)), reads=[t_cw] + t_kT[srcn], writes=[tps[i]])
                        P.op("act", lambda e, n=n, i=i: e.activation(out=hid[n][:, 0:127], in_=ps[i][:, 0:127],
                                                                     func=AF.Gelu_apprx_tanh, bias=cbias[:, i:i + 1]),
                             reads=[tps[i], t_cbias], writes=[t_hid])
                    P.op("pe", lambda e: e.matmul(ps[0][:, 0:127], lhsT=bd2["k"][:, :], rhs=hid["k"][:, 0:127], start=True, stop=True),
                         reads=[t_cw, t_hid], writes=[tps[0]])
                    P.op("dve", lambda e: e.tensor_copy(out=kcmpT[:, 0:127], in_=ps[0][:, 0:127]), reads=[tps[0]], writes=[t_kcmp])
                    P.op("pe", lambda e: e.matmul(ps[1][0:127, 0:128], lhsT=hid["v"][:, 0:127], rhs=bd2["v"][:, :], start=True, stop=True),
                         reads=[t_cw, t_hid], writes=[tps[1]])
                    P.op("dve", lambda e: e.tensor_copy(out=vc_aug[0:127, :, 0:64],
                                                        in_=ps[1][0:127, 0:128].rearrange("p (g d) -> p g d", g=2)),
                         reads=[tps[1]], writes=[t_vca])
                    if s == 0:
                        dbgdump("kcmpT", [128, 128], BF16, kcmpT[:], [t_kcmp])
                        dbgdump("vc_aug", [128, 2, 97], BF16, vc_aug[:], [t_vca])

                    bcp = [sb(f"bcp{i}", [128, 8, 128], BF16) for i in range(2)]; t_bcp = [Tok(), Tok()]
                    NSB = 4
                    sbank = [0, 1, 2, 6]
                    tS = [sb(f"tS{i}", [128, 512]) for i in range(NSB)]; t_tS = [Tok() for _ in range(NSB)]
                    pT = [sb(f"pT{i}", [128, 512], BF16) for i in range(NSB)]; t_pT = [Tok() for _ in range(NSB)]
                    o_acc = [sb(f"oacc{i}", [128, 8, 64]) for i in range(2)]; t_oacc = [Tok(), Tok()]
                    rs4 = sb("rs4", [128, 4]); wg4 = sb("wg4", [128, 4]); t_rs = Tok()
                    otmp = sb("otmp", [128, 4, 64]); t_otmp = Tok()
                    itmp = sb("itmp", [128, 4, 32]); imp = sb("imp", [128, 32]); t_imp = Tok()
                    m8 = sb("m8", [128, 8]); t_m8 = Tok()
                    negsel = [sb(f"negsel{i}", [128, 128], BF16) for i in range(2)]; t_negsel = [Tok(), Tok()]
                    for i in range(2):
                        P.op("dve", lambda e, i=i: e.memset(negsel[i][:], 0.0), writes=[t_negsel[i]])
                    nsT4 = [sb(f"nsT4{i}", [128, 4, 128], BF16) for i in range(2)]; t_nsT = [Tok(), Tok()]
                    junk2 = sb("junk2", [128, 512], BF16); t_junk2 = Tok()
                    on_b = sb("on_b", [128, 512], BF16); t_onb = Tok()
                    tpsb_a = tpsb
                    BK = {0: 0, 2: 1, 1: 2}
                    tpo = {br: [Tok()] for br in range(3)}

                    def evac_branch(g, br, qi, oa, t_oa):
                        c0 = 0
                        rd = tpo[br]
                        pob = psO[:, BK[br], :].rearrange("p (j c) -> p j c", j=4)
                        if br == 0:
                            P.op("dve", lambda e: e.tensor_scalar(out=rs4[:], in0=pob[:, :, 64], scalar1=1e-30, scalar2=None,
                                                                  op0=ALU.max), reads=rd, writes=[t_rs])
                            P.op("dve", lambda e: e.reciprocal(out=rs4[:], in_=rs4[:]), reads=[t_rs], writes=[t_rs])
                        else:
                            P.op("dve", lambda e: e.reciprocal(out=rs4[:], in_=pob[:, :, 64]), reads=rd, writes=[t_rs])
                        g0_ = 12 * g + br
                        P.op("dve", lambda e: e.tensor_tensor(out=wg4[:], in0=rs4[:], in1=gat[:, qi, g0_:g0_ + 10:3], op=ALU.mult),
                             reads=[t_rs, t_gat[qi]], writes=[t_rs])
                        if br == 0:
                            P.op("dve", lambda e: e.tensor_tensor(
                                out=oa[:, 4 * g:4 * g + 4, :], in0=pob[:, :, 0:64],
                                in1=wg4[:].unsqueeze(2).broadcast_to([128, 4, 64]), op=ALU.mult),
                                reads=rd + [t_rs], writes=[t_oa])
                            P.op("dve", lambda e: e.tensor_tensor(
                                out=itmp[:], in0=pob[:, :, 65:97], in1=rs4[:].unsqueeze(2).broadcast_to([128, 4, 32]),
                                op=ALU.mult), reads=rd + [t_rs], writes=[t_imp])
                            P.op("dve", lambda e: e.tensor_reduce(out=imp[:], in_=itmp[:].rearrange("p j n -> p n j"),
                                                                  axis=AX.X, op=ALU.add), reads=[t_imp], writes=[t_imp])
                        else:
                            P.op("dve", lambda e: e.tensor_tensor(
                                out=otmp[:], in0=pob[:, :, 0:64],
                                in1=wg4[:].unsqueeze(2).broadcast_to([128, 4, 64]), op=ALU.mult),
                                reads=rd + [t_rs], writes=[t_otmp])
                            P.op("pool", lambda e: e.tensor_tensor(out=oa[:, 4 * g:4 * g + 4, :], in0=oa[:, 4 * g:4 * g + 4, :],
                                                                   in1=otmp[:], op=ALU.add),
                                 reads=[t_otmp, t_oa], writes=[t_oa])

                    def selection(g, qi, it):
                        ns, t_ns = negsel[it % 2], t_negsel[it % 2]
                        nT, t_nT = nsT4[it % 2], t_nsT[it % 2]
                        P.op("dve", lambda e: e.tensor_tensor(out=imp[:], in0=imp[:], in1=selmul[:, qi, :], op=ALU.mult),
                             reads=[t_imp, t_selmul], writes=[t_imp])
                        P.op("dve", lambda e: e.tensor_tensor(out=imp[:], in0=imp[:], in1=seladd[:, qi, :], op=ALU.add),
                             reads=[t_imp, t_seladd], writes=[t_imp])
                        P.op("dve", lambda e: e.max(out=m8[:], in_=imp[:]), reads=[t_imp], writes=[t_m8])
                        P.op("dve", lambda e: e.tensor_scalar(out=ns[:, g * 64:g * 64 + 32], in0=imp[:], scalar1=m8[:, 7:8],
                                                              scalar2=NEG, op0=ALU.is_lt, op1=ALU.mult),
                             reads=[t_imp, t_m8], writes=[t_ns])

                        def later():
                            P.op("pe", lambda e: e.transpose(out=psb[:, 0:128], in_=ns[:, :], identity=identb[:]),
                                 reads=[t_ns, t_identb], writes=[tpsb_a])
                            P.op("act", lambda e: e.copy(out=nT[:], in_=psb[:, 0:128].unsqueeze(1).broadcast_to([128, 4, 128])),
                                 reads=[tpsb_a], writes=[t_nT])
                            sel_ready.add(it)
                        deferred.append([cur_n[0] + 2, later])

                    def finish_qi(qi, oa, t_oa):
                        gi = s * NT + qi
                        if dbg and s == 0:
                            if qi == 0:
                                dbg_out["oattn"] = dout("dbg_oattn", [NT, 128, 512], F32)
                            P.dma("sp", dbg_out["oattn"][qi], oa[:].rearrange("p h d -> p (h d)"), reads=[t_oa])
                        oaf = oa[:].rearrange("p h d -> p (h d)")
                        rms_stats(oaf, t_oa, junk2[:], t_junk2, ssqa, rstda, t_sta[gi], gi, 512)
                        P.op("dve", lambda e: e.scalar_tensor_tensor(
                            out=on_b[:], in0=oaf, scalar=rstda[:, gi:gi + 1], in1=gattn[:], op0=ALU.mult, op1=ALU.mult),
                            reads=[t_oa, t_sta[gi], t_gattn], writes=[t_onb])

                        def later():
                            for c in range(4):
                                P.op("pe", lambda e, c=c: e.transpose(out=psb[:, 512 + c * 128:512 + (c + 1) * 128],
                                                                      in_=on_b[:, c * 128:(c + 1) * 128], identity=identb[:]),
                                     reads=[t_onb, t_identb], writes=[tpsb])
                            for c in range(4):
                                evac(1, mixT[:, c, qi * 128:(qi + 1) * 128], psb[:, 512 + c * 128:512 + (c + 1) * 128],
                                     [tpsb], [t_mixa[qi]])
                        deferred.append([cur_n[0] + 4, later])

                    deferred = []
                    cur_n = [0]
                    sel_ready = set()
                    tiles = []
                    it = 0
                    for qi in range(NT):
                        oa, t_oa = o_acc[qi % 2], t_oacc[qi % 2]
                        for g in range(2):
                            pr = slice(g * 64, (g + 1) * 64)
                            rhs_q = qT[pr, :, qi * 128:(qi + 1) * 128]
                            rd_q = [t_qT[qi // 4]]
                            hs4 = slice(4 * g, 4 * g + 4)
                            bc, t_bc = bcp[qi % 2], t_bcp[qi % 2]
                            pre = None
                            if g == 0:
                                def pre(qi=qi, bc=bc, t_bc=t_bc):
                                    src = bass.AP(fc_d.tensor, 2048 + 128 * qi - 31, [[LC, 127], [127 * (LC + 16), 8], [1, 128]])
                                    P.dma("pool", bc[0:127, :, :], src, reads=[t_fc], writes=[t_bc])
                            tiles.append(dict(
                                pre=pre, rows=127, br=0, first=True, last=True, ncol=97,
                                mm=[(kcmpT[pr, 0:127], rhs_q, rd_q + [t_kcmp])],
                                bias=bc[0:127, hs4, :], t_bias=t_bc, v=vc_aug[0:127, g, :], v_rd=[t_vca],
                                post=(lambda g=g, qi=qi, oa=oa, t_oa=t_oa, it=it: (evac_branch(g, 0, qi, oa, t_oa),
                                                                                   selection(g, qi, it)))))
                            k0 = max(0, qi - 4)
                            for kj in range(k0, qi + 1):
                                tiles.append(dict(
                                    pre=None, rows=128, br=2, first=(kj == k0), last=(kj == qi), ncol=65,
                                    mm=[(kT["kw"][pr, kj * 128:(kj + 1) * 128], rhs_q, rd_q + [t_kT["kw"][kj // 4]])],
                                    bias=Bw[:, qi - kj, hs4, :], t_bias=t_Bw, v=vw_aug[:, kj, g, :], v_rd=[t_va[kj], t_ones],
                                    post=(lambda g=g, qi=qi, oa=oa, t_oa=t_oa: evac_branch(g, 2, qi, oa, t_oa)) if kj == qi else None))
                            nT, t_nT = nsT4[it % 2], t_nsT[it % 2]
                            for kj in range(qi + 1):
                                post = None
                                if kj == qi:
                                    if g == 1:
                                        post = (lambda g=g, qi=qi, oa=oa, t_oa=t_oa: (evac_branch(g, 1, qi, oa, t_oa),
                                                                                      finish_qi(qi, oa, t_oa)))
                                    else:
                                        post = (lambda g=g, qi=qi, oa=oa, t_oa=t_oa: evac_branch(g, 1, qi, oa, t_oa))
                                tiles.append(dict(
                                    need=it,
                                    pre=None, rows=128, br=1, first=(kj == 0), last=(kj == qi), ncol=65,
                                    mm=[(kT["ks"][pr, kj * 128:(kj + 1) * 128], rhs_q, rd_q + [t_kT["ks"][kj // 4]]),
                                        (esel[pr, kj * 128:(kj + 1) * 128], nT[pr, :, :], [t_esel, t_nT])],
                                    bias=Bs[:, min(qi - kj, 2), hs4, :], t_bias=t_Bs, v=vs_aug[:, kj, g, :],
                                    v_rd=[t_va[kj], t_ones], post=post))
                            it += 1

                    def emit_S(tl, sl):
                        if tl["pre"] is not None:
                            tl["pre"]()
                        rows = tl["rows"]
                        nmm = len(tl["mm"])
                        for m, (l_ap, r_ap, rd) in enumerate(tl["mm"]):
                            P.op("pe", lambda e, l_ap=l_ap, r_ap=r_ap, m=m: e.matmul(
                                ps[sbank[sl]][0:rows, :].rearrange("p (j q) -> p j q", j=4), lhsT=l_ap, rhs=r_ap,
                                start=(m == 0), stop=(m == nmm - 1)), reads=rd, writes=[tps[sbank[sl]]])
                        P.op("dve", lambda e: e.scalar_tensor_tensor(
                            out=tS[sl][0:rows, :].rearrange("p (j q) -> p j q", j=4),
                            in0=ps[sbank[sl]][0:rows, :].rearrange("p (j q) -> p j q", j=4), scalar=0.125,
                            in1=tl["bias"], op0=ALU.mult, op1=ALU.add), reads=[tps[sbank[sl]], tl["t_bias"]], writes=[t_tS[sl]])
                        P.op("act", lambda e: e.activation(out=pT[sl][0:rows, :], in_=tS[sl][0:rows, :], func=AF.Exp),
                             reads=[t_tS[sl]], writes=[t_pT[sl]])

                    def emit_PV(tl, sl):
                        rows = tl["rows"]
                        bk = BK[tl["br"]]
                        for j in range(4):
                            P.op("pe", lambda e, j=j: e.matmul(psO[:, bk, j * 128:j * 128 + tl["ncol"]],
                                                               lhsT=pT[sl][0:rows, j * 128:(j + 1) * 128], rhs=tl["v"],
                                                               start=(tl["first"] and j == 0), stop=(tl["last"] and j == 3),
                                                               skip_group_check=True),
                                 reads=[t_pT[sl]] + tl["v_rd"], writes=tpo[tl["br"]])
                        if tl["post"] is not None:
                            tl["post"]()

                    ntl = len(tiles)
                    LA = 2
                    nxt = 0
                    for n_ in range(ntl):
                        cur_n[0] = n_
                        while True:
                            for d_ in [d for d in deferred if d[0] <= n_]:
                                deferred.remove(d_)
                                d_[1]()
                            progressed = False
                            while nxt < ntl and nxt <= n_ + LA and (tiles[nxt].get("need") is None
                                                                   or tiles[nxt]["need"] in sel_ready):
                                emit_S(tiles[nxt], nxt % NSB)
                                nxt += 1
                                progressed = True
                            if nxt > n_:
                                break
                            d_ = min(deferred, key=lambda d: d[0])
                            deferred.remove(d_)
                            d_[1]()
                        emit_PV(tiles[n_], n_ % NSB)
                    for d_ in sorted(deferred, key=lambda d: d[0]):
                        d_[1]()
                    deferred.clear()

                    if s == 0:
                        dbgdump("mixT", [128, 8, S], BF16, mixT[:], t_mixc + t_mixa)
                if stop_after <= 2:
                    continue

                with Scope():
                    wout = sb("wout", [128, 8, 1024], BF16); t_wout = Tok()
                    wov = w_out_d.rearrange("(k p) n -> p k n", p=128)
                    for k in range(8):
                        P.dma("pool", wout[:, k, :], wov[:, k, :], writes=[t_wout])
                    gt1 = sb("gt1", [128, 1024]); t_gt1 = Tok()
                    P.dma("sp", gt1[:], gt_d[0, s], reads=[t_gtd], writes=[t_gt1])
                    xpool = [sb(f"xt3{i}", [128, 1024]) for i in range(2)]; t_xp = [Tok(), Tok()]
                    x1p = [sb(f"x1t{i}", [128, 1024]) for i in range(2)]; t_x1p = [Tok(), Tok()]
                    junk = sb("junk3", [128, 1024], BF16); t_junk = Tok()
                    xs = sb("xs3", [128, 1024], BF16); t_xs = Tok()
                    h2st = [sb(f"h2st{i}", [128, 8, 128], BF16) for i in range(2)]; t_h2st = [Tok(), Tok()]
                    for ti in range(NT):
                        gi = s * NT + ti
                        xt, t_xt = xpool[ti % 2], t_xp[ti % 2]
                        x1t, t_x1t = x1p[ti % 2], t_x1p[ti % 2]
                        P.dma("sp", xt[:], x_d[gi * 128:(gi + 1) * 128, :], writes=[t_xt])
                        for half in range(2):
                            for k in range(8):
                                P.op("pe", lambda e, k=k, half=half: e.matmul(
                                    ps[half][:, :], lhsT=mixT[:, k, ti * 128:(ti + 1) * 128],
                                    rhs=wout[:, k, half * 512:(half + 1) * 512], start=(k == 0), stop=(k == 7)),
                                    reads=[t_wout, t_mixa[ti], t_mixc[ti // 4]], writes=[tps[half]])
                            hs = slice(half * 512, (half + 1) * 512)
                            P.op("dve", lambda e, half=half, hs=hs: e.tensor_tensor(out=x1t[:, hs], in0=ps[half][:, :], in1=gt1[:, hs],
                                                                                    op=ALU.mult),
                                 reads=[tps[half], t_gt1], writes=[t_x1t])
                        P.op("pool", lambda e: e.tensor_tensor(out=x1t[:], in0=x1t[:], in1=xt[:], op=ALU.add),
                             reads=[t_x1t, t_xt], writes=[t_x1t])
                        P.dma("sp", x1_d[gi * 128:(gi + 1) * 128, :], x1t[:], reads=[t_x1t], writes=[t_x1d[gi]])
                        hs_, t_hs = h2st[ti % 2], t_h2st[ti % 2]
                        norm_transpose(x1t, t_x1t, gi, s, ssq2, rstd2, t_st2, gs2, 24, hs_, t_hs, 0, junk, t_junk, xs, t_xs)
                        P.dma("sp", h2T_d[:, :, gi * 128:(gi + 1) * 128], hs_[:], reads=[t_hs], writes=[t_h2d[gi]])

        if stop_after <= 3:
            P.wait_all("sp", Tok.registry)
            return nc, dbg_out

        try:
            utb_d = dscr("utb_s", [16384, 1024], BF16)
            vb_d = dscr("vb_s", [16384, 1024], BF16)
            s2_d = dscr("s2_s", [16, T, 128], BF16)
            t_utb = [Tok() for _ in range(16)]
            t_vb = [Tok() for _ in range(16)]
            with Scope():
                stg = [sb(f"cst{i}", [128, 8, 1024], BF16) for i in range(2)]; t_stg = [Tok(), Tok()]
                n = 0
                for src_d, dst_d, tks in ((ut_d, utb_d, t_utb), (pv_d, vb_d, t_vb)):
                    for c in range(16):
                        i = n % 2; n += 1
                        sv = src_d[c * 1024:(c + 1) * 1024, :].rearrange("(c p) n -> p c n", p=128)
                        dv = dst_d[c * 1024:(c + 1) * 1024, :].rearrange("(c p) n -> p c n", p=128)
                        P.dma("pool", stg[i][:], sv, writes=[t_stg[i]])
                        P.dma("sp", dv, stg[i][:], reads=[t_stg[i]], writes=[tks[c]])
            if stop_after == 3.5:
                raise _Stop()

            with Scope():
                wq, t_wq = None, Tok()
                wq = sb("wq", [128, 8, 1024], BF16)
                wqv = wq_d.rearrange("(k p) n -> p k n", p=128)
                for k in range(8):
                    P.dma("pool", wq[:, k, :], wqv[:, k, :], writes=[t_wq])
                kb, t_kb = load_const("kb", kb_d, [128, 8, 256], BF16, "pool")
                identf, t_identf = load_const("identf", identf_d, [128, 128])
                iota, t_iota = load_const("iota", iota_d, [128, 128])
                selh, t_selh = load_const("selh", selh_d, [16, 128], BF16, "pool")
                gfin, t_gfin = load_const("gfin", gfin_d, [128, 1024])
                gt2 = sb("gt2", [128, 1024]); t_gt2 = Tok()
                Wbuf = sb("Wbuf", [128, 256, 128], BF16); t_W = [Tok() for _ in range(64)]
                h2g = sb("h2g", [128, 8, 256], BF16); t_h2g = Tok()
                qTp = sb("qTp", [128, 8, 256], BF16); t_qTp = Tok()
                s_sb = sb("s_sb", [128, 8, 256]); t_s = Tok()
                work = sb("work", [128, 8, 256]); t_work = Tok()
                v16 = sb("v16", [128, 8, 2, 16]); t_v16 = Tok()
                idx = sb("idx", [128, 8, 16], U32); t_idx = Tok()
                cand = sb("cand", [128, 8, 256]); t_cand = Tok()
                cwork = sb("cwork", [128, 256]); t_cwork = Tok()
                ts16 = sb("ts16", [128, 8, 16]); t_ts = Tok()
                d16 = sb("d16", [128, 8, 16]); t_d16 = Tok()
                zz = sb("zz", [128, 8]); mu = sb("mu", [128, 8]); tauE = sb("tauE", [128, 8]); t_zz = Tok()
                tm3 = sb("tm3", [128, 3, 128]); t_tm3 = Tok()
                s2hm = [sb(f"s2hm{i}", [16, 16 * 128], BF16) for i in range(2)]; t_s2hm = [Tok(), Tok()]
                s2hb = sb("s2hb", [128, 2, 8, 128], BF16); t_s2hb = Tok()
                NRQ = 3
                e4 = [sb(f"e4{i}", [128, 4, 128], BF16) for i in range(NRQ)]; t_e4 = [Tok() for _ in range(NRQ)]
                Rt4 = [sb(f"Rt4{i}", [128, 4, 128], BF16) for i in range(NRQ)]; t_Rt4 = [Tok() for _ in range(NRQ)]
                Lt4 = [sb(f"Lt4{i}", [128, 4, 128], BF16) for i in range(NRQ)]; t_Lt4 = [Tok() for _ in range(NRQ)]
                mk4 = [sb(f"mk4{i}", [128, 4, 128], BF16) for i in range(NRQ)]; t_mk4 = [Tok() for _ in range(NRQ)]
                NC = 5
                utc = [sb(f"utc{i}", [128, 8, 128], BF16) for i in range(NC)]; t_utc = [Tok() for _ in range(NC)]
                vch = [sb(f"vch{i}", [128, 1024], BF16) for i in range(NC)]; t_vch = [Tok() for _ in range(NC)]
                abuf = [sb(f"abuf{i}", [128, 256], BF16) for i in range(2)]; t_ab = [Tok(), Tok()]
                wab = [sb(f"wab{i}", [128, 256], BF16) for i in range(2)]; t_wab = [Tok(), Tok()]
                x1t = sb("x1f", [128, 1024]); t_x1t = Tok()
                yt = sb("yf", [128, 1024]); t_yt = Tok()
                ot = sb("of", [128, 1024]); t_ot = Tok()
                junk = sb("junkf", [128, 1024], BF16); t_junk = Tok()
                ssq3 = sb("ssq3", [128, NTT]); rstd3 = sb("rstd3", [128, NTT]); t_st3 = [Tok() for _ in range(NTT)]
                t_s2d = Tok()
                t_out = Tok()

                def top16(dst_lo, dst_hi, src, wrk, rd, wr_dst, wr_wrk):
                    P.op("dve", lambda e: e.max(out=dst_lo, in_=src), reads=rd, writes=[wr_dst])
                    P.op("dve", lambda e: e.match_replace(out=wrk, in_to_replace=dst_lo, in_values=src, imm_value=-1e30),
                         reads=rd + [wr_dst], writes=[wr_wrk])
                    P.op("dve", lambda e: e.max(out=dst_hi, in_=wrk), reads=[wr_wrk], writes=[wr_dst])

                ngroups = T // 256
                s2hbb = [s2hb, sb("s2hb2", [128, 2, 8, 128], BF16)]; t_s2hbb = [t_s2hb, Tok()]
                psX = psb[:, :].bitcast(F32)
                PB = [(ps[2], tps[2]), (psX, tpsb)]
                h2gb = [h2g, sb("h2g2", [128, 8, 256], BF16)]; t_h2gb = [t_h2g, Tok()]
                tT3b = [[sb(f"tT3_{i}_{j}", [128, 3, 128]) for j in range(2)] for i in range(2)]
                t_tT3b = [[Tok() for j in range(2)] for i in range(2)]

                def prep_steps(gidx):
                    g0 = gidx * 256
                    hb, t_hb = h2gb[gidx % 2], t_h2gb[gidx % 2]
                    steps = []

                    def s_q(hh):
                        def f():
                            if hh == 0:
                                P.dma("sp", hb[:], h2T_d[:, :, g0:g0 + 256], reads=t_h2d[gidx * 2:gidx * 2 + 2], writes=[t_hb])
                            for h in (hh, hh + 1):
                                bk, tb = PB[h % 2]
                                for k in range(8):
                                    P.op("pe", lambda e, k=k, h=h, bk=bk: e.matmul(bk[:, 0:256], lhsT=wq[:, k, h * 128:(h + 1) * 128],
                                                                                   rhs=hb[:, k, :], start=(k == 0), stop=(k == 7)),
                                         reads=[t_wq, t_hb], writes=[tb])
                                P.op("act", lambda e, h=h, bk=bk: e.copy(out=qTp[:, h, :], in_=bk[:, 0:256]),
                                     reads=[tb], writes=[t_qTp])
                        return f
                    for hh in (0, 2, 4, 6):
                        steps.append((f"q{hh}", s_q(hh)))

                    for tt in range(2):
                        tsl = slice(tt * 128, (tt + 1) * 128)
                        tT3, t_tT3 = tT3b[gidx % 2][tt], t_tT3b[gidx % 2][tt]
                        s2hb, t_s2hb = s2hbb[tt], t_s2hbb[tt]

                        def sA(tsl=tsl):
                            for hp in range(4):
                                bk, tb = PB[hp % 2]
                                for h in (2 * hp, 2 * hp + 1):
                                    P.op("pe", lambda e, h=h, bk=bk: e.matmul(bk[:, (h % 2) * 256:(h % 2 + 1) * 256], lhsT=qTp[:, h, tsl],
                                                                              rhs=kb[:, h, :], start=True, stop=True),
                                         reads=[t_qTp, t_kb], writes=[tb])
                                P.op("act", lambda e, hp=hp, bk=bk: e.copy(
                                    out=s_sb[:, 2 * hp:2 * hp + 2, :], in_=bk[:, :].rearrange("p (h n) -> p h n", h=2)),
                                    reads=[tb], writes=[t_s])

                        def sB(s2hb=s2hb, t_s2hb=t_s2hb):
                            for h in range(8):
                                for p in range(2):
                                    top16(v16[:, h, p, 0:8], v16[:, h, p, 8:16], s_sb[:, h, p * 128:(p + 1) * 128],
                                          work[:, h, p * 128:(p + 1) * 128], [t_s], t_v16, t_work)
                                P.op("dve", lambda e, h=h: e.max_index(out=idx[:, h, 0:8], in_max=v16[:, h, 0, 0:8],
                                                                       in_values=s_sb[:, h, 0:128]),
                                     reads=[t_s, t_v16], writes=[t_idx])
                                P.op("dve", lambda e, h=h: e.max_index(out=idx[:, h, 8:16], in_max=v16[:, h, 0, 8:16],
                                                                       in_values=work[:, h, 0:128]),
                                     reads=[t_work, t_v16], writes=[t_idx])
                            P.op("dve", lambda e: e.tensor_tensor(
                                out=cand[:].rearrange("p h (a b) -> p h a b", a=16),
                                in0=v16[:, :, 0, :].unsqueeze(3).broadcast_to([128, 8, 16, 16]),
                                in1=v16[:, :, 1, :].unsqueeze(2).broadcast_to([128, 8, 16, 16]), op=ALU.add),
                                reads=[t_v16], writes=[t_cand])
                            for h in range(8):
                                top16(ts16[:, h, 0:8], ts16[:, h, 8:16], cand[:, h, :], cwork[:, :], [t_cand], t_ts, t_cwork)

                        def sB2(s2hb=s2hb, t_s2hb=t_s2hb):
                            P.op("dve", lambda e: e.tensor_tensor(out=d16[:], in0=ts16[:], in1=ts16[:, :, 0:1].broadcast_to([128, 8, 16]),
                                                                  op=ALU.subtract), reads=[t_ts], writes=[t_d16])
                            P.op("act", lambda e: e.activation(out=d16[:], in_=d16[:], func=AF.Exp), reads=[t_d16], writes=[t_d16])
                            P.op("dve", lambda e: e.tensor_reduce(out=zz[:], in_=d16[:], axis=AX.X, op=ALU.add),
                                 reads=[t_d16], writes=[t_zz])
                            P.op("act", lambda e: e.activation(out=zz[:], in_=zz[:], func=AF.Ln), reads=[t_zz], writes=[t_zz])
                            P.op("dve", lambda e: e.tensor_tensor(out=mu[:], in0=zz[:], in1=ts16[:, :, 0], op=ALU.add),
                                 reads=[t_zz, t_ts], writes=[t_zz])
                            P.op("dve", lambda e: e.tensor_scalar(out=tauE[:], in0=ts16[:, :, 15], scalar1=-1e-4, scalar2=None,
                                                                  op0=ALU.add), reads=[t_ts], writes=[t_zz])
                            P.op("dve", lambda e: e.tensor_tensor(
                                out=tm3[:, 0, :].rearrange("p (h a) -> p h a", h=8),
                                in0=tauE[:].unsqueeze(2).broadcast_to([128, 8, 16]), in1=v16[:, :, 0, :], op=ALU.subtract),
                                reads=[t_zz, t_v16], writes=[t_tm3])
                            P.op("dve", lambda e: e.tensor_tensor(
                                out=tm3[:, 1, :].rearrange("p (h a) -> p h a", h=8),
                                in0=v16[:, :, 0, :], in1=mu[:].unsqueeze(2).broadcast_to([128, 8, 16]), op=ALU.subtract),
                                reads=[t_zz, t_v16], writes=[t_tm3])
                            P.op("dve", lambda e: e.tensor_copy(out=tm3[:, 2, :], in_=idx[:].rearrange("p h a -> p (h a)")),
                                 reads=[t_idx], writes=[t_tm3])
                            P.op("dve", lambda e: e.tensor_copy(out=s2hb[:, 0, :, :], in_=s_sb[:, :, 128:256]),
                                 reads=[t_s], writes=[t_s2hb])
                            P.op("dve", lambda e: e.tensor_tensor(out=s2hb[:, 1, :, :], in0=s_sb[:, :, 128:256], in1=s2hb[:, 0, :, :],
                                                                  op=ALU.subtract), reads=[t_s, t_s2hb], writes=[t_s2hb])

                        def sC(tt=tt, tT3=tT3, t_tT3=t_tT3):
                            bk, tb = PB[0]
                            for w3 in range(3):
                                P.op("pe", lambda e, w3=w3: e.transpose(out=bk[:, w3 * 128:(w3 + 1) * 128], in_=tm3[:, w3, :],
                                                                        identity=identf[:]),
                                     reads=[t_tm3, t_identf], writes=[tb])
                            P.op("act", lambda e: e.copy(out=tT3[:], in_=bk[:, 0:384].rearrange("p (w t) -> p w t", w=3)),
                                 reads=[tb], writes=[t_tT3])

                        def sD(tt=tt):
                            P.dma("sp", s2_d[:, g0 + tt * 128:g0 + (tt + 1) * 128, :].rearrange("(w h) t j -> t w h j", w=2),
                                  s2hbb[tt][:], reads=[t_s2hbb[tt]], writes=[t_s2d])
                        steps.append((f"A{tt}", sA))
                        steps.append((f"B{tt}", sB))
                        steps.append((f"E{tt}", sB2))
                        steps.append((f"C{tt}", sC))
                        steps.append((f"D{tt}", sD))
                    return steps

                SCHED = {"q0": 1, "q2": 3, "q4": 5, "q6": 7, "A0": 8, "B0": 10, "E0": 45, "C0": 55, "A1": 58, "B1": 60,
                         "E1": 100, "C1": -1, "D0": -1, "D1": -1}

                for gidx in range(ngroups):
                    g0 = gidx * 256
                    sq = g0 // S
                    if g0 % S == 0:
                        P.dma("sp", gt2[:], gt_d[1, sq], reads=[t_gtd], writes=[t_gt2])
                    if gidx == 0:
                        for _, st_ in prep_steps(0):
                            st_()
                    h2g, t_h2g = h2gb[gidx % 2], t_h2gb[gidx % 2]
                    nxt_steps = prep_steps(gidx + 1) if gidx + 1 < ngroups else []
                    for tt in range(2):
                        gi = gidx * 2 + tt
                        tT3, t_tT3 = tT3b[gidx % 2][tt], t_tT3b[gidx % 2][tt]
                        quads = []
                        for sl in range(8):
                            for q4 in range(4):
                                quads.append((sl, q4))

                        def load_slab(sl):
                            i2 = sl % 2
                            t0 = g0 + tt * 128 + sl * 16
                            P.dma("sp", s2hm[i2][:, :], s2_d[:, t0:t0 + 16, :].rearrange("h t j -> h (t j)"),
                                  reads=[t_s2d], writes=[t_s2hm[i2]])

                        def emit_sel(qn):
                            sl, q4 = quads[qn]
                            i2 = sl % 2
                            if q4 == 0:
                                load_slab(sl)
                            bA = 0 if qn % 2 == 0 else 4
                            bD = 3 if qn % 2 == 0 else 5
                            for bb in (bA, bD):
                                P.op("pe", lambda e, bb=bb: e.matmul(ps[bb][:, :], lhsT=selh[:, :],
                                                                     rhs=s2hm[i2][:, q4 * 512:(q4 + 1) * 512], start=True, stop=True),
                                     reads=[t_selh, t_s2hm[i2]], writes=[tps[bb]])
                            r = qn % NRQ
                            tl0 = sl * 16 + q4 * 4
                            for t in range(4):
                                P.op("act", lambda e, t=t: e.activation(out=e4[r][:, t, :], in_=ps[bA][:, t * 128:(t + 1) * 128],
                                                                        func=AF.Exp, bias=tT3[:, 1, tl0 + t:tl0 + t + 1]),
                                     reads=[tps[bA], t_tT3], writes=[t_e4[r]])
                            P.op("dve", lambda e: e.tensor_tensor(
                                out=mk4[r][:], in0=ps[bD][:, :].rearrange("p (t j) -> p t j", t=4),
                                in1=tT3[:, 0, tl0:tl0 + 4].unsqueeze(2).broadcast_to([128, 4, 128]), op=ALU.is_ge),
                                reads=[tps[bD], t_tT3], writes=[t_mk4[r]])
                            P.op("dve", lambda e: e.tensor_tensor(
                                out=Lt4[r][:], in0=iota[:].unsqueeze(1).broadcast_to([128, 4, 128]),
                                in1=tT3[:, 2, tl0:tl0 + 4].unsqueeze(2).broadcast_to([128, 4, 128]), op=ALU.is_equal),
                                reads=[t_iota, t_tT3], writes=[t_Lt4[r]])
                            P.op("dve", lambda e: e.tensor_tensor(out=Rt4[r][:], in0=mk4[r][:], in1=e4[r][:], op=ALU.mult),
                                 reads=[t_mk4[r], t_e4[r]], writes=[t_Rt4[r]])

                        def emit_w(qn):
                            sl, q4 = quads[qn]
                            r = qn % NRQ
                            wb = 1 + (qn % 2)
                            tb0 = tt * 128 + sl * 16 + q4 * 4
                            for t in range(4):
                                P.op("pe", lambda e, t=t: e.matmul(ps[wb][:, t * 128:(t + 1) * 128], lhsT=Rt4[r][:, t, :],
                                                                   rhs=Lt4[r][:, t, :], start=True, stop=True),
                                     reads=[t_Rt4[r], t_Lt4[r]], writes=[tps[wb]])
                            evac(qn, Wbuf[:, tb0:tb0 + 4, :], ps[wb][:, :].rearrange("p (t i) -> p t i", t=4),
                                 [tps[wb]], [t_W[tb0 // 4]])

                        emit_sel(0)
                        for qn in range(len(quads)):
                            if qn + 1 < len(quads):
                                emit_sel(qn + 1)
                            emit_w(qn)

                    if dbg and gidx == 0:
                        dbgdump("Wbuf", [128, 256, 128], BF16, Wbuf[:], t_W)
                    if stop_after == 3.8:
                        raise _Stop()
                    def ld(i):
                        c = i % NC
                        P.dma("sp", utc[c][:], utb_d[i * 128:(i + 1) * 128, :].rearrange("p (k j) -> p k j", k=8),
                              reads=[t_utb[i // 8]], writes=[t_utc[c]])
                        P.dma("sp", vch[c][:], vb_d[i * 128:(i + 1) * 128, :], reads=[t_vb[i // 8]], writes=[t_vch[c]])

                    def emU(i):
                        c = i % NC
                        ba = i % 2
                        for k in range(8):
                            P.op("pe", lambda e, k=k: e.matmul(ps[ba][:, 0:256], lhsT=utc[c][:, k, :], rhs=h2g[:, k, :],
                                                               start=(k == 0), stop=(k == 7)),
                                 reads=[t_utc[c], t_h2g], writes=[tps[ba]])
                        P.op("act", lambda e: e.activation(out=abuf[ba][:], in_=ps[ba][:, 0:256], func=AF.Gelu_apprx_tanh),
                             reads=[tps[ba]], writes=[t_ab[ba]])
                        P.op("pool", lambda e: e.tensor_tensor(out=wab[ba][:], in0=abuf[ba][:], in1=Wbuf[:, :, i], op=ALU.mult),
                             reads=[t_ab[ba]] + t_W, writes=[t_wab[ba]])

                    def emV(i):
                        c = i % NC
                        ba = i % 2
                        for tt in range(2):
                            for half in range(2):
                                P.op("pe", lambda e, tt=tt, half=half: e.matmul(
                                    psO[:, tt * 2 + half, :], lhsT=wab[ba][:, tt * 128:(tt + 1) * 128],
                                    rhs=vch[c][:, half * 512:(half + 1) * 512], start=(i == 0), stop=(i == 127)),
                                    reads=[t_wab[ba], t_vch[c]], writes=[tps[3 + tt * 2 + half]])

                    PF = NC - 2
                    for i in range(PF):
                        ld(i)
                    emU(0)
                    for i in range(128):
                        if i + PF < 128:
                            ld(i + PF)
                        if i + 1 < 128:
                            emU(i + 1)
                        emV(i)
                        for nm_, fn_ in nxt_steps:
                            if SCHED[nm_] == i:
                                fn_()
                    for nm_, fn_ in nxt_steps:
                        if SCHED[nm_] == -1:
                            fn_()
                    for tt in range(2):
                        gi = gidx * 2 + tt
                        P.dma("sp", x1t[:], x1_d[gi * 128:(gi + 1) * 128, :], reads=[t_x1d[gi]], writes=[t_x1t])
                        for half in range(2):
                            hs = slice(half * 512, (half + 1) * 512)
                            P.op("dve", lambda e, tt=tt, half=half, hs=hs: e.tensor_tensor(
                                out=yt[:, hs], in0=psO[:, tt * 2 + half, :], in1=gt2[:, hs], op=ALU.mult),
                                reads=[tps[3 + tt * 2 + half], t_gt2], writes=[t_yt])
                        if dbg and gidx == 0 and tt == 0:
                            dbgdump("peer0", [128, 1024], F32, yt[:], [t_yt])
                        P.op("pool", lambda e: e.tensor_tensor(out=yt[:], in0=yt[:], in1=x1t[:], op=ALU.add),
                             reads=[t_yt, t_x1t], writes=[t_yt])
                        rms_stats(yt[:], t_yt, junk[:], t_junk, ssq3, rstd3, t_st3[gi], gi, D)
                        P.op("dve", lambda e, gi=gi: e.scalar_tensor_tensor(out=ot[:], in0=yt[:], scalar=rstd3[:, gi:gi + 1], in1=gfin[:],
                                                                            op0=ALU.mult, op1=ALU.mult),
                             reads=[t_yt, t_st3[gi], t_gfin], writes=[t_ot])
                        P.dma("sp", out_d[gi * 128:(gi + 1) * 128, :], ot[:], reads=[t_ot], writes=[t_out])
                    if stop_after == 4 and gidx == 0:
                        raise _Stop()

        except _Stop:
            pass
        P.wait_all("sp", Tok.registry)
        print("ops per engine", P.ecount, "waits", P.nwaits)
    return nc, dbg_out


_NC_CACHE = {}


def kernel(**inputs):
    inp = {k: np.asarray(v) for k, v in inputs.items()}
    sh = _prep(inp)
    if "nc" not in _NC_CACHE:
        _NC_CACHE["nc"] = build_nc(nseq=2)[0]
    nc = _NC_CACHE["nc"]
    x = np.asarray(inp["x"], np.float32)
    c = np.asarray(inp["c"], np.float32)
    in_maps = []
    for core in range(8):
        m = dict(sh)
        m["x"] = np.ascontiguousarray(x[2 * core:2 * core + 2].reshape(2 * S, D))
        m["cT"] = np.ascontiguousarray(c[2 * core:2 * core + 2].T.reshape(8, 128, 2).transpose(1, 0, 2))
        in_maps.append(m)
    res = run_bass_kernel_spmd(nc, in_maps, core_ids=list(range(8)))
    out = np.concatenate([np.asarray(r["out"]).reshape(2, S, D) for r in res.results], axis=0)
    return out.astype(np.float32)
```

```python
import math
import numpy as np
from contextlib import ExitStack
import concourse.bass as bass
import concourse.mybir as mybir
from concourse.bass_utils import run_bass_kernel_spmd

F32 = mybir.dt.float32
BF16 = mybir.dt.bfloat16
U32 = mybir.dt.uint32
AF = mybir.ActivationFunctionType
ALU = mybir.AluOpType
AX = mybir.AxisListType

S = 2048
D = 1024
NT = 16
EPS = 1e-6
NEG = -30000.0
LENG = "dve"
import os
PDBG = os.environ.get("PDBG", "")
SEM_CHUNK = 30000


class _Stop(Exception):
    pass


class Tok:
    __slots__ = ("writers", "readers")
    registry = []

    def __init__(self):
        self.writers = []
        self.readers = []
        Tok.registry.append(self)


class Op:
    __slots__ = ("eng", "sem", "val", "is_dma")


class Prog:
    ENGS = ("pe", "dve", "act", "pool", "sp")

    def __init__(self, nc, stack, n_dma_sems=8):
        self.nc = nc
        self.stack = stack
        self.eobj = {"pe": nc.tensor, "dve": nc.vector, "act": nc.scalar,
                     "pool": nc.gpsimd, "sp": nc.sync}
        self.esems = {e: [] for e in self.ENGS}
        self.ecount = {e: 0 for e in self.ENGS}
        self.waited = {e: {} for e in self.ENGS}
        self.dma_sems = {}
        self.dma_rr = {}
        self.n_dma_sems = n_dma_sems
        self.nwaits = 0

    def _esem(self, e, chunk):
        lst = self.esems[e]
        while len(lst) <= chunk:
            lst.append(self.stack.enter_context(self.nc.semaphore(f"s_{e}_{len(lst)}")))
        return lst[chunk]

    def _wait(self, e, sem, val):
        w = self.waited[e]
        k = id(sem)
        if w.get(k, 0) >= val:
            return
        w[k] = val
        self.eobj[e].wait_ge(sem, val)
        self.nwaits += 1

    def _deps(self, e, is_dma, reads, writes):
        for t in reads:
            for p in t.writers:
                if (not p.is_dma) and (not is_dma) and p.eng == e and e == "pe":
                    continue
                self._wait(e, p.sem, p.val)
        for t in writes:
            for p in t.writers + t.readers:
                if (not p.is_dma) and (not is_dma) and p.eng == e and e == "pe":
                    continue
                self._wait(e, p.sem, p.val)

    def _record(self, op, reads, writes):
        for t in reads:
            if not op.is_dma:
                t.readers = [r for r in t.readers if r.is_dma or r.eng != op.eng]
            t.readers.append(op)
        for t in writes:
            if t.readers:
                t.writers = [op]
                t.readers = []
            else:
                if not op.is_dma:
                    t.writers = [w for w in t.writers if w.is_dma or w.eng != op.eng]
                t.writers.append(op)

    def op(self, e, fn, reads=(), writes=()):
        self._deps(e, False, reads, writes)
        n = self.ecount[e]
        sem = self._esem(e, n // SEM_CHUNK)
        val = (n % SEM_CHUNK) + 1
        fn(self.eobj[e]).then_inc(sem, 1)
        self.ecount[e] = n + 1
        o = Op()
        o.eng, o.sem, o.val, o.is_dma = e, sem, val, False
        self._record(o, reads, writes)
        return o

    def dma(self, q, out, in_, reads=(), writes=(), **kw):
        if q not in self.dma_sems:
            self.dma_sems[q] = [[self.stack.enter_context(self.nc.semaphore(f"d_{q}_{i}")), 0]
                                for i in range(self.n_dma_sems)]
            self.dma_rr[q] = 0
        self._deps(q, True, reads, writes)
        i = self.dma_rr[q]
        self.dma_rr[q] = (i + 1) % self.n_dma_sems
        ent = self.dma_sems[q][i]
        sem, cur = ent
        if cur > 0:
            self._wait(q, sem, cur)
        val = cur + 16
        ent[1] = val
        self.eobj[q].dma_start(out=out, in_=in_, **kw).then_inc(sem, 16)
        o = Op()
        o.eng, o.sem, o.val, o.is_dma = q, sem, val, True
        self._record(o, reads, writes)
        return o

    def barrier(self):
        for e in self.ENGS:
            for f in self.ENGS:
                n = self.ecount[f]
                if f == e or n == 0:
                    continue
                self._wait(e, self.esems[f][(n - 1) // SEM_CHUNK], (n - 1) % SEM_CHUNK + 1)
            for q, lst in self.dma_sems.items():
                for sem, cur in lst:
                    if cur > 0:
                        self._wait(e, sem, cur)

    def wait_all(self, e, toks):
        for t in toks:
            for p in t.writers + t.readers:
                self._wait(e, p.sem, p.val)


def _rel_bucket_np(n):
    n = np.maximum(n, 0)
    exact = 16
    lr = np.log(np.maximum(n, 1).astype(np.float32) / np.float32(exact)) / np.float32(math.log(128 / exact))
    large = exact + (lr * np.float32(32 - exact)).astype(np.int32)
    return np.where(n < exact, n, np.minimum(large, 31))


def _constants():
    c = {}
    npr = np.arange(4096)
    n = npr - 2048
    bk = _rel_bucket_np(n)
    oh = np.zeros((33, 2, 4096), np.float32)
    okw = (n >= 0) & (n < 512)
    oks = (n >= 0)
    oh[bk[okw], 0, npr[okw]] = 1.0
    oh[32, 0, npr[~okw]] = 1.0
    oh[bk[oks], 1, npr[oks]] = 1.0
    oh[32, 1, npr[~oks]] = 1.0
    c["oh"] = oh
    e32 = (np.arange(2048)[None, :] // 64 == np.arange(32)[:, None]).astype(np.float32)
    e128 = np.zeros((128, 2048), np.float32)
    e128[0:32] = e32
    e128[64:96] = e32
    c["Esel"] = e128
    cs = np.arange(127) * 16
    ss = np.arange(32) * 64
    c["Cov"] = ((cs[:, None] < ss[None, :] + 64) & (cs[:, None] + 32 > ss[None, :])).astype(np.float32)
    t = np.arange(2048)
    cur = t // 64
    j = np.arange(32)
    forced = (j[None, :] == 0) | (j[None, :] == cur[:, None]) | (j[None, :] == cur[:, None] - 1)
    allowed = j[None, :] * 64 <= t[:, None]
    mul = (allowed & ~forced).astype(np.float32)
    add = np.where(forced, 1e4, np.where(allowed, 0.0, -1e30)).astype(np.float32)
    c["selmul"] = np.ascontiguousarray(mul.reshape(16, 128, 32).transpose(1, 0, 2))
    c["seladd"] = np.ascontiguousarray(add.reshape(16, 128, 32).transpose(1, 0, 2))
    c["identf"] = np.eye(128, dtype=np.float32)
    c["ones"] = np.ones((128, 128), np.float32)
    c["iota"] = np.tile(np.arange(128, dtype=np.float32)[None, :], (128, 1))
    sel = np.zeros((8, 128), np.float32)
    for h in range(8):
        sel[h, h * 16:(h + 1) * 16] = 1.0
    c["selh"] = np.concatenate([sel, sel], axis=0)
    return c


def _blockdiag2(w):
    out = np.zeros(w.shape[:-2] + (128, 128), np.float32)
    out[..., :64, :64] = w
    out[..., 64:, 64:] = w
    return out


def _prep(inp):
    sh = {}
    f = lambda a: np.ascontiguousarray(np.asarray(a, np.float32))
    sh["w_mod"] = f(inp["w_mod"][0])
    sh["bmodT"] = f(inp["b_mod"][0].reshape(48, 128).T)
    bm = inp["b_mod"][0]
    sh["bmod_bc"] = f(np.broadcast_to(np.stack([bm[2048:3072], bm[5120:6144]])[None], (128, 2, 1024)))
    sh["gmix"] = f(inp["ln_mix_g"][0].reshape(8, 128).T)
    sh["gffn"] = f(inp["ln_ffn_g"][0].reshape(8, 128).T)
    sh["gfin_bc"] = f(np.broadcast_to(inp["ln_final_g"][None, :], (128, 1024)))
    w_in = np.asarray(inp["w_in"][0], np.float32)
    sp = np.cumsum([0, 512, 128, 128, 128, 128, 128, 128, 24, 512, 512, 512])
    q, kc, vc, ks, vs, kw, vw, gt, cb, cc, ch = [w_in[:, sp[i]:sp[i + 1]] for i in range(11)]
    qh = q.reshape(1024, 8, 64)
    qperm = np.concatenate([np.concatenate([qh[:, j], qh[:, 4 + j]], axis=1) for j in range(4)], axis=1)
    cols = [qperm, kc, vc, ks, kw]
    for c4 in range(4):
        cols += [cb[:, c4 * 128:(c4 + 1) * 128], cc[:, c4 * 128:(c4 + 1) * 128], ch[:, c4 * 128:(c4 + 1) * 128]]
    cols += [vs, vw, gt]
    sh["w_in"] = f(np.concatenate(cols, axis=1))
    for nm, w1, w2, pe in (("k", "cmp_wk1", "cmp_wk2", "cmp_pe_k"), ("v", "cmp_wv1", "cmp_wv2", "cmp_pe_v")):
        a = np.asarray(inp[w1][0], np.float32).reshape(32, 64, 64)
        sh["bd1" + nm] = f(_blockdiag2(a).transpose(1, 0, 2))
        sh["bd2" + nm] = f(_blockdiag2(np.asarray(inp[w2][0], np.float32)))
        p = np.asarray(inp[pe][0], np.float32).T
        sh["pe2" + nm] = f(np.concatenate([p, p], axis=0))
    sh["convw"] = f(inp["conv_w"][0][:, 0, :].reshape(3, 4, 128).transpose(2, 1, 0))
    sh["gattn_bc"] = f(np.broadcast_to(inp["norm_attn_g"][0][None, :], (128, 512)))
    sh["gconv"] = f(inp["norm_conv_g"][0].reshape(4, 128).T)
    sh["w_out"] = f(inp["w_out"][0])
    sh["peer_wq"] = f(inp["peer_wq"][0])
    pk = np.asarray(inp["peer_keys"][0], np.float32)
    kb = np.zeros((128, 8, 256), np.float32)
    for h in range(8):
        for p in range(2):
            kb[p * 64:(p + 1) * 64, h, p * 128:(p + 1) * 128] = pk[h, p].T
    sh["peer_kb"] = kb
    u = np.asarray(inp["peer_u"][0], np.float32)
    sh["peer_ut"] = f(u.reshape(128, 128, 8, 128).transpose(0, 3, 2, 1).reshape(128 * 128, 1024))
    sh["peer_v"] = f(inp["peer_v"][0])
    rt = np.zeros((33, 8), np.float32)
    rt[:32] = inp["rel_table"]
    rt[32] = NEG
    sh["relt"] = rt
    sh.update(_constants())
    return sh


def build_nc(nseq=2, dbg=False, stop_after=99):
    nc = bass.Bass("TRN2", target_bir_lowering=False)
    Tok.registry = []
    T = nseq * S
    NTT = nseq * NT

    def din(name, shape, dt=F32):
        return nc.dram_tensor(name, list(shape), dt, kind="ExternalInput").ap()

    def dscr(name, shape, dt=F32):
        return nc.dram_tensor(name, list(shape), dt, kind="Internal").ap()

    def dout(name, shape, dt=F32):
        return nc.dram_tensor(name, list(shape), dt, kind="ExternalOutput").ap()

    x_d = din("x", [T, D])
    cT_d = din("cT", [128, 8, nseq])
    w_mod_d = din("w_mod", [1024, 6144])
    bmodT_d = din("bmodT", [128, 48])
    bmod_bc_d = din("bmod_bc", [128, 2, 1024])
    gmix_d = din("gmix", [128, 8])
    gffn_d = din("gffn", [128, 8])
    gfin_d = din("gfin_bc", [128, 1024])
    w_in_d = din("w_in", [1024, 2840])
    bd1_d = {n: din("bd1" + n, [128, 32, 128]) for n in "kv"}
    bd2_d = {n: din("bd2" + n, [128, 128]) for n in "kv"}
    pe2_d = {n: din("pe2" + n, [128, 32]) for n in "kv"}
    convw_d = din("convw", [128, 4, 3])
    gattn_d = din("gattn_bc", [128, 512])
    gconv_d = din("gconv", [128, 4])
    w_out_d = din("w_out", [1024, 1024])
    wq_d = din("peer_wq", [1024, 1024])
    kb_d = din("peer_kb", [128, 8, 256])
    ut_d = din("peer_ut", [128 * 128, 1024])
    pv_d = din("peer_v", [16384, 1024])
    relt_d = din("relt", [33, 8])
    oh_d = din("oh", [33, 2, 4096])
    esel_d = din("Esel", [128, 2048])
    cov_d = din("Cov", [127, 32])
    selmul_d = din("selmul", [128, 16, 32])
    seladd_d = din("seladd", [128, 16, 32])
    identf_d = din("identf", [128, 128])
    ones_d = din("ones", [128, 128])
    iota_d = din("iota", [128, 128])
    selh_d = din("selh", [16, 128])
    out_d = dout("out", [T, D])

    tb_d = dscr("tb_s", [2, 8, 4096])
    LW = 1024
    fw_d = dscr("fw_s", [2, 8, 128 * (LW + 1)])
    LC = 4096
    fc_d = dscr("fc_s", [8, 127 * (LC + 16)])
    gt_d = dscr("gt_s", [2, nseq, 128, 1024])
    if dbg:
        x1_d = dout("x1_s", [T, D])
        h2T_d = dout("h2T_s", [128, 8, T], BF16)
    else:
        x1_d = dscr("x1_s", [T, D])
        h2T_d = dscr("h2T_s", [128, 8, T], BF16)
    t_x1d = [Tok() for _ in range(NTT)]
    t_h2d = [Tok() for _ in range(NTT)]

    dbg_out = {}
    uniq = [0]

    with ExitStack() as st0:
        P = Prog(nc, st0)
        scopes = [st0]

        def sb(name, shape, dt=F32):
            uniq[0] += 1
            return scopes[-1].enter_context(nc.sbuf_tensor(f"sb{uniq[0]}_{name}", list(shape), dt))

        class Scope:
            def __enter__(self):
                self.st = ExitStack()
                scopes.append(self.st)
                return self

            def __exit__(self, *a):
                if a[0] is None:
                    P.barrier()
                scopes.pop()
                self.st.close()
                return False

        psg = [st0.enter_context(nc.psum_tensor(f"psum{i}", [128, 512], F32)) for i in range(3)]
        psO = st0.enter_context(nc.psum_tensor("psumO", [128, 4, 512], F32))
        ps = [psg[0][:, :], psg[1][:, :], psg[2][:, :]] + [psO[:, i, :] for i in range(4)]
        tps = [Tok() for _ in range(7)]
        psb = st0.enter_context(nc.psum_tensor("psumb", [128, 1024], BF16))
        tpsb = Tok()

        def load_const(name, src, shape, dt=F32, q="sp"):
            t = sb(name, shape, dt)
            k = Tok()
            P.dma(q, t[:], src, writes=[k])
            return t, k

        def dbgdump(name, shape, dt, src_ap, toks):
            if not dbg:
                return
            dbg_out[name] = dout("dbg_" + name, shape, dt)
            P.dma("sp", dbg_out[name], src_ap, reads=toks)

        identb, t_identb = load_const("identb", identf_d, [128, 128], BF16, "pool")
        onesb, t_onesb = load_const("onesb", ones_d, [128, 128], BF16, "pool")
        gmix, t_gmix = load_const("gmix", gmix_d, [128, 8])
        gffn, t_gffn = load_const("gffn", gffn_d, [128, 8])
        bmodT, t_bmodT = load_const("bmodT", bmodT_d, [128, 48])
        convw, t_convw = load_const("convw", convw_d, [128, 4, 3])
        gconv, t_gconv = load_const("gconv", gconv_d, [128, 4])
        relt, t_relt = load_const("relt", relt_d, [33, 8])
        modT = sb("modT", [128, 48, nseq]); t_modT = Tok()
        gs1 = sb("gs1", [128, 8, nseq]); gs2 = sb("gs2", [128, 8, nseq]); t_gs = Tok()
        t_gtd = Tok()
        t_fw = Tok(); t_fc = Tok()

        with Scope():
            cT = sb("cT", [128, 8, nseq]); t_cT = Tok()
            P.dma("sp", cT[:], cT_d, writes=[t_cT])
            c_act = sb("c_act", [128, 8, nseq]); t_cact = Tok()
            P.op("act", lambda e: e.activation(out=c_act[:], in_=cT[:], func=AF.Silu), reads=[t_cT], writes=[t_cact])
            c_rep = sb("c_rep", [128, 8, nseq, 128]); t_crep = Tok()
            P.op("dve", lambda e: e.tensor_copy(out=c_rep[:], in_=c_act[:].unsqueeze(3).broadcast_to([128, 8, nseq, 128])),
                 reads=[t_cact], writes=[t_crep])
            bmod_bc = sb("bmod_bc", [128, 2, 1024]); t_bmbc = Tok()
            P.dma("sp", bmod_bc[:], bmod_bc_d, writes=[t_bmbc])
            gstage = [sb(f"gstage{i}", [128, nseq, 128]) for i in range(2)]; t_gst = [Tok(), Tok()]
            wm_view = w_mod_d.rearrange("(k p) n -> p k n", p=128)
            wmp = [sb(f"wm{i}", [128, 8, 128]) for i in range(3)]
            t_wmp = [Tok() for _ in range(3)]
            for j in range(48):
                wt, tw = wmp[j % 3], t_wmp[j % 3]
                P.dma("sp", wt[:], wm_view[:, :, j * 128:(j + 1) * 128], writes=[tw])
                for k in range(8):
                    P.op("pe", lambda e, k=k, wt=wt: e.matmul(ps[0][:, j * nseq:(j + 1) * nseq], lhsT=wt[:, k, :],
                                                              rhs=c_act[:, k, :], start=(k == 0), stop=(k == 7)),
                         reads=[tw, t_cact], writes=[tps[0]])
                which = {2: 0, 5: 1}.get(j // 8)
                if which is not None:
                    jj = j % 8
                    bi = 1 + (j % 2)
                    for b in range(nseq):
                        for k in range(8):
                            P.op("pe", lambda e, k=k, b=b, wt=wt, bi=bi: e.matmul(
                                ps[bi][:, b * 128:(b + 1) * 128], lhsT=c_rep[:, k, b, :], rhs=wt[:, k, :],
                                start=(k == 0), stop=(k == 7)), reads=[tw, t_crep], writes=[tps[bi]])
                    gsb, tg = gstage[j % 2], t_gst[j % 2]
                    P.op("dve", lambda e, bi=bi, which=which, jj=jj, gsb=gsb: e.tensor_tensor(
                        out=gsb[:],
                        in0=ps[bi][:, 0:nseq * 128].rearrange("p (b n) -> p b n", b=nseq),
                        in1=bmod_bc[:, which, jj * 128:(jj + 1) * 128].unsqueeze(1).broadcast_to([128, nseq, 128]),
                        op=ALU.add), reads=[tps[bi], t_bmbc], writes=[tg])
                    P.dma("sp", gt_d[which].rearrange("b p n -> p b n")[:, :, jj * 128:(jj + 1) * 128], gsb[:],
                          reads=[tg], writes=[t_gtd])
            P.op("dve", lambda e: e.tensor_tensor(
                out=modT[:], in0=ps[0][:, 0:48 * nseq].rearrange("p (j b) -> p j b", b=nseq),
                in1=bmodT[:].unsqueeze(2).broadcast_to([128, 48, nseq]), op=ALU.add),
                reads=[tps[0], t_bmodT], writes=[t_modT])
            P.op("dve", lambda e: e.scalar_tensor_tensor(
                out=gs1[:], in0=modT[:, 8:16, :], scalar=1.0, in1=gmix[:].unsqueeze(2).broadcast_to([128, 8, nseq]),
                op0=ALU.add, op1=ALU.mult), reads=[t_modT, t_gmix], writes=[t_gs])
            P.op("dve", lambda e: e.scalar_tensor_tensor(
                out=gs2[:], in0=modT[:, 32:40, :], scalar=1.0, in1=gffn[:].unsqueeze(2).broadcast_to([128, 8, nseq]),
                op0=ALU.add, op1=ALU.mult), reads=[t_modT, t_gffn], writes=[t_gs])
            dbgdump("modT", [128, 48, nseq], F32, modT[:], [t_modT])

            ohp = [sb(f"ohp{i}", [33, 512]) for i in range(2)]
            t_ohp = [Tok(), Tok()]
            tbs = [sb(f"tbs{i}", [8, 512]) for i in range(2)]
            t_tbs = [Tok(), Tok()]
            t_tbd = Tok()
            for kind in range(2):
                for pc in range(8):
                    i = (kind * 8 + pc) % 2
                    P.dma("sp", ohp[i][:], oh_d[:, kind, pc * 512:(pc + 1) * 512], writes=[t_ohp[i]])
                    bi = 3 + i
                    P.op("pe", lambda e, i=i, bi=bi: e.matmul(ps[bi][0:8, :], lhsT=relt[:, :], rhs=ohp[i][:, :],
                                                               start=True, stop=True),
                         reads=[t_relt, t_ohp[i]], writes=[tps[bi]])
                    P.op("act", lambda e, i=i, bi=bi: e.copy(out=tbs[i][:], in_=ps[bi][0:8, :]),
                         reads=[tps[bi]], writes=[t_tbs[i]])
                    P.dma("sp", tb_d[kind, :, pc * 512:(pc + 1) * 512], tbs[i][:], reads=[t_tbs[i]], writes=[t_tbd])
            for kind in range(2):
                for h in range(8):
                    src = bass.AP(tb_d.tensor, (kind * 8 + h) * 4096 + (2048 - 128), [[0, 128], [1, LW]])
                    dst = bass.AP(fw_d.tensor, (kind * 8 + h) * 128 * (LW + 1), [[LW + 1, 128], [1, LW]])
                    P.dma("sp", dst, src, reads=[t_tbd], writes=[t_fw])
            for h in range(8):
                src = bass.AP(tb_d.tensor, (8 + h) * 4096, [[0, 127], [1, LC]])
                dst = bass.AP(fc_d.tensor, h * 127 * (LC + 16), [[LC + 16, 127], [1, LC]])
                P.dma("sp", dst, src, reads=[t_tbd], writes=[t_fc])

        with Scope():
            qTg = [sb(f"qTg{g}", [128, 4, S], BF16) for g in range(2)]; t_qT = [Tok() for _ in range(4)]
            t_qz = Tok()
            P.op("pool", lambda e: e.memset(qTg[0][64:128, :, :], 0.0), writes=[t_qz])
            P.op("pool", lambda e: e.memset(qTg[1][0:64, :, :], 0.0), writes=[t_qz])
            kT = {n: sb(n + "T", [128, S], BF16) for n in ("kc", "vc", "ks", "kw")}
            t_kT = {n: [Tok() for _ in range(4)] for n in kT}
            vs_aug = sb("vs_aug", [128, NT, 2, 65], BF16); vw_aug = sb("vw_aug", [128, NT, 2, 65], BF16)
            t_va = [Tok() for _ in range(NT)]
            gat = sb("gat", [128, NT, 24]); t_gat = [Tok() for _ in range(NT)]
            mixT = sb("mixT", [128, 8, S], BF16)
            t_mixc = [Tok() for _ in range(4)]
            t_mixa = [Tok() for _ in range(NT)]
            t_ones = Tok()
            P.op("pool", lambda e: e.memset(vs_aug[:, :, :, 64:65], 1.0), writes=[t_ones])
            P.op("pool", lambda e: e.memset(vw_aug[:, :, :, 64:65], 1.0), writes=[t_ones])
            ssq = sb("ssq", [128, NTT]); rstd = sb("rstd", [128, NTT]); t_st = [Tok() for _ in range(NTT)]
            ssq2 = sb("ssq2", [128, NTT]); rstd2 = sb("rstd2", [128, NTT]); t_st2 = [Tok() for _ in range(NTT)]
            ssqa = sb("ssqa", [128, NTT]); rstda = sb("rstda", [128, NTT]); t_sta = [Tok() for _ in range(NTT)]

            def rms_stats(src_ap, t_src, junk_, t_junk_, ssq_, rstd_, tk, col, width):
                P.op("act", lambda e: e.activation(out=junk_, in_=src_ap, func=AF.Square, accum_out=ssq_[:, col:col + 1]),
                     reads=[t_src], writes=[t_junk_, tk])
                P.op("dve", lambda e: e.tensor_scalar(out=rstd_[:, col:col + 1], in0=ssq_[:, col:col + 1], scalar1=1.0 / width,
                                                      scalar2=EPS, op0=ALU.mult, op1=ALU.add), reads=[tk], writes=[tk])
                P.op("act", lambda e: e.activation(out=rstd_[:, col:col + 1], in_=rstd_[:, col:col + 1], func=AF.Ln),
                     reads=[tk], writes=[tk])
                P.op("act", lambda e: e.activation(out=rstd_[:, col:col + 1], in_=rstd_[:, col:col + 1], func=AF.Exp, scale=-0.5),
                     reads=[tk], writes=[tk])

            def norm_transpose(xt, t_xt, gi, s, ssq_, rstd_, t_st_, gs_, sh_lo, dst, t_dst, col0, junk, t_junk, xs, t_xs):
                rms_stats(xt[:], t_xt, junk[:], t_junk, ssq_, rstd_, t_st_[gi], gi, D)
                P.op("dve", lambda e: e.tensor_scalar(out=xs[:], in0=xt[:], scalar1=rstd_[:, gi:gi + 1], scalar2=None,
                                                      op0=ALU.mult), reads=[t_xt, t_st_[gi]], writes=[t_xs])
                for c in range(8):
                    P.op("pe", lambda e, c=c: e.transpose(out=psb[:, c * 128:(c + 1) * 128], in_=xs[:, c * 128:(c + 1) * 128],
                                                          identity=identb[:]), reads=[t_xs, t_identb], writes=[tpsb])
                for c in range(8):
                    o_ap = dst[:, c, col0:col0 + 128]
                    i_ap = psb[:, c * 128:(c + 1) * 128]
                    sc_ap = gs_[:, c, s:s + 1]
                    bi_ap = modT[:, sh_lo + c, s:s + 1]
                    if gi % 2 == 0:
                        P.op("dve", lambda e, o_ap=o_ap, i_ap=i_ap, sc_ap=sc_ap, bi_ap=bi_ap: e.tensor_scalar(
                            out=o_ap, in0=i_ap, scalar1=sc_ap, scalar2=bi_ap, op0=ALU.mult, op1=ALU.add),
                            reads=[tpsb, t_gs, t_modT], writes=[t_dst])
                    else:
                        P.op("act", lambda e, o_ap=o_ap, i_ap=i_ap, sc_ap=sc_ap, bi_ap=bi_ap: e.activation(
                            out=o_ap, in_=i_ap, func=AF.Identity, scale=sc_ap, bias=bi_ap),
                            reads=[tpsb, t_gs, t_modT], writes=[t_dst])

            def evac(i, out_ap, in_ap, reads, writes):
                if i % 2:
                    P.op("act", lambda e: e.copy(out=out_ap, in_=in_ap), reads=reads, writes=writes)
                else:
                    P.op("dve", lambda e: e.tensor_copy(out=out_ap, in_=in_ap), reads=reads, writes=writes)

            for s in range(nseq):
                with Scope():
                    win = sb("win", [128, 8, 2840], BF16); t_win = Tok()
                    wiv = w_in_d.rearrange("(k p) n -> p k n", p=128)
                    for k in range(8):
                        P.dma("pool", win[:, k, :], wiv[:, k, :], writes=[t_win])
                    xpool = [sb(f"xt{i}", [128, 1024]) for i in range(2)]; t_xp = [Tok(), Tok()]
                    junk = sb("junk", [128, 1024], BF16); t_junk = Tok()
                    xs = sb("xs", [128, 1024], BF16); t_xs = Tok()
                    hTg = [sb(f"hTg{i}", [128, 8, 512], BF16) for i in range(2)]; t_hTg = [Tok(), Tok()]
                    cbs = sb("cbs", [128, 512]); ccs = sb("ccs", [128, 512]); t_cbs = Tok(); t_ccs = Tok()
                    zb = sb("zb", [128, 4, 514]); t_zb = [Tok() for _ in range(4)]
                    yb = sb("yb", [128, 512]); t_yb = Tok()
                    oc = sb("oc", [128, 4, 512]); t_oc = [Tok() for _ in range(4)]
                    osq = sb("osq", [128, 4, 512], BF16); t_osq = [Tok() for _ in range(4)]
                    rbc = sb("rbc", [128, 512]); t_rbc = Tok()
                    for grp in range(4):
                        hT, t_hT = hTg[grp % 2], t_hTg[grp % 2]
                        s0 = grp * 512
                        for tl in range(4):
                            ti = grp * 4 + tl
                            gi = s * NT + ti
                            xt, t_xt = xpool[gi % 2], t_xp[gi % 2]
                            P.dma("sp", xt[:], x_d[gi * 128:(gi + 1) * 128, :], writes=[t_xt])
                            norm_transpose(xt, t_xt, gi, s, ssq, rstd, t_st, gs1, 0, hT, t_hT, tl * 128, junk, t_junk, xs, t_xs)
                        if s == 0 and grp == 0:
                            dbgdump("hT0", [128, 8, 512], BF16, hT[:], [t_hT])

                        def proj_chunk(cc, bi):
                            for k in range(8):
                                P.op("pe", lambda e, k=k: e.matmul(ps[bi][:, :], lhsT=win[:, k, cc * 128:(cc + 1) * 128],
                                                                   rhs=hT[:, k, :], start=(k == 0), stop=(k == 7)),
                                     reads=[t_win, t_hT], writes=[tps[bi]])

                        nb = 0
                        for cc in range(4):
                            bi = nb % 3; nb += 1
                            proj_chunk(cc, bi)
                            if cc % 2:
                                P.op("act", lambda e, cc=cc, bi=bi: e.copy(out=qTg[0][0:64, cc, s0:s0 + 512], in_=ps[bi][0:64, :]),
                                     reads=[tps[bi], t_qz], writes=[t_qT[grp]])
                                P.op("act", lambda e, cc=cc, bi=bi: e.copy(out=qTg[1][64:128, cc, s0:s0 + 512], in_=ps[bi][64:128, :]),
                                     reads=[tps[bi], t_qz], writes=[t_qT[grp]])
                            else:
                                P.op("dve", lambda e, cc=cc, bi=bi: e.tensor_copy(out=qTg[0][0:64, cc, s0:s0 + 512],
                                                                                  in_=ps[bi][0:64, :]),
                                     reads=[tps[bi], t_qz], writes=[t_qT[grp]])
                                P.op("dve", lambda e, cc=cc, bi=bi: e.tensor_copy(out=qTg[1][64:128, cc, s0:s0 + 512],
                                                                                  in_=ps[bi][64:128, :]),
                                     reads=[tps[bi], t_qz], writes=[t_qT[grp]])
                        for idx, n in enumerate(("kc", "vc", "ks", "kw")):
                            bi = nb % 3; nb += 1
                            proj_chunk(4 + idx, bi)
                            evac(idx, kT[n][:, s0:s0 + 512], ps[bi][:, :], [tps[bi]], [t_kT[n][grp]])
                        for c4 in range(4):
                            b_cb = nb % 3; nb += 1
                            proj_chunk(8 + c4 * 3 + 0, b_cb)
                            P.op("act", lambda e, b=b_cb: e.copy(out=cbs[:], in_=ps[b][:, :]), reads=[tps[b_cb]], writes=[t_cbs])
                            b_cc = nb % 3; nb += 1
                            proj_chunk(8 + c4 * 3 + 1, b_cc)
                            P.op("act", lambda e, b=b_cc: e.copy(out=ccs[:], in_=ps[b][:, :]), reads=[tps[b_cc]], writes=[t_ccs])
                            b_ch = nb % 3; nb += 1
                            proj_chunk(8 + c4 * 3 + 2, b_ch)
                            if grp == 0:
                                P.op("dve", lambda e, c4=c4: e.memset(zb[:, c4, 0:2], 0.0), writes=[t_zb[c4]])
                            P.op("dve", lambda e, c4=c4, b=b_ch: e.tensor_tensor(out=zb[:, c4, 2:514], in0=ps[b][:, :], in1=ccs[:],
                                                                                 op=ALU.mult),
                                 reads=[tps[b_ch], t_ccs], writes=[t_zb[c4]])
                            P.op("dve", lambda e, c4=c4: e.tensor_scalar(out=yb[:], in0=zb[:, c4, 2:514], scalar1=convw[:, c4, 2:3],
                                                                         scalar2=None, op0=ALU.mult),
                                 reads=[t_zb[c4], t_convw], writes=[t_yb])
                            P.op("dve", lambda e, c4=c4: e.scalar_tensor_tensor(out=yb[:], in0=zb[:, c4, 1:513], scalar=convw[:, c4, 1:2],
                                                                                in1=yb[:], op0=ALU.mult, op1=ALU.add),
                                 reads=[t_zb[c4], t_convw, t_yb], writes=[t_yb])
                            P.op("dve", lambda e, c4=c4: e.scalar_tensor_tensor(out=yb[:], in0=zb[:, c4, 0:512], scalar=convw[:, c4, 0:1],
                                                                                in1=yb[:], op0=ALU.mult, op1=ALU.add),
                                 reads=[t_zb[c4], t_convw, t_yb], writes=[t_yb])
                            P.op("dve", lambda e, c4=c4: e.tensor_tensor(out=oc[:, c4, :], in0=yb[:], in1=cbs[:], op=ALU.mult),
                                 reads=[t_yb, t_cbs], writes=[t_oc[c4]])
                            P.op("dve", lambda e, c4=c4: e.tensor_copy(out=zb[:, c4, 0:2], in_=zb[:, c4, 512:514]),
                                 reads=[t_zb[c4]], writes=[t_zb[c4]])
                            P.op("act", lambda e, c4=c4: e.activation(out=osq[:, c4, :], in_=oc[:, c4, :], func=AF.Square),
                                 reads=[t_oc[c4]], writes=[t_osq[c4]])
                        for c4 in range(4):
                            P.op("pe", lambda e, c4=c4: e.matmul(ps[3][:, :], lhsT=onesb[:, :], rhs=osq[:, c4, :],
                                                                 start=(c4 == 0), stop=(c4 == 3)),
                                 reads=[t_onesb, t_osq[c4]], writes=[tps[3]])
                        P.op("dve", lambda e: e.tensor_scalar(out=rbc[:], in0=ps[3][:, :], scalar1=1.0 / 512, scalar2=EPS,
                                                              op0=ALU.mult, op1=ALU.add), reads=[tps[3]], writes=[t_rbc])
                        P.op("act", lambda e: e.activation(out=rbc[:], in_=rbc[:], func=AF.Sqrt), reads=[t_rbc], writes=[t_rbc])
                        P.op("dve", lambda e: e.reciprocal(out=rbc[:], in_=rbc[:]), reads=[t_rbc], writes=[t_rbc])
                        for c4 in range(4):
                            P.op("dve", lambda e, c4=c4: e.scalar_tensor_tensor(
                                out=mixT[:, 4 + c4, s0:s0 + 512], in0=oc[:, c4, :], scalar=gconv[:, c4:c4 + 1], in1=rbc[:],
                                op0=ALU.mult, op1=ALU.mult), reads=[t_oc[c4], t_gconv, t_rbc], writes=[t_mixc[grp]])
                        for tl in range(4):
                            ti = grp * 4 + tl
                            bi = 4 + (tl % 2)
                            for k in range(8):
                                P.op("pe", lambda e, k=k, tl=tl, bi=bi: e.matmul(
                                    ps[bi][:, 0:280], lhsT=hT[:, k, tl * 128:(tl + 1) * 128], rhs=win[:, k, 2560:2840],
                                    start=(k == 0), stop=(k == 7)), reads=[t_win, t_hT], writes=[tps[bi]])
                            P.op("act", lambda e, ti=ti, bi=bi: e.copy(
                                out=vs_aug[:, ti, :, 0:64], in_=ps[bi][:, 0:128].rearrange("p (g d) -> p g d", g=2)),
                                reads=[tps[bi]], writes=[t_va[ti]])
                            P.op("act", lambda e, ti=ti, bi=bi: e.copy(
                                out=vw_aug[:, ti, :, 0:64], in_=ps[bi][:, 128:256].rearrange("p (g d) -> p g d", g=2)),
                                reads=[tps[bi]], writes=[t_va[ti]])
                            P.op("act", lambda e, ti=ti, bi=bi: e.activation(out=gat[:, ti, :], in_=ps[bi][:, 256:280],
                                                                             func=AF.Sigmoid),
                                 reads=[tps[bi]], writes=[t_gat[ti]])
                    if s == 0:
                        dbgdump("gat", [128, NT, 24], F32, gat[:], t_gat)
                        dbgdump("vs", [128, NT, 2, 65], BF16, vs_aug[:], t_va + [t_ones])
                        if stop_after <= 1:
                            dbgdump("mixT", [128, 8, S], BF16, mixT[:], t_mixc + t_mixa)
                if stop_after <= 1:
                    continue

                with Scope():
                    bd1 = {}; bd2 = {}; pe2 = {}; t_cw = Tok()
                    for n in "kv":
                        bd1[n] = sb("bd1" + n, [128, 32, 128], BF16)
                        P.dma("pool", bd1[n][:], bd1_d[n], writes=[t_cw])
                        bd2[n] = sb("bd2" + n, [128, 128], BF16)
                        P.dma("pool", bd2[n][:], bd2_d[n], writes=[t_cw])
                        pe2[n] = sb("pe2" + n, [128, 32], BF16)
                        P.dma("pool", pe2[n][:], pe2_d[n], writes=[t_cw])
                    Bw = sb("Bw", [128, 5, 8, 128], BF16); t_Bw = Tok()
                    Bs = sb("Bs", [128, 3, 8, 128], BF16); t_Bs = Tok()
                    for dl in range(5):
                        src = bass.AP(fw_d.tensor, 128 + dl * 128, [[LW, 128], [128 * (LW + 1), 8], [1, 128]])
                        P.dma("pool", Bw[:, dl, :, :], src, reads=[t_fw], writes=[t_Bw])
                    for dl in range(3):
                        src = bass.AP(fw_d.tensor, 8 * 128 * (LW + 1) + 128 + dl * 128,
                                      [[LW, 128], [128 * (LW + 1), 8], [1, 128]])
                        P.dma("pool", Bs[:, dl, :, :], src, reads=[t_fw], writes=[t_Bs])
                    P.op("act", lambda e: e.activation(out=Bw[:], in_=Bw[:], func=AF.Exp), reads=[t_Bw], writes=[t_Bw])
                    P.op("act", lambda e: e.activation(out=Bs[:], in_=Bs[:], func=AF.Exp), reads=[t_Bs], writes=[t_Bs])
                    if False:
                        dbgdump("Bw", [128, 5, 8, 128], BF16, Bw[:], [t_Bw])
                        dbgdump("Bs", [128, 3, 8, 128], BF16, Bs[:], [t_Bs])
                    esel, t_esel = load_const("esel", esel_d, [128, 2048], BF16, "pool")
                    selmul, t_selmul = load_const("selmul", selmul_d, [128, 16, 32])
                    seladd, t_seladd = load_const("seladd", seladd_d, [128, 16, 32])
                    gattn, t_gattn = load_const("gattn", gattn_d, [128, 512])
                    cbias = sb("cbias", [128, 2]); t_cbias = Tok()
                    for i, n in enumerate("kv"):
                        for l in range(32):
                            P.op("pe", lambda e, n=n, l=l, i=i: e.matmul(ps[2][:, i:i + 1], lhsT=bd1[n][:, l, :],
                                                                         rhs=pe2[n][:, l:l + 1], start=(l == 0), stop=(l == 31)),
                                 reads=[t_cw], writes=[tps[2]])
                    P.op("dve", lambda e: e.tensor_copy(out=cbias[:], in_=ps[2][:, 0:2]), reads=[tps[2]], writes=[t_cbias])
                    kcmpT = sb("kcmpT", [128, 128], BF16); t_kcmp = Tok()
                    vc_aug = sb("vc_aug", [128, 2, 97], BF16); t_vca = Tok()
                    for g in range(2):
                        P.dma("pool", vc_aug[0:127, g, 65:97], cov_d, writes=[t_vca])
                    P.op("dve", lambda e: e.memset(vc_aug[:, :, 64:65], 1.0), writes=[t_vca])
                    hid = {n: sb("hid" + n, [128, 128], BF16) for n in "kv"}; t_hid = Tok()
                    for i, (n, srcn) in enumerate((("k", "kc"), ("v", "vc"))):
                        src = kT[srcn]
                        for l in range(32):
                            P.op("pe", lambda e, n=n, l=l, i=i, src=src: e.matmul(
                                ps[i][:, 0:127], lhsT=bd1[n][:, l, :], rhs=src[:, l:l + 2017:16],
                                start=(l == 0), stop=(l == 31)), reads=[t_cw] + t_kT[srcn], writes=[tps[i]])
                        P.op("act", lambda e, n=n, i=i: e.activation(out=hid[n][:, 0:127], in_=ps[i][:, 0:127],
                                                                     func=AF.Gelu_apprx_tanh, bias=cbias[:, i:i + 1]),
                             reads=[tps[i], t_cbias], writes=[t_hid])
                    P.op("pe", lambda e: e.matmul(ps[0][:, 0:127], lhsT=bd2["k"][:, :], rhs=hid["k"][:, 0:127], start=True, stop=True),
                         reads=[t_cw, t_hid], writes=[tps[0]])
                    P.op("dve", lambda e: e.tensor_copy(out=kcmpT[:, 0:127], in_=ps[0][:, 0:127]), reads=[tps[0]], writes=[t_kcmp])
                    P.op("pe", lambda e: e.matmul(ps[1][0:127, 0:128], lhsT=hid["v"][:, 0:127], rhs=bd2["v"][:, :], start=True, stop=True),
                         reads=[t_cw, t_hid], writes=[tps[1]])
                    P.op("dve", lambda e: e.tensor_copy(out=vc_aug[0:127, :, 0:64],
                                                        in_=ps[1][0:127, 0:128].rearrange("p (g d) -> p g d", g=2)),
                         reads=[tps[1]], writes=[t_vca])
                    if s == 0:
                        dbgdump("kcmpT", [128, 128], BF16, kcmpT[:], [t_kcmp])
                        dbgdump("vc_aug", [128, 2, 97], BF16, vc_aug[:], [t_vca])

                    bc_all = sb("bc_all", [128, NT, 8, 128], BF16); t_bcq = [Tok() for _ in range(NT)]
                    for q_ in range(NT):
                        src = bass.AP(fc_d.tensor, 2048 + 128 * q_ - 31, [[LC, 127], [127 * (LC + 16), 8], [1, 128]])
                        P.dma("pool", bc_all[0:127, q_, :, :], src, reads=[t_fc], writes=[t_bcq[q_]])

                    def bc_exp(q_):
                        P.op("act", lambda e: e.activation(out=bc_all[0:127, q_, :, :], in_=bc_all[0:127, q_, :, :], func=AF.Exp),
                             reads=[t_bcq[q_]], writes=[t_bcq[q_]])
                    bc_exp(0)
                    NSB = 4
                    sbank = [0, 1, 2, 6]
                    tS = [sb(f"tS{i}", [128, 512], BF16) for i in range(NSB)]; t_tS = [Tok() for _ in range(NSB)]
                    pT = [sb(f"pT{i}", [128, 512], BF16) for i in range(NSB)]; t_pT = [Tok() for _ in range(NSB)]
                    o_acc = [sb(f"oacc{i}", [128, 8, 64]) for i in range(2)]; t_oacc = [Tok(), Tok()]
                    rs4 = sb("rs4", [128, 4]); wg4 = sb("wg4", [128, 4]); t_rs = Tok()
                    otmp = sb("otmp", [128, 4, 64]); t_otmp = Tok()
                    itmp = sb("itmp", [128, 4, 32]); imp = sb("imp", [128, 32]); t_imp = Tok()
                    m8 = sb("m8", [128, 8]); t_m8 = Tok()
                    negsel = [sb(f"negsel{i}", [128, 128], BF16) for i in range(2)]; t_negsel = [Tok(), Tok()]
                    for i in range(2):
                        P.op("dve", lambda e, i=i: e.memset(negsel[i][:], 0.0), writes=[t_negsel[i]])
                    nsT4 = [sb(f"nsT4{i}", [128, 4, 128], BF16) for i in range(2)]; t_nsT = [Tok(), Tok()]
                    junk2 = sb("junk2", [128, 512], BF16); t_junk2 = Tok()
                    on_b = sb("on_b", [128, 512], BF16); t_onb = Tok()
                    tpsb_a = tpsb
                    BK = {0: 0, 2: 1, 1: 2}
                    tpo = {br: [Tok()] for br in range(3)}

                    def evac_branch(g, br, qi, oa, t_oa):
                        c0 = 0
                        rd = tpo[br]
                        pob = psO[:, BK[br], :].rearrange("p (j c) -> p j c", j=4)
                        if br == 0:
                            P.op("dve", lambda e: e.tensor_scalar(out=rs4[:], in0=pob[:, :, 64], scalar1=1e-30, scalar2=None,
                                                                  op0=ALU.max), reads=rd, writes=[t_rs])
                            P.op("dve", lambda e: e.reciprocal(out=rs4[:], in_=rs4[:]), reads=[t_rs], writes=[t_rs])
                        else:
                            P.op("dve", lambda e: e.reciprocal(out=rs4[:], in_=pob[:, :, 64]), reads=rd, writes=[t_rs])
                        g0_ = 12 * g + br
                        P.op("dve", lambda e: e.tensor_tensor(out=wg4[:], in0=rs4[:], in1=gat[:, qi, g0_:g0_ + 10:3], op=ALU.mult),
                             reads=[t_rs, t_gat[qi]], writes=[t_rs])
                        if br == 0:
                            P.op("dve", lambda e: e.tensor_tensor(
                                out=oa[:, 4 * g:4 * g + 4, :], in0=pob[:, :, 0:64],
                                in1=wg4[:].unsqueeze(2).broadcast_to([128, 4, 64]), op=ALU.mult),
                                reads=rd + [t_rs], writes=[t_oa])
                            P.op("dve", lambda e: e.tensor_tensor(
                                out=itmp[:], in0=pob[:, :, 65:97], in1=rs4[:].unsqueeze(2).broadcast_to([128, 4, 32]),
                                op=ALU.mult), reads=rd + [t_rs], writes=[t_imp])
                            P.op("dve", lambda e: e.tensor_reduce(out=imp[:], in_=itmp[:].rearrange("p j n -> p n j"),
                                                                  axis=AX.X, op=ALU.add), reads=[t_imp], writes=[t_imp])
                        else:
                            P.op("dve", lambda e: e.tensor_tensor(
                                out=otmp[:], in0=pob[:, :, 0:64],
                                in1=wg4[:].unsqueeze(2).broadcast_to([128, 4, 64]), op=ALU.mult),
                                reads=rd + [t_rs], writes=[t_otmp])
                            P.op("pool", lambda e: e.tensor_tensor(out=oa[:, 4 * g:4 * g + 4, :], in0=oa[:, 4 * g:4 * g + 4, :],
                                                                   in1=otmp[:], op=ALU.add),
                                 reads=[t_otmp, t_oa], writes=[t_oa])

                    def selection(g, qi, it):
                        ns, t_ns = negsel[it % 2], t_negsel[it % 2]
                        nT, t_nT = nsT4[it % 2], t_nsT[it % 2]
                        P.op("dve", lambda e: e.tensor_tensor(out=imp[:], in0=imp[:], in1=selmul[:, qi, :], op=ALU.mult),
                             reads=[t_imp, t_selmul], writes=[t_imp])
                        P.op("dve", lambda e: e.tensor_tensor(out=imp[:], in0=imp[:], in1=seladd[:, qi, :], op=ALU.add),
                             reads=[t_imp, t_seladd], writes=[t_imp])
                        P.op("dve", lambda e: e.max(out=m8[:], in_=imp[:]), reads=[t_imp], writes=[t_m8])
                        P.op("dve", lambda e: e.tensor_scalar(out=ns[:, g * 64:g * 64 + 32], in0=imp[:], scalar1=m8[:, 7:8],
                                                              scalar2=NEG, op0=ALU.is_lt, op1=ALU.mult),
                             reads=[t_imp, t_m8], writes=[t_ns])

                        def later():
                            P.op("pe", lambda e: e.transpose(out=psb[:, 0:128], in_=ns[:, :], identity=identb[:]),
                                 reads=[t_ns, t_identb], writes=[tpsb_a])
                            P.op("act", lambda e: e.copy(out=nT[:], in_=psb[:, 0:128].unsqueeze(1).broadcast_to([128, 4, 128])),
                                 reads=[tpsb_a], writes=[t_nT])
                            sel_ready.add(it)
                        deferred.append([cur_n[0] + 8, later])

                    def finish_qi(qi, oa, t_oa):
                        gi = s * NT + qi
                        if dbg and s == 0:
                            if qi == 0:
                                dbg_out["oattn"] = dout("dbg_oattn", [NT, 128, 512], F32)
                            P.dma("sp", dbg_out["oattn"][qi], oa[:].rearrange("p h d -> p (h d)"), reads=[t_oa])
                        oaf = oa[:].rearrange("p h d -> p (h d)")
                        rms_stats(oaf, t_oa, junk2[:], t_junk2, ssqa, rstda, t_sta[gi], gi, 512)
                        P.op("dve", lambda e: e.scalar_tensor_tensor(
                            out=on_b[:], in0=oaf, scalar=rstda[:, gi:gi + 1], in1=gattn[:], op0=ALU.mult, op1=ALU.mult),
                            reads=[t_oa, t_sta[gi], t_gattn], writes=[t_onb])

                        def later():
                            for c in range(4):
                                P.op("pe", lambda e, c=c: e.transpose(out=psb[:, 512 + c * 128:512 + (c + 1) * 128],
                                                                      in_=on_b[:, c * 128:(c + 1) * 128], identity=identb[:]),
                                     reads=[t_onb, t_identb], writes=[tpsb])
                            for c in range(4):
                                evac(1, mixT[:, c, qi * 128:(qi + 1) * 128], psb[:, 512 + c * 128:512 + (c + 1) * 128],
                                     [tpsb], [t_mixa[qi]])
                        deferred.append([cur_n[0] + 8, later])

                    deferred = []
                    cur_n = [0]
                    sel_ready = set()
                    tiles = []
                    groups_ = []
                    it = 0
                    for qi in range(NT):
                        oa, t_oa = o_acc[qi % 2], t_oacc[qi % 2]
                        for g in range(2):
                            pr = slice(0, 128)
                            rhs_q = qTg[g][:, :, qi * 128:(qi + 1) * 128]
                            rd_q = [t_qT[qi // 4]]
                            hs4 = slice(4 * g, 4 * g + 4)
                            bc, t_bc = bc_all[:, qi, :, :], t_bcq[qi]
                            pre = None
                            if g == 0 and qi + 1 < NT:
                                def pre(qi=qi):
                                    bc_exp(qi + 1)
                            tiles.append(dict(
                                pre=pre, rows=127, br=0, first=True, last=True, ncol=97,
                                mm=[(kcmpT[pr, 0:127], rhs_q, rd_q + [t_kcmp])],
                                bias=bc[0:127, hs4, :], t_bias=t_bc, v=vc_aug[0:127, g, :], v_rd=[t_vca],
                                post=(lambda g=g, qi=qi, oa=oa, t_oa=t_oa, it=it: (evac_branch(g, 0, qi, oa, t_oa),
                                                                                   selection(g, qi, it)))))
                            k0 = max(0, qi - 4)
                            for kj in range(k0, qi + 1):
                                tiles.append(dict(
                                    pre=None, rows=128, br=2, first=(kj == k0), last=(kj == qi), ncol=65,
                                    mm=[(kT["kw"][pr, kj * 128:(kj + 1) * 128], rhs_q, rd_q + [t_kT["kw"][kj // 4]])],
                                    bias=Bw[:, qi - kj, hs4, :], t_bias=t_Bw, v=vw_aug[:, kj, g, :], v_rd=[t_va[kj], t_ones],
                                    post=(lambda g=g, qi=qi, oa=oa, t_oa=t_oa: evac_branch(g, 2, qi, oa, t_oa)) if kj == qi else None))
                            nT, t_nT = nsT4[it % 2], t_nsT[it % 2]
                            for kj in range(qi + 1):
                                post = None
                                if kj == qi:
                                    if g == 1:
                                        post = (lambda g=g, qi=qi, oa=oa, t_oa=t_oa: (evac_branch(g, 1, qi, oa, t_oa),
                                                                                      finish_qi(qi, oa, t_oa)))
                                    else:
                                        post = (lambda g=g, qi=qi, oa=oa, t_oa=t_oa: evac_branch(g, 1, qi, oa, t_oa))
                                tiles.append(dict(
                                    need=it,
                                    pre=None, rows=128, br=1, first=(kj == 0), last=(kj == qi), ncol=65,
                                    mm=[(kT["ks"][pr, kj * 128:(kj + 1) * 128], rhs_q, rd_q + [t_kT["ks"][kj // 4]]),
                                        (esel[pr, kj * 128:(kj + 1) * 128], nT[pr, :, :], [t_esel, t_nT])],
                                    bias=Bs[:, min(qi - kj, 2), hs4, :], t_bias=t_Bs, v=vs_aug[:, kj, g, :],
                                    v_rd=[t_va[kj], t_ones], post=post))
                            it += 1
                            groups_.append(tiles)
                            tiles = []
                    cmp_ = [g_[0] for g_ in groups_]
                    win_ = [[t_ for t_ in g_[1:] if t_["br"] == 2] for g_ in groups_]
                    sel_ = [[t_ for t_ in g_[1:] if t_["br"] == 1] for g_ in groups_]
                    tiles = [cmp_[0]]
                    for i_ in range(len(groups_)):
                        tiles += win_[i_]
                        if i_ + 1 < len(groups_):
                            tiles.append(cmp_[i_ + 1])
                        tiles += sel_[i_]

                    def emit_S(tl, sl):
                        if tl["pre"] is not None:
                            tl["pre"]()
                        rows = tl["rows"]
                        nmm = len(tl["mm"])
                        for m, (l_ap, r_ap, rd) in enumerate(tl["mm"]):
                            P.op("pe", lambda e, l_ap=l_ap, r_ap=r_ap, m=m: e.matmul(
                                ps[sbank[sl]][0:rows, :].rearrange("p (j q) -> p j q", j=4), lhsT=l_ap, rhs=r_ap,
                                start=(m == 0), stop=(m == nmm - 1)), reads=rd, writes=[tps[sbank[sl]]])
                        P.op("act", lambda e: e.activation(out=tS[sl][0:rows, :], in_=ps[sbank[sl]][0:rows, :], func=AF.Exp,
                                                           scale=0.125), reads=[tps[sbank[sl]]], writes=[t_tS[sl]])
                        P.op("dve", lambda e: e.tensor_tensor(
                            out=pT[sl][0:rows, :].rearrange("p (j q) -> p j q", j=4),
                            in0=tS[sl][0:rows, :].rearrange("p (j q) -> p j q", j=4),
                            in1=tl["bias"], op=ALU.mult), reads=[t_tS[sl], tl["t_bias"]], writes=[t_pT[sl]])

                    def emit_PV(tl, sl):
                        rows = tl["rows"]
                        bk = BK[tl["br"]]
                        for j in range(4):
                            P.op("pe", lambda e, j=j: e.matmul(psO[:, bk, j * 128:j * 128 + tl["ncol"]],
                                                               lhsT=pT[sl][0:rows, j * 128:(j + 1) * 128], rhs=tl["v"],
                                                               start=(tl["first"] and j == 0), stop=(tl["last"] and j == 3),
                                                               skip_group_check=True),
                                 reads=[t_pT[sl]] + tl["v_rd"], writes=tpo[tl["br"]])
                        if tl["post"] is not None:
                            tl["post"]()

                    ntl = len(tiles)
                    LA = 2
                    nxt = 0
                    for n_ in range(ntl):
                        cur_n[0] = n_
                        while True:
                            for d_ in [d for d in deferred if d[0] <= n_]:
                                deferred.remove(d_)
                                d_[1]()
                            progressed = False
                            while nxt < ntl and nxt <= n_ + LA and (tiles[nxt].get("need") is None
                                                                   or tiles[nxt]["need"] in sel_ready):
                                emit_S(tiles[nxt], nxt % NSB)
                                nxt += 1
                                progressed = True
                            if nxt > n_:
                                break
                            d_ = min(deferred, key=lambda d: d[0])
                            deferred.remove(d_)
                            d_[1]()
                        emit_PV(tiles[n_], n_ % NSB)
                    for d_ in sorted(deferred, key=lambda d: d[0]):
                        d_[1]()
                    deferred.clear()

                    if s == 0:
                        dbgdump("mixT", [128, 8, S], BF16, mixT[:], t_mixc + t_mixa)
                if stop_after <= 2:
                    continue

                with Scope():
                    wout = sb("wout", [128, 8, 1024], BF16); t_wout = Tok()
                    wov = w_out_d.rearrange("(k p) n -> p k n", p=128)
                    for k in range(8):
                        P.dma("pool", wout[:, k, :], wov[:, k, :], writes=[t_wout])
                    gt1 = sb("gt1", [128, 1024]); t_gt1 = Tok()
                    P.dma("sp", gt1[:], gt_d[0, s], reads=[t_gtd], writes=[t_gt1])
                    xpool = [sb(f"xt3{i}", [128, 1024]) for i in range(2)]; t_xp = [Tok(), Tok()]
                    x1p = [sb(f"x1t{i}", [128, 1024]) for i in range(2)]; t_x1p = [Tok(), Tok()]
                    junk = sb("junk3", [128, 1024], BF16); t_junk = Tok()
                    xs = sb("xs3", [128, 1024], BF16); t_xs = Tok()
                    h2st = [sb(f"h2st{i}", [128, 8, 128], BF16) for i in range(2)]; t_h2st = [Tok(), Tok()]
                    for ti in range(NT):
                        gi = s * NT + ti
                        xt, t_xt = xpool[ti % 2], t_xp[ti % 2]
                        x1t, t_x1t = x1p[ti % 2], t_x1p[ti % 2]
                        P.dma("sp", xt[:], x_d[gi * 128:(gi + 1) * 128, :], writes=[t_xt])
                        for half in range(2):
                            for k in range(8):
                                P.op("pe", lambda e, k=k, half=half: e.matmul(
                                    ps[half][:, :], lhsT=mixT[:, k, ti * 128:(ti + 1) * 128],
                                    rhs=wout[:, k, half * 512:(half + 1) * 512], start=(k == 0), stop=(k == 7)),
                                    reads=[t_wout, t_mixa[ti], t_mixc[ti // 4]], writes=[tps[half]])
                            hs = slice(half * 512, (half + 1) * 512)
                            P.op("dve", lambda e, half=half, hs=hs: e.tensor_tensor(out=x1t[:, hs], in0=ps[half][:, :], in1=gt1[:, hs],
                                                                                    op=ALU.mult),
                                 reads=[tps[half], t_gt1], writes=[t_x1t])
                        P.op("pool", lambda e: e.tensor_tensor(out=x1t[:], in0=x1t[:], in1=xt[:], op=ALU.add),
                             reads=[t_x1t, t_xt], writes=[t_x1t])
                        P.dma("sp", x1_d[gi * 128:(gi + 1) * 128, :], x1t[:], reads=[t_x1t], writes=[t_x1d[gi]])
                        hs_, t_hs = h2st[ti % 2], t_h2st[ti % 2]
                        norm_transpose(x1t, t_x1t, gi, s, ssq2, rstd2, t_st2, gs2, 24, hs_, t_hs, 0, junk, t_junk, xs, t_xs)
                        P.dma("sp", h2T_d[:, :, gi * 128:(gi + 1) * 128], hs_[:], reads=[t_hs], writes=[t_h2d[gi]])

        if stop_after <= 3:
            P.wait_all("sp", Tok.registry)
            return nc, dbg_out

        try:
            utb_d = dscr("utb_s", [16384, 1024], BF16)
            vb_d = dscr("vb_s", [16384, 1024], BF16)
            s2_d = dscr("s2_s", [16, T, 128], BF16)
            t_utb = [Tok() for _ in range(16)]
            t_vb = [Tok() for _ in range(16)]
            with Scope():
                stg = [sb(f"cst{i}", [128, 8, 1024], BF16) for i in range(2)]; t_stg = [Tok(), Tok()]
                n = 0
                for src_d, dst_d, tks in ((ut_d, utb_d, t_utb), (pv_d, vb_d, t_vb)):
                    for c in range(16):
                        i = n % 2; n += 1
                        sv = src_d[c * 1024:(c + 1) * 1024, :].rearrange("(c p) n -> p c n", p=128)
                        dv = dst_d[c * 1024:(c + 1) * 1024, :].rearrange("(c p) n -> p c n", p=128)
                        P.dma("pool", stg[i][:], sv, writes=[t_stg[i]])
                        P.dma("sp", dv, stg[i][:], reads=[t_stg[i]], writes=[tks[c]])
            if stop_after == 3.5:
                raise _Stop()

            with Scope():
                wq, t_wq = None, Tok()
                wq = sb("wq", [128, 8, 1024], BF16)
                wqv = wq_d.rearrange("(k p) n -> p k n", p=128)
                for k in range(8):
                    P.dma("pool", wq[:, k, :], wqv[:, k, :], writes=[t_wq])
                kb, t_kb = load_const("kb", kb_d, [128, 8, 256], BF16, "pool")
                identf, t_identf = load_const("identf", identf_d, [128, 128])
                iota, t_iota = load_const("iota", iota_d, [128, 128])
                selh, t_selh = load_const("selh", selh_d, [16, 128], BF16, "pool")
                gfin, t_gfin = load_const("gfin", gfin_d, [128, 1024])
                gt2 = sb("gt2", [128, 1024]); t_gt2 = Tok()
                Wbuf = sb("Wbuf", [128, 256, 128], BF16); t_W = [Tok() for _ in range(64)]
                h2g = sb("h2g", [128, 8, 256], BF16); t_h2g = Tok()
                qTp = sb("qTp", [128, 8, 256], BF16); t_qTp = Tok()
                s_sb = sb("s_sb", [128, 8, 256]); t_s = Tok()
                work = sb("work", [128, 8, 256]); t_work = Tok()
                v16 = sb("v16", [128, 8, 2, 16]); t_v16 = Tok()
                idx = sb("idx", [128, 8, 16], U32); t_idx = Tok()
                cand = sb("cand", [128, 8, 256]); t_cand = Tok()
                cwork = sb("cwork", [128, 256]); t_cwork = Tok()
                ts16 = sb("ts16", [128, 8, 16]); t_ts = Tok()
                d16 = sb("d16", [128, 8, 16]); t_d16 = Tok()
                zz = sb("zz", [128, 8]); mu = sb("mu", [128, 8]); tauE = sb("tauE", [128, 8]); t_zz = Tok()
                tm3 = sb("tm3", [128, 3, 128]); t_tm3 = Tok()
                s2hm = [sb(f"s2hm{i}", [16, 16 * 128], BF16) for i in range(2)]; t_s2hm = [Tok(), Tok()]
                s2hb = sb("s2hb", [128, 2, 8, 128], BF16); t_s2hb = Tok()
                NRQ = 3
                e4 = [sb(f"e4{i}", [128, 4, 128], BF16) for i in range(NRQ)]; t_e4 = [Tok() for _ in range(NRQ)]
                Rt4 = [sb(f"Rt4{i}", [128, 4, 128], BF16) for i in range(NRQ)]; t_Rt4 = [Tok() for _ in range(NRQ)]
                Lt4 = [sb(f"Lt4{i}", [128, 4, 128], BF16) for i in range(NRQ)]; t_Lt4 = [Tok() for _ in range(NRQ)]
                mk4 = [sb(f"mk4{i}", [128, 4, 128], BF16) for i in range(NRQ)]; t_mk4 = [Tok() for _ in range(NRQ)]
                NC = 5
                utc = [sb(f"utc{i}", [128, 8, 128], BF16) for i in range(NC)]; t_utc = [Tok() for _ in range(NC)]
                vch = [sb(f"vch{i}", [128, 1024], BF16) for i in range(NC)]; t_vch = [Tok() for _ in range(NC)]
                abuf = [sb(f"abuf{i}", [128, 256], BF16) for i in range(2)]; t_ab = [Tok(), Tok()]
                wab = [sb(f"wab{i}", [128, 256], BF16) for i in range(2)]; t_wab = [Tok(), Tok()]
                x1t = sb("x1f", [128, 1024]); t_x1t = Tok()
                yt = sb("yf", [128, 1024]); t_yt = Tok()
                ot = sb("of", [128, 1024]); t_ot = Tok()
                junk = sb("junkf", [128, 1024], BF16); t_junk = Tok()
                ssq3 = sb("ssq3", [128, NTT]); rstd3 = sb("rstd3", [128, NTT]); t_st3 = [Tok() for _ in range(NTT)]
                t_s2d = Tok()
                t_out = Tok()

                def top16(dst_lo, dst_hi, src, wrk, rd, wr_dst, wr_wrk):
                    P.op("dve", lambda e: e.max(out=dst_lo, in_=src), reads=rd, writes=[wr_dst])
                    P.op("dve", lambda e: e.match_replace(out=wrk, in_to_replace=dst_lo, in_values=src, imm_value=-1e30),
                         reads=rd + [wr_dst], writes=[wr_wrk])
                    P.op("dve", lambda e: e.max(out=dst_hi, in_=wrk), reads=[wr_wrk], writes=[wr_dst])

                ngroups = T // 256
                s2hbb = [s2hb, sb("s2hb2", [128, 2, 8, 128], BF16)]; t_s2hbb = [t_s2hb, Tok()]
                psX = psb[:, :].bitcast(F32)
                PB = [(ps[2], tps[2]), (psX, tpsb)]
                h2gb = [h2g, sb("h2g2", [128, 8, 256], BF16)]; t_h2gb = [t_h2g, Tok()]
                tT3b = [[sb(f"tT3_{i}_{j}", [128, 3, 128]) for j in range(2)] for i in range(2)]
                t_tT3b = [[Tok() for j in range(2)] for i in range(2)]

                def prep_steps(gidx):
                    g0 = gidx * 256
                    hb, t_hb = h2gb[gidx % 2], t_h2gb[gidx % 2]
                    steps = []

                    def s_q(hh):
                        def f():
                            if hh == 0:
                                P.dma("sp", hb[:], h2T_d[:, :, g0:g0 + 256], reads=t_h2d[gidx * 2:gidx * 2 + 2], writes=[t_hb])
                            for h in (hh, hh + 1):
                                bk, tb = PB[h % 2]
                                for k in range(8):
                                    P.op("pe", lambda e, k=k, h=h, bk=bk: e.matmul(bk[:, 0:256], lhsT=wq[:, k, h * 128:(h + 1) * 128],
                                                                                   rhs=hb[:, k, :], start=(k == 0), stop=(k == 7)),
                                         reads=[t_wq, t_hb], writes=[tb])
                                P.op("act", lambda e, h=h, bk=bk: e.copy(out=qTp[:, h, :], in_=bk[:, 0:256]),
                                     reads=[tb], writes=[t_qTp])
                        return f
                    for hh in (0, 2, 4, 6):
                        steps.append((f"q{hh}", s_q(hh)))

                    for tt in range(2):
                        tsl = slice(tt * 128, (tt + 1) * 128)
                        tT3, t_tT3 = tT3b[gidx % 2][tt], t_tT3b[gidx % 2][tt]
                        s2hb, t_s2hb = s2hbb[tt], t_s2hbb[tt]

                        def sA(tsl=tsl):
                            for hp in range(4):
                                bk, tb = PB[hp % 2]
                                for h in (2 * hp, 2 * hp + 1):
                                    P.op("pe", lambda e, h=h, bk=bk: e.matmul(bk[:, (h % 2) * 256:(h % 2 + 1) * 256], lhsT=qTp[:, h, tsl],
                                                                              rhs=kb[:, h, :], start=True, stop=True),
                                         reads=[t_qTp, t_kb], writes=[tb])
                                P.op("act", lambda e, hp=hp, bk=bk: e.copy(
                                    out=s_sb[:, 2 * hp:2 * hp + 2, :], in_=bk[:, :].rearrange("p (h n) -> p h n", h=2)),
                                    reads=[tb], writes=[t_s])

                        def sB(s2hb=s2hb, t_s2hb=t_s2hb):
                            for h in range(8):
                                for p in range(2):
                                    top16(v16[:, h, p, 0:8], v16[:, h, p, 8:16], s_sb[:, h, p * 128:(p + 1) * 128],
                                          work[:, h, p * 128:(p + 1) * 128], [t_s], t_v16, t_work)
                                P.op("dve", lambda e, h=h: e.max_index(out=idx[:, h, 0:8], in_max=v16[:, h, 0, 0:8],
                                                                       in_values=s_sb[:, h, 0:128]),
                                     reads=[t_s, t_v16], writes=[t_idx])
                                P.op("dve", lambda e, h=h: e.max_index(out=idx[:, h, 8:16], in_max=v16[:, h, 0, 8:16],
                                                                       in_values=work[:, h, 0:128]),
                                     reads=[t_work, t_v16], writes=[t_idx])
                            P.op("dve", lambda e: e.tensor_tensor(
                                out=cand[:].rearrange("p h (a b) -> p h a b", a=16),
                                in0=v16[:, :, 0, :].unsqueeze(3).broadcast_to([128, 8, 16, 16]),
                                in1=v16[:, :, 1, :].unsqueeze(2).broadcast_to([128, 8, 16, 16]), op=ALU.add),
                                reads=[t_v16], writes=[t_cand])
                            for h in range(8):
                                top16(ts16[:, h, 0:8], ts16[:, h, 8:16], cand[:, h, :], cwork[:, :], [t_cand], t_ts, t_cwork)

                        def sB2(s2hb=s2hb, t_s2hb=t_s2hb):
                            P.op("dve", lambda e: e.tensor_tensor(out=d16[:], in0=ts16[:], in1=ts16[:, :, 0:1].broadcast_to([128, 8, 16]),
                                                                  op=ALU.subtract), reads=[t_ts], writes=[t_d16])
                            P.op("act", lambda e: e.activation(out=d16[:], in_=d16[:], func=AF.Exp), reads=[t_d16], writes=[t_d16])
                            P.op("dve", lambda e: e.tensor_reduce(out=zz[:], in_=d16[:], axis=AX.X, op=ALU.add),
                                 reads=[t_d16], writes=[t_zz])
                            P.op("act", lambda e: e.activation(out=zz[:], in_=zz[:], func=AF.Ln), reads=[t_zz], writes=[t_zz])
                            P.op("dve", lambda e: e.tensor_tensor(out=mu[:], in0=zz[:], in1=ts16[:, :, 0], op=ALU.add),
                                 reads=[t_zz, t_ts], writes=[t_zz])
                            P.op("dve", lambda e: e.tensor_scalar(out=tauE[:], in0=ts16[:, :, 15], scalar1=-1e-4, scalar2=None,
                                                                  op0=ALU.add), reads=[t_ts], writes=[t_zz])
                            P.op("dve", lambda e: e.tensor_tensor(
                                out=tm3[:, 0, :].rearrange("p (h a) -> p h a", h=8),
                                in0=tauE[:].unsqueeze(2).broadcast_to([128, 8, 16]), in1=v16[:, :, 0, :], op=ALU.subtract),
                                reads=[t_zz, t_v16], writes=[t_tm3])
                            P.op("dve", lambda e: e.tensor_tensor(
                                out=tm3[:, 1, :].rearrange("p (h a) -> p h a", h=8),
                                in0=v16[:, :, 0, :], in1=mu[:].unsqueeze(2).broadcast_to([128, 8, 16]), op=ALU.subtract),
                                reads=[t_zz, t_v16], writes=[t_tm3])
                            P.op("dve", lambda e: e.tensor_copy(out=tm3[:, 2, :], in_=idx[:].rearrange("p h a -> p (h a)")),
                                 reads=[t_idx], writes=[t_tm3])
                            P.op("dve", lambda e: e.tensor_copy(out=s2hb[:, 0, :, :], in_=s_sb[:, :, 128:256]),
                                 reads=[t_s], writes=[t_s2hb])
                            P.op("dve", lambda e: e.tensor_tensor(out=s2hb[:, 1, :, :], in0=s_sb[:, :, 128:256], in1=s2hb[:, 0, :, :],
                                                                  op=ALU.subtract), reads=[t_s, t_s2hb], writes=[t_s2hb])

                        def sC(tt=tt, tT3=tT3, t_tT3=t_tT3):
                            bk, tb = PB[0]
                            for w3 in range(3):
                                P.op("pe", lambda e, w3=w3: e.transpose(out=bk[:, w3 * 128:(w3 + 1) * 128], in_=tm3[:, w3, :],
                                                                        identity=identf[:]),
                                     reads=[t_tm3, t_identf], writes=[tb])
                            P.op("act", lambda e: e.copy(out=tT3[:], in_=bk[:, 0:384].rearrange("p (w t) -> p w t", w=3)),
                                 reads=[tb], writes=[t_tT3])

                        def sD(tt=tt):
                            P.dma("sp", s2_d[:, g0 + tt * 128:g0 + (tt + 1) * 128, :].rearrange("(w h) t j -> t w h j", w=2),
                                  s2hbb[tt][:], reads=[t_s2hbb[tt]], writes=[t_s2d])
                        steps.append((f"A{tt}", sA))
                        steps.append((f"B{tt}", sB))
                        steps.append((f"E{tt}", sB2))
                        steps.append((f"C{tt}", sC))
                        steps.append((f"D{tt}", sD))
                    return steps

                SCHED = {"q0": 1, "q2": 3, "q4": 5, "q6": 7, "A0": 8, "B0": 10, "E0": 45, "C0": 55, "A1": 58, "B1": 60,
                         "E1": 100, "C1": -1, "D0": -1, "D1": -1}

                for gidx in range(ngroups):
                    g0 = gidx * 256
                    sq = g0 // S
                    if g0 % S == 0:
                        P.dma("sp", gt2[:], gt_d[1, sq], reads=[t_gtd], writes=[t_gt2])
                    if gidx == 0:
                        for _, st_ in prep_steps(0):
                            st_()
                    h2g, t_h2g = h2gb[gidx % 2], t_h2gb[gidx % 2]
                    nxt_steps = prep_steps(gidx + 1) if gidx + 1 < ngroups else []
                    for tt in range(2):
                        gi = gidx * 2 + tt
                        tT3, t_tT3 = tT3b[gidx % 2][tt], t_tT3b[gidx % 2][tt]
                        quads = []
                        for sl in range(8):
                            for q4 in range(4):
                                quads.append((sl, q4))

                        def load_slab(sl):
                            i2 = sl % 2
                            t0 = g0 + tt * 128 + sl * 16
                            P.dma("sp", s2hm[i2][:, :], s2_d[:, t0:t0 + 16, :].rearrange("h t j -> h (t j)"),
                                  reads=[t_s2d], writes=[t_s2hm[i2]])

                        def emit_sel(qn):
                            sl, q4 = quads[qn]
                            i2 = sl % 2
                            if q4 == 0:
                                load_slab(sl)
                            bA = 0 if qn % 2 == 0 else 4
                            bD = 3 if qn % 2 == 0 else 5
                            for bb in (bA, bD):
                                P.op("pe", lambda e, bb=bb: e.matmul(ps[bb][:, :], lhsT=selh[:, :],
                                                                     rhs=s2hm[i2][:, q4 * 512:(q4 + 1) * 512], start=True, stop=True),
                                     reads=[t_selh, t_s2hm[i2]], writes=[tps[bb]])
                            r = qn % NRQ
                            tl0 = sl * 16 + q4 * 4
                            for t in range(4):
                                P.op("act", lambda e, t=t: e.activation(out=e4[r][:, t, :], in_=ps[bA][:, t * 128:(t + 1) * 128],
                                                                        func=AF.Exp, bias=tT3[:, 1, tl0 + t:tl0 + t + 1]),
                                     reads=[tps[bA], t_tT3], writes=[t_e4[r]])
                            P.op("dve", lambda e: e.tensor_tensor(
                                out=mk4[r][:], in0=ps[bD][:, :].rearrange("p (t j) -> p t j", t=4),
                                in1=tT3[:, 0, tl0:tl0 + 4].unsqueeze(2).broadcast_to([128, 4, 128]), op=ALU.is_ge),
                                reads=[tps[bD], t_tT3], writes=[t_mk4[r]])
                            P.op("dve", lambda e: e.tensor_tensor(
                                out=Lt4[r][:], in0=iota[:].unsqueeze(1).broadcast_to([128, 4, 128]),
                                in1=tT3[:, 2, tl0:tl0 + 4].unsqueeze(2).broadcast_to([128, 4, 128]), op=ALU.is_equal),
                                reads=[t_iota, t_tT3], writes=[t_Lt4[r]])
                            P.op("dve", lambda e: e.tensor_tensor(out=Rt4[r][:], in0=mk4[r][:], in1=e4[r][:], op=ALU.mult),
                                 reads=[t_mk4[r], t_e4[r]], writes=[t_Rt4[r]])

                        def emit_w(qn):
                            sl, q4 = quads[qn]
                            r = qn % NRQ
                            wb = 1 + (qn % 2)
                            tb0 = tt * 128 + sl * 16 + q4 * 4
                            for t in range(4):
                                P.op("pe", lambda e, t=t: e.matmul(ps[wb][:, t * 128:(t + 1) * 128], lhsT=Rt4[r][:, t, :],
                                                                   rhs=Lt4[r][:, t, :], start=True, stop=True),
                                     reads=[t_Rt4[r], t_Lt4[r]], writes=[tps[wb]])
                            evac(qn, Wbuf[:, tb0:tb0 + 4, :], ps[wb][:, :].rearrange("p (t i) -> p t i", t=4),
                                 [tps[wb]], [t_W[tb0 // 4]])

                        emit_sel(0)
                        for qn in range(len(quads)):
                            if qn + 1 < len(quads):
                                emit_sel(qn + 1)
                            emit_w(qn)

                    if dbg and gidx == 0:
                        dbgdump("Wbuf", [128, 256, 128], BF16, Wbuf[:], t_W)
                    if stop_after == 3.8:
                        raise _Stop()
                    def ld(i):
                        c = i % NC
                        P.dma("sp", utc[c][:], utb_d[i * 128:(i + 1) * 128, :].rearrange("p (k j) -> p k j", k=8),
                              reads=[t_utb[i // 8]], writes=[t_utc[c]])
                        P.dma("sp", vch[c][:], vb_d[i * 128:(i + 1) * 128, :], reads=[t_vb[i // 8]], writes=[t_vch[c]])

                    def emU(i):
                        c = i % NC
                        ba = i % 2
                        for k in range(8):
                            P.op("pe", lambda e, k=k: e.matmul(ps[ba][:, 0:256], lhsT=utc[c][:, k, :], rhs=h2g[:, k, :],
                                                               start=(k == 0), stop=(k == 7)),
                                 reads=[t_utc[c], t_h2g], writes=[tps[ba]])
                        P.op("act", lambda e: e.activation(out=abuf[ba][:], in_=ps[ba][:, 0:256], func=AF.Gelu_apprx_tanh),
                             reads=[tps[ba]], writes=[t_ab[ba]])
                        P.op("pool", lambda e: e.tensor_tensor(out=wab[ba][:], in0=abuf[ba][:], in1=Wbuf[:, :, i], op=ALU.mult),
                             reads=[t_ab[ba]] + t_W, writes=[t_wab[ba]])

                    def emV(i):
                        c = i % NC
                        ba = i % 2
                        for tt in range(2):
                            for half in range(2):
                                P.op("pe", lambda e, tt=tt, half=half: e.matmul(
                                    psO[:, tt * 2 + half, :], lhsT=wab[ba][:, tt * 128:(tt + 1) * 128],
                                    rhs=vch[c][:, half * 512:(half + 1) * 512], start=(i == 0), stop=(i == 127)),
                                    reads=[t_wab[ba], t_vch[c]], writes=[tps[3 + tt * 2 + half]])

                    PF = NC - 2
                    for i in range(PF):
                        ld(i)
                    emU(0)
                    for i in range(128):
                        if i + PF < 128:
                            ld(i + PF)
                        if i + 1 < 128:
                            emU(i + 1)
                        emV(i)
                        for nm_, fn_ in nxt_steps:
                            if SCHED[nm_] == i:
                                fn_()
                    for nm_, fn_ in nxt_steps:
                        if SCHED[nm_] == -1:
                            fn_()
                    for tt in range(2):
                        gi = gidx * 2 + tt
                        P.dma("sp", x1t[:], x1_d[gi * 128:(gi + 1) * 128, :], reads=[t_x1d[gi]], writes=[t_x1t])
                        for half in range(2):
                            hs = slice(half * 512, (half + 1) * 512)
                            P.op("dve", lambda e, tt=tt, half=half, hs=hs: e.tensor_tensor(
                                out=yt[:, hs], in0=psO[:, tt * 2 + half, :], in1=gt2[:, hs], op=ALU.mult),
                                reads=[tps[3 + tt * 2 + half], t_gt2], writes=[t_yt])
                        if dbg and gidx == 0 and tt == 0:
                            dbgdump("peer0", [128, 1024], F32, yt[:], [t_yt])
                        P.op("pool", lambda e: e.tensor_tensor(out=yt[:], in0=yt[:], in1=x1t[:], op=ALU.add),
                             reads=[t_yt, t_x1t], writes=[t_yt])
                        rms_stats(yt[:], t_yt, junk[:], t_junk, ssq3, rstd3, t_st3[gi], gi, D)
                        P.op("dve", lambda e, gi=gi: e.scalar_tensor_tensor(out=ot[:], in0=yt[:], scalar=rstd3[:, gi:gi + 1], in1=gfin[:],
                                                                            op0=ALU.mult, op1=ALU.mult),
                             reads=[t_yt, t_st3[gi], t_gfin], writes=[t_ot])
                        P.dma("sp", out_d[gi * 128:(gi + 1) * 128, :], ot[:], reads=[t_ot], writes=[t_out])
                    if stop_after == 4 and gidx == 0:
                        raise _Stop()

        except _Stop:
            pass
        P.wait_all("sp", Tok.registry)
        print("ops per engine", P.ecount, "waits", P.nwaits)
    return nc, dbg_out


_NC_CACHE = {}


def kernel(**inputs):
    inp = {k: np.asarray(v) for k, v in inputs.items()}
    sh = _prep(inp)
    if "nc" not in _NC_CACHE:
        _NC_CACHE["nc"] = build_nc(nseq=2)[0]
    nc = _NC_CACHE["nc"]
    x = np.asarray(inp["x"], np.float32)
    c = np.asarray(inp["c"], np.float32)
    in_maps = []
    for core in range(8):
        m = dict(sh)
        m["x"] = np.ascontiguousarray(x[2 * core:2 * core + 2].reshape(2 * S, D))
        m["cT"] = np.ascontiguousarray(c[2 * core:2 * core + 2].T.reshape(8, 128, 2).transpose(1, 0, 2))
        in_maps.append(m)
    res = run_bass_kernel_spmd(nc, in_maps, core_ids=list(range(8)))
    out = np.concatenate([np.asarray(r["out"]).reshape(2, S, D) for r in res.results], axis=0)
    return out.astype(np.float32)
```

```python
import math
import numpy as np
from contextlib import ExitStack
import concourse.bass as bass
import concourse.mybir as mybir
from concourse.bass_utils import run_bass_kernel_spmd

F32 = mybir.dt.float32
BF16 = mybir.dt.bfloat16
U32 = mybir.dt.uint32
AF = mybir.ActivationFunctionType
ALU = mybir.AluOpType
AX = mybir.AxisListType

S = 2048
D = 1024
NT = 16
EPS = 1e-6
NEG = -30000.0
LENG = "dve"
import os
PDBG = os.environ.get("PDBG", "")
SEM_CHUNK = 30000


class _Stop(Exception):
    pass


class Tok:
    __slots__ = ("writers", "readers")
    registry = []

    def __init__(self):
        self.writers = []
        self.readers = []
        Tok.registry.append(self)


class Op:
    __slots__ = ("eng", "sem", "val", "is_dma")


class Prog:
    ENGS = ("pe", "dve", "act", "pool", "sp")

    def __init__(self, nc, stack, n_dma_sems=8):
        self.nc = nc
        self.stack = stack
        self.eobj = {"pe": nc.tensor, "dve": nc.vector, "act": nc.scalar,
                     "pool": nc.gpsimd, "sp": nc.sync}
        self.esems = {e: [] for e in self.ENGS}
        self.ecount = {e: 0 for e in self.ENGS}
        self.waited = {e: {} for e in self.ENGS}
        self.dma_sems = {}
        self.dma_rr = {}
        self.n_dma_sems = n_dma_sems
        self.nwaits = 0

    def _esem(self, e, chunk):
        lst = self.esems[e]
        while len(lst) <= chunk:
            lst.append(self.stack.enter_context(self.nc.semaphore(f"s_{e}_{len(lst)}")))
        return lst[chunk]

    def _wait(self, e, sem, val):
        w = self.waited[e]
        k = id(sem)
        if w.get(k, 0) >= val:
            return
        w[k] = val
        self.eobj[e].wait_ge(sem, val)
        self.nwaits += 1

    def _deps(self, e, is_dma, reads, writes):
        for t in reads:
            for p in t.writers:
                if (not p.is_dma) and (not is_dma) and p.eng == e and e == "pe":
                    continue
                self._wait(e, p.sem, p.val)
        for t in writes:
            for p in t.writers + t.readers:
                if (not p.is_dma) and (not is_dma) and p.eng == e and e == "pe":
                    continue
                self._wait(e, p.sem, p.val)

    def _record(self, op, reads, writes):
        for t in reads:
            if not op.is_dma:
                t.readers = [r for r in t.readers if r.is_dma or r.eng != op.eng]
            t.readers.append(op)
        for t in writes:
            if t.readers:
                t.writers = [op]
                t.readers = []
            else:
                if not op.is_dma:
                    t.writers = [w for w in t.writers if w.is_dma or w.eng != op.eng]
                t.writers.append(op)

    def op(self, e, fn, reads=(), writes=()):
        self._deps(e, False, reads, writes)
        n = self.ecount[e]
        sem = self._esem(e, n // SEM_CHUNK)
        val = (n % SEM_CHUNK) + 1
        fn(self.eobj[e]).then_inc(sem, 1)
        self.ecount[e] = n + 1
        o = Op()
        o.eng, o.sem, o.val, o.is_dma = e, sem, val, False
        self._record(o, reads, writes)
        return o

    def dma(self, q, out, in_, reads=(), writes=(), **kw):
        if q not in self.dma_sems:
            self.dma_sems[q] = [[self.stack.enter_context(self.nc.semaphore(f"d_{q}_{i}")), 0]
                                for i in range(self.n_dma_sems)]
            self.dma_rr[q] = 0
        self._deps(q, True, reads, writes)
        i = self.dma_rr[q]
        self.dma_rr[q] = (i + 1) % self.n_dma_sems
        ent = self.dma_sems[q][i]
        sem, cur = ent
        if cur > 0:
            self._wait(q, sem, cur)
        val = cur + 16
        ent[1] = val
        self.eobj[q].dma_start(out=out, in_=in_, **kw).then_inc(sem, 16)
        o = Op()
        o.eng, o.sem, o.val, o.is_dma = q, sem, val, True
        self._record(o, reads, writes)
        return o

    def barrier(self):
        for e in self.ENGS:
            for f in self.ENGS:
                n = self.ecount[f]
                if f == e or n == 0:
                    continue
                self._wait(e, self.esems[f][(n - 1) // SEM_CHUNK], (n - 1) % SEM_CHUNK + 1)
            for q, lst in self.dma_sems.items():
                for sem, cur in lst:
                    if cur > 0:
                        self._wait(e, sem, cur)

    def wait_all(self, e, toks):
        for t in toks:
            for p in t.writers + t.readers:
                self._wait(e, p.sem, p.val)


def _rel_bucket_np(n):
    n = np.maximum(n, 0)
    exact = 16
    lr = np.log(np.maximum(n, 1).astype(np.float32) / np.float32(exact)) / np.float32(math.log(128 / exact))
    large = exact + (lr * np.float32(32 - exact)).astype(np.int32)
    return np.where(n < exact, n, np.minimum(large, 31))


def _constants():
    c = {}
    npr = np.arange(4096)
    n = npr - 2048
    bk = _rel_bucket_np(n)
    oh = np.zeros((33, 2, 4096), np.float32)
    okw = (n >= 0) & (n < 512)
    oks = (n >= 0)
    oh[bk[okw], 0, npr[okw]] = 1.0
    oh[32, 0, npr[~okw]] = 1.0
    oh[bk[oks], 1, npr[oks]] = 1.0
    oh[32, 1, npr[~oks]] = 1.0
    c["oh"] = oh
    e32 = (np.arange(2048)[None, :] // 64 == np.arange(32)[:, None]).astype(np.float32)
    e128 = np.zeros((128, 2048), np.float32)
    e128[0:32] = e32
    e128[64:96] = e32
    c["Esel"] = e128
    cs = np.arange(127) * 16
    ss = np.arange(32) * 64
    c["Cov"] = ((cs[:, None] < ss[None, :] + 64) & (cs[:, None] + 32 > ss[None, :])).astype(np.float32)
    t = np.arange(2048)
    cur = t // 64
    j = np.arange(32)
    forced = (j[None, :] == 0) | (j[None, :] == cur[:, None]) | (j[None, :] == cur[:, None] - 1)
    allowed = j[None, :] * 64 <= t[:, None]
    mul = (allowed & ~forced).astype(np.float32)
    add = np.where(forced, 1e4, np.where(allowed, 0.0, -1e30)).astype(np.float32)
    c["selmul"] = np.ascontiguousarray(mul.reshape(16, 128, 32).transpose(1, 0, 2))
    c["seladd"] = np.ascontiguousarray(add.reshape(16, 128, 32).transpose(1, 0, 2))
    c["identf"] = np.eye(128, dtype=np.float32)
    c["ones"] = np.ones((128, 128), np.float32)
    c["iota"] = np.tile(np.arange(128, dtype=np.float32)[None, :], (128, 1))
    sel = np.zeros((8, 128), np.float32)
    for h in range(8):
        sel[h, h * 16:(h + 1) * 16] = 1.0
    c["selh"] = np.concatenate([sel, sel], axis=0)
    return c


def _blockdiag2(w):
    out = np.zeros(w.shape[:-2] + (128, 128), np.float32)
    out[..., :64, :64] = w
    out[..., 64:, 64:] = w
    return out


def _prep(inp):
    sh = {}
    f = lambda a: np.ascontiguousarray(np.asarray(a, np.float32))
    sh["w_mod"] = f(inp["w_mod"][0])
    sh["bmodT"] = f(inp["b_mod"][0].reshape(48, 128).T)
    bm = inp["b_mod"][0]
    sh["bmod_bc"] = f(np.broadcast_to(np.stack([bm[2048:3072], bm[5120:6144]])[None], (128, 2, 1024)))
    sh["gmix"] = f(inp["ln_mix_g"][0].reshape(8, 128).T)
    sh["gffn"] = f(inp["ln_ffn_g"][0].reshape(8, 128).T)
    sh["gfin_bc"] = f(np.broadcast_to(inp["ln_final_g"][None, :], (128, 1024)))
    w_in = np.asarray(inp["w_in"][0], np.float32)
    sp = np.cumsum([0, 512, 128, 128, 128, 128, 128, 128, 24, 512, 512, 512])
    q, kc, vc, ks, vs, kw, vw, gt, cb, cc, ch = [w_in[:, sp[i]:sp[i + 1]] for i in range(11)]
    qh = q.reshape(1024, 8, 64)
    qperm = np.concatenate([np.concatenate([qh[:, j], qh[:, 4 + j]], axis=1) for j in range(4)], axis=1)
    cols = [qperm, kc, vc, ks, kw]
    for c4 in range(4):
        cols += [cb[:, c4 * 128:(c4 + 1) * 128], cc[:, c4 * 128:(c4 + 1) * 128], ch[:, c4 * 128:(c4 + 1) * 128]]
    cols += [vs, vw, gt]
    sh["w_in"] = f(np.concatenate(cols, axis=1))
    for nm, w1, w2, pe in (("k", "cmp_wk1", "cmp_wk2", "cmp_pe_k"), ("v", "cmp_wv1", "cmp_wv2", "cmp_pe_v")):
        a = np.asarray(inp[w1][0], np.float32).reshape(32, 64, 64)
        sh["bd1" + nm] = f(_blockdiag2(a).transpose(1, 0, 2))
        sh["bd2" + nm] = f(_blockdiag2(np.asarray(inp[w2][0], np.float32)))
        p = np.asarray(inp[pe][0], np.float32).T
        sh["pe2" + nm] = f(np.concatenate([p, p], axis=0))
    sh["convw"] = f(inp["conv_w"][0][:, 0, :].reshape(3, 4, 128).transpose(2, 1, 0))
    sh["gattn_bc"] = f(np.broadcast_to(inp["norm_attn_g"][0][None, :], (128, 512)))
    sh["gconv"] = f(inp["norm_conv_g"][0].reshape(4, 128).T)
    sh["w_out"] = f(inp["w_out"][0])
    sh["peer_wq"] = f(inp["peer_wq"][0])
    pk = np.asarray(inp["peer_keys"][0], np.float32)
    kb = np.zeros((128, 8, 256), np.float32)
    for h in range(8):
        for p in range(2):
            kb[p * 64:(p + 1) * 64, h, p * 128:(p + 1) * 128] = pk[h, p].T
    sh["peer_kb"] = kb
    u = np.asarray(inp["peer_u"][0], np.float32)
    sh["peer_ut"] = f(u.reshape(128, 128, 8, 128).transpose(0, 3, 2, 1).reshape(128 * 128, 1024))
    sh["peer_v"] = f(inp["peer_v"][0])
    rt = np.zeros((33, 8), np.float32)
    rt[:32] = inp["rel_table"]
    rt[32] = NEG
    sh["relt"] = rt
    sh.update(_constants())
    return sh


def build_nc(nseq=2, dbg=False, stop_after=99):
    nc = bass.Bass("TRN2", target_bir_lowering=False)
    Tok.registry = []
    T = nseq * S
    NTT = nseq * NT

    def din(name, shape, dt=F32):
        return nc.dram_tensor(name, list(shape), dt, kind="ExternalInput").ap()

    def dscr(name, shape, dt=F32):
        return nc.dram_tensor(name, list(shape), dt, kind="Internal").ap()

    def dout(name, shape, dt=F32):
        return nc.dram_tensor(name, list(shape), dt, kind="ExternalOutput").ap()

    x_d = din("x", [T, D])
    cT_d = din("cT", [128, 8, nseq])
    w_mod_d = din("w_mod", [1024, 6144])
    bmodT_d = din("bmodT", [128, 48])
    bmod_bc_d = din("bmod_bc", [128, 2, 1024])
    gmix_d = din("gmix", [128, 8])
    gffn_d = din("gffn", [128, 8])
    gfin_d = din("gfin_bc", [128, 1024])
    w_in_d = din("w_in", [1024, 2840])
    bd1_d = {n: din("bd1" + n, [128, 32, 128]) for n in "kv"}
    bd2_d = {n: din("bd2" + n, [128, 128]) for n in "kv"}
    pe2_d = {n: din("pe2" + n, [128, 32]) for n in "kv"}
    convw_d = din("convw", [128, 4, 3])
    gattn_d = din("gattn_bc", [128, 512])
    gconv_d = din("gconv", [128, 4])
    w_out_d = din("w_out", [1024, 1024])
    wq_d = din("peer_wq", [1024, 1024])
    kb_d = din("peer_kb", [128, 8, 256])
    ut_d = din("peer_ut", [128 * 128, 1024])
    pv_d = din("peer_v", [16384, 1024])
    relt_d = din("relt", [33, 8])
    oh_d = din("oh", [33, 2, 4096])
    esel_d = din("Esel", [128, 2048])
    cov_d = din("Cov", [127, 32])
    selmul_d = din("selmul", [128, 16, 32])
    seladd_d = din("seladd", [128, 16, 32])
    identf_d = din("identf", [128, 128])
    ones_d = din("ones", [128, 128])
    iota_d = din("iota", [128, 128])
    selh_d = din("selh", [16, 128])
    out_d = dout("out", [T, D])

    tb_d = dscr("tb_s", [2, 8, 4096])
    LW = 1024
    fw_d = dscr("fw_s", [2, 8, 128 * (LW + 1)])
    LC = 4096
    fc_d = dscr("fc_s", [8, 127 * (LC + 16)])
    gt_d = dscr("gt_s", [2, nseq, 128, 1024])
    if dbg:
        x1_d = dout("x1_s", [T, D])
        h2T_d = dout("h2T_s", [128, 8, T], BF16)
    else:
        x1_d = dscr("x1_s", [T, D])
        h2T_d = dscr("h2T_s", [128, 8, T], BF16)
    t_x1d = [Tok() for _ in range(NTT)]
    t_h2d = [Tok() for _ in range(NTT)]

    dbg_out = {}
    uniq = [0]

    with ExitStack() as st0:
        P = Prog(nc, st0)
        scopes = [st0]

        def sb(name, shape, dt=F32):
            uniq[0] += 1
            return scopes[-1].enter_context(nc.sbuf_tensor(f"sb{uniq[0]}_{name}", list(shape), dt))

        class Scope:
            def __enter__(self):
                self.st = ExitStack()
                scopes.append(self.st)
                return self

            def __exit__(self, *a):
                if a[0] is None:
                    P.barrier()
                scopes.pop()
                self.st.close()
                return False

        psg = [st0.enter_context(nc.psum_tensor(f"psum{i}", [128, 512], F32)) for i in range(3)]
        psO = st0.enter_context(nc.psum_tensor("psumO", [128, 4, 512], F32))
        ps = [psg[0][:, :], psg[1][:, :], psg[2][:, :]] + [psO[:, i, :] for i in range(4)]
        tps = [Tok() for _ in range(7)]
        psb = st0.enter_context(nc.psum_tensor("psumb", [128, 1024], BF16))
        tpsb = Tok()

        def load_const(name, src, shape, dt=F32, q="sp"):
            t = sb(name, shape, dt)
            k = Tok()
            P.dma(q, t[:], src, writes=[k])
            return t, k

        def dbgdump(name, shape, dt, src_ap, toks):
            if not dbg:
                return
            dbg_out[name] = dout("dbg_" + name, shape, dt)
            P.dma("sp", dbg_out[name], src_ap, reads=toks)

        identb, t_identb = load_const("identb", identf_d, [128, 128], BF16, "pool")
        onesb, t_onesb = load_const("onesb", ones_d, [128, 128], BF16, "pool")
        gmix, t_gmix = load_const("gmix", gmix_d, [128, 8])
        gffn, t_gffn = load_const("gffn", gffn_d, [128, 8])
        bmodT, t_bmodT = load_const("bmodT", bmodT_d, [128, 48])
        convw, t_convw = load_const("convw", convw_d, [128, 4, 3])
        gconv, t_gconv = load_const("gconv", gconv_d, [128, 4])
        relt, t_relt = load_const("relt", relt_d, [33, 8])
        modT = sb("modT", [128, 48, nseq]); t_modT = Tok()
        gs1 = sb("gs1", [128, 8, nseq]); gs2 = sb("gs2", [128, 8, nseq]); t_gs = Tok()
        t_gtd = Tok()
        t_fw = Tok(); t_fc = Tok()

        with Scope():
            cT = sb("cT", [128, 8, nseq]); t_cT = Tok()
            P.dma("sp", cT[:], cT_d, writes=[t_cT])
            c_act = sb("c_act", [128, 8, nseq]); t_cact = Tok()
            P.op("act", lambda e: e.activation(out=c_act[:], in_=cT[:], func=AF.Silu), reads=[t_cT], writes=[t_cact])
            c_rep = sb("c_rep", [128, 8, nseq, 128]); t_crep = Tok()
            P.op("dve", lambda e: e.tensor_copy(out=c_rep[:], in_=c_act[:].unsqueeze(3).broadcast_to([128, 8, nseq, 128])),
                 reads=[t_cact], writes=[t_crep])
            bmod_bc = sb("bmod_bc", [128, 2, 1024]); t_bmbc = Tok()
            P.dma("sp", bmod_bc[:], bmod_bc_d, writes=[t_bmbc])
            gstage = [sb(f"gstage{i}", [128, nseq, 128]) for i in range(2)]; t_gst = [Tok(), Tok()]
            wm_view = w_mod_d.rearrange("(k p) n -> p k n", p=128)
            wmp = [sb(f"wm{i}", [128, 8, 128]) for i in range(3)]
            t_wmp = [Tok() for _ in range(3)]
            for j in range(48):
                wt, tw = wmp[j % 3], t_wmp[j % 3]
                P.dma("sp", wt[:], wm_view[:, :, j * 128:(j + 1) * 128], writes=[tw])
                for k in range(8):
                    P.op("pe", lambda e, k=k, wt=wt: e.matmul(ps[0][:, j * nseq:(j + 1) * nseq], lhsT=wt[:, k, :],
                                                              rhs=c_act[:, k, :], start=(k == 0), stop=(k == 7)),
                         reads=[tw, t_cact], writes=[tps[0]])
                which = {2: 0, 5: 1}.get(j // 8)
                if which is not None:
                    jj = j % 8
                    bi = 1 + (j % 2)
                    for b in range(nseq):
                        for k in range(8):
                            P.op("pe", lambda e, k=k, b=b, wt=wt, bi=bi: e.matmul(
                                ps[bi][:, b * 128:(b + 1) * 128], lhsT=c_rep[:, k, b, :], rhs=wt[:, k, :],
                                start=(k == 0), stop=(k == 7)), reads=[tw, t_crep], writes=[tps[bi]])
                    gsb, tg = gstage[j % 2], t_gst[j % 2]
                    P.op("dve", lambda e, bi=bi, which=which, jj=jj, gsb=gsb: e.tensor_tensor(
                        out=gsb[:],
                        in0=ps[bi][:, 0:nseq * 128].rearrange("p (b n) -> p b n", b=nseq),
                        in1=bmod_bc[:, which, jj * 128:(jj + 1) * 128].unsqueeze(1).broadcast_to([128, nseq, 128]),
                        op=ALU.add), reads=[tps[bi], t_bmbc], writes=[tg])
                    P.dma("sp", gt_d[which].rearrange("b p n -> p b n")[:, :, jj * 128:(jj + 1) * 128], gsb[:],
                          reads=[tg], writes=[t_gtd])
            P.op("dve", lambda e: e.tensor_tensor(
                out=modT[:], in0=ps[0][:, 0:48 * nseq].rearrange("p (j b) -> p j b", b=nseq),
                in1=bmodT[:].unsqueeze(2).broadcast_to([128, 48, nseq]), op=ALU.add),
                reads=[tps[0], t_bmodT], writes=[t_modT])
            P.op("dve", lambda e: e.scalar_tensor_tensor(
                out=gs1[:], in0=modT[:, 8:16, :], scalar=1.0, in1=gmix[:].unsqueeze(2).broadcast_to([128, 8, nseq]),
                op0=ALU.add, op1=ALU.mult), reads=[t_modT, t_gmix], writes=[t_gs])
            P.op("dve", lambda e: e.scalar_tensor_tensor(
                out=gs2[:], in0=modT[:, 32:40, :], scalar=1.0, in1=gffn[:].unsqueeze(2).broadcast_to([128, 8, nseq]),
                op0=ALU.add, op1=ALU.mult), reads=[t_modT, t_gffn], writes=[t_gs])
            dbgdump("modT", [128, 48, nseq], F32, modT[:], [t_modT])

            ohp = [sb(f"ohp{i}", [33, 512]) for i in range(2)]
            t_ohp = [Tok(), Tok()]
            tbs = [sb(f"tbs{i}", [8, 512]) for i in range(2)]
            t_tbs = [Tok(), Tok()]
            t_tbd = Tok()
            for kind in range(2):
                for pc in range(8):
                    i = (kind * 8 + pc) % 2
                    P.dma("sp", ohp[i][:], oh_d[:, kind, pc * 512:(pc + 1) * 512], writes=[t_ohp[i]])
                    bi = 3 + i
                    P.op("pe", lambda e, i=i, bi=bi: e.matmul(ps[bi][0:8, :], lhsT=relt[:, :], rhs=ohp[i][:, :],
                                                               start=True, stop=True),
                         reads=[t_relt, t_ohp[i]], writes=[tps[bi]])
                    P.op("act", lambda e, i=i, bi=bi: e.copy(out=tbs[i][:], in_=ps[bi][0:8, :]),
                         reads=[tps[bi]], writes=[t_tbs[i]])
                    P.dma("sp", tb_d[kind, :, pc * 512:(pc + 1) * 512], tbs[i][:], reads=[t_tbs[i]], writes=[t_tbd])
            for kind in range(2):
                for h in range(8):
                    src = bass.AP(tb_d.tensor, (kind * 8 + h) * 4096 + (2048 - 128), [[0, 128], [1, LW]])
                    dst = bass.AP(fw_d.tensor, (kind * 8 + h) * 128 * (LW + 1), [[LW + 1, 128], [1, LW]])
                    P.dma("sp", dst, src, reads=[t_tbd], writes=[t_fw])
            for h in range(8):
                src = bass.AP(tb_d.tensor, (8 + h) * 4096, [[0, 127], [1, LC]])
                dst = bass.AP(fc_d.tensor, h * 127 * (LC + 16), [[LC + 16, 127], [1, LC]])
                P.dma("sp", dst, src, reads=[t_tbd], writes=[t_fc])

        with Scope():
            qTg = [sb(f"qTg{g}", [128, 4, S], BF16) for g in range(2)]; t_qT = [Tok() for _ in range(4)]
            t_qz = Tok()
            P.op("pool", lambda e: e.memset(qTg[0][64:128, :, :], 0.0), writes=[t_qz])
            P.op("pool", lambda e: e.memset(qTg[1][0:64, :, :], 0.0), writes=[t_qz])
            kT = {n: sb(n + "T", [128, S], BF16) for n in ("kc", "vc", "ks", "kw")}
            t_kT = {n: [Tok() for _ in range(4)] for n in kT}
            vs_aug = sb("vs_aug", [128, NT, 2, 65], BF16); vw_aug = sb("vw_aug", [128, NT, 2, 65], BF16)
            t_va = [Tok() for _ in range(NT)]
            gat = sb("gat", [128, NT, 24]); t_gat = [Tok() for _ in range(NT)]
            mixT = sb("mixT", [128, 8, S], BF16)
            t_mixc = [Tok() for _ in range(4)]
            t_mixa = [Tok() for _ in range(NT)]
            t_ones = Tok()
            P.op("pool", lambda e: e.memset(vs_aug[:, :, :, 64:65], 1.0), writes=[t_ones])
            P.op("pool", lambda e: e.memset(vw_aug[:, :, :, 64:65], 1.0), writes=[t_ones])
            ssq = sb("ssq", [128, NTT]); rstd = sb("rstd", [128, NTT]); t_st = [Tok() for _ in range(NTT)]
            ssq2 = sb("ssq2", [128, NTT]); rstd2 = sb("rstd2", [128, NTT]); t_st2 = [Tok() for _ in range(NTT)]
            ssqa = sb("ssqa", [128, NTT]); rstda = sb("rstda", [128, NTT]); t_sta = [Tok() for _ in range(NTT)]

            def rms_stats(src_ap, t_src, junk_, t_junk_, ssq_, rstd_, tk, col, width):
                P.op("act", lambda e: e.activation(out=junk_, in_=src_ap, func=AF.Square, accum_out=ssq_[:, col:col + 1]),
                     reads=[t_src], writes=[t_junk_, tk])
                P.op("dve", lambda e: e.tensor_scalar(out=rstd_[:, col:col + 1], in0=ssq_[:, col:col + 1], scalar1=1.0 / width,
                                                      scalar2=EPS, op0=ALU.mult, op1=ALU.add), reads=[tk], writes=[tk])
                P.op("act", lambda e: e.activation(out=rstd_[:, col:col + 1], in_=rstd_[:, col:col + 1], func=AF.Ln),
                     reads=[tk], writes=[tk])
                P.op("act", lambda e: e.activation(out=rstd_[:, col:col + 1], in_=rstd_[:, col:col + 1], func=AF.Exp, scale=-0.5),
                     reads=[tk], writes=[tk])

            def norm_transpose(xt, t_xt, gi, s, ssq_, rstd_, t_st_, gs_, sh_lo, dst, t_dst, col0, junk, t_junk, xs, t_xs):
                rms_stats(xt[:], t_xt, junk[:], t_junk, ssq_, rstd_, t_st_[gi], gi, D)
                P.op("dve", lambda e: e.tensor_scalar(out=xs[:], in0=xt[:], scalar1=rstd_[:, gi:gi + 1], scalar2=None,
                                                      op0=ALU.mult), reads=[t_xt, t_st_[gi]], writes=[t_xs])
                for c in range(8):
                    P.op("pe", lambda e, c=c: e.transpose(out=psb[:, c * 128:(c + 1) * 128], in_=xs[:, c * 128:(c + 1) * 128],
                                                          identity=identb[:]), reads=[t_xs, t_identb], writes=[tpsb])
                for c in range(8):
                    o_ap = dst[:, c, col0:col0 + 128]
                    i_ap = psb[:, c * 128:(c + 1) * 128]
                    sc_ap = gs_[:, c, s:s + 1]
                    bi_ap = modT[:, sh_lo + c, s:s + 1]
                    if gi % 2 == 0:
                        P.op("dve", lambda e, o_ap=o_ap, i_ap=i_ap, sc_ap=sc_ap, bi_ap=bi_ap: e.tensor_scalar(
                            out=o_ap, in0=i_ap, scalar1=sc_ap, scalar2=bi_ap, op0=ALU.mult, op1=ALU.add),
                            reads=[tpsb, t_gs, t_modT], writes=[t_dst])
                    else:
                        P.op("act", lambda e, o_ap=o_ap, i_ap=i_ap, sc_ap=sc_ap, bi_ap=bi_ap: e.activation(
                            out=o_ap, in_=i_ap, func=AF.Identity, scale=sc_ap, bias=bi_ap),
                            reads=[tpsb, t_gs, t_modT], writes=[t_dst])

            def evac(i, out_ap, in_ap, reads, writes):
                if i % 2:
                    P.op("act", lambda e: e.copy(out=out_ap, in_=in_ap), reads=reads, writes=writes)
                else:
                    P.op("dve", lambda e: e.tensor_copy(out=out_ap, in_=in_ap), reads=reads, writes=writes)

            for s in range(nseq):
                with Scope():
                    win = sb("win", [128, 8, 2840], BF16); t_win = Tok()
                    wiv = w_in_d.rearrange("(k p) n -> p k n", p=128)
                    for k in range(8):
                        P.dma("pool", win[:, k, :], wiv[:, k, :], writes=[t_win])
                    xpool = [sb(f"xt{i}", [128, 1024]) for i in range(2)]; t_xp = [Tok(), Tok()]
                    junk = sb("junk", [128, 1024], BF16); t_junk = Tok()
                    xs = sb("xs", [128, 1024], BF16); t_xs = Tok()
                    hTg = [sb(f"hTg{i}", [128, 8, 512], BF16) for i in range(2)]; t_hTg = [Tok(), Tok()]
                    cbs = sb("cbs", [128, 512]); ccs = sb("ccs", [128, 512]); t_cbs = Tok(); t_ccs = Tok()
                    zb = sb("zb", [128, 4, 514]); t_zb = [Tok() for _ in range(4)]
                    yb = sb("yb", [128, 512]); t_yb = Tok()
                    oc = sb("oc", [128, 4, 512]); t_oc = [Tok() for _ in range(4)]
                    osq = sb("osq", [128, 4, 512], BF16); t_osq = [Tok() for _ in range(4)]
                    rbc = sb("rbc", [128, 512]); t_rbc = Tok()
                    for grp in range(4):
                        hT, t_hT = hTg[grp % 2], t_hTg[grp % 2]
                        s0 = grp * 512
                        for tl in range(4):
                            ti = grp * 4 + tl
                            gi = s * NT + ti
                            xt, t_xt = xpool[gi % 2], t_xp[gi % 2]
                            P.dma("sp", xt[:], x_d[gi * 128:(gi + 1) * 128, :], writes=[t_xt])
                            norm_transpose(xt, t_xt, gi, s, ssq, rstd, t_st, gs1, 0, hT, t_hT, tl * 128, junk, t_junk, xs, t_xs)
                        if s == 0 and grp == 0:
                            dbgdump("hT0", [128, 8, 512], BF16, hT[:], [t_hT])

                        def proj_chunk(cc, bi):
                            for k in range(8):
                                P.op("pe", lambda e, k=k: e.matmul(ps[bi][:, :], lhsT=win[:, k, cc * 128:(cc + 1) * 128],
                                                                   rhs=hT[:, k, :], start=(k == 0), stop=(k == 7)),
                                     reads=[t_win, t_hT], writes=[tps[bi]])

                        nb = 0
                        for cc in range(4):
                            bi = nb % 3; nb += 1
                            proj_chunk(cc, bi)
                            if cc % 2:
                                P.op("act", lambda e, cc=cc, bi=bi: e.copy(out=qTg[0][0:64, cc, s0:s0 + 512], in_=ps[bi][0:64, :]),
                                     reads=[tps[bi], t_qz], writes=[t_qT[grp]])
                                P.op("act", lambda e, cc=cc, bi=bi: e.copy(out=qTg[1][64:128, cc, s0:s0 + 512], in_=ps[bi][64:128, :]),
                                     reads=[tps[bi], t_qz], writes=[t_qT[grp]])
                            else:
                                P.op("dve", lambda e, cc=cc, bi=bi: e.tensor_copy(out=qTg[0][0:64, cc, s0:s0 + 512],
                                                                                  in_=ps[bi][0:64, :]),
                                     reads=[tps[bi], t_qz], writes=[t_qT[grp]])
                                P.op("dve", lambda e, cc=cc, bi=bi: e.tensor_copy(out=qTg[1][64:128, cc, s0:s0 + 512],
                                                                                  in_=ps[bi][64:128, :]),
                                     reads=[tps[bi], t_qz], writes=[t_qT[grp]])
                        for idx, n in enumerate(("kc", "vc", "ks", "kw")):
                            bi = nb % 3; nb += 1
                            proj_chunk(4 + idx, bi)
                            evac(idx, kT[n][:, s0:s0 + 512], ps[bi][:, :], [tps[bi]], [t_kT[n][grp]])
                        for c4 in range(4):
                            b_cb = nb % 3; nb += 1
                            proj_chunk(8 + c4 * 3 + 0, b_cb)
                            P.op("act", lambda e, b=b_cb: e.copy(out=cbs[:], in_=ps[b][:, :]), reads=[tps[b_cb]], writes=[t_cbs])
                            b_cc = nb % 3; nb += 1
                            proj_chunk(8 + c4 * 3 + 1, b_cc)
                            P.op("act", lambda e, b=b_cc: e.copy(out=ccs[:], in_=ps[b][:, :]), reads=[tps[b_cc]], writes=[t_ccs])
                            b_ch = nb % 3; nb += 1
                            proj_chunk(8 + c4 * 3 + 2, b_ch)
                            if grp == 0:
                                P.op("dve", lambda e, c4=c4: e.memset(zb[:, c4, 0:2], 0.0), writes=[t_zb[c4]])
                            P.op("dve", lambda e, c4=c4, b=b_ch: e.tensor_tensor(out=zb[:, c4, 2:514], in0=ps[b][:, :], in1=ccs[:],
                                                                                 op=ALU.mult),
                                 reads=[tps[b_ch], t_ccs], writes=[t_zb[c4]])
                            P.op("dve", lambda e, c4=c4: e.tensor_scalar(out=yb[:], in0=zb[:, c4, 2:514], scalar1=convw[:, c4, 2:3],
                                                                         scalar2=None, op0=ALU.mult),
                                 reads=[t_zb[c4], t_convw], writes=[t_yb])
                            P.op("dve", lambda e, c4=c4: e.scalar_tensor_tensor(out=yb[:], in0=zb[:, c4, 1:513], scalar=convw[:, c4, 1:2],
                                                                                in1=yb[:], op0=ALU.mult, op1=ALU.add),
                                 reads=[t_zb[c4], t_convw, t_yb], writes=[t_yb])
                            P.op("dve", lambda e, c4=c4: e.scalar_tensor_tensor(out=yb[:], in0=zb[:, c4, 0:512], scalar=convw[:, c4, 0:1],
                                                                                in1=yb[:], op0=ALU.mult, op1=ALU.add),
                                 reads=[t_zb[c4], t_convw, t_yb], writes=[t_yb])
                            P.op("dve", lambda e, c4=c4: e.tensor_tensor(out=oc[:, c4, :], in0=yb[:], in1=cbs[:], op=ALU.mult),
                                 reads=[t_yb, t_cbs], writes=[t_oc[c4]])
                            P.op("dve", lambda e, c4=c4: e.tensor_copy(out=zb[:, c4, 0:2], in_=zb[:, c4, 512:514]),
                                 reads=[t_zb[c4]], writes=[t_zb[c4]])
                            P.op("act", lambda e, c4=c4: e.activation(out=osq[:, c4, :], in_=oc[:, c4, :], func=AF.Square),
                                 reads=[t_oc[c4]], writes=[t_osq[c4]])
                        for c4 in range(4):
                            P.op("pe", lambda e, c4=c4: e.matmul(ps[3][:, :], lhsT=onesb[:, :], rhs=osq[:, c4, :],
                                                                 start=(c4 == 0), stop=(c4 == 3)),
                                 reads=[t_onesb, t_osq[c4]], writes=[tps[3]])
                        P.op("dve", lambda e: e.tensor_scalar(out=rbc[:], in0=ps[3][:, :], scalar1=1.0 / 512, scalar2=EPS,
                                                              op0=ALU.mult, op1=ALU.add), reads=[tps[3]], writes=[t_rbc])
                        P.op("act", lambda e: e.activation(out=rbc[:], in_=rbc[:], func=AF.Sqrt), reads=[t_rbc], writes=[t_rbc])
                        P.op("dve", lambda e: e.reciprocal(out=rbc[:], in_=rbc[:]), reads=[t_rbc], writes=[t_rbc])
                        for c4 in range(4):
                            P.op("dve", lambda e, c4=c4: e.scalar_tensor_tensor(
                                out=mixT[:, 4 + c4, s0:s0 + 512], in0=oc[:, c4, :], scalar=gconv[:, c4:c4 + 1], in1=rbc[:],
                                op0=ALU.mult, op1=ALU.mult), reads=[t_oc[c4], t_gconv, t_rbc], writes=[t_mixc[grp]])
                        for tl in range(4):
                            ti = grp * 4 + tl
                            bi = 4 + (tl % 2)
                            for k in range(8):
                                P.op("pe", lambda e, k=k, tl=tl, bi=bi: e.matmul(
                                    ps[bi][:, 0:280], lhsT=hT[:, k, tl * 128:(tl + 1) * 128], rhs=win[:, k, 2560:2840],
                                    start=(k == 0), stop=(k == 7)), reads=[t_win, t_hT], writes=[tps[bi]])
                            P.op("act", lambda e, ti=ti, bi=bi: e.copy(
                                out=vs_aug[:, ti, :, 0:64], in_=ps[bi][:, 0:128].rearrange("p (g d) -> p g d", g=2)),
                                reads=[tps[bi]], writes=[t_va[ti]])
                            P.op("act", lambda e, ti=ti, bi=bi: e.copy(
                                out=vw_aug[:, ti, :, 0:64], in_=ps[bi][:, 128:256].rearrange("p (g d) -> p g d", g=2)),
                                reads=[tps[bi]], writes=[t_va[ti]])
                            P.op("act", lambda e, ti=ti, bi=bi: e.activation(out=gat[:, ti, :], in_=ps[bi][:, 256:280],
                                                                             func=AF.Sigmoid),
                                 reads=[tps[bi]], writes=[t_gat[ti]])
                    if s == 0:
                        dbgdump("gat", [128, NT, 24], F32, gat[:], t_gat)
                        dbgdump("vs", [128, NT, 2, 65], BF16, vs_aug[:], t_va + [t_ones])
                        if stop_after <= 1:
                            dbgdump("mixT", [128, 8, S], BF16, mixT[:], t_mixc + t_mixa)
                if stop_after <= 1:
                    continue

                with Scope():
                    bd1 = {}; bd2 = {}; pe2 = {}; t_cw = Tok()
                    for n in "kv":
                        bd1[n] = sb("bd1" + n, [128, 32, 128], BF16)
                        P.dma("pool", bd1[n][:], bd1_d[n], writes=[t_cw])
                        bd2[n] = sb("bd2" + n, [128, 128], BF16)
                        P.dma("pool", bd2[n][:], bd2_d[n], writes=[t_cw])
                        pe2[n] = sb("pe2" + n, [128, 32], BF16)
                        P.dma("pool", pe2[n][:], pe2_d[n], writes=[t_cw])
                    Bw = sb("Bw", [128, 5, 8, 128], BF16); t_Bw = Tok()
                    Bs = sb("Bs", [128, 3, 8, 128], BF16); t_Bs = Tok()
                    for dl in range(5):
                        src = bass.AP(fw_d.tensor, 128 + dl * 128, [[LW, 128], [128 * (LW + 1), 8], [1, 128]])
                        P.dma("pool", Bw[:, dl, :, :], src, reads=[t_fw], writes=[t_Bw])
                    for dl in range(3):
                        src = bass.AP(fw_d.tensor, 8 * 128 * (LW + 1) + 128 + dl * 128,
                                      [[LW, 128], [128 * (LW + 1), 8], [1, 128]])
                        P.dma("pool", Bs[:, dl, :, :], src, reads=[t_fw], writes=[t_Bs])
                    P.op("act", lambda e: e.activation(out=Bw[:], in_=Bw[:], func=AF.Exp), reads=[t_Bw], writes=[t_Bw])
                    P.op("act", lambda e: e.activation(out=Bs[:], in_=Bs[:], func=AF.Exp), reads=[t_Bs], writes=[t_Bs])
                    if False:
                        dbgdump("Bw", [128, 5, 8, 128], BF16, Bw[:], [t_Bw])
                        dbgdump("Bs", [128, 3, 8, 128], BF16, Bs[:], [t_Bs])
                    esel, t_esel = load_const("esel", esel_d, [128, 2048], BF16, "pool")
                    selmul, t_selmul = load_const("selmul", selmul_d, [128, 16, 32])
                    seladd, t_seladd = load_const("seladd", seladd_d, [128, 16, 32])
                    gattn, t_gattn = load_const("gattn", gattn_d, [128, 512])
                    cbias = sb("cbias", [128, 2]); t_cbias = Tok()
                    for i, n in enumerate("kv"):
                        for l in range(32):
                            P.op("pe", lambda e, n=n, l=l, i=i: e.matmul(ps[2][:, i:i + 1], lhsT=bd1[n][:, l, :],
                                                                         rhs=pe2[n][:, l:l + 1], start=(l == 0), stop=(l == 31)),
                                 reads=[t_cw], writes=[tps[2]])
                    P.op("dve", lambda e: e.tensor_copy(out=cbias[:], in_=ps[2][:, 0:2]), reads=[tps[2]], writes=[t_cbias])
                    kcmpT = sb("kcmpT", [128, 128], BF16); t_kcmp = Tok()
                    vc_aug = sb("vc_aug", [128, 2, 97], BF16); t_vca = Tok()
                    for g in range(2):
                        P.dma("pool", vc_aug[0:127, g, 65:97], cov_d, writes=[t_vca])
                    P.op("dve", lambda e: e.memset(vc_aug[:, :, 64:65], 1.0), writes=[t_vca])
                    hid = {n: sb("hid" + n, [128, 128], BF16) for n in "kv"}; t_hid = Tok()
                    for i, (n, srcn) in enumerate((("k", "kc"), ("v", "vc"))):
                        src = kT[srcn]
                        for l in range(32):
                            P.op("pe", lambda e, n=n, l=l, i=i, src=src: e.matmul(
                                ps[i][:, 0:127], lhsT=bd1[n][:, l, :], rhs=src[:, l:l + 2017:16],
                                start=(l == 0), stop=(l == 31)), reads=[t_cw] + t_kT[srcn], writes=[tps[i]])
                        P.op("act", lambda e, n=n, i=i: e.activation(out=hid[n][:, 0:127], in_=ps[i][:, 0:127],
                                                                     func=AF.Gelu_apprx_tanh, bias=cbias[:, i:i + 1]),
                             reads=[tps[i], t_cbias], writes=[t_hid])
                    P.op("pe", lambda e: e.matmul(ps[0][:, 0:127], lhsT=bd2["k"][:, :], rhs=hid["k"][:, 0:127], start=True, stop=True),
                         reads=[t_cw, t_hid], writes=[tps[0]])
                    P.op("dve", lambda e: e.tensor_copy(out=kcmpT[:, 0:127], in_=ps[0][:, 0:127]), reads=[tps[0]], writes=[t_kcmp])
                    P.op("pe", lambda e: e.matmul(ps[1][0:127, 0:128], lhsT=hid["v"][:, 0:127], rhs=bd2["v"][:, :], start=True, stop=True),
                         reads=[t_cw, t_hid], writes=[tps[1]])
                    P.op("dve", lambda e: e.tensor_copy(out=vc_aug[0:127, :, 0:64],
                                                        in_=ps[1][0:127, 0:128].rearrange("p (g d) -> p g d", g=2)),
                         reads=[tps[1]], writes=[t_vca])
                    if s == 0:
                        dbgdump("kcmpT", [128, 128], BF16, kcmpT[:], [t_kcmp])
                        dbgdump("vc_aug", [128, 2, 97], BF16, vc_aug[:], [t_vca])

                    bc_all = sb("bc_all", [128, NT, 8, 128], BF16); t_bcq = [Tok() for _ in range(NT)]
                    for q_ in range(NT):
                        src = bass.AP(fc_d.tensor, 2048 + 128 * q_ - 31, [[LC, 127], [127 * (LC + 16), 8], [1, 128]])
                        P.dma("pool", bc_all[0:127, q_, :, :], src, reads=[t_fc], writes=[t_bcq[q_]])

                    def bc_exp(q_):
                        P.op("act", lambda e: e.activation(out=bc_all[0:127, q_, :, :], in_=bc_all[0:127, q_, :, :], func=AF.Exp),
                             reads=[t_bcq[q_]], writes=[t_bcq[q_]])
                    bc_exp(0)
                    NSB = 4
                    sbank = [0, 1, 2, 6]
                    tS = [sb(f"tS{i}", [128, 512], BF16) for i in range(NSB)]; t_tS = [Tok() for _ in range(NSB)]
                    pT = [sb(f"pT{i}", [128, 512], BF16) for i in range(NSB)]; t_pT = [Tok() for _ in range(NSB)]
                    o_acc = [sb(f"oacc{i}", [128, 8, 64]) for i in range(2)]; t_oacc = [Tok(), Tok()]
                    rs4 = sb("rs4", [128, 4]); wg4 = sb("wg4", [128, 4]); t_rs = Tok()
                    otmp = sb("otmp", [128, 4, 64]); t_otmp = Tok()
                    itmp = sb("itmp", [128, 4, 32]); imp = sb("imp", [128, 32]); t_imp = Tok()
                    m8 = sb("m8", [128, 8]); t_m8 = Tok()
                    negsel = [sb(f"negsel{i}", [128, 128], BF16) for i in range(2)]; t_negsel = [Tok(), Tok()]
                    for i in range(2):
                        P.op("dve", lambda e, i=i: e.memset(negsel[i][:], 0.0), writes=[t_negsel[i]])
                    nsT4 = [sb(f"nsT4{i}", [128, 4, 128], BF16) for i in range(2)]; t_nsT = [Tok(), Tok()]
                    junk2 = sb("junk2", [128, 512], BF16); t_junk2 = Tok()
                    on_b = sb("on_b", [128, 512], BF16); t_onb = Tok()
                    tpsb_a = tpsb
                    BK = {0: 0, 2: 1, 1: 2}
                    tpo = {br: [Tok()] for br in range(3)}

                    def evac_branch(g, br, qi, oa, t_oa):
                        c0 = 0
                        rd = tpo[br]
                        pob = psO[:, BK[br], :].rearrange("p (j c) -> p j c", j=4)
                        if br == 0:
                            P.op("dve", lambda e: e.tensor_scalar(out=rs4[:], in0=pob[:, :, 64], scalar1=1e-30, scalar2=None,
                                                                  op0=ALU.max), reads=rd, writes=[t_rs])
                            P.op("dve", lambda e: e.reciprocal(out=rs4[:], in_=rs4[:]), reads=[t_rs], writes=[t_rs])
                        else:
                            P.op("dve", lambda e: e.reciprocal(out=rs4[:], in_=pob[:, :, 64]), reads=rd, writes=[t_rs])
                        g0_ = 12 * g + br
                        P.op("dve", lambda e: e.tensor_tensor(out=wg4[:], in0=rs4[:], in1=gat[:, qi, g0_:g0_ + 10:3], op=ALU.mult),
                             reads=[t_rs, t_gat[qi]], writes=[t_rs])
                        if br == 0:
                            P.op("dve", lambda e: e.tensor_tensor(
                                out=oa[:, 4 * g:4 * g + 4, :], in0=pob[:, :, 0:64],
                                in1=wg4[:].unsqueeze(2).broadcast_to([128, 4, 64]), op=ALU.mult),
                                reads=rd + [t_rs], writes=[t_oa])
                            P.op("dve", lambda e: e.tensor_tensor(
                                out=itmp[:], in0=pob[:, :, 65:97], in1=rs4[:].unsqueeze(2).broadcast_to([128, 4, 32]),
                                op=ALU.mult), reads=rd + [t_rs], writes=[t_imp])
                            P.op("dve", lambda e: e.tensor_reduce(out=imp[:], in_=itmp[:].rearrange("p j n -> p n j"),
                                                                  axis=AX.X, op=ALU.add), reads=[t_imp], writes=[t_imp])
                        else:
                            P.op("dve", lambda e: e.tensor_tensor(
                                out=otmp[:], in0=pob[:, :, 0:64],
                                in1=wg4[:].unsqueeze(2).broadcast_to([128, 4, 64]), op=ALU.mult),
                                reads=rd + [t_rs], writes=[t_otmp])
                            P.op("pool", lambda e: e.tensor_tensor(out=oa[:, 4 * g:4 * g + 4, :], in0=oa[:, 4 * g:4 * g + 4, :],
                                                                   in1=otmp[:], op=ALU.add),
                                 reads=[t_otmp, t_oa], writes=[t_oa])

                    def selection(g, qi, it):
                        ns, t_ns = negsel[it % 2], t_negsel[it % 2]
                        nT, t_nT = nsT4[it % 2], t_nsT[it % 2]
                        P.op("dve", lambda e: e.tensor_tensor(out=imp[:], in0=imp[:], in1=selmul[:, qi, :], op=ALU.mult),
                             reads=[t_imp, t_selmul], writes=[t_imp])
                        P.op("dve", lambda e: e.tensor_tensor(out=imp[:], in0=imp[:], in1=seladd[:, qi, :], op=ALU.add),
                             reads=[t_imp, t_seladd], writes=[t_imp])
                        P.op("dve", lambda e: e.max(out=m8[:], in_=imp[:]), reads=[t_imp], writes=[t_m8])
                        P.op("dve", lambda e: e.tensor_scalar(out=ns[:, g * 64:g * 64 + 32], in0=imp[:], scalar1=m8[:, 7:8],
                                                              scalar2=NEG, op0=ALU.is_lt, op1=ALU.mult),
                             reads=[t_imp, t_m8], writes=[t_ns])

                        def later():
                            P.op("pe", lambda e: e.transpose(out=psb[:, 0:128], in_=ns[:, :], identity=identb[:]),
                                 reads=[t_ns, t_identb], writes=[tpsb_a])
                            P.op("act", lambda e: e.copy(out=nT[:], in_=psb[:, 0:128].unsqueeze(1).broadcast_to([128, 4, 128])),
                                 reads=[tpsb_a], writes=[t_nT])
                            sel_ready.add(it)
                        deferred.append([cur_n[0] + 8, later])

                    def finish_qi(qi, oa, t_oa):
                        gi = s * NT + qi
                        if dbg and s == 0:
                            if qi == 0:
                                dbg_out["oattn"] = dout("dbg_oattn", [NT, 128, 512], F32)
                            P.dma("sp", dbg_out["oattn"][qi], oa[:].rearrange("p h d -> p (h d)"), reads=[t_oa])
                        oaf = oa[:].rearrange("p h d -> p (h d)")
                        rms_stats(oaf, t_oa, junk2[:], t_junk2, ssqa, rstda, t_sta[gi], gi, 512)
                        P.op("dve", lambda e: e.scalar_tensor_tensor(
                            out=on_b[:], in0=oaf, scalar=rstda[:, gi:gi + 1], in1=gattn[:], op0=ALU.mult, op1=ALU.mult),
                            reads=[t_oa, t_sta[gi], t_gattn], writes=[t_onb])

                        def later():
                            for c in range(4):
                                P.op("pe", lambda e, c=c: e.transpose(out=psb[:, 512 + c * 128:512 + (c + 1) * 128],
                                                                      in_=on_b[:, c * 128:(c + 1) * 128], identity=identb[:]),
                                     reads=[t_onb, t_identb], writes=[tpsb])
                            for c in range(4):
                                evac(1, mixT[:, c, qi * 128:(qi + 1) * 128], psb[:, 512 + c * 128:512 + (c + 1) * 128],
                                     [tpsb], [t_mixa[qi]])
                        deferred.append([cur_n[0] + 8, later])

                    deferred = []
                    cur_n = [0]
                    sel_ready = set()
                    tiles = []
                    groups_ = []
                    it = 0
                    for qi in range(NT):
                        oa, t_oa = o_acc[qi % 2], t_oacc[qi % 2]
                        for g in range(2):
                            pr = slice(0, 128)
                            rhs_q = qTg[g][:, :, qi * 128:(qi + 1) * 128]
                            rd_q = [t_qT[qi // 4]]
                            hs4 = slice(4 * g, 4 * g + 4)
                            bc, t_bc = bc_all[:, qi, :, :], t_bcq[qi]
                            pre = None
                            if g == 0 and qi + 1 < NT:
                                def pre(qi=qi):
                                    bc_exp(qi + 1)
                            tiles.append(dict(
                                pre=pre, rows=127, br=0, first=True, last=True, ncol=97,
                                mm=[(kcmpT[pr, 0:127], rhs_q, rd_q + [t_kcmp])],
                                bias=bc[0:127, hs4, :], t_bias=t_bc, v=vc_aug[0:127, g, :], v_rd=[t_vca],
                                post=(lambda g=g, qi=qi, oa=oa, t_oa=t_oa, it=it: (evac_branch(g, 0, qi, oa, t_oa),
                                                                                   selection(g, qi, it)))))
                            k0 = max(0, qi - 4)
                            for kj in range(k0, qi + 1):
                                tiles.append(dict(
                                    pre=None, rows=128, br=2, first=(kj == k0), last=(kj == qi), ncol=65,
                                    mm=[(kT["kw"][pr, kj * 128:(kj + 1) * 128], rhs_q, rd_q + [t_kT["kw"][kj // 4]])],
                                    bias=Bw[:, qi - kj, hs4, :], t_bias=t_Bw, v=vw_aug[:, kj, g, :], v_rd=[t_va[kj], t_ones],
                                    post=(lambda g=g, qi=qi, oa=oa, t_oa=t_oa: evac_branch(g, 2, qi, oa, t_oa)) if kj == qi else None))
                            nT, t_nT = nsT4[it % 2], t_nsT[it % 2]
                            for kj in range(qi + 1):
                                post = None
                                if kj == qi:
                                    if g == 1:
                                        post = (lambda g=g, qi=qi, oa=oa, t_oa=t_oa: (evac_branch(g, 1, qi, oa, t_oa),
                                                                                      finish_qi(qi, oa, t_oa)))
                                    else:
                                        post = (lambda g=g, qi=qi, oa=oa, t_oa=t_oa: evac_branch(g, 1, qi, oa, t_oa))
                                tiles.append(dict(
                                    need=it,
                                    pre=None, rows=128, br=1, first=(kj == 0), last=(kj == qi), ncol=65,
                                    mm=[(kT["ks"][pr, kj * 128:(kj + 1) * 128], rhs_q, rd_q + [t_kT["ks"][kj // 4]]),
                                        (esel[pr, kj * 128:(kj + 1) * 128], nT[pr, :, :], [t_esel, t_nT])],
                                    bias=Bs[:, min(qi - kj, 2), hs4, :], t_bias=t_Bs, v=vs_aug[:, kj, g, :],
                                    v_rd=[t_va[kj], t_ones], post=post))
                            it += 1
                            groups_.append(tiles)
                            tiles = []
                    cmp_ = [g_[0] for g_ in groups_]
                    win_ = [[t_ for t_ in g_[1:] if t_["br"] == 2] for g_ in groups_]
                    sel_ = [[t_ for t_ in g_[1:] if t_["br"] == 1] for g_ in groups_]
                    tiles = [cmp_[0]]
                    for i_ in range(len(groups_)):
                        tiles += win_[i_]
                        if i_ + 1 < len(groups_):
                            tiles.append(cmp_[i_ + 1])
                        tiles += sel_[i_]

                    def emit_S(tl, sl):
                        if tl["pre"] is not None:
                            tl["pre"]()
                        rows = tl["rows"]
                        nmm = len(tl["mm"])
                        for m, (l_ap, r_ap, rd) in enumerate(tl["mm"]):
                            P.op("pe", lambda e, l_ap=l_ap, r_ap=r_ap, m=m: e.matmul(
                                ps[sbank[sl]][0:rows, :].rearrange("p (j q) -> p j q", j=4), lhsT=l_ap, rhs=r_ap,
                                start=(m == 0), stop=(m == nmm - 1)), reads=rd, writes=[tps[sbank[sl]]])
                        P.op("act", lambda e: e.activation(out=tS[sl][0:rows, :], in_=ps[sbank[sl]][0:rows, :], func=AF.Exp,
                                                           scale=0.125), reads=[tps[sbank[sl]]], writes=[t_tS[sl]])
                        P.op("dve", lambda e: e.tensor_tensor(
                            out=pT[sl][0:rows, :].rearrange("p (j q) -> p j q", j=4),
                            in0=tS[sl][0:rows, :].rearrange("p (j q) -> p j q", j=4),
                            in1=tl["bias"], op=ALU.mult), reads=[t_tS[sl], tl["t_bias"]], writes=[t_pT[sl]])

                    def emit_PV(tl, sl):
                        rows = tl["rows"]
                        bk = BK[tl["br"]]
                        for j in range(4):
                            P.op("pe", lambda e, j=j: e.matmul(psO[:, bk, j * 128:j * 128 + tl["ncol"]],
                                                               lhsT=pT[sl][0:rows, j * 128:(j + 1) * 128], rhs=tl["v"],
                                                               start=(tl["first"] and j == 0), stop=(tl["last"] and j == 3),
                                                               skip_group_check=True),
                                 reads=[t_pT[sl]] + tl["v_rd"], writes=tpo[tl["br"]])
                        if tl["post"] is not None:
                            tl["post"]()

                    ntl = len(tiles)
                    LA = 3
                    nxt = 0
                    for n_ in range(ntl):
                        cur_n[0] = n_
                        while True:
                            for d_ in [d for d in deferred if d[0] <= n_]:
                                deferred.remove(d_)
                                d_[1]()
                            progressed = False
                            while nxt < ntl and nxt <= n_ + LA and (tiles[nxt].get("need") is None
                                                                   or tiles[nxt]["need"] in sel_ready):
                                emit_S(tiles[nxt], nxt % NSB)
                                nxt += 1
                                progressed = True
                            if nxt > n_:
                                break
                            d_ = min(deferred, key=lambda d: d[0])
                            deferred.remove(d_)
                            d_[1]()
                        emit_PV(tiles[n_], n_ % NSB)
                    for d_ in sorted(deferred, key=lambda d: d[0]):
                        d_[1]()
                    deferred.clear()

                    if s == 0:
                        dbgdump("mixT", [128, 8, S], BF16, mixT[:], t_mixc + t_mixa)
                if stop_after <= 2:
                    continue

                with Scope():
                    wout = sb("wout", [128, 8, 1024], BF16); t_wout = Tok()
                    wov = w_out_d.rearrange("(k p) n -> p k n", p=128)
                    for k in range(8):
                        P.dma("pool", wout[:, k, :], wov[:, k, :], writes=[t_wout])
                    gt1 = sb("gt1", [128, 1024]); t_gt1 = Tok()
                    P.dma("sp", gt1[:], gt_d[0, s], reads=[t_gtd], writes=[t_gt1])
                    xpool = [sb(f"xt3{i}", [128, 1024]) for i in range(2)]; t_xp = [Tok(), Tok()]
                    x1p = [sb(f"x1t{i}", [128, 1024]) for i in range(2)]; t_x1p = [Tok(), Tok()]
                    junk = sb("junk3", [128, 1024], BF16); t_junk = Tok()
                    xs = sb("xs3", [128, 1024], BF16); t_xs = Tok()
                    h2st = [sb(f"h2st{i}", [128, 8, 128], BF16) for i in range(2)]; t_h2st = [Tok(), Tok()]
                    for ti in range(NT):
                        gi = s * NT + ti
                        xt, t_xt = xpool[ti % 2], t_xp[ti % 2]
                        x1t, t_x1t = x1p[ti % 2], t_x1p[ti % 2]
                        P.dma("sp", xt[:], x_d[gi * 128:(gi + 1) * 128, :], writes=[t_xt])
                        for half in range(2):
                            for k in range(8):
                                P.op("pe", lambda e, k=k, half=half: e.matmul(
                                    ps[half][:, :], lhsT=mixT[:, k, ti * 128:(ti + 1) * 128],
                                    rhs=wout[:, k, half * 512:(half + 1) * 512], start=(k == 0), stop=(k == 7)),
                                    reads=[t_wout, t_mixa[ti], t_mixc[ti // 4]], writes=[tps[half]])
                            hs = slice(half * 512, (half + 1) * 512)
                            P.op("dve", lambda e, half=half, hs=hs: e.tensor_tensor(out=x1t[:, hs], in0=ps[half][:, :], in1=gt1[:, hs],
                                                                                    op=ALU.mult),
                                 reads=[tps[half], t_gt1], writes=[t_x1t])
                        P.op("pool", lambda e: e.tensor_tensor(out=x1t[:], in0=x1t[:], in1=xt[:], op=ALU.add),
                             reads=[t_x1t, t_xt], writes=[t_x1t])
                        P.dma("sp", x1_d[gi * 128:(gi + 1) * 128, :], x1t[:], reads=[t_x1t], writes=[t_x1d[gi]])
                        hs_, t_hs = h2st[ti % 2], t_h2st[ti % 2]
                        norm_transpose(x1t, t_x1t, gi, s, ssq2, rstd2, t_st2, gs2, 24, hs_, t_hs, 0, junk, t_junk, xs, t_xs)
                        P.dma("sp", h2T_d[:, :, gi * 128:(gi + 1) * 128], hs_[:], reads=[t_hs], writes=[t_h2d[gi]])

        if stop_after <= 3:
            P.wait_all("sp", Tok.registry)
            return nc, dbg_out

        try:
            utb_d = dscr("utb_s", [16384, 1024], BF16)
            vb_d = dscr("vb_s", [16384, 1024], BF16)
            s2_d = dscr("s2_s", [16, T, 128], BF16)
            t_utb = [Tok() for _ in range(16)]
            t_vb = [Tok() for _ in range(16)]
            with Scope():
                stg = [sb(f"cst{i}", [128, 8, 1024], BF16) for i in range(2)]; t_stg = [Tok(), Tok()]
                n = 0
                for src_d, dst_d, tks in ((ut_d, utb_d, t_utb), (pv_d, vb_d, t_vb)):
                    for c in range(16):
                        i = n % 2; n += 1
                        sv = src_d[c * 1024:(c + 1) * 1024, :].rearrange("(c p) n -> p c n", p=128)
                        dv = dst_d[c * 1024:(c + 1) * 1024, :].rearrange("(c p) n -> p c n", p=128)
                        P.dma("pool", stg[i][:], sv, writes=[t_stg[i]])
                        P.dma("sp", dv, stg[i][:], reads=[t_stg[i]], writes=[tks[c]])
            if stop_after == 3.5:
                raise _Stop()

            with Scope():
                wq, t_wq = None, Tok()
                wq = sb("wq", [128, 8, 1024], BF16)
                wqv = wq_d.rearrange("(k p) n -> p k n", p=128)
                for k in range(8):
                    P.dma("pool", wq[:, k, :], wqv[:, k, :], writes=[t_wq])
                kb, t_kb = load_const("kb", kb_d, [128, 8, 256], BF16, "pool")
                identf, t_identf = load_const("identf", identf_d, [128, 128])
                iota, t_iota = load_const("iota", iota_d, [128, 128])
                selh, t_selh = load_const("selh", selh_d, [16, 128], BF16, "pool")
                gfin, t_gfin = load_const("gfin", gfin_d, [128, 1024])
                gt2 = sb("gt2", [128, 1024]); t_gt2 = Tok()
                Wbuf = sb("Wbuf", [128, 256, 128], BF16); t_W = [Tok() for _ in range(64)]
                h2g = sb("h2g", [128, 8, 256], BF16); t_h2g = Tok()
                qTp = sb("qTp", [128, 8, 256], BF16); t_qTp = Tok()
                s_sb = sb("s_sb", [128, 8, 256]); t_s = Tok()
                work = sb("work", [128, 8, 256]); t_work = Tok()
                v16 = sb("v16", [128, 8, 2, 16]); t_v16 = Tok()
                idx = sb("idx", [128, 8, 16], U32); t_idx = Tok()
                cand = sb("cand", [128, 8, 256]); t_cand = Tok()
                cwork = sb("cwork", [128, 256]); t_cwork = Tok()
                ts16 = sb("ts16", [128, 8, 16]); t_ts = Tok()
                d16 = sb("d16", [128, 8, 16]); t_d16 = Tok()
                zz = sb("zz", [128, 8]); mu = sb("mu", [128, 8]); tauE = sb("tauE", [128, 8]); t_zz = Tok()
                tm3 = sb("tm3", [128, 3, 128]); t_tm3 = Tok()
                s2hm = [sb(f"s2hm{i}", [16, 16 * 128], BF16) for i in range(2)]; t_s2hm = [Tok(), Tok()]
                s2hb = sb("s2hb", [128, 2, 8, 128], BF16); t_s2hb = Tok()
                NRQ = 3
                e4 = [sb(f"e4{i}", [128, 4, 128], BF16) for i in range(NRQ)]; t_e4 = [Tok() for _ in range(NRQ)]
                Rt4 = [sb(f"Rt4{i}", [128, 4, 128], BF16) for i in range(NRQ)]; t_Rt4 = [Tok() for _ in range(NRQ)]
                Lt4 = [sb(f"Lt4{i}", [128, 4, 128], BF16) for i in range(NRQ)]; t_Lt4 = [Tok() for _ in range(NRQ)]
                mk4 = [sb(f"mk4{i}", [128, 4, 128], BF16) for i in range(NRQ)]; t_mk4 = [Tok() for _ in range(NRQ)]
                NC = 5
                utc = [sb(f"utc{i}", [128, 8, 128], BF16) for i in range(NC)]; t_utc = [Tok() for _ in range(NC)]
                vch = [sb(f"vch{i}", [128, 1024], BF16) for i in range(NC)]; t_vch = [Tok() for _ in range(NC)]
                abuf = [sb(f"abuf{i}", [128, 256], BF16) for i in range(2)]; t_ab = [Tok(), Tok()]
                wab = [sb(f"wab{i}", [128, 256], BF16) for i in range(2)]; t_wab = [Tok(), Tok()]
                x1t = sb("x1f", [128, 1024]); t_x1t = Tok()
                yt = sb("yf", [128, 1024]); t_yt = Tok()
                ot = sb("of", [128, 1024]); t_ot = Tok()
                junk = sb("junkf", [128, 1024], BF16); t_junk = Tok()
                ssq3 = sb("ssq3", [128, NTT]); rstd3 = sb("rstd3", [128, NTT]); t_st3 = [Tok() for _ in range(NTT)]
                t_s2d = Tok()
                t_out = Tok()

                def top16(dst_lo, dst_hi, src, wrk, rd, wr_dst, wr_wrk):
                    P.op("dve", lambda e: e.max(out=dst_lo, in_=src), reads=rd, writes=[wr_dst])
                    P.op("dve", lambda e: e.match_replace(out=wrk, in_to_replace=dst_lo, in_values=src, imm_value=-1e30),
                         reads=rd + [wr_dst], writes=[wr_wrk])
                    P.op("dve", lambda e: e.max(out=dst_hi, in_=wrk), reads=[wr_wrk], writes=[wr_dst])

                ngroups = T // 256
                s2hbb = [s2hb, sb("s2hb2", [128, 2, 8, 128], BF16)]; t_s2hbb = [t_s2hb, Tok()]
                psX = psb[:, :].bitcast(F32)
                PB = [(ps[2], tps[2]), (psX, tpsb)]
                h2gb = [h2g, sb("h2g2", [128, 8, 256], BF16)]; t_h2gb = [t_h2g, Tok()]
                tT3b = [[sb(f"tT3_{i}_{j}", [128, 3, 128]) for j in range(2)] for i in range(2)]
                t_tT3b = [[Tok() for j in range(2)] for i in range(2)]

                def prep_steps(gidx):
                    g0 = gidx * 256
                    hb, t_hb = h2gb[gidx % 2], t_h2gb[gidx % 2]
                    steps = []

                    def s_q(hh):
                        def f():
                            if hh == 0:
                                P.dma("sp", hb[:], h2T_d[:, :, g0:g0 + 256], reads=t_h2d[gidx * 2:gidx * 2 + 2], writes=[t_hb])
                            for h in (hh, hh + 1):
                                bk, tb = PB[h % 2]
                                for k in range(8):
                                    P.op("pe", lambda e, k=k, h=h, bk=bk: e.matmul(bk[:, 0:256], lhsT=wq[:, k, h * 128:(h + 1) * 128],
                                                                                   rhs=hb[:, k, :], start=(k == 0), stop=(k == 7)),
                                         reads=[t_wq, t_hb], writes=[tb])
                                P.op("act", lambda e, h=h, bk=bk: e.copy(out=qTp[:, h, :], in_=bk[:, 0:256]),
                                     reads=[tb], writes=[t_qTp])
                        return f
                    for hh in (0, 2, 4, 6):
                        steps.append((f"q{hh}", s_q(hh)))

                    for tt in range(2):
                        tsl = slice(tt * 128, (tt + 1) * 128)
                        tT3, t_tT3 = tT3b[gidx % 2][tt], t_tT3b[gidx % 2][tt]
                        s2hb, t_s2hb = s2hbb[tt], t_s2hbb[tt]

                        def sA(tsl=tsl):
                            for hp in range(4):
                                bk, tb = PB[hp % 2]
                                for h in (2 * hp, 2 * hp + 1):
                                    P.op("pe", lambda e, h=h, bk=bk: e.matmul(bk[:, (h % 2) * 256:(h % 2 + 1) * 256], lhsT=qTp[:, h, tsl],
                                                                              rhs=kb[:, h, :], start=True, stop=True),
                                         reads=[t_qTp, t_kb], writes=[tb])
                                P.op("act", lambda e, hp=hp, bk=bk: e.copy(
                                    out=s_sb[:, 2 * hp:2 * hp + 2, :], in_=bk[:, :].rearrange("p (h n) -> p h n", h=2)),
                                    reads=[tb], writes=[t_s])

                        def sB(s2hb=s2hb, t_s2hb=t_s2hb):
                            for h in range(8):
                                for p in range(2):
                                    top16(v16[:, h, p, 0:8], v16[:, h, p, 8:16], s_sb[:, h, p * 128:(p + 1) * 128],
                                          work[:, h, p * 128:(p + 1) * 128], [t_s], t_v16, t_work)
                                P.op("dve", lambda e, h=h: e.max_index(out=idx[:, h, 0:8], in_max=v16[:, h, 0, 0:8],
                                                                       in_values=s_sb[:, h, 0:128]),
                                     reads=[t_s, t_v16], writes=[t_idx])
                                P.op("dve", lambda e, h=h: e.max_index(out=idx[:, h, 8:16], in_max=v16[:, h, 0, 8:16],
                                                                       in_values=work[:, h, 0:128]),
                                     reads=[t_work, t_v16], writes=[t_idx])
                            P.op("dve", lambda e: e.tensor_tensor(
                                out=cand[:].rearrange("p h (a b) -> p h a b", a=16),
                                in0=v16[:, :, 0, :].unsqueeze(3).broadcast_to([128, 8, 16, 16]),
                                in1=v16[:, :, 1, :].unsqueeze(2).broadcast_to([128, 8, 16, 16]), op=ALU.add),
                                reads=[t_v16], writes=[t_cand])
                            for h in range(8):
                                top16(ts16[:, h, 0:8], ts16[:, h, 8:16], cand[:, h, :], cwork[:, :], [t_cand], t_ts, t_cwork)

                        def sB2(s2hb=s2hb, t_s2hb=t_s2hb):
                            P.op("dve", lambda e: e.tensor_tensor(out=d16[:], in0=ts16[:], in1=ts16[:, :, 0:1].broadcast_to([128, 8, 16]),
                                                                  op=ALU.subtract), reads=[t_ts], writes=[t_d16])
                            P.op("act", lambda e: e.activation(out=d16[:], in_=d16[:], func=AF.Exp), reads=[t_d16], writes=[t_d16])
                            P.op("dve", lambda e: e.tensor_reduce(out=zz[:], in_=d16[:], axis=AX.X, op=ALU.add),
                                 reads=[t_d16], writes=[t_zz])
                            P.op("act", lambda e: e.activation(out=zz[:], in_=zz[:], func=AF.Ln), reads=[t_zz], writes=[t_zz])
                            P.op("dve", lambda e: e.tensor_tensor(out=mu[:], in0=zz[:], in1=ts16[:, :, 0], op=ALU.add),
                                 reads=[t_zz, t_ts], writes=[t_zz])
                            P.op("dve", lambda e: e.tensor_scalar(out=tauE[:], in0=ts16[:, :, 15], scalar1=-1e-4, scalar2=None,
                                                                  op0=ALU.add), reads=[t_ts], writes=[t_zz])
                            P.op("dve", lambda e: e.tensor_tensor(
                                out=tm3[:, 0, :].rearrange("p (h a) -> p h a", h=8),
                                in0=tauE[:].unsqueeze(2).broadcast_to([128, 8, 16]), in1=v16[:, :, 0, :], op=ALU.subtract),
                                reads=[t_zz, t_v16], writes=[t_tm3])
                            P.op("dve", lambda e: e.tensor_tensor(
                                out=tm3[:, 1, :].rearrange("p (h a) -> p h a", h=8),
                                in0=v16[:, :, 0, :], in1=mu[:].unsqueeze(2).broadcast_to([128, 8, 16]), op=ALU.subtract),
                                reads=[t_zz, t_v16], writes=[t_tm3])
                            P.op("dve", lambda e: e.tensor_copy(out=tm3[:, 2, :], in_=idx[:].rearrange("p h a -> p (h a)")),
                                 reads=[t_idx], writes=[t_tm3])
                            P.op("dve", lambda e: e.tensor_copy(out=s2hb[:, 0, :, :], in_=s_sb[:, :, 128:256]),
                                 reads=[t_s], writes=[t_s2hb])
                            P.op("dve", lambda e: e.tensor_tensor(out=s2hb[:, 1, :, :], in0=s_sb[:, :, 128:256], in1=s2hb[:, 0, :, :],
                                                                  op=ALU.subtract), reads=[t_s, t_s2hb], writes=[t_s2hb])

                        def sC(tt=tt, tT3=tT3, t_tT3=t_tT3):
                            bk, tb = PB[0]
                            for w3 in range(3):
                                P.op("pe", lambda e, w3=w3: e.transpose(out=bk[:, w3 * 128:(w3 + 1) * 128], in_=tm3[:, w3, :],
                                                                        identity=identf[:]),
                                     reads=[t_tm3, t_identf], writes=[tb])
                            P.op("act", lambda e: e.copy(out=tT3[:], in_=bk[:, 0:384].rearrange("p (w t) -> p w t", w=3)),
                                 reads=[tb], writes=[t_tT3])

                        def sD(tt=tt):
                            P.dma("sp", s2_d[:, g0 + tt * 128:g0 + (tt + 1) * 128, :].rearrange("(w h) t j -> t w h j", w=2),
                                  s2hbb[tt][:], reads=[t_s2hbb[tt]], writes=[t_s2d])
                        steps.append((f"A{tt}", sA))
                        steps.append((f"B{tt}", sB))
                        steps.append((f"E{tt}", sB2))
                        steps.append((f"C{tt}", sC))
                        steps.append((f"D{tt}", sD))
                    return steps

                SCHED = {"q0": 1, "q2": 3, "q4": 5, "q6": 7, "A0": 8, "B0": 10, "E0": 45, "C0": 55, "A1": 58, "B1": 60,
                         "E1": 100, "C1": -1, "D0": -1, "D1": -1}

                for gidx in range(ngroups):
                    g0 = gidx * 256
                    sq = g0 // S
                    if g0 % S == 0:
                        P.dma("sp", gt2[:], gt_d[1, sq], reads=[t_gtd], writes=[t_gt2])
                    if gidx == 0:
                        for _, st_ in prep_steps(0):
                            st_()
                    h2g, t_h2g = h2gb[gidx % 2], t_h2gb[gidx % 2]
                    nxt_steps = prep_steps(gidx + 1) if gidx + 1 < ngroups else []
                    for tt in range(2):
                        gi = gidx * 2 + tt
                        tT3, t_tT3 = tT3b[gidx % 2][tt], t_tT3b[gidx % 2][tt]
                        quads = []
                        for sl in range(8):
                            for q4 in range(4):
                                quads.append((sl, q4))

                        def load_slab(sl):
                            i2 = sl % 2
                            t0 = g0 + tt * 128 + sl * 16
                            P.dma("sp", s2hm[i2][:, :], s2_d[:, t0:t0 + 16, :].rearrange("h t j -> h (t j)"),
                                  reads=[t_s2d], writes=[t_s2hm[i2]])

                        def emit_sel(qn):
                            sl, q4 = quads[qn]
                            i2 = sl % 2
                            if q4 == 0:
                                load_slab(sl)
                            bA = 0 if qn % 2 == 0 else 4
                            bD = 3 if qn % 2 == 0 else 5
                            for bb in (bA, bD):
                                P.op("pe", lambda e, bb=bb: e.matmul(ps[bb][:, :], lhsT=selh[:, :],
                                                                     rhs=s2hm[i2][:, q4 * 512:(q4 + 1) * 512], start=True, stop=True),
                                     reads=[t_selh, t_s2hm[i2]], writes=[tps[bb]])
                            r = qn % NRQ
                            tl0 = sl * 16 + q4 * 4
                            for t in range(4):
                                P.op("act", lambda e, t=t: e.activation(out=e4[r][:, t, :], in_=ps[bA][:, t * 128:(t + 1) * 128],
                                                                        func=AF.Exp, bias=tT3[:, 1, tl0 + t:tl0 + t + 1]),
                                     reads=[tps[bA], t_tT3], writes=[t_e4[r]])
                            P.op("dve", lambda e: e.tensor_tensor(
                                out=mk4[r][:], in0=ps[bD][:, :].rearrange("p (t j) -> p t j", t=4),
                                in1=tT3[:, 0, tl0:tl0 + 4].unsqueeze(2).broadcast_to([128, 4, 128]), op=ALU.is_ge),
                                reads=[tps[bD], t_tT3], writes=[t_mk4[r]])
                            P.op("dve", lambda e: e.tensor_tensor(
                                out=Lt4[r][:], in0=iota[:].unsqueeze(1).broadcast_to([128, 4, 128]),
                                in1=tT3[:, 2, tl0:tl0 + 4].unsqueeze(2).broadcast_to([128, 4, 128]), op=ALU.is_equal),
                                reads=[t_iota, t_tT3], writes=[t_Lt4[r]])
                            P.op("dve", lambda e: e.tensor_tensor(out=Rt4[r][:], in0=mk4[r][:], in1=e4[r][:], op=ALU.mult),
                                 reads=[t_mk4[r], t_e4[r]], writes=[t_Rt4[r]])

                        def emit_w(qn):
                            sl, q4 = quads[qn]
                            r = qn % NRQ
                            wb = 1 + (qn % 2)
                            tb0 = tt * 128 + sl * 16 + q4 * 4
                            for t in range(4):
                                P.op("pe", lambda e, t=t: e.matmul(ps[wb][:, t * 128:(t + 1) * 128], lhsT=Rt4[r][:, t, :],
                                                                   rhs=Lt4[r][:, t, :], start=True, stop=True),
                                     reads=[t_Rt4[r], t_Lt4[r]], writes=[tps[wb]])
                            evac(qn, Wbuf[:, tb0:tb0 + 4, :], ps[wb][:, :].rearrange("p (t i) -> p t i", t=4),
                                 [tps[wb]], [t_W[tb0 // 4]])

                        emit_sel(0)
                        for qn in range(len(quads)):
                            if qn + 1 < len(quads):
                                emit_sel(qn + 1)
                            emit_w(qn)

                    if dbg and gidx == 0:
                        dbgdump("Wbuf", [128, 256, 128], BF16, Wbuf[:], t_W)
                    if stop_after == 3.8:
                        raise _Stop()
                    def ld(i):
                        c = i % NC
                        P.dma("sp", utc[c][:], utb_d[i * 128:(i + 1) * 128, :].rearrange("p (k j) -> p k j", k=8),
                              reads=[t_utb[i // 8]], writes=[t_utc[c]])
                        P.dma("sp", vch[c][:], vb_d[i * 128:(i + 1) * 128, :], reads=[t_vb[i // 8]], writes=[t_vch[c]])

                    def emU(i):
                        c = i % NC
                        ba = i % 2
                        for k in range(8):
                            P.op("pe", lambda e, k=k: e.matmul(ps[ba][:, 0:256], lhsT=utc[c][:, k, :], rhs=h2g[:, k, :],
                                                               start=(k == 0), stop=(k == 7)),
                                 reads=[t_utc[c], t_h2g], writes=[tps[ba]])
                        P.op("act", lambda e: e.activation(out=abuf[ba][:], in_=ps[ba][:, 0:256], func=AF.Gelu_apprx_tanh),
                             reads=[tps[ba]], writes=[t_ab[ba]])
                        P.op("pool", lambda e: e.tensor_tensor(out=wab[ba][:], in0=abuf[ba][:], in1=Wbuf[:, :, i], op=ALU.mult),
                             reads=[t_ab[ba]] + t_W, writes=[t_wab[ba]])

                    def emV(i):
                        c = i % NC
                        ba = i % 2
                        for tt in range(2):
                            for half in range(2):
                                P.op("pe", lambda e, tt=tt, half=half: e.matmul(
                                    psO[:, tt * 2 + half, :], lhsT=wab[ba][:, tt * 128:(tt + 1) * 128],
                                    rhs=vch[c][:, half * 512:(half + 1) * 512], start=(i == 0), stop=(i == 127)),
                                    reads=[t_wab[ba], t_vch[c]], writes=[tps[3 + tt * 2 + half]])

                    PF = NC - 2
                    for i in range(PF):
                        ld(i)
                    emU(0)
                    for i in range(128):
                        if i + PF < 128:
                            ld(i + PF)
                        if i + 1 < 128:
                            emU(i + 1)
                        emV(i)
                        for nm_, fn_ in nxt_steps:
                            if SCHED[nm_] == i:
                                fn_()
                    for nm_, fn_ in nxt_steps:
                        if SCHED[nm_] == -1:
                            fn_()
                    for tt in range(2):
                        gi = gidx * 2 + tt
                        P.dma("sp", x1t[:], x1_d[gi * 128:(gi + 1) * 128, :], reads=[t_x1d[gi]], writes=[t_x1t])
                        for half in range(2):
                            hs = slice(half * 512, (half + 1) * 512)
                            P.op("dve", lambda e, tt=tt, half=half, hs=hs: e.tensor_tensor(
                                out=yt[:, hs], in0=psO[:, tt * 2 + half, :], in1=gt2[:, hs], op=ALU.mult),
                                reads=[tps[3 + tt * 2 + half], t_gt2], writes=[t_yt])
                        if dbg and gidx == 0 and tt == 0:
                            dbgdump("peer0", [128, 1024], F32, yt[:], [t_yt])
                        P.op("pool", lambda e: e.tensor_tensor(out=yt[:], in0=yt[:], in1=x1t[:], op=ALU.add),
                             reads=[t_yt, t_x1t], writes=[t_yt])
                        rms_stats(yt[:], t_yt, junk[:], t_junk, ssq3, rstd3, t_st3[gi], gi, D)
                        P.op("dve", lambda e, gi=gi: e.scalar_tensor_tensor(out=ot[:], in0=yt[:], scalar=rstd3[:, gi:gi + 1], in1=gfin[:],
                                                                            op0=ALU.mult, op1=ALU.mult),
                             reads=[t_yt, t_st3[gi], t_gfin], writes=[t_ot])
                        P.dma("sp", out_d[gi * 128:(gi + 1) * 128, :], ot[:], reads=[t_ot], writes=[t_out])
                    if stop_after == 4 and gidx == 0:
                        raise _Stop()

        except _Stop:
            pass
        P.wait_all("sp", Tok.registry)
        print("ops per engine", P.ecount, "waits", P.nwaits)
    return nc, dbg_out


_NC_CACHE = {}


def kernel(**inputs):
    inp = {k: np.asarray(v) for k, v in inputs.items()}
    sh = _prep(inp)
    if "nc" not in _NC_CACHE:
        _NC_CACHE["nc"] = build_nc(nseq=2)[0]
    nc = _NC_CACHE["nc"]
    x = np.asarray(inp["x"], np.float32)
    c = np.asarray(inp["c"], np.float32)
    in_maps = []
    for core in range(8):
        m = dict(sh)
        m["x"] = np.ascontiguousarray(x[2 * core:2 * core + 2].reshape(2 * S, D))
        m["cT"] = np.ascontiguousarray(c[2 * core:2 * core + 2].T.reshape(8, 128, 2).transpose(1, 0, 2))
        in_maps.append(m)
    res = run_bass_kernel_spmd(nc, in_maps, core_ids=list(range(8)))
    out = np.concatenate([np.asarray(r["out"]).reshape(2, S, D) for r in res.results], axis=0)
    return out.astype(np.float32)
```
